# Optimizing a Trainium2 kernel written in Bass

```python
import math
import jax
import jax.numpy as jnp
from jax import lax
import numpy as np

D_MODEL = 1024
BATCH = 4
SEQ = 4096
DEPTH = 2

CHUNK = 64
Q_BLOCK = 128
NORM_EPS = 1e-6
NEG_INF = -1e30

N_EVEN = (DEPTH + 1) // 2
N_ODD = DEPTH // 2

RWKV_HEAD_DIM = 64
RWKV_WIDTH = D_MODEL // 2
RWKV_HEADS = RWKV_WIDTH // RWKV_HEAD_DIM
DECAY_LORA = 64
ICLR_LORA = 64
GATE_LORA = 160
RWKV_GN_EPS = 1e-5 * RWKV_HEAD_DIM
RWKV_SPLITS = (RWKV_WIDTH, 2 * RWKV_WIDTH, 3 * RWKV_WIDTH,
               3 * RWKV_WIDTH + DECAY_LORA, 3 * RWKV_WIDTH + DECAY_LORA + ICLR_LORA)
RWKV_COLS = 3 * RWKV_WIDTH + DECAY_LORA + ICLR_LORA + GATE_LORA

S5_WIDTH = D_MODEL - RWKV_WIDTH
S5_GROUP = 16
S5_GROUPS = S5_WIDTH // S5_GROUP
S5_STATE = 64
S5_DT_MIN = 1e-3
S5_DT_MAX = 1e-1

IN_COLS = RWKV_COLS + S5_WIDTH
MIX_WIDTH = RWKV_WIDTH + S5_WIDTH

DIFF_HEAD_DIM = 64
DIFF_V_DIM = 2 * DIFF_HEAD_DIM
DIFF_HEADS = D_MODEL // DIFF_V_DIM
DIFF_WIDTH = DIFF_HEADS * DIFF_V_DIM

MEM_LEN = 256
MEM_HEADS = 4
MEM_HEAD_DIM = D_MODEL // MEM_HEADS

D_FF = 2816
N_EXPERTS = 8
TOP_K = 2
D_FF_EXPERT = 3584

kernel_name = 'hybrid_rwkv7_s5_diffattn_moe_encoder'

F32 = jnp.float32


def rms_norm(x, gain):
    xf = x.astype(F32)
    y = xf * lax.rsqrt(jnp.mean(xf * xf, axis=-1, keepdims=True) + NORM_EPS)
    return (y * gain.astype(F32)).astype(x.dtype)


def token_shift(z):
    return jnp.pad(z, ((0, 0), (1, 0), (0, 0)))[:, :-1]


def alibi_slopes(n_heads):
    return 2.0 ** (-8.0 * jnp.arange(1, n_heads + 1, dtype=F32) / n_heads)


def rwkv7_recurrence(r, decay, k, v, kk, a):
    b, _, h, n = r.shape

    def step(state, inp):
        r_t, w_t, k_t, v_t, kk_t, a_t = inp
        s_kk = jnp.einsum('bhvk,bhk->bhv', state, kk_t)
        state = (state * w_t[:, :, None, :]
                 - s_kk[..., None] * (kk_t * a_t)[:, :, None, :]
                 + v_t[..., None] * k_t[:, :, None, :])
        return state, jnp.einsum('bhvk,bhk->bhv', state, r_t)

    xs = tuple(jnp.moveaxis(t, 1, 0) for t in (r, decay, k, v, kk, a))
    _, out = lax.scan(step, jnp.zeros((b, h, n, n), F32), xs)
    return jnp.moveaxis(out, 0, 1)


def rwkv7_time_mix(zr, mu, w0, w_up, a0, a_up, g_up, k_k, k_a, r_k, ln_w, ln_b):
    b, t, _ = zr.shape
    zr = zr.astype(F32)
    zr = zr + (token_shift(zr) - zr) * mu.astype(F32)
    r, k, v, wd, ad, gd = jnp.split(zr, RWKV_SPLITS, axis=-1)
    heads = lambda u: u.reshape(b, t, RWKV_HEADS, RWKV_HEAD_DIM)
    w_log = -jax.nn.softplus(-(w0.astype(F32) + jnp.tanh(wd) @ w_up.astype(F32))) - 0.5
    decay = jnp.exp(-jnp.exp(w_log))
    a = jax.nn.sigmoid(a0.astype(F32) + ad @ a_up.astype(F32))
    g = jax.nn.sigmoid(gd) @ g_up.astype(F32)
    kk = heads(k * k_k.astype(F32))
    kk = kk * lax.rsqrt(jnp.maximum(jnp.sum(kk * kk, axis=-1, keepdims=True), 1e-24))
    k = k * (1.0 + (a - 1.0) * k_a.astype(F32))
    r_h, k_h, v_h = heads(r), heads(k), heads(v)
    o = rwkv7_recurrence(r_h, heads(decay), k_h, v_h, kk, heads(a))
    mean = jnp.mean(o, axis=-1, keepdims=True)
    var = jnp.mean(jnp.square(o - mean), axis=-1, keepdims=True)
    o = ((o - mean) * lax.rsqrt(var + RWKV_GN_EPS)).reshape(b, t, RWKV_WIDTH)
    o = o * ln_w.astype(F32) + ln_b.astype(F32)
    bonus = jnp.sum(r_h * k_h * r_k.astype(F32), axis=-1, keepdims=True) * v_h
    return (o + bonus.reshape(b, t, RWKV_WIDTH)) * g


def s5_ssm(u, lam_re, lam_im, log_dt, b_re, b_im, c_re, c_im, d_skip, w_glu, b_glu):
    b, t, _ = u.shape
    ug = u.astype(F32).reshape(b, t, S5_GROUPS, S5_GROUP)
    lam = lax.complex(lam_re.astype(F32), lam_im.astype(F32))
    dt = jnp.exp(log_dt.astype(F32))[:, None]
    lam_bar = jnp.exp(lam * dt)
    b_bar = ((lam_bar - 1.0) / lam)[..., None] * lax.complex(b_re.astype(F32), b_im.astype(F32))
    bu = jnp.einsum('gpi,btgi->btgp', b_bar, ug.astype(jnp.complex64))

    def combine(left, right):
        a_l, x_l = left
        a_r, x_r = right
        return a_r * a_l, a_r * x_l + x_r

    _, states = lax.associative_scan(combine, (jnp.broadcast_to(lam_bar, bu.shape), bu), axis=1)
    c_mat = lax.complex(c_re.astype(F32), c_im.astype(F32))
    y = jnp.real(jnp.einsum('gip,btgp->btgi', c_mat, states))
    y = y + d_skip.astype(F32).reshape(S5_GROUPS, S5_GROUP) * ug
    y = jax.nn.gelu(y.reshape(b, t, S5_WIDTH))
    return y * jax.nn.sigmoid(y @ w_glu.astype(F32) + b_glu.astype(F32))


def diff_attention(h, w_qkv, q_gain, k_gain, lam_q1, lam_k1, lam_q2, lam_k2, sub_gain, w_o, lambda_init):
    b, t, _ = h.shape
    q, k, v = jnp.split(h @ w_qkv, 3, axis=-1)
    q = rms_norm(q.reshape(b, t, DIFF_HEADS, 2, DIFF_HEAD_DIM), q_gain).astype(F32)
    k = rms_norm(k.reshape(b, t, DIFF_HEADS, 2, DIFF_HEAD_DIM), k_gain).astype(F32)
    v = v.reshape(b, t, DIFF_HEADS, DIFF_V_DIM).astype(F32)
    lam = (jnp.exp(jnp.sum(lam_q1.astype(F32) * lam_k1.astype(F32)))
           - jnp.exp(jnp.sum(lam_q2.astype(F32) * lam_k2.astype(F32))) + lambda_init)
    slopes = alibi_slopes(DIFF_HEADS)
    key_pos = jnp.arange(t)
    n_blocks = t // Q_BLOCK
    q_blocks = jnp.moveaxis(q.reshape(b, n_blocks, Q_BLOCK, DIFF_HEADS, 2, DIFF_HEAD_DIM), 1, 0)
    starts = jnp.arange(n_blocks) * Q_BLOCK
    scale = DIFF_HEAD_DIM ** -0.5

    def attend_block(args):
        qb, start = args
        q_pos = start + jnp.arange(Q_BLOCK)
        s = jnp.einsum('bqhcd,bshcd->bhcqs', qb, k) * scale
        dist = jnp.abs(q_pos[:, None] - key_pos[None, :]).astype(F32)
        bias = -slopes[:, None, None, None] * dist
        visible = (key_pos[None, :] // CHUNK) <= (q_pos[:, None] // CHUNK)
        p = jax.nn.softmax(jnp.where(visible, s + bias, NEG_INF), axis=-1)
        attn = p[:, :, 0] - lam * p[:, :, 1]
        return jnp.einsum('bhqs,bshe->bqhe', attn, v)

    o = lax.map(attend_block, (q_blocks, starts))
    o = jnp.moveaxis(o, 0, 1).reshape(b, t, DIFF_HEADS, DIFF_V_DIM)
    o = rms_norm(o, sub_gain) * (1.0 - lambda_init)
    return o.reshape(b, t, DIFF_WIDTH).astype(h.dtype) @ w_o


def memory_cross_attention(h, mem_n, w_q, w_kv, q_gain, k_gain, w_o):
    b, t, _ = h.shape
    m = mem_n.shape[1]
    q = rms_norm((h @ w_q).reshape(b, t, MEM_HEADS, MEM_HEAD_DIM), q_gain).astype(F32)
    k, v = jnp.split(mem_n @ w_kv, 2, axis=-1)
    k = rms_norm(k.reshape(b, m, MEM_HEADS, MEM_HEAD_DIM), k_gain).astype(F32)
    v = v.reshape(b, m, MEM_HEADS, MEM_HEAD_DIM).astype(F32)
    p = jax.nn.softmax(jnp.einsum('bthd,bmhd->bhtm', q, k) * MEM_HEAD_DIM ** -0.5, axis=-1)
    o = jnp.einsum('bhtm,bmhd->bthd', p, v).reshape(b, t, D_MODEL)
    return o.astype(h.dtype) @ w_o


def swiglu(h, w_gate, w_up, w_down):
    return (jax.nn.silu(h @ w_gate) * (h @ w_up)) @ w_down


def moe_swiglu(h, w_router, b_router, w_gate, w_up, w_down):
    logits = (h @ w_router).astype(F32) + b_router.astype(F32)
    top_logits, top_idx = lax.top_k(logits, TOP_K)
    top_w = jax.nn.softmax(top_logits, axis=-1)
    gates = jnp.sum(jax.nn.one_hot(top_idx, N_EXPERTS, dtype=F32) * top_w[..., None], axis=-2)
    out = jnp.zeros(h.shape, F32)
    for e in range(N_EXPERTS):
        y_e = swiglu(h, w_gate[e], w_up[e], w_down[e]).astype(F32)
        out = out + gates[..., e:e + 1] * y_e
    return out.astype(h.dtype)


def setup_inputs(seed: int = 0) -> dict:
    key = jax.random.key(seed)
    keys = iter(jax.random.split(key, 80))

    def nrm(shape, scale):
        return jax.random.normal(next(keys), shape, F32) * scale

    def gain(shape):
        return 1.0 + nrm(shape, 0.02)

    D, L, NE, NO = D_MODEL, DEPTH, N_EVEN, N_ODD
    G, P, I = S5_GROUPS, S5_STATE, S5_GROUP
    decay_ramp = -6.5 + 5.0 * jnp.linspace(0.0, 1.0, RWKV_WIDTH, dtype=F32) ** 0.85
    return {
        'x': nrm((BATCH, SEQ, D), 1.0),
        'mem': nrm((BATCH, MEM_LEN, D), 1.0),
        'norm_mix': gain((L, D)),
        'norm_xattn': gain((L, D)),
        'norm_mem': gain((L, D)),
        'norm_ffn': gain((L, D)),
        'xa_w_q': nrm((L, D, D), D ** -0.5),
        'xa_w_kv': nrm((L, D, 2 * D), D ** -0.5),
        'xa_q_gain': gain((L, MEM_HEAD_DIM)),
        'xa_k_gain': gain((L, MEM_HEAD_DIM)),
        'xa_w_o': nrm((L, D, D), D ** -0.5),
        'hy_w_in': nrm((NE, D, IN_COLS), D ** -0.5),
        'rw_mu': jax.random.uniform(next(keys), (NE, RWKV_COLS), F32),
        'rw_w0': decay_ramp + nrm((NE, RWKV_WIDTH), 0.01),
        'rw_w_up': nrm((NE, DECAY_LORA, RWKV_WIDTH), 0.1),
        'rw_a0': nrm((NE, RWKV_WIDTH), 0.1),
        'rw_a_up': nrm((NE, ICLR_LORA, RWKV_WIDTH), ICLR_LORA ** -0.5),
        'rw_g_up': nrm((NE, GATE_LORA, RWKV_WIDTH), GATE_LORA ** -0.5),
        'rw_k_k': 0.85 + nrm((NE, RWKV_WIDTH), 0.02),
        'rw_k_a': gain((NE, RWKV_WIDTH)),
        'rw_r_k': nrm((NE, RWKV_HEADS, RWKV_HEAD_DIM), 0.1),
        'rw_ln_w': gain((NE, RWKV_WIDTH)),
        'rw_ln_b': nrm((NE, RWKV_WIDTH), 0.02),
        's5_lam_re': -0.5 + nrm((NE, G, P), 0.01),
        's5_lam_im': jnp.pi * jnp.arange(P, dtype=F32) + nrm((NE, G, P), 0.01),
        's5_log_dt': jax.random.uniform(next(keys), (NE, G), F32, math.log(S5_DT_MIN), math.log(S5_DT_MAX)),
        's5_b_re': nrm((NE, G, P, I), (2 * I) ** -0.5),
        's5_b_im': nrm((NE, G, P, I), (2 * I) ** -0.5),
        's5_c_re': nrm((NE, G, I, P), P ** -0.5),
        's5_c_im': nrm((NE, G, I, P), P ** -0.5),
        's5_d': nrm((NE, S5_WIDTH), 1.0),
        's5_w_glu': nrm((NE, S5_WIDTH, S5_WIDTH), S5_WIDTH ** -0.5),
        's5_b_glu': nrm((NE, S5_WIDTH), 0.02),
        'hy_w_out': nrm((NE, MIX_WIDTH, D), MIX_WIDTH ** -0.5),
        'ff_w_gate': nrm((NE, D, D_FF), D ** -0.5),
        'ff_w_up': nrm((NE, D, D_FF), D ** -0.5),
        'ff_w_down': nrm((NE, D_FF, D), D_FF ** -0.5),
        'da_w_qkv': nrm((NO, D, 3 * DIFF_WIDTH), D ** -0.5),
        'da_q_gain': gain((NO, DIFF_HEAD_DIM)),
        'da_k_gain': gain((NO, DIFF_HEAD_DIM)),
        'da_lam_q1': nrm((NO, DIFF_HEAD_DIM), 0.1),
        'da_lam_k1': nrm((NO, DIFF_HEAD_DIM), 0.1),
        'da_lam_q2': nrm((NO, DIFF_HEAD_DIM), 0.1),
        'da_lam_k2': nrm((NO, DIFF_HEAD_DIM), 0.1),
        'da_sub_gain': gain((NO, DIFF_V_DIM)),
        'da_w_o': nrm((NO, DIFF_WIDTH, D), DIFF_WIDTH ** -0.5),
        'moe_w_router': nrm((NO, D, N_EXPERTS), D ** -0.5),
        'moe_b_router': nrm((NO, N_EXPERTS), 0.01),
        'moe_w_gate': nrm((NO, N_EXPERTS, D, D_FF_EXPERT), D ** -0.5),
        'moe_w_up': nrm((NO, N_EXPERTS, D, D_FF_EXPERT), D ** -0.5),
        'moe_w_down': nrm((NO, N_EXPERTS, D_FF_EXPERT, D), D_FF_EXPERT ** -0.5),
    }


def reference(x, mem, norm_mix, norm_xattn, norm_mem, norm_ffn,
              xa_w_q, xa_w_kv, xa_q_gain, xa_k_gain, xa_w_o,
              hy_w_in, rw_mu, rw_w0, rw_w_up, rw_a0, rw_a_up, rw_g_up, rw_k_k, rw_k_a, rw_r_k,
              rw_ln_w, rw_ln_b,
              s5_lam_re, s5_lam_im, s5_log_dt, s5_b_re, s5_b_im, s5_c_re, s5_c_im, s5_d,
              s5_w_glu, s5_b_glu, hy_w_out,
              ff_w_gate, ff_w_up, ff_w_down,
              da_w_qkv, da_q_gain, da_k_gain, da_lam_q1, da_lam_k1, da_lam_q2, da_lam_k2,
              da_sub_gain, da_w_o,
              moe_w_router, moe_b_router, moe_w_gate, moe_w_up, moe_w_down):
    h = x
    for l in range(DEPTH):
        i = l // 2
        hn = rms_norm(h, norm_mix[l])
        if l % 2 == 0:
            z = hn @ hy_w_in[i]
            y_a = rwkv7_time_mix(z[..., :RWKV_COLS], rw_mu[i], rw_w0[i], rw_w_up[i], rw_a0[i],
                                 rw_a_up[i], rw_g_up[i], rw_k_k[i], rw_k_a[i], rw_r_k[i],
                                 rw_ln_w[i], rw_ln_b[i])
            y_b = s5_ssm(z[..., RWKV_COLS:], s5_lam_re[i], s5_lam_im[i], s5_log_dt[i], s5_b_re[i],
                         s5_b_im[i], s5_c_re[i], s5_c_im[i], s5_d[i], s5_w_glu[i], s5_b_glu[i])
            y_mix = jnp.concatenate([y_a, y_b], axis=-1).astype(h.dtype) @ hy_w_out[i]
        else:
            lambda_init = 0.8 - 0.6 * math.exp(-0.3 * l)
            y_mix = diff_attention(hn, da_w_qkv[i], da_q_gain[i], da_k_gain[i], da_lam_q1[i],
                                   da_lam_k1[i], da_lam_q2[i], da_lam_k2[i], da_sub_gain[i],
                                   da_w_o[i], lambda_init)
        h = h + y_mix.astype(h.dtype)
        y_mem = memory_cross_attention(rms_norm(h, norm_xattn[l]), rms_norm(mem, norm_mem[l]),
                                       xa_w_q[l], xa_w_kv[l], xa_q_gain[l], xa_k_gain[l], xa_w_o[l])
        h = h + y_mem.astype(h.dtype)
        hn = rms_norm(h, norm_ffn[l])
        if l % 2 == 0:
            y_ff = swiglu(hn, ff_w_gate[i], ff_w_up[i], ff_w_down[i])
        else:
            y_ff = moe_swiglu(hn, moe_w_router[i], moe_b_router[i], moe_w_gate[i],
                              moe_w_up[i], moe_w_down[i])
        h = h + y_ff.astype(h.dtype)
    return h
```

```python
from contextlib import ExitStack
import numpy as np
import concourse.bass as bass
import concourse.mybir as mybir
from concourse.bass_utils import run_bass_kernel_spmd

F32 = mybir.dt.float32
BF16 = mybir.dt.bfloat16
AF = mybir.ActivationFunctionType
ALU = mybir.AluOpType
AX = mybir.AxisListType

ENGS = ("pe", "act", "dve", "pool", "sp")


def PK(*a, **k):
    return (a, k)


class Buf:
    __slots__ = ("name", "w", "rs", "excl")

    def __init__(self, name, excl=False):
        self.name = name
        self.excl = excl
        self.w = None
        self.rs = []


class Op:
    __slots__ = ("eng", "fn", "deps", "is_dma", "needed", "idx", "semval", "dsem")

    def __init__(self, eng, fn, is_dma):
        self.eng = eng
        self.fn = fn
        self.deps = set()
        self.is_dma = is_dma
        self.needed = False
        self.semval = None
        self.dsem = None


class Prog:
    def __init__(self, nc, n_dma_sems=24):
        self.nc = nc
        self.ops = []
        self.n_dma_sems = n_dma_sems
        self.out_dma_ops = []
        self.fence_idx = None
        self.last = {}
        self.dmas_since = []

    def op(self, eng, fn, pack=None, reads=(), writes=(), is_dma=False, is_out=False, after=()):
        if isinstance(fn, str):
            _name, _a, _k = fn, pack[0], pack[1]
            fn = lambda e: getattr(e, _name)(*_a, **_k)
        o = Op(eng, fn, is_dma)
        o.idx = len(self.ops)
        ops = self.ops
        ex = [b for b in reads if b.excl and b not in writes]
        if ex:
            reads = [b for b in reads if not b.excl]
            writes = list(writes) + ex

        def add(d, raw):
            p = ops[d]
            if not (p.is_dma or is_dma) and p.eng == eng:
                if eng == "pe":
                    return
            o.deps.add(d)
        for b in reads:
            if b.w is not None:
                add(b.w, True)
        for b in writes:
            if b.w is not None:
                add(b.w, False)
            for r in b.rs:
                add(r, False)
        if self.fence_idx is not None:
            o.deps.add(self.fence_idx)
        for x in after:
            o.deps.add(x.idx)
        for b in reads:
            b.rs.append(o.idx)
        for b in writes:
            b.w = o.idx
            b.rs = []
        if is_dma:
            self.dmas_since.append(o.idx)
        else:
            self.last[eng] = o.idx
        self.ops.append(o)
        if is_out:
            self.out_dma_ops.append(o.idx)
        return o

    def fence(self, dummy_ap):
        deps = set(self.last.values()) | set(self.dmas_since)
        o = self.op("dve", "memset", PK(dummy_ap, 0.0))
        o.deps |= deps
        self.fence_idx = o.idx
        self.dmas_since = []
        return o

    def emit(self, stack):
        nc = self.nc
        ops = self.ops
        for o in ops:
            best = {}
            keep = set()
            for d in o.deps:
                p = ops[d]
                if p.is_dma:
                    keep.add(d)
                else:
                    if p.eng not in best or d > best[p.eng]:
                        best[p.eng] = d
            for e, d in best.items():
                keep.add(d)
            o.deps = keep
            for d in keep:
                ops[d].needed = True
        for i in self.out_dma_ops:
            ops[i].needed = True
        sems = {e: stack.enter_context(nc.semaphore("s_" + e)) for e in ENGS if e != "sp"}
        dsems = [stack.enter_context(nc.semaphore("d%d" % i)) for i in range(self.n_dma_sems)]
        cnt = {e: 0 for e in sems}
        dcnt = [0] * len(dsems)
        rr = {"sw": 0, "hw": 0}
        nsw = len(dsems) // 2
        pools = {"sw": list(range(0, nsw)), "hw": list(range(nsw, len(dsems)))}
        lastd = {}
        per_eng = {e: [] for e in ENGS}
        for o in ops:
            if o.is_dma:
                o.needed = True
                kind = "sw" if o.eng == "pool" else "hw"
                pl = pools[kind]
                k = pl[rr[kind] % len(pl)]
                rr[kind] += 1
                prev = lastd.get(k)
                if prev is not None:
                    o.deps.add(prev)
                lastd[k] = o.idx
                dcnt[k] += 16
                o.dsem = dsems[k]
                o.semval = dcnt[k]
            elif o.needed:
                cnt[o.eng] += 1
                o.semval = cnt[o.eng]
            per_eng[o.eng].append(o)
        self.stats = {e: len(per_eng[e]) for e in ENGS}
        self.stats["sem_max"] = dict(cnt)
        block = stack.enter_context(nc.Block())

        def run(engname, eng):
            seen = {}
            for o in per_eng[engname]:
                for d in sorted(o.deps):
                    p = ops[d]
                    s = p.dsem if p.is_dma else sems[p.eng]
                    key = id(s)
                    if seen.get(key, -1) >= p.semval:
                        continue
                    seen[key] = p.semval
                    eng.wait_ge(s, p.semval)
                ins = o.fn(eng)
                if o.is_dma:
                    ins.then_inc(o.dsem, 16)
                elif o.needed:
                    ins.then_inc(sems[o.eng], 1)
            if engname == "sp":
                for i in self.out_dma_ops:
                    p = ops[i]
                    eng.wait_ge(p.dsem, p.semval)

        @block.tensor
        def _(e):
            run("pe", e)

        @block.scalar
        def _(e):
            run("act", e)

        @block.vector
        def _(e):
            run("dve", e)

        @block.gpsimd
        def _(e):
            run("pool", e)

        @block.sync
        def _(e):
            run("sp", e)


D = 1024
NTOK = 2048
TT = 512
NTT = NTOK // TT
EPS = 1e-6


class Ctx:
    def __init__(self, nc, st, P):
        self.nc, self.st, self.P = nc, st, P
        self.ps = []
        for i in range(8):
            t = st.enter_context(nc.psum_tensor("ps%d" % i, [128, 512], F32))
            self.ps.append((t, Buf("ps%d" % i, excl=True)))
        self.psi = 0
        self.epsc, self.epsb = self.sb("epsc", [128, 2], F32)
        P.op("dve", "memset", PK(self.epsc[:, 0:1], EPS), writes=[self.epsb])
        P.op("dve", "memset", PK(self.epsc[:, 1:2], 64e-5), writes=[self.epsb])
        self.wbufs = []
        self.wi = 0
        self.dmaq = 0

    def sb(self, name, shape, dt):
        t = self.st.enter_context(self.nc.sbuf_tensor(name, shape, dt))
        return t, Buf(name)

    def psum(self):
        r = self.ps[self.psi % 8]
        self.psi += 1
        return r

    def init_w(self, n, cols):
        for i in range(n):
            self.wbufs.append(self.sb("wb%d" % i, [128, cols], BF16))

    def wbuf(self):
        r = self.wbufs[self.wi % len(self.wbufs)]
        self.wi += 1
        return r

    def load_w(self, src_ap, kc, ncols):
        t, b = self.wbuf()
        view = t[:, 0:kc * ncols].rearrange("p (c n) -> p c n", c=kc)
        src = src_ap.rearrange("(c p) n -> p c n", p=128)
        self.P.op("pool", "dma_start", PK(out=view, in_=src), writes=[b], is_dma=True)
        return view, b

    def dma(self, out, in_, R=(), W=(), is_out=False, q=None):
        if q is None:
            q = "sp"
        return self.P.op(q, "dma_start", PK(out=out, in_=in_), reads=R, writes=W, is_dma=True, is_out=is_out)


def rmsnorm_fm(C, xT, xb, gcol, outT, outb, tt, scr, dim_chunks=8, extra_scale=1.0):
    P = C.P
    sq, sqb = scr["sq"]
    rs, rsb = scr["rs"]
    ones, onesb = scr["ones"]
    sl = slice(tt * TT, (tt + 1) * TT)
    pt, pb = C.psum()
    for c in range(dim_chunks):
        P.op("act", "activation", PK(out=sq[:, c, :], in_=xT[:, c, sl], func=AF.Square), reads=[xb], writes=[sqb])
    for c in range(dim_chunks):
        P.op("pe", "matmul", PK(pt[:, :], ones[:, :], sq[:, c, :], start=(c == 0), stop=(c == dim_chunks - 1)),
             reads=[sqb, onesb], writes=[pb])
    dd = dim_chunks * 128
    P.op("dve", "tensor_scalar", PK(out=rs[:, :], in0=pt[:, :], scalar1=float(dd * EPS), scalar2=-0.5, op0=ALU.add, op1=ALU.pow),
         reads=[pb], writes=[rsb])
    sc = float(np.sqrt(dd) * extra_scale)
    for c in range(dim_chunks):
        eng = "dve"
        P.op(eng, "scalar_tensor_tensor", PK(out=outT[:, c, sl], in0=xT[:, c, sl], scalar=gcol[:, c:c + 1], in1=rs[:, :],
                                                       op0=ALU.mult, op1=ALU.mult),
             reads=[xb, rsb], writes=[outb])
    return sc


def linear_fm(C, W_dram, kc, xin, xinb, n_out, evac, tts=range(NTT), col0=0, blk=512, mcols=128, xinbs=None):
    P = C.P
    nblk = (n_out + blk - 1) // blk
    oc = 0
    for bi in range(nblk):
        c0 = col0 + bi * blk
        w = min(blk, n_out - bi * blk)
        wv, wb = C.load_w(W_dram[:, c0:c0 + w], kc, w)
        for j in range(0, w, mcols):
            m = min(mcols, w - j)
            for tt in tts:
                pt, pb = C.psum()
                sl = slice(tt * TT, (tt + 1) * TT)
                for k in range(kc):
                    P.op("pe", "matmul", PK(pt[0:m, :], wv[:, k, j:j + m], xin[:, k, sl],
                                                                              start=(k == 0), stop=(k == kc - 1)),
                         reads=[wb, (xinbs[tt] if xinbs is not None else xinb)], writes=[pb])
                evac(oc, tt, pt, pb, m)
            oc += 1


def rstd_from(C, rs_ap, ps_ap, pb, rsb, eps=EPS):
    np_ = rs_ap.shape[0]
    C.P.op("act", "activation", PK(out=rs_ap, in_=ps_ap, func=AF.Sqrt, bias=C.epsc[0:np_, 0:1] if eps == EPS else C.epsc[0:np_, 1:2]),
           reads=[pb, C.epsb], writes=[rsb])
    C.P.op("dve", "reciprocal", PK(out=rs_ap, in_=rs_ap), reads=[rsb], writes=[rsb])


def norm_fm(C, xin, xb, n, dim_chunks, onesm, onesb, gcol, out, outb, scr, sq_from_psum=False):
    P = C.P
    sq, sqb = scr["sq"]
    rs, rsb = scr["rs"]
    pt, pb = C.psum()
    for c in range(dim_chunks):
        P.op("act", "activation", PK(out=sq[:, c, 0:n], in_=xin(c), func=AF.Square), reads=xb, writes=[sqb])
    for c in range(dim_chunks):
        P.op("pe", "matmul", PK(pt[:, 0:n], onesm, sq[:, c, 0:n], start=(c == 0), stop=(c == dim_chunks - 1)),
             reads=[sqb, onesb], writes=[pb])
    rstd_from(C, rs[:, 0:n], pt[:, 0:n], pb, rsb)
    for c in range(dim_chunks):
        eng = "dve"
        P.op(eng, "scalar_tensor_tensor", PK(out=out(c), in0=xin(c), scalar=gcol(c), in1=rs[:, 0:n],
                                                       op0=ALU.mult, op1=ALU.mult),
             reads=list(xb) + [rsb] + ([C.vb] if getattr(C, 'vb', None) is not None else []), writes=outb)


def mem_kv(C, memT_d, wkv_d, V, vb, col_nm, col_kg, cst, cstb, scr, name):
    P = C.P
    mT, mTb = C.sb(name + "_mT", [128, 8, 256], BF16)
    mn, mnb = C.sb(name + "_mn", [128, 8, 256], BF16)
    kf, kfb = C.sb(name + "_kf", [128, 8, 256], BF16)
    kn, knb = C.sb(name + "_kn", [128, 8, 256], BF16)
    vt, vtb = C.sb(name + "_vt", [128, 2, 1024], BF16)
    P.op("pool", "dma_start", PK(out=mT[:, :, :], in_=memT_d.rearrange("(c p) n -> p c n", p=128)), writes=[mTb], is_dma=True)
    norm_fm(C, lambda c: mT[:, c, :], [mTb], 256, 8, cst[:, 0, :], cstb, lambda c: V[:, col_nm + c:col_nm + c + 1],
            lambda c: mn[:, c, :], [mnb], scr)
    for half in range(2):
        wv, wb = C.load_w(wkv_d[:, half * 512:(half + 1) * 512], 8, 512)
        for j in range(4):
            oc = half * 4 + j
            pt, pb = C.psum()
            for k in range(8):
                P.op("pe", "matmul", PK(pt[:, 0:256], wv[:, k, j * 128:(j + 1) * 128], mn[:, k, :],
                                                                      start=(k == 0), stop=(k == 7)), reads=[wb, mnb], writes=[pb])
            P.op("dve", "tensor_copy", PK(out=kf[:, oc, :], in_=pt[:, 0:256]), reads=[pb], writes=[kfb])
    for hh in range(4):
        norm_fm(C, lambda c, hh=hh: kf[:, hh * 2 + c, :], [kfb], 256, 2, cst[:, 1, :], cstb,
                lambda c: V[:, col_kg + c:col_kg + c + 1], lambda c, hh=hh: kn[:, hh * 2 + c, :], [knb], scr)
    for half in range(2):
        wv, wb = C.load_w(wkv_d[:, 1024 + half * 512:1024 + (half + 1) * 512], 8, 512)
        for j in range(2):
            pt, pb = C.psum()
            for k in range(8):
                P.op("pe", "matmul", PK(pt[:, :], mn[:, k, j * 128:(j + 1) * 128], wv[:, k, :],
                                                                      start=(k == 0), stop=(k == 7)), reads=[wb, mnb], writes=[pb])
            P.op("act", "activation", PK(out=vt[:, j, half * 512:(half + 1) * 512], in_=pt[:, :], func=AF.Copy),
                 reads=[pb], writes=[vtb])
    return (kn, knb), (vt, vtb)


def mem_xattn(C, hT, hTb, bufA, bufAb, bufQ, bufQb, V, vb, col_nx, col_qg, wq_d, wo_d, kn, knb, vt, vtb, cst, cstb, scr, stop=0):
    P = C.P
    for tt in range(NTT):
        sl = slice(tt * TT, (tt + 1) * TT)
        norm_fm(C, lambda c: hT[:, c, sl], [hTb[tt]], TT, 8, cst[:, 0, :], cstb, lambda c: V[:, col_nx + c:col_nx + c + 1],
                lambda c: bufA[:, c, sl], [bufAb[tt]], scr)
    if stop == 16:
        return
    sqq, sqqb = scr["sqq"]
    rs, rsb = scr["rs"]

    def evac_q(oc, tt, pt, pb, m):
        sl = slice(tt * TT, (tt + 1) * TT)
        P.op("act", "activation", PK(out=sqq[:, oc % 2, tt, :], in_=pt[:, :], func=AF.Square), reads=[pb], writes=[sqqb[tt]])
        P.op("dve", "tensor_copy", PK(out=bufQ[:, oc, sl], in_=pt[:, :]), reads=[pb], writes=[bufQb[tt]])
        import os
        V17 = os.environ.get('V17', '')
        if oc % 2 == 1 and V17 != 'a':
            p2, p2b = C.psum()
            for c in range(2):
                P.op("pe", "matmul", PK(p2[:, :], cst[:, 1, :], sqq[:, c, tt, :], start=(c == 0), stop=(c == 1)),
                     reads=[sqqb[tt], cstb], writes=[p2b])
            rstd_from(C, rs[:, :], p2[:, :], p2b, rsb)
            for c in range(2):
                o2 = oc - 1 + c
                P.op("dve", "tensor_tensor", PK(out=bufQ[:, o2, sl], in0=bufQ[:, o2, sl], in1=rs[:, :], op=ALU.mult),
                     reads=[bufQb[tt], rsb], writes=[bufQb[tt]])
                P.op("act", "activation", PK(out=bufQ[:, o2, sl], in_=bufQ[:, o2, sl], func=AF.Copy, scale=V[:, col_qg + c:col_qg + c + 1]),
                     reads=[bufQb[tt], vb], writes=[bufQb[tt]])
    linear_fm(C, wq_d, 8, bufA, None, 1024, evac_q, xinbs=bufAb)
    if stop == 17:
        return
    et, etb = scr["et"]
    rd, rdb = scr["rd"]
    for hh in range(4):
        for tt in range(NTT):
            sl = slice(tt * TT, (tt + 1) * TT)
            pden, pdenb = C.psum()
            pn = [C.psum(), C.psum()]
            for j in range(2):
                psc, pscb = C.psum()
                for dc in range(2):
                    P.op("pe", "matmul", PK(psc[:, :], kn[:, hh * 2 + dc, j * 128:(j + 1) * 128], bufQ[:, hh * 2 + dc, sl],
                                                                        start=(dc == 0), stop=(dc == 1)), reads=[knb, bufQb[tt]], writes=[pscb])
                P.op("act", "activation", PK(out=et[:, j, :], in_=psc[:, :], func=AF.Exp, scale=1.0 / 16.0),
                     reads=[pscb], writes=[etb[j]])
                P.op("pe", "matmul", PK(pden[:, :], cst[:, 4, :], et[:, j, :], start=(j == 0), stop=(j == 1)),
                     reads=[etb[j], cstb], writes=[pdenb])
                for c in range(2):
                    P.op("pe", "matmul", PK(pn[c][0][:, :], vt[:, j, hh * 256 + c * 128:hh * 256 + (c + 1) * 128], et[:, j, :],
                                                            start=(j == 0), stop=(j == 1)), reads=[etb[j], vtb], writes=[pn[c][1]])
            P.op("dve", "reciprocal", PK(out=rd[:, :], in_=pden[:, :]), reads=[pdenb], writes=[rdb])
            for c in range(2):
                P.op("dve", "tensor_tensor", PK(out=bufA[:, hh * 2 + c, sl], in0=pn[c][0][:, :], in1=rd[:, :], op=ALU.mult),
                     reads=[pn[c][1], rdb], writes=[bufAb[tt]])

    if stop == 18:
        return

    def evac_o(oc, tt, pt, pb, m):
        sl = slice(tt * TT, (tt + 1) * TT)
        P.op("dve", "tensor_tensor", PK(out=hT[:, oc, sl], in0=pt[:, :], in1=hT[:, oc, sl], op=ALU.add), reads=[pb, hTb[tt]], writes=[hTb[tt]])
    linear_fm(C, wo_d, 8, bufA, None, 1024, evac_o, xinbs=bufAb)


def swiglu_ffn(C, hT, hTb, bufA, bufAb, bufH, bufHb, V, vb, col_nf, wg_d, wu_d, wd_d, dff, cst, cstb, scr, gate_bc=None):
    P = C.P
    if col_nf is not None:
        for tt in range(NTT):
            sl = slice(tt * TT, (tt + 1) * TT)
            norm_fm(C, lambda c: hT[:, c, sl], [hTb[tt]], TT, 8, cst[:, 0, :], cstb, lambda c: V[:, col_nf + c:col_nf + c + 1],
                    lambda c: bufA[:, c, sl], [bufAb[tt]], scr)
    sg, sgb = scr["sg"]
    nch = dff // 128
    GRP = bufH.shape[1]
    g0 = 0
    while g0 < nch:
        gn = min(GRP, nch - g0)
        c = 0
        while c < gn:
            cb = min(4, gn - c)
            col = (g0 + c) * 128
            wgv, wgb = C.load_w(wg_d[:, col:col + cb * 128], 8, cb * 128)
            wuv, wub = C.load_w(wu_d[:, col:col + cb * 128], 8, cb * 128)
            for j in range(cb):
                for tt in range(NTT):
                    sl = slice(tt * TT, (tt + 1) * TT)
                    pg, pgb = C.psum()
                    pu, pub = C.psum()
                    for k in range(8):
                        P.op("pe", "matmul", PK(pg[:, :], wgv[:, k, j * 128:(j + 1) * 128], bufA[:, k, sl],
                                                                                       start=(k == 0), stop=(k == 7)), reads=[wgb, bufAb[tt]], writes=[pgb])
                    for k in range(8):
                        P.op("pe", "matmul", PK(pu[:, :], wuv[:, k, j * 128:(j + 1) * 128], bufA[:, k, sl],
                                                                                       start=(k == 0), stop=(k == 7)), reads=[wub, bufAb[tt]], writes=[pub])
                    P.op("act", "activation", PK(out=sg[:, :], in_=pg[:, :], func=AF.Silu), reads=[pgb], writes=[sgb])
                    if gate_bc is not None:
                        gt, gtb = gate_bc
                        P.op("dve", "tensor_tensor", PK(out=sg[:, :], in0=sg[:, :], in1=gt[:, sl], op=ALU.mult),
                             reads=[sgb, gtb[tt]], writes=[sgb])
                    P.op("dve", "tensor_tensor", PK(out=bufH[:, c + j, sl], in0=pu[:, :], in1=sg[:, :], op=ALU.mult),
                         reads=[pub, sgb], writes=[bufHb[tt]])
            c += cb
        wblks = []
        r = 0
        while r < gn:
            rb = min(4, gn - r)
            row = (g0 + r) * 128
            wblks.append((r, rb, C.load_w(wd_d[row:row + rb * 128, :], rb, 1024)))
            r += rb
        for oc in range(8):
            for tt in range(NTT):
                sl = slice(tt * TT, (tt + 1) * TT)
                pt, pb = C.psum()
                first = True
                for (r, rb, (wv, wb)) in wblks:
                    for q in range(rb):
                        last = (r + q == gn - 1)
                        P.op("pe", "matmul", PK(pt[:, :], wv[:, q, oc * 128:(oc + 1) * 128], bufH[:, r + q, sl], start=first, stop=last),
                             reads=[wb, bufHb[tt]], writes=[pb])
                        first = False
                P.op("dve", "tensor_tensor", PK(out=hT[:, oc, sl], in0=pt[:, :], in1=hT[:, oc, sl], op=ALU.add),
                     reads=[pb, hTb[tt]], writes=[hTb[tt]])
        g0 += gn


VB = dict(b_glu=0, n_xattn=4, n_mem=12, q_gain=20, k_gain=22, n_ffn=24, n_mix1=32, da_qg=40, da_kg=41)
NVB = 42


def build_B():
    nc = bass.Bass("TRN2", target_bir_lowering=False)
    dr = lambda n, s, k="ExternalInput", dt=F32: nc.dram_tensor(n, s, dt, kind=k).ap()
    xT_d = dr("xT", [D, NTOK]); ymT_d = dr("ymT", [D, NTOK]); memT_d = dr("memT", [D, 256])
    vec_d = dr("vecs", [128, NVB]); cst_d = dr("cst", [128, 5 * 128])
    wglu_d = dr("w_glu", [512, 512]); wout_d = dr("w_out", [D, D])
    wq_d = dr("w_q", [D, D]); wkv_d = dr("w_kv", [D, 2 * D]); wo_d = dr("w_o", [D, D])
    wg_d = dr("ff_g", [D, 2816]); wu_d = dr("ff_u", [D, 2816]); wd_d = dr("ff_d", [2816, D])
    wqkv_d = dr("w_qkv", [D, 3 * D])
    h1T_d = dr("h1T", [D, NTOK], "ExternalOutput")
    qT_d = dr("qT", [16, 64, NTOK], "ExternalOutput")
    kT_d = dr("kT", [16, 64, NTOK], "ExternalOutput")
    vT_d = dr("vT", [D, NTOK], "ExternalOutput")
    with ExitStack() as st:
        P = Prog(nc)
        C = Ctx(nc, st, P)
        C.init_w(4, 4096)
        hT, _ = C.sb("hT", [128, 8, NTOK], F32); hTb = [Buf("hT%d" % i) for i in range(NTT)]
        bufA, _ = C.sb("bufA", [128, 8, NTOK], BF16); bufAb = [Buf("bA%d" % i) for i in range(NTT)]
        bufH, _ = C.sb("bufH", [128, 8, NTOK], BF16); bufHb = [Buf("bH%d" % i) for i in range(NTT)]
        V, vb = C.sb("V_sb", [128, NVB], F32); C.vb = vb
        cst3, cstb = C.sb("cst_sb", [128, 5, 128], BF16)
        scr = dict(sq=C.sb("sq", [128, 8, 512], BF16), rs=C.sb("rs", [128, 512], F32), sg=C.sb("sg", [128, 512], F32),
                   rd=C.sb("rd", [128, 512], F32))
        sqq, _ = C.sb("sqq", [128, 2, NTT, 512], BF16)
        scr["sqq"] = (sqq, [Buf("sqq%d" % i) for i in range(NTT)])
        et, _ = C.sb("et", [128, 2, 512], BF16)
        scr["et"] = (et, [Buf("et0"), Buf("et1")])
        C.dma(V[:, :], vec_d[:, :], W=[vb])
        P.op("pool", "dma_start", PK(out=cst3[:, :, :], in_=cst_d.rearrange("p (a b) -> p a b", a=5)), writes=[cstb], is_dma=True)
        for tt in range(NTT):
            sl = slice(tt * TT, (tt + 1) * TT)
            C.dma(hT[:, :, sl], xT_d[:, sl].rearrange("(c p) n -> p c n", p=128), W=[hTb[tt]])
            P.op("pool", "dma_start", PK(out=bufA[:, :, sl], in_=ymT_d[:, sl].rearrange("(c p) n -> p c n", p=128)),
                 writes=[bufAb[tt]], is_dma=True)
        import os
        STAGE = int(os.environ.get("STAGE", "9"))
        def finish():
            for tt in range(NTT):
                sl = slice(tt * TT, (tt + 1) * TT)
                C.dma(h1T_d[:, sl].rearrange("(c p) n -> p c n", p=128), hT[:, :, sl], R=[hTb[tt]], is_out=True)
            P.emit(st)
            print("B stats", P.stats)
            return nc
        if STAGE == 0:
            return finish()
        wv, wb = C.load_w(wglu_d[:, :], 4, 512)
        sg, sgb = scr["sg"]
        for tt in range(NTT):
            sl = slice(tt * TT, (tt + 1) * TT)
            pts = []
            for oc in range(4):
                pt, pb = C.psum()
                for k in range(4):
                    P.op("pe", "matmul", PK(pt[:, :], wv[:, k, oc * 128:(oc + 1) * 128], bufA[:, 4 + k, sl],
                                                                            start=(k == 0), stop=(k == 3)), reads=[wb, bufAb[tt]], writes=[pb])
                pts.append((pt, pb))
            for oc in range(4):
                pt, pb = pts[oc]
                P.op("act", "activation", PK(out=sg[:, :], in_=pt[:, :], func=AF.Sigmoid,
                                                                bias=V[:, VB["b_glu"] + oc:VB["b_glu"] + oc + 1]), reads=[pb, vb], writes=[sgb])
                P.op("dve", "tensor_tensor", PK(out=bufA[:, 4 + oc, sl], in0=bufA[:, 4 + oc, sl], in1=sg[:, :], op=ALU.mult),
                     reads=[sgb, bufAb[tt]], writes=[bufAb[tt]])

        def evac_res(oc, tt, pt, pb, m):
            sl = slice(tt * TT, (tt + 1) * TT)
            P.op("dve", "tensor_tensor", PK(out=hT[:, oc, sl], in0=pt[:, :], in1=hT[:, oc, sl], op=ALU.add), reads=[pb, hTb[tt]], writes=[hTb[tt]])
        linear_fm(C, wout_d, 8, bufA, None, 1024, evac_res, xinbs=bufAb)
        import os
        STAGE = int(os.environ.get("STAGE", "9"))
        def finish():
            for tt in range(NTT):
                sl = slice(tt * TT, (tt + 1) * TT)
                C.dma(h1T_d[:, sl].rearrange("(c p) n -> p c n", p=128), hT[:, :, sl], R=[hTb[tt]], is_out=True)
            P.emit(st)
            return nc
        if STAGE == 1:
            return finish()
        (kn, knb), (vt, vtb) = mem_kv(C, memT_d, wkv_d, V, vb, VB["n_mem"], VB["k_gain"], cst3, cstb, scr, "m0")
        if STAGE == 15:
            return finish()
        mem_xattn(C, hT, hTb, bufA, bufAb, bufH, bufHb, V, vb, VB["n_xattn"], VB["q_gain"], wq_d, wo_d, kn, knb, vt, vtb, cst3, cstb, scr, stop=STAGE)
        if STAGE in (2, 16, 17, 18):
            return finish()
        swiglu_ffn(C, hT, hTb, bufA, bufAb, bufH, bufHb, V, vb, VB["n_ffn"], wg_d, wu_d, wd_d, 2816, cst3, cstb, scr)
        for tt in range(NTT):
            sl = slice(tt * TT, (tt + 1) * TT)
            C.dma(h1T_d[:, sl].rearrange("(c p) n -> p c n", p=128), hT[:, :, sl], R=[hTb[tt]], is_out=True)
        for tt in range(NTT):
            sl = slice(tt * TT, (tt + 1) * TT)
            norm_fm(C, lambda c: hT[:, c, sl], [hTb[tt]], TT, 8, cst3[:, 0, :], cstb, lambda c: V[:, VB["n_mix1"] + c:VB["n_mix1"] + c + 1],
                    lambda c: bufA[:, c, sl], [bufAb[tt]], scr)
        qs, qsb = scr["sg"]
        qo, qob = scr["rd"]
        sq1, sq1b = C.sb("sq1", [64, 512], BF16)
        rs, rsb = scr["rs"]
        for which, (dst, gcol, scl) in enumerate(((qT_d, VB["da_qg"], 0.125), (kT_d, VB["da_kg"], 1.0))):
            def evac_qk(oc, tt, pt, pb, m, dst=dst, gcol=gcol, scl=scl):
                sl = slice(tt * TT, (tt + 1) * TT)
                P.op("act", "activation", PK(out=sq1[:, :], in_=pt[0:64, :], func=AF.Square), reads=[pb], writes=[sq1b])
                p2, p2b = C.psum()
                P.op("pe", "matmul", PK(p2[0:64, :], cst3[0:64, 2, 0:64], sq1[:, :], start=True, stop=True), reads=[sq1b, cstb], writes=[p2b])
                rstd_from(C, rs[0:64, :], p2[0:64, :], p2b, rsb)
                P.op("dve", "scalar_tensor_tensor", PK(out=qs[0:64, :], in0=pt[0:64, :], scalar=V[0:64, gcol:gcol + 1], in1=rs[0:64, :],
                                                             op0=ALU.mult, op1=ALU.mult), reads=[pb, rsb, vb], writes=[qsb])
                P.op("act", "activation", PK(out=qo[0:64, :], in_=qs[0:64, :], func=AF.Copy, scale=float(scl)), reads=[qsb], writes=[qob])
                C.dma(dst[oc, :, sl], qo[0:64, :], R=[qob], is_out=True)
            linear_fm(C, wqkv_d, 8, bufA, None, 1024, evac_qk, col0=which * 1024, mcols=64, xinbs=bufAb)
        vo, vob = scr["sg"]

        def evac_v(oc, tt, pt, pb, m):
            sl = slice(tt * TT, (tt + 1) * TT)
            P.op("act", "activation", PK(out=vo[:, :], in_=pt[:, :], func=AF.Copy), reads=[pb], writes=[vob])
            C.dma(vT_d[oc * 128:(oc + 1) * 128, sl], vo[:, :], R=[vob], is_out=True)
        linear_fm(C, wqkv_d, 8, bufA, None, 1024, evac_v, col0=2048, xinbs=bufAb)
        P.emit(st)
        print("B stats", P.stats)
    return nc


def cst_table():
    c = np.zeros((128, 5, 128), np.float32)
    c[:, 0, :] = 1.0 / 1024
    c[:, 1, :] = 1.0 / 256
    c[0:64, 2, 0:64] = 1.0 / 64
    c[64:128, 2, 64:128] = 1.0 / 64
    c[:, 3, :] = 1.0 / 128
    c[:, 4, :] = 1.0
    return c.reshape(128, 640)


def col(v):
    v = np.asarray(v, np.float32).reshape(-1)
    if v.size < 128:
        o = np.zeros((128, 1), np.float32); o[:v.size, 0] = v
        return o
    return np.ascontiguousarray(v.reshape(-1, 128).T)


def vecs_B(inp):
    V = np.zeros((128, NVB), np.float32)
    def put(name, arr):
        a = col(arr); V[:, VB[name]:VB[name] + a.shape[1]] = a
    put("b_glu", inp["s5_b_glu"][0]); put("n_xattn", inp["norm_xattn"][0]); put("n_mem", inp["norm_mem"][0])
    put("q_gain", inp["xa_q_gain"][0]); put("k_gain", inp["xa_k_gain"][0]); put("n_ffn", inp["norm_ffn"][0])
    put("n_mix1", inp["norm_mix"][1])
    put("da_qg", np.tile(np.asarray(inp["da_q_gain"][0]), 2)); put("da_kg", np.tile(np.asarray(inp["da_k_gain"][0]), 2))
    return V


def tok_index(p):
    return np.concatenate([np.arange((2 * m + p) * 512, (2 * m + p + 1) * 512) for m in range(4)])


VC = dict(n_xattn=0, n_mem=8, q_gain=16, k_gain=18, n_ffn=20, b_router=28)
NVC = 36


def build_C2():
    nc = bass.Bass("TRN2", target_bir_lowering=False)
    dr = lambda n, s, k="ExternalInput", dt=F32: nc.dram_tensor(n, s, dt, kind=k).ap()
    h1T_d = dr("h1T", [D, NTOK]); atT_d = dr("attnT", [D, NTOK]); memT_d = dr("memT", [D, 256])
    vec_d = dr("vecs", [128, NVC]); cst_d = dr("cst", [128, 5 * 128]); c32_d = dr("c32", [128, 256])
    wdo_d = dr("da_w_o", [D, D])
    wq_d = dr("w_q", [D, D]); wkv_d = dr("w_kv", [D, 2 * D]); wo_d = dr("w_o", [D, D])
    wr_d = dr("w_router", [D, 8])
    wg_d = dr("moe_g", [8, D, 3584]); wu_d = dr("moe_u", [8, D, 3584]); wd_d = dr("moe_d", [8, 3584, D])
    outT_d = dr("outT", [D, NTOK], "ExternalOutput")
    with ExitStack() as st:
        P = Prog(nc)
        C = Ctx(nc, st, P)
        C.init_w(3, 4096)
        hT, _ = C.sb("hT", [128, 8, NTOK], F32); hTb = [Buf("hT%d" % i) for i in range(NTT)]
        bufA, _ = C.sb("bufA", [128, 8, NTOK], BF16); bufAb = [Buf("bA%d" % i) for i in range(NTT)]
        bufH, _ = C.sb("bufH", [128, 8, NTOK], BF16); bufHb = [Buf("bH%d" % i) for i in range(NTT)]
        V, vb = C.sb("V_sb", [128, NVC], F32); C.vb = vb
        cst3, cstb = C.sb("cst_sb", [128, 5, 128], BF16)
        c32, c32b = C.sb("c32_sb", [128, 2, 128], F32)
        scr = dict(sq=C.sb("sq", [128, 8, 512], BF16), rs=C.sb("rs", [128, 512], F32), sg=C.sb("sg", [128, 512], F32),
                   rd=C.sb("rd", [128, 512], F32))
        sqq, _ = C.sb("sqq", [128, 2, NTT, 512], BF16)
        scr["sqq"] = (sqq, [Buf("sqq%d" % i) for i in range(NTT)])
        et, _ = C.sb("et", [128, 2, 512], BF16)
        scr["et"] = (et, [Buf("et0"), Buf("et1")])
        C.dma(V[:, :], vec_d[:, :], W=[vb])
        C.dma(c32[:, :, :], c32_d.rearrange("p (a b) -> p a b", a=2), W=[c32b])
        P.op("pool", "dma_start", PK(out=cst3[:, :, :], in_=cst_d.rearrange("p (a b) -> p a b", a=5)), writes=[cstb], is_dma=True)
        for tt in range(NTT):
            sl = slice(tt * TT, (tt + 1) * TT)
            C.dma(hT[:, :, sl], h1T_d[:, sl].rearrange("(c p) n -> p c n", p=128), W=[hTb[tt]])
            P.op("pool", "dma_start", PK(out=bufA[:, :, sl], in_=atT_d[:, sl].rearrange("(c p) n -> p c n", p=128)),
                 writes=[bufAb[tt]], is_dma=True)

        def evac_res(oc, tt, pt, pb, m):
            sl = slice(tt * TT, (tt + 1) * TT)
            P.op("dve", "tensor_tensor", PK(out=hT[:, oc, sl], in0=pt[:, :], in1=hT[:, oc, sl], op=ALU.add), reads=[pb, hTb[tt]], writes=[hTb[tt]])
        linear_fm(C, wdo_d, 8, bufA, None, 1024, evac_res, xinbs=bufAb)
        (kn, knb), (vt, vtb) = mem_kv(C, memT_d, wkv_d, V, vb, VC["n_mem"], VC["k_gain"], cst3, cstb, scr, "m1")
        mem_xattn(C, hT, hTb, bufA, bufAb, bufH, bufHb, V, vb, VC["n_xattn"], VC["q_gain"], wq_d, wo_d, kn, knb, vt, vtb, cst3, cstb, scr)
        for tt in range(NTT):
            sl = slice(tt * TT, (tt + 1) * TT)
            norm_fm(C, lambda c: hT[:, c, sl], [hTb[tt]], TT, 8, cst3[:, 0, :], cstb, lambda c: V[:, VC["n_ffn"] + c:VC["n_ffn"] + c + 1],
                    lambda c: bufA[:, c, sl], [bufAb[tt]], scr)
        hn32, hn32b = C.sb("hn32", [128, 8, 128], F32)
        wr, wrb = C.sb("wr", [128, 8, 8], F32)
        G, Gb = C.sb("G", [128, 16, 8], F32)
        lg, lgb = C.sb("lg", [128, 8], F32)
        mx, mxb = C.sb("mx", [128, 8], F32)
        sm, smb = C.sb("sm", [128, 4], F32)
        C.dma(wr[:, :, :], wr_d.rearrange("(c p) n -> p c n", p=128), W=[wrb])
        for s in range(16):
            tt = s // 4
            sl = slice(s * 128, (s + 1) * 128)
            norm_fm(C, lambda c: hT[:, c, sl], [hTb[tt]], 128, 8, cst3[:, 0, :], cstb, lambda c: V[:, VC["n_ffn"] + c:VC["n_ffn"] + c + 1],
                    lambda c: hn32[:, c, :], [hn32b], scr)
            pt, pb = C.psum()
            for k in range(8):
                P.op("pe", "matmul", PK(pt[:, 0:8], hn32[:, k, :], wr[:, k, :], start=(k == 0), stop=(k == 7)), reads=[hn32b, wrb], writes=[pb])
            P.op("dve", "tensor_tensor", PK(out=lg[:, :], in0=pt[:, 0:8], in1=V[:, VC["b_router"]:VC["b_router"] + 8], op=ALU.add),
                 reads=[pb, vb], writes=[lgb])
            P.op("dve", "max", PK(out=mx[:, :], in_=lg[:, :]), reads=[lgb], writes=[mxb])
            P.op("dve", "tensor_scalar", PK(out=sm[:, 0:1], in0=mx[:, 0:1], scalar1=-1.0, scalar2=None, op0=ALU.mult), reads=[mxb], writes=[smb])
            ex, exb = scr["sg"]
            P.op("act", "activation", PK(out=ex[:, 0:8], in_=lg[:, :], func=AF.Exp, bias=sm[:, 0:1]), reads=[lgb, smb], writes=[exb])
            P.op("dve", "tensor_scalar", PK(out=lg[:, :], in0=lg[:, :], scalar1=mx[:, 1:2], scalar2=None, op0=ALU.is_ge), reads=[lgb, mxb], writes=[lgb])
            P.op("dve", "tensor_tensor", PK(out=ex[:, 0:8], in0=ex[:, 0:8], in1=lg[:, :], op=ALU.mult), reads=[exb, lgb], writes=[exb])
            P.op("dve", "reduce_sum", PK(out=sm[:, 1:2], in_=ex[:, 0:8], axis=AX.X), reads=[exb], writes=[smb])
            P.op("dve", "reciprocal", PK(out=sm[:, 2:3], in_=sm[:, 1:2]), reads=[smb], writes=[smb])
            P.op("dve", "tensor_scalar", PK(out=G[:, s, :], in0=ex[:, 0:8], scalar1=sm[:, 2:3], scalar2=None, op0=ALU.mult), reads=[exb, smb], writes=[Gb])
        gbc, _ = C.sb("gbc", [128, NTOK], BF16); gbcb = [Buf("gbc%d" % i) for i in range(NTT)]
        dg, dgb = C.sb("dg", [128, 128], F32)
        for e_ in range(8):
            for tt in range(NTT):
                pt, pb = C.psum()
                for q in range(4):
                    s = tt * 4 + q
                    P.op("dve", "tensor_scalar", PK(out=dg[:, :], in0=c32[:, 0, :], scalar1=G[:, s, e_:e_ + 1], scalar2=None, op0=ALU.mult),
                         reads=[c32b, Gb], writes=[dgb])
                    P.op("pe", "matmul", PK(pt[:, q * 128:(q + 1) * 128], c32[:, 1, :], dg[:, :], start=True, stop=True), reads=[dgb, c32b], writes=[pb])
                P.op("act", "activation", PK(out=gbc[:, tt * TT:(tt + 1) * TT], in_=pt[:, :], func=AF.Copy), reads=[pb], writes=[gbcb[tt]])
            swiglu_ffn(C, hT, hTb, bufA, bufAb, bufH, bufHb, V, vb, None, wg_d[e_], wu_d[e_], wd_d[e_], 3584, cst3, cstb, scr, gate_bc=(gbc, gbcb))
        for tt in range(NTT):
            sl = slice(tt * TT, (tt + 1) * TT)
            C.dma(outT_d[:, sl].rearrange("(c p) n -> p c n", p=128), hT[:, :, sl], R=[hTb[tt]], is_out=True)
        P.emit(st)
    return nc


def c32_table():
    c = np.zeros((128, 2, 128), np.float32)
    c[:, 0, :] = np.eye(128, dtype=np.float32)
    c[:, 1, :] = 1.0
    return c.reshape(128, 256)


def vecs_C(inp):
    V = np.zeros((128, NVC), np.float32)
    def put(name, arr):
        a = col(arr); V[:, VC[name]:VC[name] + a.shape[1]] = a
    put("n_xattn", inp["norm_xattn"][1]); put("n_mem", inp["norm_mem"][1])
    put("q_gain", inp["xa_q_gain"][1]); put("k_gain", inp["xa_k_gain"][1]); put("n_ffn", inp["norm_ffn"][1])
    V[:, VC["b_router"]:VC["b_router"] + 8] = np.asarray(inp["moe_b_router"][0], np.float32)[None, :]
    return V


LAMBDA_INIT = 0.8 - 0.6 * float(np.exp(-0.3 * 1))


def build_C1():
    nc = bass.Bass("TRN2", target_bir_lowering=False)
    dr = lambda n, s, k="ExternalInput", dt=F32: nc.dram_tensor(n, s, dt, kind=k).ap()
    qa_d = dr("qaug", [16, 68, NTOK]); ka_d = dr("kaug", [16, 68, 4096]); vf_d = dr("vfull", [4096, D])
    bias_d = dr("biasT", [128, 8 * 512]); lamb_d = dr("lamb", [128, 4 * 64]); sgc_d = dr("sgc", [128, 1]); cst_d = dr("cst", [128, 5 * 128])
    at_d = dr("attnT", [D, NTOK], "ExternalOutput")
    with ExitStack() as st:
        P = Prog(nc)
        C = Ctx(nc, st, P)
        cst3, cstb = C.sb("cst_sb", [128, 5, 128], BF16)
        P.op("pool", "dma_start", PK(out=cst3[:, :, :], in_=cst_d.rearrange("p (a b) -> p a b", a=5)), writes=[cstb], is_dma=True)
        biasT, biasb = C.sb("bias_sb", [128, 8, 512], F32)
        C.dma(biasT[:, :, :], bias_d.rearrange("p (a b) -> p a b", a=8), W=[biasb])
        lamb, lambb = C.sb("lamb_sb", [128, 4, 64], F32)
        C.dma(lamb[:, :, :], lamb_d.rearrange("p (a b) -> p a b", a=4), W=[lambb])
        sgc, sgcb = C.sb("sgc_sb", [128, 1], F32)
        C.dma(sgc[:, :], sgc_d[:, :], W=[sgcb])
        scr = dict(sq=C.sb("sq", [128, 1, 512], BF16), rs=C.sb("rs", [128, 512], F32))
        lt, ltb = C.sb("lt", [128, 2, 64], F32)
        lc, lcb = C.sb("lc", [128, 4], F32)
        for i in range(2):
            P.op("dve", "tensor_tensor", PK(out=lt[:, i, :], in0=lamb[:, 2 * i, :], in1=lamb[:, 2 * i + 1, :], op=ALU.mult), reads=[lambb], writes=[ltb])
            P.op("dve", "reduce_sum", PK(out=lc[:, i:i + 1], in_=lt[:, i, :], axis=AX.X), reads=[ltb], writes=[lcb])
        P.op("act", "activation", PK(out=lc[:, 0:2], in_=lc[:, 0:2], func=AF.Exp), reads=[lcb], writes=[lcb])
        P.op("dve", "tensor_tensor", PK(out=lc[:, 2:3], in0=lc[:, 1:2], in1=lc[:, 0:1], op=ALU.subtract), reads=[lcb], writes=[lcb])
        P.op("dve", "tensor_scalar_add", PK(out=lc[:, 3:4], in0=lc[:, 2:3], scalar1=-LAMBDA_INIT), reads=[lcb], writes=[lcb])
        P.op("dve", "tensor_scalar", PK(out=sgc[:, :], in0=sgc[:, :], scalar1=float(1.0 - LAMBDA_INIT), scalar2=None, op0=ALU.mult), reads=[sgcb], writes=[sgcb])
        kA = [C.sb("kA%d" % i, [68, 2, 4096], BF16) for i in range(2)]
        qA = [C.sb("qA%d" % i, [68, 2, NTOK], BF16) for i in range(2)]
        vA = [C.sb("vA%d" % i, [128, 32, 128], BF16) for i in range(2)]
        et, _ = C.sb("et", [128, 4, 512], BF16); etb = [Buf("et%d" % i) for i in range(4)]
        tmp, _ = C.sb("tmp", [128, 2, 512], F32); tmpb = [Buf("tmp%d" % i) for i in range(2)]
        rd, rdb = C.sb("rd", [128, 512], F32)
        o0, o0b = C.sb("o0", [128, 512], F32)
        o1, o1b = C.sb("o1", [128, 512], F32)
        ot, otb = C.sb("ot", [128, 512], F32)
        ei = 0
        ti_ = 0
        for h in range(8):
            slope = float(2.0 ** (-(h + 1)))
            kt, ktb = kA[h % 2]; qt, qtb = qA[h % 2]; vt, vtb = vA[h % 2]
            P.op("pool", "dma_start", PK(out=kt[:, :, :], in_=ka_d[2 * h:2 * h + 2].rearrange("c r n -> r c n")), writes=[ktb], is_dma=True)
            P.op("pool", "dma_start", PK(out=qt[:, :, :], in_=qa_d[2 * h:2 * h + 2].rearrange("c r n -> r c n")), writes=[qtb], is_dma=True)
            P.op("pool", "dma_start", PK(out=vt[:, :, :], in_=vf_d[:, h * 128:(h + 1) * 128].rearrange("(j p) d -> p j d", p=128)), writes=[vtb], is_dma=True)
            for m in range(4):
                nkb = 8 * (m + 1)
                qsl = slice(m * 512, (m + 1) * 512)
                N = [C.ps[0], C.ps[1]]
                Dn = [C.ps[2], C.ps[3]]
                for j in range(nkb):
                    for c in range(2):
                        sc, scb = C.ps[4 + ((2 * j + c) % 4)]
                        P.op("pe", "matmul", PK(sc[:, :], kt[:, c, j * 128:(j + 1) * 128], qt[:, c, qsl], start=True, stop=True),
                             reads=[ktb, qtb], writes=[scb])
                        e_slot = ei % 4; ei += 1
                        if j >= nkb - 8:
                            s = j - (nkb - 8)
                            tb = ti_ % 2; ti_ += 1
                            P.op("dve", "scalar_tensor_tensor", PK(out=tmp[:, tb, :], in0=biasT[:, s, :], scalar=slope, in1=sc[:, :], op0=ALU.mult, op1=ALU.add),
                                 reads=[biasb, scb], writes=[tmpb[tb]])
                            P.op("act", "activation", PK(out=et[:, e_slot, :], in_=tmp[:, tb, :], func=AF.Exp), reads=[tmpb[tb]], writes=[etb[e_slot]])
                        else:
                            P.op("act", "activation", PK(out=et[:, e_slot, :], in_=sc[:, :], func=AF.Exp), reads=[scb], writes=[etb[e_slot]])
                        P.op("pe", "matmul", PK(N[c][0][:, :], vt[:, j, :], et[:, e_slot, :], start=(j == 0), stop=(j == nkb - 1)),
                             reads=[vtb, etb[e_slot]], writes=[N[c][1]])
                        P.op("pe", "matmul", PK(Dn[c][0][:, :], cst3[:, 4, :], et[:, e_slot, :], start=(j == 0), stop=(j == nkb - 1)),
                             reads=[cstb, etb[e_slot]], writes=[Dn[c][1]])
                P.op("dve", "reciprocal", PK(out=rd[:, :], in_=Dn[0][0][:, :]), reads=[Dn[0][1]], writes=[rdb])
                P.op("dve", "tensor_tensor", PK(out=o0[:, :], in0=N[0][0][:, :], in1=rd[:, :], op=ALU.mult), reads=[N[0][1], rdb], writes=[o0b])
                P.op("dve", "reciprocal", PK(out=rd[:, :], in_=Dn[1][0][:, :]), reads=[Dn[1][1]], writes=[rdb])
                P.op("dve", "tensor_tensor", PK(out=o1[:, :], in0=N[1][0][:, :], in1=rd[:, :], op=ALU.mult), reads=[N[1][1], rdb], writes=[o1b])
                P.op("dve", "scalar_tensor_tensor", PK(out=o0[:, :], in0=o1[:, :], scalar=lc[:, 3:4], in1=o0[:, :], op0=ALU.mult, op1=ALU.add),
                     reads=[o1b, o0b, lcb], writes=[o0b])
                sq, sqb = scr["sq"]; rs, rsb = scr["rs"]
                P.op("act", "activation", PK(out=sq[:, 0, :], in_=o0[:, :], func=AF.Square), reads=[o0b], writes=[sqb])
                pn, pnb = C.ps[4]
                P.op("pe", "matmul", PK(pn[:, :], cst3[:, 3, :], sq[:, 0, :], start=True, stop=True), reads=[sqb, cstb], writes=[pnb])
                rstd_from(C, rs[:, :], pn[:, :], pnb, rsb)
                P.op("dve", "scalar_tensor_tensor", PK(out=ot[:, :], in0=o0[:, :], scalar=sgc[:, 0:1], in1=rs[:, :], op0=ALU.mult, op1=ALU.mult),
                     reads=[o0b, rsb, sgcb], writes=[otb])
                C.dma(at_d[h * 128:(h + 1) * 128, qsl], ot[:, :], R=[otb], is_out=True)
        P.emit(st)
    return nc


def bias_table(p):
    t = np.zeros((8, 128, 512), np.float32)
    kq = np.arange(128)[:, None]
    qq = np.arange(512)[None, :]
    for s in range(8):
        if p == 0:
            r = s if s < 4 else None
            full_mask = s >= 4
        else:
            r = s - 4 if s >= 4 else None
            full_mask = False
        if full_mask:
            t[s] = -1e30
        elif r is not None:
            kpos = 128 * r + kq
            kc = kpos // 64; qc = qq // 64
            d = -2.0 * np.maximum(kpos - qq, 0).astype(np.float32)
            t[s] = np.where(kc < qc, 0.0, np.where(kc == qc, d, -1e30))
    return np.ascontiguousarray(t.transpose(1, 0, 2).reshape(128, 8 * 512))


def q_aug_rows(p):
    pos = tok_index(p)
    return np.stack([pos // 64, pos % 64, np.ones_like(pos), np.ones_like(pos)]).astype(np.float32)


def k_aug_rows(h):
    s = np.arange(4096)
    sl = 2.0 ** (-(h + 1))
    return np.stack([np.full(4096, -64.0 * sl), np.full(4096, -sl), 64.0 * sl * (s // 64), sl * (s % 64)]).astype(np.float32)


T_SEQ = 4096
SEG = 512
NSEG_FULL = T_SEQ // SEG
NCOLS = 1312
ZCH = [(0, 128), (128, 128), (256, 128), (384, 128), (512, 128), (640, 128), (768, 128), (896, 128), (1024, 32), (1056, 128), (1184, 128)]
VA = dict(n_mix=0, mu=8, w0=17, a0=19, k_k=21, k_a=23, r_k=25, ln_w=27, ln_b=29, s5_d=31, lam_re=33, lam_im=41, log_dt=49, halfpi=57)
NVA = 58
C_ID, C_B64, C_ONES = 0, 1, 2
NEG_E05 = -float(np.exp(-0.5))


def build_A(nseg=NSEG_FULL):
    nc = bass.Bass("TRN2", target_bir_lowering=False)
    dr = lambda n, s, k="ExternalInput", dt=F32: nc.dram_tensor(n, s, dt, kind=k).ap()
    xT_d = dr("xT", [D, T_SEQ]); wc_d = dr("wc", [D, NCOLS]); vec_d = dr("vecs", [128, NVA])
    cst_d = dr("cst", [128, 5 * 128]); c32_d = dr("c32", [128, 3 * 128]); msk_d = dr("mask5", [64, 320]); rmask_d = dr("rmask", [128, SEG])
    wup_d = dr("w_up", [64, 256]); aup_d = dr("a_up", [128, 256]); gupa_d = dr("g_upa", [128, 256]); gupb_d = dr("g_upb", [32, 256])
    bre_d = dr("breT", [8, 128, 128]); bim_d = dr("bimT", [8, 128, 128]); cre_d = dr("creT", [8, 128, 128]); cim_d = dr("cimT", [8, 128, 128])
    ya_d = dr("yaT", [256, T_SEQ], "ExternalOutput"); yb_d = dr("ybT", [256, T_SEQ], "ExternalOutput")
    with ExitStack() as st:
        P = Prog(nc)
        C = Ctx(nc, st, P)
        rot = [0, 1, 2, 3, 4, 5, 7]
        rs_ = [0]

        def psum():
            r = C.ps[rot[rs_[0] % len(rot)]]
            rs_[0] += 1
            return r
        C.psum = psum
        T = lambda name, shape, dt=F32: C.sb(name, shape, dt)
        V, vb = T("V_sb", [128, NVA]); C.vb = vb
        C.dma(V[:, :], vec_d[:, :], W=[vb])
        cst3, cstb = T("cst_sb", [128, 5, 128], BF16)
        P.op("pool", "dma_start", PK(out=cst3[:, :, :], in_=cst_d.rearrange("p (a b) -> p a b", a=5)), writes=[cstb], is_dma=True)
        c32, c32b = T("c32_sb", [128, 3, 128]); C.dma(c32[:, :, :], c32_d.rearrange("p (a b) -> p a b", a=3), W=[c32b])
        msk, mskb = T("msk_sb", [64, 320]); C.dma(msk[:, :], msk_d[:, :], W=[mskb])
        rmask, rmaskb = T("rmask_sb", [128, SEG]); C.dma(rmask[:, :], rmask_d[:, :], W=[rmaskb])
        wup, wupb = T("wup_sb", [64, 256]); C.dma(wup[:, :], wup_d[:, :], W=[wupb])
        aup, aupb = T("aup_sb", [128, 256]); C.dma(aup[:, :], aup_d[:, :], W=[aupb])
        gupa, gupab = T("gupa_sb", [128, 256]); C.dma(gupa[:, :], gupa_d[:, :], W=[gupab])
        gupb, gupbb = T("gupb_sb", [32, 256]); C.dma(gupb[:, :], gupb_d[:, :], W=[gupbb])
        wc, wcb = T("wc_sb", [128, 8, NCOLS], BF16)
        for k in range(8):
            P.op("pool", "dma_start", PK(out=wc[:, k, :], in_=wc_d[k * 128:(k + 1) * 128, :]), writes=[wcb], is_dma=True)
        scr = dict(sq=T("sq", [128, 8, 512], BF16), rs=T("rs", [128, 512]))
        ident = c32[:, C_ID, :]

        breT, breb = T("breT_sb", [128, 8, 128], BF16); bimT, bimb = T("bimT_sb", [128, 8, 128], BF16)
        P.op("pool", "dma_start", PK(out=breT[:, :, :], in_=bre_d.rearrange("q k m -> k q m")), writes=[breb], is_dma=True)
        P.op("pool", "dma_start", PK(out=bimT[:, :, :], in_=bim_d.rearrange("q k m -> k q m")), writes=[bimb], is_dma=True)
        creT, creb = T("creT_sb", [128, 8, 128]); cimT, cimb = T("cimT_sb", [128, 8, 128])
        C.dma(creT[:, :, :], cre_d.rearrange("q k m -> k q m"), W=[creb])
        C.dma(cimT[:, :, :], cim_d.rearrange("q k m -> k q m"), W=[cimb])
        s5c, s5cb = T("s5c", [128, 12, 8])
        lre = V[:, VA["lam_re"]:VA["lam_re"] + 8]; lim = V[:, VA["lam_im"]:VA["lam_im"] + 8]
        S = lambda i: s5c[:, i, :]
        dv = lambda name, **kw: P.op("dve", name, PK(**kw), reads=[s5cb, vb], writes=[s5cb])
        P.op("act", "activation", PK(out=S(0), in_=V[:, VA["log_dt"]:VA["log_dt"] + 8], func=AF.Exp), reads=[vb], writes=[s5cb])
        dv("tensor_tensor", out=S(1), in0=lim, in1=S(0), op=ALU.mult)
        dv("tensor_tensor", out=S(9), in0=lre, in1=S(0), op=ALU.mult)
        P.op("act", "activation", PK(out=S(2), in_=S(9), func=AF.Exp), reads=[s5cb], writes=[s5cb])
        P.op("act", "activation", PK(out=S(4), in_=S(1), func=AF.Sin, scale=0.125), reads=[s5cb], writes=[s5cb])
        P.op("act", "activation", PK(out=S(3), in_=S(1), func=AF.Sin, scale=-0.125, bias=V[:, VA["halfpi"]:VA["halfpi"] + 1]), reads=[s5cb, vb], writes=[s5cb])
        for _ in range(3):
            dv("tensor_tensor", out=S(9), in0=S(3), in1=S(3), op=ALU.mult)
            dv("tensor_tensor", out=S(10), in0=S(4), in1=S(4), op=ALU.mult)
            dv("tensor_tensor", out=S(11), in0=S(3), in1=S(4), op=ALU.mult)
            dv("tensor_tensor", out=S(3), in0=S(9), in1=S(10), op=ALU.subtract)
            dv("tensor_scalar", out=S(4), in0=S(11), scalar1=2.0, scalar2=None, op0=ALU.mult)
        dv("tensor_tensor", out=S(5), in0=S(2), in1=S(3), op=ALU.mult)
        dv("tensor_tensor", out=S(6), in0=S(2), in1=S(4), op=ALU.mult)
        dv("tensor_scalar_add", out=S(5), in0=S(5), scalar1=-1.0)
        dv("tensor_tensor", out=S(9), in0=lre, in1=lre, op=ALU.mult)
        dv("tensor_tensor", out=S(10), in0=lim, in1=lim, op=ALU.mult)
        dv("tensor_tensor", out=S(9), in0=S(9), in1=S(10), op=ALU.add)
        dv("reciprocal", out=S(9), in_=S(9))
        dv("tensor_tensor", out=S(10), in0=S(5), in1=lre, op=ALU.mult)
        dv("tensor_tensor", out=S(11), in0=S(6), in1=lim, op=ALU.mult)
        dv("tensor_tensor", out=S(10), in0=S(10), in1=S(11), op=ALU.add)
        dv("tensor_tensor", out=S(7), in0=S(10), in1=S(9), op=ALU.mult)
        dv("tensor_tensor", out=S(10), in0=S(6), in1=lre, op=ALU.mult)
        dv("tensor_tensor", out=S(11), in0=S(5), in1=lim, op=ALU.mult)
        dv("tensor_tensor", out=S(10), in0=S(10), in1=S(11), op=ALU.subtract)
        dv("tensor_tensor", out=S(8), in0=S(10), in1=S(9), op=ALU.mult)
        cpre, cpreb = T("cpre", [128, 8, 128], BF16); cpim, cpimb = T("cpim", [128, 8, 128], BF16)
        ctmp, ctmpb = T("ctmp", [128, 128])
        for q in range(8):
            cr = s5c[:, 7, q:q + 1]; ci = s5c[:, 8, q:q + 1]
            P.op("dve", "tensor_scalar", PK(out=ctmp[:, :], in0=cimT[:, q, :], scalar1=ci, scalar2=None, op0=ALU.mult), reads=[cimb, s5cb], writes=[ctmpb])
            P.op("dve", "scalar_tensor_tensor", PK(out=cpre[:, q, :], in0=creT[:, q, :], scalar=cr, in1=ctmp[:, :], op0=ALU.mult, op1=ALU.subtract),
                 reads=[creb, ctmpb, s5cb], writes=[cpreb])
            P.op("dve", "tensor_scalar", PK(out=ctmp[:, :], in0=cimT[:, q, :], scalar1=cr, scalar2=-1.0, op0=ALU.mult, op1=ALU.mult), reads=[cimb, s5cb], writes=[ctmpb])
            P.op("dve", "scalar_tensor_tensor", PK(out=ctmp[:, :], in0=creT[:, q, :], scalar=ci, in1=ctmp[:, :], op0=ALU.mult, op1=ALU.subtract),
                 reads=[creb, ctmpb, s5cb], writes=[ctmpb])
            P.op("dve", "tensor_scalar", PK(out=cpim[:, q, :], in0=ctmp[:, :], scalar1=-1.0, scalar2=None, op0=ALU.mult), reads=[ctmpb], writes=[cpimb])
        Fc, Fcb = T("Fc", [128, 8, SEG]); Fs, Fsb = T("Fs", [128, 8, SEG])
        ncol, ncolb = T("ncol", [128, 2])
        ftmp, ftmpb = T("ftmp", [128, SEG // 2])
        for q in range(8):
            P.op("dve", "tensor_copy", PK(out=Fc[:, q, 0:1], in_=s5c[:, 3, q:q + 1]), reads=[s5cb], writes=[Fcb])
            P.op("dve", "tensor_copy", PK(out=Fs[:, q, 0:1], in_=s5c[:, 4, q:q + 1]), reads=[s5cb], writes=[Fsb])
            n = 1
            while n < SEG:
                cn = Fc[:, q, n - 1:n]; sn = Fs[:, q, n - 1:n]
                P.op("dve", "tensor_scalar", PK(out=ftmp[:, 0:n], in0=Fs[:, q, 0:n], scalar1=sn, scalar2=None, op0=ALU.mult), reads=[Fsb], writes=[ftmpb])
                P.op("dve", "scalar_tensor_tensor", PK(out=Fc[:, q, n:2 * n], in0=Fc[:, q, 0:n], scalar=cn, in1=ftmp[:, 0:n], op0=ALU.mult, op1=ALU.subtract),
                     reads=[Fcb, ftmpb], writes=[Fcb])
                P.op("dve", "tensor_scalar", PK(out=ftmp[:, 0:n], in0=Fc[:, q, 0:n], scalar1=sn, scalar2=None, op0=ALU.mult), reads=[Fcb, Fsb], writes=[ftmpb])
                P.op("dve", "scalar_tensor_tensor", PK(out=Fs[:, q, n:2 * n], in0=Fs[:, q, 0:n], scalar=cn, in1=ftmp[:, 0:n], op0=ALU.mult, op1=ALU.add),
                     reads=[Fsb, Fcb, ftmpb], writes=[Fsb])
                n *= 2
        xst_re, xreb = T("xst_re", [128, 8, 2]); xst_im, ximb = T("xst_im", [128, 8, 2])
        P.op("dve", "memset", PK(xst_re[:, :, :], 0.0), writes=[xreb]); P.op("dve", "memset", PK(xst_im[:, :, :], 0.0), writes=[ximb])

        zT, _ = T("zT", [128, 11, SEG + 1]); zTb = [Buf("zT%d" % c) for c in range(11)]
        for c in range(11):
            P.op("dve", "memset", PK(zT[:, c, 0:1], 0.0), writes=[zTb[c]])
        Sst = [[T("S%d_%d" % (hp, i), [128, 64]) for i in range(2)] for hp in range(2)]
        for hp in range(2):
            P.op("dve", "memset", PK(Sst[hp][0][0][:, :], 0.0), writes=[Sst[hp][0][1]])
        sidx = [0, 0]
        xT_t, xTb = T("xT_sb", [128, 8, SEG], BF16); hn, hnb = T("hn", [128, 8, SEG], BF16)
        names = "ld a g al be km cum Gi tA tB Rb Kb Ab Bb Kh Bh rk".split()
        W_ = {n: T("w_" + n, [128, SEG]) for n in names}
        gC, gCb = T("gC", [128, 8])
        AAs, AAsb = T("AAs", [64, 320]); TM, TMb = T("TM", [64, 4, 128])
        Asq = [T("Asq%d" % i, [64, 128]) for i in range(2)]
        Zt = [T("Zt%d" % i, [64, 64]) for i in range(2)]
        Wsb, Wsbb = T("Wsb", [64, 64]); Ut0, Ut0b = T("Ut0", [64, 64]); Ut, Utb = T("Ut", [64, 64])
        Phi, Phib = T("Phi", [128, 64])
        ubuf, ubufb = T("ubuf", [128, 2, SEG], BF16)
        s5t = {n: T("s5_" + n, [128, SEG]) for n in "t1 t2 cre cim zre zim xre xim".split()}
        tw, twb = s5t["t1"]; sg0, sg0b = s5t["t2"]; sg1, sg1b = s5t["zre"]
        xreb16, xreb16b = T("xre16", [128, 8, SEG], BF16); ximb16, ximb16b = T("xim16", [128, 8, SEG], BF16)
        yo, yob = s5t["cre"]

        for seg in range(nseg):
            tsl = slice(seg * SEG, (seg + 1) * SEG)
            P.op("pool", "dma_start", PK(out=xT_t[:, :, :], in_=xT_d[:, tsl].rearrange("(c p) n -> p c n", p=128)), writes=[xTb], is_dma=True)
            norm_fm(C, lambda c: xT_t[:, c, :], [xTb], SEG, 8, cst3[:, 0, :], cstb, lambda c: V[:, VA["n_mix"] + c:VA["n_mix"] + c + 1],
                    lambda c: hn[:, c, :], [hnb], scr)
            for c, (c0, wdt) in enumerate(ZCH):
                pt, pb = C.psum()
                for k in range(8):
                    P.op("pe", "matmul", PK(pt[0:wdt, :], wc[:, k, c0:c0 + wdt], hn[:, k, :], start=(k == 0), stop=(k == 7)), reads=[wcb, hnb], writes=[pb])
                P.op("act", "activation", PK(out=zT[0:wdt, c, 1:SEG + 1], in_=pt[0:wdt, :], func=AF.Copy), reads=[pb], writes=[zTb[c]])
            tA, tAb = W_["tA"]
            for c in range(9):
                wdt = ZCH[c][1]
                P.op("dve", "tensor_tensor", PK(out=tA[0:wdt, :], in0=zT[0:wdt, c, 0:SEG], in1=zT[0:wdt, c, 1:SEG + 1], op=ALU.subtract), reads=[zTb[c]], writes=[tAb])
                P.op("dve", "tensor_copy", PK(out=zT[0:wdt, c, 0:1], in_=zT[0:wdt, c, SEG:SEG + 1]), reads=[tAb], writes=[zTb[c]])
                P.op("dve", "scalar_tensor_tensor", PK(out=zT[0:wdt, c, 1:SEG + 1], in0=tA[0:wdt, :], scalar=V[0:wdt, VA["mu"] + c:VA["mu"] + c + 1],
                                                      in1=zT[0:wdt, c, 1:SEG + 1], op0=ALU.mult, op1=ALU.add), reads=[tAb, zTb[c], vb], writes=[zTb[c]])
            Z = lambda c, lo=0, hi=128: zT[lo:hi, c, 1:SEG + 1]
            P.op("act", "activation", PK(out=tw[0:64, :], in_=Z(6, 0, 64), func=AF.Tanh), reads=[zTb[6]], writes=[twb])
            P.op("act", "activation", PK(out=sg0[:, :], in_=Z(7), func=AF.Sigmoid), reads=[zTb[7]], writes=[sg0b])
            P.op("act", "activation", PK(out=sg1[0:32, :], in_=Z(8, 0, 32), func=AF.Sigmoid), reads=[zTb[8]], writes=[sg1b])
            for hp in range(2):
                cols = slice(hp * 128, (hp + 1) * 128)
                vcol = lambda nm: V[:, VA[nm] + hp:VA[nm] + hp + 1]
                r_ = Z(hp); k_ = Z(2 + hp); v_ = Z(4 + hp)
                rb_, kb_, vb_ = zTb[hp], zTb[2 + hp], zTb[4 + hp]
                X = lambda n: W_[n][0]
                B_ = lambda n: W_[n][1]
                DV = lambda name, R, Wn, **kw: P.op("dve", name, PK(**kw), reads=R, writes=[B_(Wn)])
                pt, pb = C.psum()
                P.op("pe", "matmul", PK(pt[:, :], wup[:, cols], tw[0:64, :], start=True, stop=True), reads=[wupb, twb], writes=[pb])
                P.op("act", "activation", PK(out=X("ld")[:, :], in_=pt[:, :], func=AF.Sigmoid, bias=vcol("w0")), reads=[pb, vb], writes=[B_("ld")])
                DV("tensor_scalar", [B_("ld")], "ld", out=X("ld")[:, :], in0=X("ld")[:, :], scalar1=NEG_E05, scalar2=None, op0=ALU.mult)
                pt, pb = C.psum()
                P.op("pe", "matmul", PK(pt[:, :], aup[64:128, cols], Z(6, 64, 128), start=True, stop=True), reads=[aupb, zTb[6]], writes=[pb])
                P.op("act", "activation", PK(out=X("a")[:, :], in_=pt[:, :], func=AF.Sigmoid, bias=vcol("a0")), reads=[pb, vb], writes=[B_("a")])
                pt, pb = C.psum()
                P.op("pe", "matmul", PK(pt[:, :], gupa[:, cols], sg0[:, :], start=True, stop=False), reads=[gupab, sg0b], writes=[pb])
                P.op("pe", "matmul", PK(pt[:, :], gupb[:, cols], sg1[0:32, :], start=False, stop=True), reads=[gupbb, sg1b], writes=[pb])
                P.op("act", "activation", PK(out=X("g")[:, :], in_=pt[:, :], func=AF.Copy), reads=[pb], writes=[B_("g")])
                DV("tensor_scalar", [kb_, vb], "tA", out=X("tA")[:, :], in0=k_, scalar1=vcol("k_k"), scalar2=None, op0=ALU.mult)
                P.op("act", "activation", PK(out=X("tB")[:, :], in_=X("tA")[:, :], func=AF.Square), reads=[B_("tA")], writes=[B_("tB")])
                pt, pb = C.psum()
                P.op("pe", "matmul", PK(pt[:, :], c32[:, C_B64, :], X("tB")[:, :], start=True, stop=True), reads=[c32b, B_("tB")], writes=[pb])
                rs, rsb = scr["rs"]
                rstd_from(C, rs[:, :], pt[:, :], pb, rsb)
                DV("scalar_tensor_tensor", [B_("tA"), rsb], "al", out=X("al")[:, :], in0=X("tA")[:, :], scalar=0.125, in1=rs[:, :], op0=ALU.mult, op1=ALU.mult)
                DV("scalar_tensor_tensor", [B_("al"), B_("a")], "be", out=X("be")[:, :], in0=X("al")[:, :], scalar=-1.0, in1=X("a")[:, :], op0=ALU.mult, op1=ALU.mult)
                DV("tensor_scalar", [B_("a"), vb], "tA", out=X("tA")[:, :], in0=X("a")[:, :], scalar1=-1.0, scalar2=vcol("k_a"), op0=ALU.add, op1=ALU.mult)
                DV("scalar_tensor_tensor", [B_("tA"), kb_], "km", out=X("km")[:, :], in0=X("tA")[:, :], scalar=1.0, in1=k_, op0=ALU.add, op1=ALU.mult)
                DV("scalar_tensor_tensor", [rb_, B_("km"), vb], "rk", out=X("rk")[:, :], in0=r_, scalar=vcol("r_k"), in1=X("km")[:, :], op0=ALU.mult, op1=ALU.mult)
                DV("tensor_tensor_scan", [rmaskb, B_("ld")], "cum", out=X("cum")[:, :], data0=rmask[:, :], data1=X("ld")[:, :], initial=0.0, op0=ALU.mult, op1=ALU.add)
                cum3 = X("cum")[:, :].rearrange("p (c t) -> p c t", t=64)
                P.op("act", "activation", PK(out=X("Gi")[:, :], in_=X("cum")[:, :], func=AF.Exp), reads=[B_("cum")], writes=[B_("Gi")])
                DV("tensor_tensor", [B_("Gi"), rb_], "Rb", out=X("Rb")[:, :], in0=r_, in1=X("Gi")[:, :], op=ALU.mult)
                DV("tensor_tensor", [B_("cum"), B_("ld")], "tA", out=X("tA")[:, :], in0=X("cum")[:, :], in1=X("ld")[:, :], op=ALU.subtract)
                P.op("act", "activation", PK(out=X("Gi")[:, :], in_=X("tA")[:, :], func=AF.Exp), reads=[B_("tA")], writes=[B_("Gi")])
                DV("tensor_tensor", [B_("Gi"), B_("al")], "Ab", out=X("Ab")[:, :], in0=X("al")[:, :], in1=X("Gi")[:, :], op=ALU.mult)
                P.op("act", "activation", PK(out=X("Gi")[:, :], in_=X("cum")[:, :], func=AF.Exp, scale=-1.0), reads=[B_("cum")], writes=[B_("Gi")])
                DV("tensor_tensor", [B_("Gi"), B_("km")], "Kb", out=X("Kb")[:, :], in0=X("km")[:, :], in1=X("Gi")[:, :], op=ALU.mult)
                DV("tensor_tensor", [B_("Gi"), B_("be")], "Bb", out=X("Bb")[:, :], in0=X("be")[:, :], in1=X("Gi")[:, :], op=ALU.mult)
                tA3 = X("tA")[:, :].rearrange("p (c t) -> p c t", t=64)
                DV("tensor_tensor", [B_("cum")], "tA", out=tA3, in0=cum3[:, :, 63:64].to_broadcast([128, 8, 64]), in1=cum3, op=ALU.subtract)
                P.op("act", "activation", PK(out=X("Gi")[:, :], in_=X("tA")[:, :], func=AF.Exp), reads=[B_("tA")], writes=[B_("Gi")])
                DV("tensor_tensor", [B_("Gi"), B_("km")], "Kh", out=X("Kh")[:, :], in0=X("km")[:, :], in1=X("Gi")[:, :], op=ALU.mult)
                DV("tensor_tensor", [B_("Gi"), B_("be")], "Bh", out=X("Bh")[:, :], in0=X("be")[:, :], in1=X("Gi")[:, :], op=ALU.mult)
                P.op("act", "activation", PK(out=gC[:, :], in_=cum3[:, :, 63], func=AF.Exp), reads=[B_("cum")], writes=[gCb])
                po, pob = C.ps[6]
                for c in range(SEG // 64):
                    cs = slice(c * 64, (c + 1) * 64)
                    pt, pb = C.psum()
                    for i, (src, sb_) in enumerate(((v_, vb_), (X("Ab")[:, :], B_("Ab")), (X("Bh")[:, :], B_("Bh")), (X("Kh")[:, :], B_("Kh")))):
                        P.op("pe", "transpose", PK(pt[0:64, i * 128:(i + 1) * 128], src[:, cs], ident), reads=[sb_, c32b], writes=[pb])
                    P.op("act", "activation", PK(out=TM[:, :, :], in_=pt[0:64, :].rearrange("p (i k) -> p i k", i=4), func=AF.Copy), reads=[pb], writes=[TMb])
                    for e in range(2):
                        pr = slice(e * 64, (e + 1) * 64)
                        es = slice(e * 64, (e + 1) * 64)
                        pt, pb = C.psum()
                        Bb_, Kb_, Ab_, Rb_ = X("Bb")[pr, cs], X("Kb")[pr, cs], X("Ab")[pr, cs], X("Rb")[pr, cs]
                        P.op("pe", "matmul", PK(pt[0:64, 0:64], Bb_, Ab_, start=True, stop=True), reads=[B_("Bb"), B_("Ab")], writes=[pb])
                        P.op("pe", "matmul", PK(pt[0:64, 64:128], Bb_, Rb_, start=True, stop=True), reads=[B_("Bb"), B_("Rb")], writes=[pb])
                        P.op("pe", "matmul", PK(pt[0:64, 128:192], Kb_, Ab_, start=True, stop=True), reads=[B_("Kb"), B_("Ab")], writes=[pb])
                        P.op("pe", "matmul", PK(pt[0:64, 192:256], Kb_, Rb_, start=True, stop=True), reads=[B_("Kb"), B_("Rb")], writes=[pb])
                        P.op("pe", "matmul", PK(pt[0:64, 256:320], Ab_, Bb_, start=True, stop=True), reads=[B_("Bb"), B_("Ab")], writes=[pb])
                        P.op("dve", "tensor_tensor", PK(out=AAs[:, :], in0=pt[0:64, 0:320], in1=msk[:, :], op=ALU.mult), reads=[pb, mskb], writes=[AAsb])
                        A_ab, A_rb, A_ak, A_rk, A_abT = (AAs[:, 0:64], AAs[:, 64:128], AAs[:, 128:192], AAs[:, 192:256], AAs[:, 256:320])
                        Vt = TM[:, 0, es]; AbT = TM[:, 1, es]; BhT = TM[:, 2, es]; KhT = TM[:, 3, es]
                        zi = 0
                        P.op("dve", "tensor_tensor", PK(out=Zt[0][0][:, :], in0=A_ab, in1=ident[0:64, 0:64], op=ALU.add), reads=[AAsb, c32b], writes=[Zt[0][1]])
                        curA, curAT, curb = A_ab, A_abT, AAsb
                        for lvl in range(1, 6):
                            psq, psqb = C.psum()
                            if lvl < 5:
                                P.op("pe", "matmul", PK(psq[0:64, 0:64], curAT, curA, start=True, stop=True), reads=[curb], writes=[psqb])
                            P.op("pe", "matmul", PK(psq[0:64, 64:128], curA, curAT, start=True, stop=True), reads=[curb], writes=[psqb])
                            at, atb = Asq[lvl % 2]
                            lo = 0 if lvl < 5 else 64
                            P.op("act", "activation", PK(out=at[:, lo:128], in_=psq[0:64, lo:128], func=AF.Copy), reads=[psqb], writes=[atb])
                            curA, curAT, curb = at[:, 0:64], at[:, 64:128], atb
                            pz, pzb = C.psum()
                            P.op("pe", "matmul", PK(pz[0:64, 0:64], curAT, Zt[zi][0][:, :], start=True, stop=True), reads=[curb, Zt[zi][1]], writes=[pzb])
                            P.op("dve", "tensor_tensor", PK(out=Zt[1 - zi][0][:, :], in0=pz[0:64, 0:64], in1=Zt[zi][0][:, :], op=ALU.add),
                                 reads=[pzb, Zt[zi][1]], writes=[Zt[1 - zi][1]])
                            zi = 1 - zi
                        Tm, Tmb = Zt[zi]
                        pt, pb = C.psum()
                        P.op("pe", "matmul", PK(pt[0:64, 0:64], A_ak, Vt, start=True, stop=True), reads=[AAsb, TMb], writes=[pb])
                        P.op("act", "activation", PK(out=Wsb[:, :], in_=pt[0:64, 0:64], func=AF.Copy), reads=[pb], writes=[Wsbb])
                        pt, pb = C.psum()
                        P.op("pe", "matmul", PK(pt[0:64, 0:64], Tm[:, :], Wsb[:, :], start=True, stop=True), reads=[Tmb, Wsbb], writes=[pb])
                        P.op("pe", "matmul", PK(pt[pr, 64:128], AbT, Tm[:, :], start=True, stop=True), reads=[Tmb, TMb], writes=[pb])
                        P.op("act", "activation", PK(out=Ut0[:, :], in_=pt[0:64, 0:64], func=AF.Copy), reads=[pb], writes=[Ut0b])
                        P.op("act", "activation", PK(out=Phi[pr, :], in_=pt[pr, 64:128], func=AF.Copy), reads=[pb], writes=[Phib])
                        Sc, Scb = Sst[hp][sidx[hp] % 2] if e == 0 else Sst[hp][sidx[hp] % 2]
                        Sn, Snb = Sst[hp][(sidx[hp] + 1) % 2]
                        pt, pb = C.psum()
                        P.op("pe", "matmul", PK(pt[0:64, 0:64], Phi[pr, :], Sc[pr, :], start=True, stop=True), reads=[Phib, Scb], writes=[pb])
                        P.op("dve", "tensor_tensor", PK(out=Ut[:, :], in0=pt[0:64, 0:64], in1=Ut0[:, :], op=ALU.add), reads=[pb, Ut0b], writes=[Utb])
                        P.op("pe", "matmul", PK(po[pr, cs], Sc[pr, :], Rb_, start=True, stop=False), reads=[Scb, B_("Rb")], writes=[pob])
                        P.op("pe", "matmul", PK(po[pr, cs], Ut[:, :], A_rb, start=False, stop=False), reads=[Utb, AAsb], writes=[pob])
                        P.op("pe", "matmul", PK(po[pr, cs], Vt, A_rk, start=False, stop=True), reads=[TMb, AAsb], writes=[pob])
                        pt, pb = C.psum()
                        P.op("pe", "matmul", PK(pt[pr, 0:64], BhT, Ut[:, :], start=True, stop=False), reads=[TMb, Utb], writes=[pb])
                        P.op("pe", "matmul", PK(pt[pr, 0:64], KhT, Vt, start=False, stop=True), reads=[TMb], writes=[pb])
                        P.op("dve", "scalar_tensor_tensor", PK(out=Sn[pr, :], in0=Sc[pr, :], scalar=gC[pr, c:c + 1], in1=pt[pr, 0:64], op0=ALU.mult, op1=ALU.add),
                             reads=[Scb, gCb, pb], writes=[Snb])
                    sidx[hp] += 1
                Osb, Osbb = W_["Gi"]
                P.op("act", "activation", PK(out=Osb[:, :], in_=po[:, :], func=AF.Copy), reads=[pob], writes=[Osbb])
                pm, pmb = C.psum()
                P.op("pe", "matmul", PK(pm[:, :], c32[:, C_B64, :], Osb[:, :], start=True, stop=True), reads=[c32b, Osbb], writes=[pmb])
                DV("tensor_tensor", [Osbb, pmb], "tA", out=X("tA")[:, :], in0=Osb[:, :], in1=pm[:, :], op=ALU.subtract)
                P.op("act", "activation", PK(out=X("tB")[:, :], in_=X("tA")[:, :], func=AF.Square), reads=[B_("tA")], writes=[B_("tB")])
                pv, pvb = C.psum()
                P.op("pe", "matmul", PK(pv[:, :], c32[:, C_B64, :], X("tB")[:, :], start=True, stop=True), reads=[c32b, B_("tB")], writes=[pvb])
                rstd_from(C, rs[:, :], pv[:, :], pvb, rsb, eps=64e-5)
                DV("tensor_tensor", [B_("tA"), rsb], "tA", out=X("tA")[:, :], in0=X("tA")[:, :], in1=rs[:, :], op=ALU.mult)
                P.op("act", "activation", PK(out=X("tB")[:, :], in_=X("tA")[:, :], func=AF.Identity, scale=vcol("ln_w"), bias=vcol("ln_b")),
                     reads=[B_("tA"), vb], writes=[B_("tB")])
                pk, pkb = C.psum()
                P.op("pe", "matmul", PK(pk[:, :], c32[:, C_B64, :], X("rk")[:, :], start=True, stop=True), reads=[c32b, B_("rk")], writes=[pkb])
                DV("scalar_tensor_tensor", [pkb, vb_], "tA", out=X("tA")[:, :], in0=pk[:, :], scalar=64.0, in1=v_, op0=ALU.mult, op1=ALU.mult)
                DV("tensor_tensor", [B_("tA"), B_("tB")], "tB", out=X("tB")[:, :], in0=X("tB")[:, :], in1=X("tA")[:, :], op=ALU.add)
                DV("tensor_tensor", [B_("tB"), B_("g")], "Gi", out=Osb[:, :], in0=X("tB")[:, :], in1=X("g")[:, :], op=ALU.mult)
                C.dma(ya_d[hp * 128:(hp + 1) * 128, tsl], Osb[:, :], R=[Osbb], is_out=True)

            for uc in range(2):
                P.op("act", "activation", PK(out=ubuf[:, uc, :], in_=Z(9 + uc), func=AF.Copy), reads=[zTb[9 + uc]], writes=[ubufb])
            for q in range(8):
                uc = q // 4
                t1, t1b = s5t["t1"]; t2, t2b = s5t["t2"]
                cre_, creb_ = s5t["cre"]; cim_, cimb_ = s5t["cim"]
                zre, zreb = s5t["zre"]; zim, zimb = s5t["zim"]
                pr_, prb_ = C.psum(); pi_, pib_ = C.psum()
                P.op("pe", "matmul", PK(pr_[:, :], breT[:, q, :], ubuf[:, uc, :], start=True, stop=True), reads=[breb, ubufb], writes=[prb_])
                P.op("pe", "matmul", PK(pi_[:, :], bimT[:, q, :], ubuf[:, uc, :], start=True, stop=True), reads=[bimb, ubufb], writes=[pib_])
                fc = Fc[:, q, :]; fs = Fs[:, q, :]
                P.op("dve", "tensor_tensor", PK(out=t1[:, :], in0=pr_[:, :], in1=fc, op=ALU.mult), reads=[prb_, Fcb], writes=[t1b])
                P.op("dve", "tensor_tensor", PK(out=t2[:, :], in0=pi_[:, :], in1=fs, op=ALU.mult), reads=[pib_, Fsb], writes=[t2b])
                P.op("dve", "tensor_tensor", PK(out=cre_[:, :], in0=t1[:, :], in1=t2[:, :], op=ALU.add), reads=[t1b, t2b], writes=[creb_])
                P.op("dve", "tensor_tensor", PK(out=t1[:, :], in0=pi_[:, :], in1=fc, op=ALU.mult), reads=[pib_, Fcb], writes=[t1b])
                P.op("dve", "tensor_tensor", PK(out=t2[:, :], in0=pr_[:, :], in1=fs, op=ALU.mult), reads=[prb_, Fsb], writes=[t2b])
                P.op("dve", "tensor_tensor", PK(out=cim_[:, :], in0=t1[:, :], in1=t2[:, :], op=ALU.subtract), reads=[t1b, t2b], writes=[cimb_])
                P.op("dve", "tensor_tensor_scan", PK(out=zre[:, :], data0=s5c[:, 2, q:q + 1].to_broadcast([128, SEG]), data1=cre_[:, :], initial=xst_re[:, q, 0:1], op0=ALU.mult, op1=ALU.add),
                     reads=[s5cb, creb_, xreb], writes=[zreb])
                P.op("dve", "tensor_tensor_scan", PK(out=zim[:, :], data0=s5c[:, 2, q:q + 1].to_broadcast([128, SEG]), data1=cim_[:, :], initial=xst_im[:, q, 0:1], op0=ALU.mult, op1=ALU.add),
                     reads=[s5cb, cimb_, ximb], writes=[zimb])
                xre_, xreb_ = s5t["xre"]; xim_, ximb_ = s5t["xim"]
                P.op("dve", "tensor_tensor", PK(out=t1[:, :], in0=zre[:, :], in1=fc, op=ALU.mult), reads=[zreb, Fcb], writes=[t1b])
                P.op("dve", "tensor_tensor", PK(out=t2[:, :], in0=zim[:, :], in1=fs, op=ALU.mult), reads=[zimb, Fsb], writes=[t2b])
                P.op("dve", "tensor_tensor", PK(out=xre_[:, :], in0=t1[:, :], in1=t2[:, :], op=ALU.subtract), reads=[t1b, t2b], writes=[xreb_])
                P.op("dve", "tensor_tensor", PK(out=t1[:, :], in0=zim[:, :], in1=fc, op=ALU.mult), reads=[zimb, Fcb], writes=[t1b])
                P.op("dve", "tensor_tensor", PK(out=t2[:, :], in0=zre[:, :], in1=fs, op=ALU.mult), reads=[zreb, Fsb], writes=[t2b])
                P.op("dve", "tensor_tensor", PK(out=xim_[:, :], in0=t1[:, :], in1=t2[:, :], op=ALU.add), reads=[t1b, t2b], writes=[ximb_])
                P.op("dve", "tensor_copy", PK(out=xst_re[:, q, 0:1], in_=xre_[:, SEG - 1:SEG]), reads=[xreb_], writes=[xreb])
                P.op("dve", "tensor_copy", PK(out=xst_im[:, q, 0:1], in_=xim_[:, SEG - 1:SEG]), reads=[ximb_], writes=[ximb])
                P.op("act", "activation", PK(out=xreb16[:, q, :], in_=xre_[:, :], func=AF.Copy), reads=[xreb_], writes=[xreb16b])
                P.op("act", "activation", PK(out=ximb16[:, q, :], in_=xim_[:, :], func=AF.Copy), reads=[ximb_], writes=[ximb16b])
            for yc in range(2):
                py, pyb = C.psum()
                for qq in range(4):
                    q = yc * 4 + qq
                    P.op("pe", "matmul", PK(py[:, :], cpre[:, q, :], xreb16[:, q, :], start=(qq == 0), stop=False), reads=[cpreb, xreb16b], writes=[pyb])
                    P.op("pe", "matmul", PK(py[:, :], cpim[:, q, :], ximb16[:, q, :], start=False, stop=(qq == 3)), reads=[cpimb, ximb16b], writes=[pyb])
                t1, t1b = s5t["t1"]; t2, t2b = s5t["t2"]
                P.op("dve", "scalar_tensor_tensor", PK(out=yo[:, :], in0=Z(9 + yc), scalar=V[:, VA["s5_d"] + yc:VA["s5_d"] + yc + 1], in1=py[:, :], op0=ALU.mult, op1=ALU.add),
                     reads=[zTb[9 + yc], vb, pyb], writes=[yob])
                P.op("act", "activation", PK(out=t1[:, :], in_=yo[:, :], func=AF.Square), reads=[yob], writes=[t1b])
                P.op("dve", "tensor_scalar", PK(out=t1[:, :], in0=t1[:, :], scalar1=0.044715, scalar2=1.0, op0=ALU.mult, op1=ALU.add), reads=[t1b], writes=[t1b])
                P.op("dve", "tensor_tensor", PK(out=t1[:, :], in0=t1[:, :], in1=yo[:, :], op=ALU.mult), reads=[t1b, yob], writes=[t1b])
                P.op("act", "activation", PK(out=t2[:, :], in_=t1[:, :], func=AF.Sigmoid, scale=1.5957691216057308), reads=[t1b], writes=[t2b])
                P.op("dve", "tensor_tensor", PK(out=t2[:, :], in0=t2[:, :], in1=yo[:, :], op=ALU.mult), reads=[t2b, yob], writes=[t2b])
                C.dma(yb_d[yc * 128:(yc + 1) * 128, tsl], t2[:, :], R=[t2b], is_out=True)
        P.emit(st)
    return nc


def pack_A(inp, b, hh):
    W = np.asarray(inp["hy_w_in"][0]); mu = np.asarray(inp["rw_mu"][0])
    hc = slice(hh * 256, (hh + 1) * 256)
    idx = np.concatenate([np.arange(0, 512)[hc], 512 + np.arange(0, 512)[hc], 1024 + np.arange(0, 512)[hc],
                          np.arange(1536, 1824), 1824 + np.arange(0, 512)[hc]])
    wc = np.ascontiguousarray(W[:, idx])
    muc = np.concatenate([mu[idx[:1056]], np.zeros(96, np.float32)])
    Vt = np.zeros((128, NVA), np.float32)

    def put(name, arr):
        a = col(arr); Vt[:, VA[name]:VA[name] + a.shape[1]] = a
    put("n_mix", inp["norm_mix"][0])
    put("mu", muc)
    for nm, key in (("w0", "rw_w0"), ("a0", "rw_a0"), ("k_k", "rw_k_k"), ("k_a", "rw_k_a"), ("ln_w", "rw_ln_w"), ("ln_b", "rw_ln_b")):
        put(nm, np.asarray(inp[key][0])[hc])
    put("r_k", np.asarray(inp["rw_r_k"][0]).reshape(-1)[hc])
    put("s5_d", np.asarray(inp["s5_d"][0])[hc])
    gs = slice(hh * 16, (hh + 1) * 16)
    lre = np.asarray(inp["s5_lam_re"][0])[gs]; lim = np.asarray(inp["s5_lam_im"][0])[gs]; ldt = np.asarray(inp["s5_log_dt"][0])[gs]
    Vt[:, VA["lam_re"]:VA["lam_re"] + 8] = lre.reshape(8, 128).T
    Vt[:, VA["lam_im"]:VA["lam_im"] + 8] = lim.reshape(8, 128).T
    Vt[:, VA["log_dt"]:VA["log_dt"] + 8] = np.repeat(ldt, 64).reshape(8, 128).T
    Vt[:, VA["halfpi"]] = np.float32(np.pi / 2)
    bre = np.asarray(inp["s5_b_re"][0])[gs]; bim = np.asarray(inp["s5_b_im"][0])[gs]
    cre = np.asarray(inp["s5_c_re"][0])[gs]; cim = np.asarray(inp["s5_c_im"][0])[gs]
    breT = np.zeros((8, 128, 128), np.float32); bimT = np.zeros_like(breT); creT = np.zeros_like(breT); cimT = np.zeros_like(breT)
    for q in range(8):
        for e in range(2):
            gl = 2 * q + e
            rows = slice((gl % 8) * 16, (gl % 8) * 16 + 16)
            breT[q, rows, e * 64:(e + 1) * 64] = bre[gl].T
            bimT[q, rows, e * 64:(e + 1) * 64] = bim[gl].T
            creT[q, e * 64:(e + 1) * 64, rows] = cre[gl].T
            cimT[q, e * 64:(e + 1) * 64, rows] = cim[gl].T
    aup = np.zeros((128, 256), np.float32); aup[64:] = np.asarray(inp["rw_a_up"][0])[:, hc]
    gup = np.asarray(inp["rw_g_up"][0])[:, hc]
    c32 = np.zeros((128, 3, 128), np.float32)
    c32[:, 0, :] = np.eye(128, dtype=np.float32)
    c32[0:64, 1, 0:64] = 1.0 / 64; c32[64:, 1, 64:] = 1.0 / 64
    c32[:, 2, :] = 1.0
    s = np.arange(64)[:, None]; t = np.arange(64)[None, :]
    su = (s < t).astype(np.float32); ui = (s <= t).astype(np.float32)
    mask5 = np.concatenate([su, ui, su, ui, su.T], 1)
    rmask = np.ones((128, SEG), np.float32); rmask[:, ::64] = 0.0
    return dict(xT=np.ascontiguousarray(np.asarray(inp["x"][b]).T), wc=wc, vecs=Vt, cst=cst_table(), c32=c32.reshape(128, 384), mask5=mask5, rmask=rmask,
                w_up=np.ascontiguousarray(np.asarray(inp["rw_w_up"][0])[:, hc]), a_up=aup, g_upa=np.ascontiguousarray(gup[:128]),
                g_upb=np.ascontiguousarray(gup[128:]), breT=breT, bimT=bimT, creT=creT, cimT=cimT)


class ACtx(Ctx):
    def __init__(self, nc, st, P, words):
        self.nc, self.st, self.P = nc, st, P
        self.arena = st.enter_context(nc.sbuf_tensor("arena", [128, words], F32))
        self.words = words
        self.off = 0
        self.ps = []
        for i in range(8):
            t = st.enter_context(nc.psum_tensor("ps%d" % i, [128, 512], F32))
            self.ps.append((t, Buf("ps%d" % i, excl=True)))
        self.psi = 0
        self.wbufs = []
        self.wi = 0
        self.dmaq = 0
        self.vb = None
        self.nphase = 0
        self.epsc, self.epsb = self.sb("epsc", [128, 2], F32)
        self.dummy, _ = self.sb("dummy", [128, 2], F32)
        P.op("dve", "memset", PK(self.epsc[:, 0:1], EPS), writes=[self.epsb])
        P.op("dve", "memset", PK(self.epsc[:, 1:2], 64e-5), writes=[self.epsb])
        self.mark = self.off
        self.psum_default = self.psum

    def sb(self, name, shape, dt):
        n = 1
        for s_ in shape[1:]:
            n *= s_
        nbytes = n * (2 if dt == BF16 else 4)
        nw = (nbytes + 31) // 32 * 8
        assert self.off + nw <= self.words, ("arena overflow", name, self.off, nw, self.words)
        v = self.arena[0:shape[0], self.off:self.off + nw]
        self.off += nw
        if dt == BF16:
            v = v.bitcast(BF16)
        v = v[:, 0:n]
        if len(shape) == 3:
            v = v.rearrange("p (a b) -> p a b", a=shape[1])
        elif len(shape) == 4:
            v = v.rearrange("p (a b c) -> p a b c", a=shape[1], b=shape[2])
        return v, Buf("%s_%d" % (name, self.nphase))

    def new_phase(self, keep=None):
        self.P.fence(self.dummy[:, 0:1])
        self.off = self.mark if keep is None else keep
        self.wbufs = []
        self.wi = 0
        self.nphase += 1
        self.vb = None

    def init_w(self, n, cols):
        self.wbufs = []
        self.wi = 0
        for i in range(n):
            self.wbufs.append(self.sb("wb%d" % i, [128, cols], BF16))


def body_A(C, d, hh, ymb, nseg=NSEG_FULL):
    P = C.P
    if True:
        xT_d = d["xT"]; wc_d = d["wc%d" % hh]; vec_d = d["vecsA%d" % hh]
        cst_d = d["cst"]; c32_d = d["c32A"]; msk_d = d["mask5"]; rmask_d = d["rmask"]
        wup_d = d["w_up%d" % hh]; aup_d = d["a_up%d" % hh]; gupa_d = d["g_upa%d" % hh]; gupb_d = d["g_upb%d" % hh]
        bre_d = d["breT%d" % hh]; bim_d = d["bimT%d" % hh]; cre_d = d["creT%d" % hh]; cim_d = d["cimT%d" % hh]
        ya_d = d["ymT"][hh * 256:(hh + 1) * 256, :]; yb_d = d["ymT"][512 + hh * 256:512 + (hh + 1) * 256, :]
        rot = [0, 1, 2, 3, 4, 7]
        rs_ = [0]

        def psum():
            r = C.ps[rot[rs_[0] % len(rot)]]
            rs_[0] += 1
            return r
        C.psum = psum
        T = lambda name, shape, dt=F32: C.sb(name, shape, dt)
        V, vb = T("V_sb", [128, NVA]); C.vb = vb
        C.dma(V[:, :], vec_d[:, :], W=[vb])
        cst3, cstb = T("cst_sb", [128, 5, 128], BF16)
        P.op("pool", "dma_start", PK(out=cst3[:, :, :], in_=cst_d.rearrange("p (a b) -> p a b", a=5)), writes=[cstb], is_dma=True)
        c32, c32b = T("c32_sb", [128, 3, 128]); C.dma(c32[:, :, :], c32_d.rearrange("p (a b) -> p a b", a=3), W=[c32b])
        msk, mskb = T("msk_sb", [64, 320]); C.dma(msk[:, :], msk_d[:, :], W=[mskb])
        rmask, rmaskb = T("rmask_sb", [128, SEG]); C.dma(rmask[:, :], rmask_d[:, :], W=[rmaskb])
        wup, wupb = T("wup_sb", [64, 256]); C.dma(wup[:, :], wup_d[:, :], W=[wupb])
        aup, aupb = T("aup_sb", [128, 256]); C.dma(aup[:, :], aup_d[:, :], W=[aupb])
        gupa, gupab = T("gupa_sb", [128, 256]); C.dma(gupa[:, :], gupa_d[:, :], W=[gupab])
        gupb, gupbb = T("gupb_sb", [32, 256]); C.dma(gupb[:, :], gupb_d[:, :], W=[gupbb])
        wc, wcb = T("wc_sb", [128, 8, NCOLS], BF16)
        for k in range(8):
            P.op("pool", "dma_start", PK(out=wc[:, k, :], in_=wc_d[k * 128:(k + 1) * 128, :]), writes=[wcb], is_dma=True)
        scr = dict(sq=T("sq", [128, 8, 512], BF16), rs=T("rs", [128, 512]))
        ident = c32[:, C_ID, :]

        breT, breb = T("breT_sb", [128, 8, 128], BF16); bimT, bimb = T("bimT_sb", [128, 8, 128], BF16)
        P.op("pool", "dma_start", PK(out=breT[:, :, :], in_=bre_d.rearrange("q k m -> k q m")), writes=[breb], is_dma=True)
        P.op("pool", "dma_start", PK(out=bimT[:, :, :], in_=bim_d.rearrange("q k m -> k q m")), writes=[bimb], is_dma=True)
        creT, creb = T("creT_sb", [128, 8, 128]); cimT, cimb = T("cimT_sb", [128, 8, 128])
        C.dma(creT[:, :, :], cre_d.rearrange("q k m -> k q m"), W=[creb])
        C.dma(cimT[:, :, :], cim_d.rearrange("q k m -> k q m"), W=[cimb])
        s5c, s5cb = T("s5c", [128, 12, 8])
        lre = V[:, VA["lam_re"]:VA["lam_re"] + 8]; lim = V[:, VA["lam_im"]:VA["lam_im"] + 8]
        S = lambda i: s5c[:, i, :]
        dv = lambda name, **kw: P.op("dve", name, PK(**kw), reads=[s5cb, vb], writes=[s5cb])
        P.op("act", "activation", PK(out=S(0), in_=V[:, VA["log_dt"]:VA["log_dt"] + 8], func=AF.Exp), reads=[vb], writes=[s5cb])
        dv("tensor_tensor", out=S(1), in0=lim, in1=S(0), op=ALU.mult)
        dv("tensor_tensor", out=S(9), in0=lre, in1=S(0), op=ALU.mult)
        P.op("act", "activation", PK(out=S(2), in_=S(9), func=AF.Exp), reads=[s5cb], writes=[s5cb])
        P.op("act", "activation", PK(out=S(4), in_=S(1), func=AF.Sin, scale=0.125), reads=[s5cb], writes=[s5cb])
        P.op("act", "activation", PK(out=S(3), in_=S(1), func=AF.Sin, scale=-0.125, bias=V[:, VA["halfpi"]:VA["halfpi"] + 1]), reads=[s5cb, vb], writes=[s5cb])
        for _ in range(3):
            dv("tensor_tensor", out=S(9), in0=S(3), in1=S(3), op=ALU.mult)
            dv("tensor_tensor", out=S(10), in0=S(4), in1=S(4), op=ALU.mult)
            dv("tensor_tensor", out=S(11), in0=S(3), in1=S(4), op=ALU.mult)
            dv("tensor_tensor", out=S(3), in0=S(9), in1=S(10), op=ALU.subtract)
            dv("tensor_scalar", out=S(4), in0=S(11), scalar1=2.0, scalar2=None, op0=ALU.mult)
        dv("tensor_tensor", out=S(5), in0=S(2), in1=S(3), op=ALU.mult)
        dv("tensor_tensor", out=S(6), in0=S(2), in1=S(4), op=ALU.mult)
        dv("tensor_scalar_add", out=S(5), in0=S(5), scalar1=-1.0)
        dv("tensor_tensor", out=S(9), in0=lre, in1=lre, op=ALU.mult)
        dv("tensor_tensor", out=S(10), in0=lim, in1=lim, op=ALU.mult)
        dv("tensor_tensor", out=S(9), in0=S(9), in1=S(10), op=ALU.add)
        dv("reciprocal", out=S(9), in_=S(9))
        dv("tensor_tensor", out=S(10), in0=S(5), in1=lre, op=ALU.mult)
        dv("tensor_tensor", out=S(11), in0=S(6), in1=lim, op=ALU.mult)
        dv("tensor_tensor", out=S(10), in0=S(10), in1=S(11), op=ALU.add)
        dv("tensor_tensor", out=S(7), in0=S(10), in1=S(9), op=ALU.mult)
        dv("tensor_tensor", out=S(10), in0=S(6), in1=lre, op=ALU.mult)
        dv("tensor_tensor", out=S(11), in0=S(5), in1=lim, op=ALU.mult)
        dv("tensor_tensor", out=S(10), in0=S(10), in1=S(11), op=ALU.subtract)
        dv("tensor_tensor", out=S(8), in0=S(10), in1=S(9), op=ALU.mult)
        cpre, cpreb = T("cpre", [128, 8, 128], BF16); cpim, cpimb = T("cpim", [128, 8, 128], BF16)
        ctmp, ctmpb = T("ctmp", [128, 128])
        for q in range(8):
            cr = s5c[:, 7, q:q + 1]; ci = s5c[:, 8, q:q + 1]
            P.op("dve", "tensor_scalar", PK(out=ctmp[:, :], in0=cimT[:, q, :], scalar1=ci, scalar2=None, op0=ALU.mult), reads=[cimb, s5cb], writes=[ctmpb])
            P.op("dve", "scalar_tensor_tensor", PK(out=cpre[:, q, :], in0=creT[:, q, :], scalar=cr, in1=ctmp[:, :], op0=ALU.mult, op1=ALU.subtract),
                 reads=[creb, ctmpb, s5cb], writes=[cpreb])
            P.op("dve", "tensor_scalar", PK(out=ctmp[:, :], in0=cimT[:, q, :], scalar1=cr, scalar2=-1.0, op0=ALU.mult, op1=ALU.mult), reads=[cimb, s5cb], writes=[ctmpb])
            P.op("dve", "scalar_tensor_tensor", PK(out=ctmp[:, :], in0=creT[:, q, :], scalar=ci, in1=ctmp[:, :], op0=ALU.mult, op1=ALU.subtract),
                 reads=[creb, ctmpb, s5cb], writes=[ctmpb])
            P.op("dve", "tensor_scalar", PK(out=cpim[:, q, :], in0=ctmp[:, :], scalar1=-1.0, scalar2=None, op0=ALU.mult), reads=[ctmpb], writes=[cpimb])
        Fc, Fcb = T("Fc", [128, 8, SEG]); Fs, Fsb = T("Fs", [128, 8, SEG])
        ncol, ncolb = T("ncol", [128, 2])
        ftmp, ftmpb = T("ftmp", [128, SEG // 2])
        for q in range(8):
            P.op("dve", "tensor_copy", PK(out=Fc[:, q, 0:1], in_=s5c[:, 3, q:q + 1]), reads=[s5cb], writes=[Fcb])
            P.op("dve", "tensor_copy", PK(out=Fs[:, q, 0:1], in_=s5c[:, 4, q:q + 1]), reads=[s5cb], writes=[Fsb])
            n = 1
            while n < SEG:
                cn = Fc[:, q, n - 1:n]; sn = Fs[:, q, n - 1:n]
                P.op("dve", "tensor_scalar", PK(out=ftmp[:, 0:n], in0=Fs[:, q, 0:n], scalar1=sn, scalar2=None, op0=ALU.mult), reads=[Fsb], writes=[ftmpb])
                P.op("dve", "scalar_tensor_tensor", PK(out=Fc[:, q, n:2 * n], in0=Fc[:, q, 0:n], scalar=cn, in1=ftmp[:, 0:n], op0=ALU.mult, op1=ALU.subtract),
                     reads=[Fcb, ftmpb], writes=[Fcb])
                P.op("dve", "tensor_scalar", PK(out=ftmp[:, 0:n], in0=Fc[:, q, 0:n], scalar1=sn, scalar2=None, op0=ALU.mult), reads=[Fcb, Fsb], writes=[ftmpb])
                P.op("dve", "scalar_tensor_tensor", PK(out=Fs[:, q, n:2 * n], in0=Fs[:, q, 0:n], scalar=cn, in1=ftmp[:, 0:n], op0=ALU.mult, op1=ALU.add),
                     reads=[Fsb, Fcb, ftmpb], writes=[Fsb])
                n *= 2
        xst_re, xreb = T("xst_re", [128, 8, 2]); xst_im, ximb = T("xst_im", [128, 8, 2])
        P.op("dve", "memset", PK(xst_re[:, :, :], 0.0), writes=[xreb]); P.op("dve", "memset", PK(xst_im[:, :, :], 0.0), writes=[ximb])

        zT, _ = T("zT", [128, 11, SEG + 1]); zTb = [Buf("zT%d" % c) for c in range(11)]
        for c in range(11):
            P.op("dve", "memset", PK(zT[:, c, 0:1], 0.0), writes=[zTb[c]])
        Sst = [[T("S%d_%d" % (hp, i), [128, 64]) for i in range(2)] for hp in range(2)]
        for hp in range(2):
            P.op("dve", "memset", PK(Sst[hp][0][0][:, :], 0.0), writes=[Sst[hp][0][1]])
        sidx = [0, 0]
        xT_t, xTb = T("xT_sb", [128, 8, SEG], BF16); hn, hnb = T("hn", [128, 8, SEG], BF16)
        tw = xT_t[:, 0:2, :].rearrange("p a b -> p (a b)").bitcast(F32); sg0 = xT_t[:, 2:4, :].rearrange("p a b -> p (a b)").bitcast(F32)
        sg1 = xT_t[:, 4:6, :].rearrange("p a b -> p (a b)").bitcast(F32); twb = sg0b = sg1b = xTb
        names = "ld a g al be km cum Gi tA tB Rb Kb Ab Bb Kh Bh rk".split()
        W_ = {n: T("w_" + n, [128, SEG]) for n in names}
        gC, gCb = T("gC", [128, 8])
        NG = 2
        NU = 2 * NG
        AAsL = [T("AAs%d" % u, [64, 320]) for u in range(NU)]
        TML = [T("TM%d" % g, [64, 4, 128]) for g in range(NG)]
        AsqL = [[T("Asq%d_%d" % (u, i), [64, 128]) for i in range(2)] for u in range(NU)]
        ZtL = [[T("Zt%d_%d" % (u, i), [64, 64]) for i in range(2)] for u in range(NU)]
        WsbL = [T("Wsb%d" % u, [64, 64]) for u in range(NU)]
        Ut0L = [T("Ut0%d" % u, [64, 64]) for u in range(NU)]
        UtL = [T("Ut%d" % u, [64, 64]) for u in range(NU)]
        PhiL = [T("Phi%d" % g, [128, 64]) for g in range(NG)]
        ubuf, ubufb = T("ubuf", [128, 2, SEG], BF16)
        s5t = {n: T("s5_" + n, [128, SEG]) for n in "t1 t2 cre cim zre zim xre xim".split()}
        xreb16, _ = T("xre16", [128, 2, SEG], BF16); ximb16, _ = T("xim16", [128, 2, SEG], BF16)
        yacc, yaccb = T("yacc", [128, SEG])
        xre16bL = [Buf("xre16_0"), Buf("xre16_1")]; xim16bL = [Buf("xim16_0"), Buf("xim16_1")]
        yo, yob = s5t["cre"]

        for seg in range(nseg):
            tsl = slice(seg * SEG, (seg + 1) * SEG)
            P.op("pool", "dma_start", PK(out=xT_t[:, :, :], in_=xT_d[:, tsl].rearrange("(c p) n -> p c n", p=128)), writes=[xTb], is_dma=True)
            norm_fm(C, lambda c: xT_t[:, c, :], [xTb], SEG, 8, cst3[:, 0, :], cstb, lambda c: V[:, VA["n_mix"] + c:VA["n_mix"] + c + 1],
                    lambda c: hn[:, c, :], [hnb], scr)
            for c, (c0, wdt) in enumerate(ZCH):
                pt, pb = C.psum()
                for k in range(8):
                    P.op("pe", "matmul", PK(pt[0:wdt, :], wc[:, k, c0:c0 + wdt], hn[:, k, :], start=(k == 0), stop=(k == 7)), reads=[wcb, hnb], writes=[pb])
                P.op("act", "activation", PK(out=zT[0:wdt, c, 1:SEG + 1], in_=pt[0:wdt, :], func=AF.Copy), reads=[pb], writes=[zTb[c]])
            tA, tAb = W_["tA"]
            for c in range(9):
                wdt = ZCH[c][1]
                P.op("dve", "tensor_tensor", PK(out=tA[0:wdt, :], in0=zT[0:wdt, c, 0:SEG], in1=zT[0:wdt, c, 1:SEG + 1], op=ALU.subtract), reads=[zTb[c]], writes=[tAb])
                P.op("dve", "tensor_copy", PK(out=zT[0:wdt, c, 0:1], in_=zT[0:wdt, c, SEG:SEG + 1]), reads=[tAb], writes=[zTb[c]])
                P.op("dve", "scalar_tensor_tensor", PK(out=zT[0:wdt, c, 1:SEG + 1], in0=tA[0:wdt, :], scalar=V[0:wdt, VA["mu"] + c:VA["mu"] + c + 1],
                                                      in1=zT[0:wdt, c, 1:SEG + 1], op0=ALU.mult, op1=ALU.add), reads=[tAb, zTb[c], vb], writes=[zTb[c]])
            Z = lambda c, lo=0, hi=128: zT[lo:hi, c, 1:SEG + 1]
            P.op("act", "activation", PK(out=tw[0:64, :], in_=Z(6, 0, 64), func=AF.Tanh), reads=[zTb[6]], writes=[twb])
            P.op("act", "activation", PK(out=sg0[:, :], in_=Z(7), func=AF.Sigmoid), reads=[zTb[7]], writes=[sg0b])
            P.op("act", "activation", PK(out=sg1[0:32, :], in_=Z(8, 0, 32), func=AF.Sigmoid), reads=[zTb[8]], writes=[sg1b])
            for uc in range(2):
                P.op("act", "activation", PK(out=ubuf[:, uc, :], in_=Z(9 + uc), func=AF.Copy), reads=[zTb[9 + uc]], writes=[ubufb])
            def s5_pair(q):
                    uc = q // 4
                    t1, t1b = s5t["t1"]; t2, t2b = s5t["t2"]
                    cre_, creb_ = s5t["cre"]; cim_, cimb_ = s5t["cim"]
                    zre, zreb = s5t["zre"]; zim, zimb = s5t["zim"]
                    pr_, prb_ = C.psum(); pi_, pib_ = C.psum()
                    P.op("pe", "matmul", PK(pr_[:, :], breT[:, q, :], ubuf[:, uc, :], start=True, stop=True), reads=[breb, ubufb], writes=[prb_])
                    P.op("pe", "matmul", PK(pi_[:, :], bimT[:, q, :], ubuf[:, uc, :], start=True, stop=True), reads=[bimb, ubufb], writes=[pib_])
                    fc = Fc[:, q, :]; fs = Fs[:, q, :]
                    xre_, xreb_ = s5t["xre"]; xim_, ximb_ = s5t["xim"]
                    P.op("act", "activation", PK(out=xre_[:, :], in_=pr_[:, :], func=AF.Copy), reads=[prb_], writes=[xreb_])
                    P.op("act", "activation", PK(out=xim_[:, :], in_=pi_[:, :], func=AF.Copy), reads=[pib_], writes=[ximb_])
                    G_ = lambda o_, ob_, a_, ab_, b_, bb_, op_: P.op("pool", "tensor_tensor", PK(out=o_, in0=a_, in1=b_, op=op_), reads=[ab_, bb_], writes=[ob_])
                    G_(t1[:, :], t1b, xre_[:, :], xreb_, fc, Fcb, ALU.mult)
                    G_(t2[:, :], t2b, xim_[:, :], ximb_, fs, Fsb, ALU.mult)
                    G_(cre_[:, :], creb_, t1[:, :], t1b, t2[:, :], t2b, ALU.add)
                    G_(t1[:, :], t1b, xim_[:, :], ximb_, fc, Fcb, ALU.mult)
                    G_(t2[:, :], t2b, xre_[:, :], xreb_, fs, Fsb, ALU.mult)
                    G_(cim_[:, :], cimb_, t1[:, :], t1b, t2[:, :], t2b, ALU.subtract)
                    P.op("dve", "tensor_tensor_scan", PK(out=zre[:, :], data0=s5c[:, 2, q:q + 1].to_broadcast([128, SEG]), data1=cre_[:, :], initial=xst_re[:, q, 0:1], op0=ALU.mult, op1=ALU.add),
                         reads=[s5cb, creb_, xreb], writes=[zreb])
                    P.op("dve", "tensor_tensor_scan", PK(out=zim[:, :], data0=s5c[:, 2, q:q + 1].to_broadcast([128, SEG]), data1=cim_[:, :], initial=xst_im[:, q, 0:1], op0=ALU.mult, op1=ALU.add),
                         reads=[s5cb, cimb_, ximb], writes=[zimb])
                    G_(t1[:, :], t1b, zre[:, :], zreb, fc, Fcb, ALU.mult)
                    G_(t2[:, :], t2b, zim[:, :], zimb, fs, Fsb, ALU.mult)
                    G_(xre_[:, :], xreb_, t1[:, :], t1b, t2[:, :], t2b, ALU.subtract)
                    G_(t1[:, :], t1b, zim[:, :], zimb, fc, Fcb, ALU.mult)
                    G_(t2[:, :], t2b, zre[:, :], zreb, fs, Fsb, ALU.mult)
                    G_(xim_[:, :], ximb_, t1[:, :], t1b, t2[:, :], t2b, ALU.add)
                    P.op("pool", "tensor_copy", PK(out=xst_re[:, q, 0:1], in_=xre_[:, SEG - 1:SEG]), reads=[xreb_], writes=[xreb])
                    P.op("pool", "tensor_copy", PK(out=xst_im[:, q, 0:1], in_=xim_[:, SEG - 1:SEG]), reads=[ximb_], writes=[ximb])
                    xb_ = q % 2
                    P.op("act", "activation", PK(out=xreb16[:, xb_, :], in_=xre_[:, :], func=AF.Copy), reads=[xreb_], writes=[xre16bL[xb_]])
                    P.op("act", "activation", PK(out=ximb16[:, xb_, :], in_=xim_[:, :], func=AF.Copy), reads=[ximb_], writes=[xim16bL[xb_]])
                    py, pyb = C.psum()
                    qq = q % 4
                    P.op("pe", "matmul", PK(py[:, :], cpre[:, q, :], xreb16[:, xb_, :], start=True, stop=False), reads=[cpreb, xre16bL[xb_]], writes=[pyb])
                    P.op("pe", "matmul", PK(py[:, :], cpim[:, q, :], ximb16[:, xb_, :], start=False, stop=True), reads=[cpimb, xim16bL[xb_]], writes=[pyb])
                    if qq == 0:
                        P.op("act", "activation", PK(out=yacc[:, :], in_=py[:, :], func=AF.Copy), reads=[pyb], writes=[yaccb])
                    else:
                        P.op("dve", "tensor_tensor", PK(out=yacc[:, :], in0=py[:, :], in1=yacc[:, :], op=ALU.add), reads=[pyb, yaccb], writes=[yaccb])
                    if qq != 3:
                        return
                    yc = q // 4
                    t1, t1b = s5t["t1"]; t2, t2b = s5t["t2"]
                    P.op("dve", "scalar_tensor_tensor", PK(out=yo[:, :], in0=Z(9 + yc), scalar=V[:, VA["s5_d"] + yc:VA["s5_d"] + yc + 1], in1=yacc[:, :], op0=ALU.mult, op1=ALU.add),
                         reads=[zTb[9 + yc], vb, yaccb], writes=[yob])
                    P.op("act", "activation", PK(out=t1[:, :], in_=yo[:, :], func=AF.Square), reads=[yob], writes=[t1b])
                    P.op("dve", "tensor_scalar", PK(out=t1[:, :], in0=t1[:, :], scalar1=0.044715, scalar2=1.0, op0=ALU.mult, op1=ALU.add), reads=[t1b], writes=[t1b])
                    P.op("dve", "tensor_tensor", PK(out=t1[:, :], in0=t1[:, :], in1=yo[:, :], op=ALU.mult), reads=[t1b, yob], writes=[t1b])
                    P.op("act", "activation", PK(out=t2[:, :], in_=t1[:, :], func=AF.Sigmoid, scale=1.5957691216057308), reads=[t1b], writes=[t2b])
                    P.op("dve", "tensor_tensor", PK(out=t2[:, :], in0=t2[:, :], in1=yo[:, :], op=ALU.mult), reads=[t2b, yob], writes=[t2b])
                    C.dma(yb_d[yc * 128:(yc + 1) * 128, tsl], t2[:, :], R=[t2b])
            s5_next = [0]
            for hp in range(2):
                cols = slice(hp * 128, (hp + 1) * 128)
                vcol = lambda nm: V[:, VA[nm] + hp:VA[nm] + hp + 1]
                r_ = Z(hp); k_ = Z(2 + hp); v_ = Z(4 + hp)
                rb_, kb_, vb_ = zTb[hp], zTb[2 + hp], zTb[4 + hp]
                X = lambda n: W_[n][0]
                B_ = lambda n: W_[n][1]
                DV = lambda name, R, Wn, **kw: P.op("dve", name, PK(**kw), reads=R, writes=[B_(Wn)])
                pt, pb = C.psum()
                P.op("pe", "matmul", PK(pt[:, :], wup[:, cols], tw[0:64, :], start=True, stop=True), reads=[wupb, twb], writes=[pb])
                P.op("act", "activation", PK(out=X("ld")[:, :], in_=pt[:, :], func=AF.Sigmoid, bias=vcol("w0")), reads=[pb, vb], writes=[B_("ld")])
                DV("tensor_scalar", [B_("ld")], "ld", out=X("ld")[:, :], in0=X("ld")[:, :], scalar1=NEG_E05, scalar2=None, op0=ALU.mult)
                pt, pb = C.psum()
                P.op("pe", "matmul", PK(pt[:, :], aup[64:128, cols], Z(6, 64, 128), start=True, stop=True), reads=[aupb, zTb[6]], writes=[pb])
                P.op("act", "activation", PK(out=X("a")[:, :], in_=pt[:, :], func=AF.Sigmoid, bias=vcol("a0")), reads=[pb, vb], writes=[B_("a")])
                pt, pb = C.psum()
                P.op("pe", "matmul", PK(pt[:, :], gupa[:, cols], sg0[:, :], start=True, stop=False), reads=[gupab, sg0b], writes=[pb])
                P.op("pe", "matmul", PK(pt[:, :], gupb[:, cols], sg1[0:32, :], start=False, stop=True), reads=[gupbb, sg1b], writes=[pb])
                P.op("act", "activation", PK(out=X("g")[:, :], in_=pt[:, :], func=AF.Copy), reads=[pb], writes=[B_("g")])
                DV("tensor_scalar", [kb_, vb], "tA", out=X("tA")[:, :], in0=k_, scalar1=vcol("k_k"), scalar2=None, op0=ALU.mult)
                P.op("act", "activation", PK(out=X("tB")[:, :], in_=X("tA")[:, :], func=AF.Square), reads=[B_("tA")], writes=[B_("tB")])
                pt, pb = C.psum()
                P.op("pe", "matmul", PK(pt[:, :], c32[:, C_B64, :], X("tB")[:, :], start=True, stop=True), reads=[c32b, B_("tB")], writes=[pb])
                rs, rsb = scr["rs"]
                rstd_from(C, rs[:, :], pt[:, :], pb, rsb)
                DV("scalar_tensor_tensor", [B_("tA"), rsb], "al", out=X("al")[:, :], in0=X("tA")[:, :], scalar=0.125, in1=rs[:, :], op0=ALU.mult, op1=ALU.mult)
                DV("scalar_tensor_tensor", [B_("al"), B_("a")], "be", out=X("be")[:, :], in0=X("al")[:, :], scalar=-1.0, in1=X("a")[:, :], op0=ALU.mult, op1=ALU.mult)
                DV("tensor_scalar", [B_("a"), vb], "tA", out=X("tA")[:, :], in0=X("a")[:, :], scalar1=-1.0, scalar2=vcol("k_a"), op0=ALU.add, op1=ALU.mult)
                DV("scalar_tensor_tensor", [B_("tA"), kb_], "km", out=X("km")[:, :], in0=X("tA")[:, :], scalar=1.0, in1=k_, op0=ALU.add, op1=ALU.mult)
                DV("scalar_tensor_tensor", [rb_, B_("km"), vb], "rk", out=X("rk")[:, :], in0=r_, scalar=vcol("r_k"), in1=X("km")[:, :], op0=ALU.mult, op1=ALU.mult)
                DV("tensor_tensor_scan", [rmaskb, B_("ld")], "cum", out=X("cum")[:, :], data0=rmask[:, :], data1=X("ld")[:, :], initial=0.0, op0=ALU.mult, op1=ALU.add)
                cum3 = X("cum")[:, :].rearrange("p (c t) -> p c t", t=64)
                P.op("act", "activation", PK(out=X("Gi")[:, :], in_=X("cum")[:, :], func=AF.Exp), reads=[B_("cum")], writes=[B_("Gi")])
                DV("tensor_tensor", [B_("Gi"), rb_], "Rb", out=X("Rb")[:, :], in0=r_, in1=X("Gi")[:, :], op=ALU.mult)
                DV("tensor_tensor", [B_("cum"), B_("ld")], "tA", out=X("tA")[:, :], in0=X("cum")[:, :], in1=X("ld")[:, :], op=ALU.subtract)
                P.op("act", "activation", PK(out=X("Gi")[:, :], in_=X("tA")[:, :], func=AF.Exp), reads=[B_("tA")], writes=[B_("Gi")])
                DV("tensor_tensor", [B_("Gi"), B_("al")], "Ab", out=X("Ab")[:, :], in0=X("al")[:, :], in1=X("Gi")[:, :], op=ALU.mult)
                P.op("act", "activation", PK(out=X("Gi")[:, :], in_=X("cum")[:, :], func=AF.Exp, scale=-1.0), reads=[B_("cum")], writes=[B_("Gi")])
                DV("tensor_tensor", [B_("Gi"), B_("km")], "Kb", out=X("Kb")[:, :], in0=X("km")[:, :], in1=X("Gi")[:, :], op=ALU.mult)
                DV("tensor_tensor", [B_("Gi"), B_("be")], "Bb", out=X("Bb")[:, :], in0=X("be")[:, :], in1=X("Gi")[:, :], op=ALU.mult)
                tA3 = X("tA")[:, :].rearrange("p (c t) -> p c t", t=64)
                DV("tensor_tensor", [B_("cum")], "tA", out=tA3, in0=cum3[:, :, 63:64].to_broadcast([128, 8, 64]), in1=cum3, op=ALU.subtract)
                P.op("act", "activation", PK(out=X("Gi")[:, :], in_=X("tA")[:, :], func=AF.Exp), reads=[B_("tA")], writes=[B_("Gi")])
                DV("tensor_tensor", [B_("Gi"), B_("km")], "Kh", out=X("Kh")[:, :], in0=X("km")[:, :], in1=X("Gi")[:, :], op=ALU.mult)
                DV("tensor_tensor", [B_("Gi"), B_("be")], "Bh", out=X("Bh")[:, :], in0=X("be")[:, :], in1=X("Gi")[:, :], op=ALU.mult)
                P.op("act", "activation", PK(out=gC[:, :], in_=cum3[:, :, 63], func=AF.Exp), reads=[B_("cum")], writes=[gCb])
                poL = [C.ps[6], C.ps[5]]
                vsrcs = ((v_, vb_), (X("Ab")[:, :], B_("Ab")), (X("Bh")[:, :], B_("Bh")), (X("Kh")[:, :], B_("Kh")))
                for g0 in range(0, SEG // 64, NG):
                    units = [(gi, e) for gi in range(NG) for e in range(2)]
                    CS = [slice((g0 + gi) * 64, (g0 + gi + 1) * 64) for gi in range(NG)]
                    PR = [slice(0, 64), slice(64, 128)]
                    for gi in range(NG):
                        pt, pb = C.psum()
                        for i, (src, sb_) in enumerate(vsrcs):
                            P.op("pe", "transpose", PK(pt[0:64, i * 128:(i + 1) * 128], src[:, CS[gi]], ident), reads=[sb_, c32b], writes=[pb])
                        P.op("act", "activation", PK(out=TML[gi][0][:, :, :], in_=pt[0:64, :].rearrange("p (i k) -> p i k", i=4), func=AF.Copy), reads=[pb], writes=[TML[gi][1]])
                    pts = []
                    for u, (gi, e) in enumerate(units):
                        pr, cs = PR[e], CS[gi]
                        pt, pb = C.psum()
                        Bb_, Kb_, Ab_, Rb_ = X("Bb")[pr, cs], X("Kb")[pr, cs], X("Ab")[pr, cs], X("Rb")[pr, cs]
                        P.op("pe", "matmul", PK(pt[0:64, 0:64], Bb_, Ab_, start=True, stop=True), reads=[B_("Bb"), B_("Ab")], writes=[pb])
                        P.op("pe", "matmul", PK(pt[0:64, 64:128], Bb_, Rb_, start=True, stop=True), reads=[B_("Bb"), B_("Rb")], writes=[pb])
                        P.op("pe", "matmul", PK(pt[0:64, 128:192], Kb_, Ab_, start=True, stop=True), reads=[B_("Kb"), B_("Ab")], writes=[pb])
                        P.op("pe", "matmul", PK(pt[0:64, 192:256], Kb_, Rb_, start=True, stop=True), reads=[B_("Kb"), B_("Rb")], writes=[pb])
                        P.op("pe", "matmul", PK(pt[0:64, 256:320], Ab_, Bb_, start=True, stop=True), reads=[B_("Bb"), B_("Ab")], writes=[pb])
                        pts.append((pt, pb))
                    for u in range(NU):
                        pt, pb = pts[u]
                        P.op("dve", "tensor_tensor", PK(out=AAsL[u][0][:, :], in0=pt[0:64, 0:320], in1=msk[:, :], op=ALU.mult), reads=[pb, mskb], writes=[AAsL[u][1]])
                    for u in range(NU):
                        AAs, AAsb = AAsL[u]
                        P.op("dve", "tensor_tensor", PK(out=ZtL[u][0][0][:, :], in0=AAs[:, 0:64], in1=ident[0:64, 0:64], op=ALU.add), reads=[AAsb, c32b], writes=[ZtL[u][0][1]])
                    cur = [(AAsL[u][0][:, 0:64], AAsL[u][0][:, 256:320], AAsL[u][1]) for u in range(NU)]
                    zi = 0
                    for lvl in range(1, 6):
                        lo = 0 if lvl < 5 else 64
                        pts = []
                        for u in range(NU):
                            curA, curAT, curb = cur[u]
                            psq, psqb = C.psum()
                            if lvl < 5:
                                P.op("pe", "matmul", PK(psq[0:64, 0:64], curAT, curA, start=True, stop=True), reads=[curb], writes=[psqb])
                            P.op("pe", "matmul", PK(psq[0:64, 64:128], curA, curAT, start=True, stop=True), reads=[curb], writes=[psqb])
                            pts.append((psq, psqb))
                        for u in range(NU):
                            psq, psqb = pts[u]
                            at, atb = AsqL[u][lvl % 2]
                            P.op("act", "activation", PK(out=at[:, lo:128], in_=psq[0:64, lo:128], func=AF.Copy), reads=[psqb], writes=[atb])
                            cur[u] = (at[:, 0:64], at[:, 64:128], atb)
                        pts = []
                        for u in range(NU):
                            pz, pzb = C.psum()
                            P.op("pe", "matmul", PK(pz[0:64, 0:64], cur[u][1], ZtL[u][zi][0][:, :], start=True, stop=True), reads=[cur[u][2], ZtL[u][zi][1]], writes=[pzb])
                            pts.append((pz, pzb))
                        for u in range(NU):
                            pz, pzb = pts[u]
                            P.op("dve", "tensor_tensor", PK(out=ZtL[u][1 - zi][0][:, :], in0=pz[0:64, 0:64], in1=ZtL[u][zi][0][:, :], op=ALU.add),
                                 reads=[pzb, ZtL[u][zi][1]], writes=[ZtL[u][1 - zi][1]])
                        zi = 1 - zi
                    pts = []
                    for u, (gi, e) in enumerate(units):
                        es = PR[e]
                        pt, pb = C.psum()
                        P.op("pe", "matmul", PK(pt[0:64, 0:64], AAsL[u][0][:, 128:192], TML[gi][0][:, 0, es], start=True, stop=True), reads=[AAsL[u][1], TML[gi][1]], writes=[pb])
                        pts.append((pt, pb))
                    for u in range(NU):
                        pt, pb = pts[u]
                        P.op("act", "activation", PK(out=WsbL[u][0][:, :], in_=pt[0:64, 0:64], func=AF.Copy), reads=[pb], writes=[WsbL[u][1]])
                    pts = []
                    for u, (gi, e) in enumerate(units):
                        pr, es = PR[e], PR[e]
                        Tm, Tmb = ZtL[u][zi]
                        pt, pb = C.psum()
                        P.op("pe", "matmul", PK(pt[0:64, 0:64], Tm[:, :], WsbL[u][0][:, :], start=True, stop=True), reads=[Tmb, WsbL[u][1]], writes=[pb])
                        P.op("pe", "matmul", PK(pt[pr, 64:128], TML[gi][0][:, 1, es], Tm[:, :], start=True, stop=True), reads=[Tmb, TML[gi][1]], writes=[pb])
                        pts.append((pt, pb))
                    for u, (gi, e) in enumerate(units):
                        pt, pb = pts[u]
                        pr = PR[e]
                        P.op("act", "activation", PK(out=Ut0L[u][0][:, :], in_=pt[0:64, 0:64], func=AF.Copy), reads=[pb], writes=[Ut0L[u][1]])
                        P.op("act", "activation", PK(out=PhiL[gi][0][pr, :], in_=pt[pr, 64:128], func=AF.Copy), reads=[pb], writes=[PhiL[gi][1]])
                    for gi in range(NG):
                        c = g0 + gi
                        cs = CS[gi]
                        Sc, Scb = Sst[hp][sidx[hp] % 2]
                        Sn, Snb = Sst[hp][(sidx[hp] + 1) % 2]
                        TM, TMb = TML[gi]
                        Phi, Phib = PhiL[gi]
                        pts = []
                        for e in range(2):
                            pr = PR[e]
                            pt, pb = C.psum()
                            P.op("pe", "matmul", PK(pt[0:64, 0:64], Phi[pr, :], Sc[pr, :], start=True, stop=True), reads=[Phib, Scb], writes=[pb])
                            pts.append((pt, pb))
                        for e in range(2):
                            u = gi * 2 + e
                            pt, pb = pts[e]
                            P.op("dve", "tensor_tensor", PK(out=UtL[u][0][:, :], in0=pt[0:64, 0:64], in1=Ut0L[u][0][:, :], op=ALU.add), reads=[pb, Ut0L[u][1]], writes=[UtL[u][1]])
                        for e in range(2):
                            u = gi * 2 + e
                            pr = PR[e]
                            AAs, AAsb = AAsL[u]
                            Ut, Utb = UtL[u]
                            po, pob = poL[e]
                            mm1 = P.op("pe", "matmul", PK(po[pr, cs], Sc[pr, :], X("Rb")[pr, cs], start=True, stop=False), reads=[Scb, B_("Rb")], writes=[pob])
                            P.op("pe", "matmul", PK(po[pr, cs], Ut[:, :], AAs[:, 64:128], start=False, stop=False), reads=[Utb, AAsb], writes=[pob],
                                 after=([mm1] if e == 1 else []))
                            P.op("pe", "matmul", PK(po[pr, cs], TM[:, 0, pr], AAs[:, 192:256], start=False, stop=True), reads=[TMb, AAsb], writes=[pob])
                        pts = []
                        for e in range(2):
                            u = gi * 2 + e
                            pr = PR[e]
                            Ut, Utb = UtL[u]
                            pt, pb = C.psum()
                            P.op("pe", "matmul", PK(pt[pr, 0:64], TM[:, 2, pr], Ut[:, :], start=True, stop=False), reads=[TMb, Utb], writes=[pb])
                            P.op("pe", "matmul", PK(pt[pr, 0:64], TM[:, 3, pr], TM[:, 0, pr], start=False, stop=True), reads=[TMb], writes=[pb])
                            pts.append((pt, pb))
                        for e in range(2):
                            pr = PR[e]
                            pt, pb = pts[e]
                            P.op("dve", "scalar_tensor_tensor", PK(out=Sn[pr, :], in0=Sc[pr, :], scalar=gC[pr, c:c + 1], in1=pt[pr, 0:64], op0=ALU.mult, op1=ALU.add),
                                 reads=[Scb, gCb, pb], writes=[Snb])
                        sidx[hp] += 1
                    s5_pair(s5_next[0]); s5_next[0] += 1
                Osb, Osbb = W_["Gi"]
                P.op("act", "activation", PK(out=Osb[0:64, :], in_=poL[0][0][0:64, :], func=AF.Copy), reads=[poL[0][1]], writes=[Osbb])
                P.op("act", "activation", PK(out=Osb[64:128, :], in_=poL[1][0][64:128, :], func=AF.Copy), reads=[poL[1][1]], writes=[Osbb])
                pm, pmb = C.psum()
                P.op("pe", "matmul", PK(pm[:, :], c32[:, C_B64, :], Osb[:, :], start=True, stop=True), reads=[c32b, Osbb], writes=[pmb])
                DV("tensor_tensor", [Osbb, pmb], "tA", out=X("tA")[:, :], in0=Osb[:, :], in1=pm[:, :], op=ALU.subtract)
                P.op("act", "activation", PK(out=X("tB")[:, :], in_=X("tA")[:, :], func=AF.Square), reads=[B_("tA")], writes=[B_("tB")])
                pv, pvb = C.psum()
                P.op("pe", "matmul", PK(pv[:, :], c32[:, C_B64, :], X("tB")[:, :], start=True, stop=True), reads=[c32b, B_("tB")], writes=[pvb])
                rstd_from(C, rs[:, :], pv[:, :], pvb, rsb, eps=64e-5)
                DV("tensor_tensor", [B_("tA"), rsb], "tA", out=X("tA")[:, :], in0=X("tA")[:, :], in1=rs[:, :], op=ALU.mult)
                P.op("act", "activation", PK(out=X("tB")[:, :], in_=X("tA")[:, :], func=AF.Identity, scale=vcol("ln_w"), bias=vcol("ln_b")),
                     reads=[B_("tA"), vb], writes=[B_("tB")])
                pk, pkb = C.psum()
                P.op("pe", "matmul", PK(pk[:, :], c32[:, C_B64, :], X("rk")[:, :], start=True, stop=True), reads=[c32b, B_("rk")], writes=[pkb])
                DV("scalar_tensor_tensor", [pkb, vb_], "tA", out=X("tA")[:, :], in0=pk[:, :], scalar=64.0, in1=v_, op0=ALU.mult, op1=ALU.mult)
                DV("tensor_tensor", [B_("tA"), B_("tB")], "tB", out=X("tB")[:, :], in0=X("tB")[:, :], in1=X("tA")[:, :], op=ALU.add)
                DV("tensor_tensor", [B_("tB"), B_("g")], "Gi", out=Osb[:, :], in0=X("tB")[:, :], in1=X("g")[:, :], op=ALU.mult)
                C.dma(ya_d[hp * 128:(hp + 1) * 128, tsl], Osb[:, :], R=[Osbb])

        C.psum = C.psum_default


def body_B(C, d, half, bufs):
    P = C.P
    t0 = half * NTOK
    if True:
        xT_d = d["xT"][:, t0:t0 + NTOK]; ymT_d = d["ymT"][:, t0:t0 + NTOK]; memT_d = d["memT"]
        vec_d = d["vecsB"]; cst_d = d["cst"]
        wglu_d = d["w_glu"]; wout_d = d["w_out"]; wq_d = d["w_q0"]; wkv_d = d["w_kv0"]; wo_d = d["w_o0"]
        wg_d = d["ff_g"]; wu_d = d["ff_u"]; wd_d = d["ff_d"]; wqkv_d = d["w_qkv"]
        h1T_d = d["h1T"][:, t0:t0 + NTOK]; qT_d = d["qT"][:, :, t0:t0 + NTOK]; kT_d = d["kT"][:, :, t0:t0 + NTOK]
        vtok_d = d["vtok"][t0:t0 + NTOK, :]
        ymb, h1b, qb_, kb_, vtb_ = bufs
        C.init_w(4, 4096)
        hT, _ = C.sb("hT", [128, 8, NTOK], F32); hTb = [Buf("hT%d" % i) for i in range(NTT)]
        bufA, _ = C.sb("bufA", [128, 8, NTOK], BF16); bufAb = [Buf("bA%d" % i) for i in range(NTT)]
        bufH, _ = C.sb("bufH", [128, 8, NTOK], BF16); bufHb = [Buf("bH%d" % i) for i in range(NTT)]
        V, vb = C.sb("V_sb", [128, NVB], F32); C.vb = vb
        cst3, cstb = C.sb("cst_sb", [128, 5, 128], BF16)
        scr = dict(sq=C.sb("sq", [128, 8, 512], BF16), rs=C.sb("rs", [128, 512], F32), sg=C.sb("sg", [128, 512], F32),
                   rd=C.sb("rd", [128, 512], F32))
        sqq, _ = C.sb("sqq", [128, 2, NTT, 512], BF16)
        scr["sqq"] = (sqq, [Buf("sqq%d" % i) for i in range(NTT)])
        et, _ = C.sb("et", [128, 2, 512], BF16)
        scr["et"] = (et, [Buf("et0"), Buf("et1")])
        C.dma(V[:, :], vec_d[:, :], W=[vb])
        P.op("pool", "dma_start", PK(out=cst3[:, :, :], in_=cst_d.rearrange("p (a b) -> p a b", a=5)), writes=[cstb], is_dma=True)
        for tt in range(NTT):
            sl = slice(tt * TT, (tt + 1) * TT)
            C.dma(hT[:, :, sl], xT_d[:, sl].rearrange("(c p) n -> p c n", p=128), W=[hTb[tt]])
            P.op("pool", "dma_start", PK(out=bufA[:, :, sl], in_=ymT_d[:, sl].rearrange("(c p) n -> p c n", p=128)),
                 reads=[ymb], writes=[bufAb[tt]], is_dma=True)
        wv, wb = C.load_w(wglu_d[:, :], 4, 512)
        sg, sgb = scr["sg"]
        for tt in range(NTT):
            sl = slice(tt * TT, (tt + 1) * TT)
            pts = []
            for oc in range(4):
                pt, pb = C.psum()
                for k in range(4):
                    P.op("pe", "matmul", PK(pt[:, :], wv[:, k, oc * 128:(oc + 1) * 128], bufA[:, 4 + k, sl],
                                                                            start=(k == 0), stop=(k == 3)), reads=[wb, bufAb[tt]], writes=[pb])
                pts.append((pt, pb))
            for oc in range(4):
                pt, pb = pts[oc]
                P.op("act", "activation", PK(out=sg[:, :], in_=pt[:, :], func=AF.Sigmoid,
                                                                bias=V[:, VB["b_glu"] + oc:VB["b_glu"] + oc + 1]), reads=[pb, vb], writes=[sgb])
                P.op("dve", "tensor_tensor", PK(out=bufA[:, 4 + oc, sl], in0=bufA[:, 4 + oc, sl], in1=sg[:, :], op=ALU.mult),
                     reads=[sgb, bufAb[tt]], writes=[bufAb[tt]])

        def evac_res(oc, tt, pt, pb, m):
            sl = slice(tt * TT, (tt + 1) * TT)
            P.op("dve", "tensor_tensor", PK(out=hT[:, oc, sl], in0=pt[:, :], in1=hT[:, oc, sl], op=ALU.add), reads=[pb, hTb[tt]], writes=[hTb[tt]])
        linear_fm(C, wout_d, 8, bufA, None, 1024, evac_res, xinbs=bufAb)
        (kn, knb), (vt, vtb) = mem_kv(C, memT_d, wkv_d, V, vb, VB["n_mem"], VB["k_gain"], cst3, cstb, scr, "m0")
        mem_xattn(C, hT, hTb, bufA, bufAb, bufH, bufHb, V, vb, VB["n_xattn"], VB["q_gain"], wq_d, wo_d, kn, knb, vt, vtb, cst3, cstb, scr)
        swiglu_ffn(C, hT, hTb, bufA, bufAb, bufH, bufHb, V, vb, VB["n_ffn"], wg_d, wu_d, wd_d, 2816, cst3, cstb, scr)
        for tt in range(NTT):
            sl = slice(tt * TT, (tt + 1) * TT)
            C.dma(h1T_d[:, sl].rearrange("(c p) n -> p c n", p=128), hT[:, :, sl], R=[hTb[tt]])
        for tt in range(NTT):
            sl = slice(tt * TT, (tt + 1) * TT)
            norm_fm(C, lambda c: hT[:, c, sl], [hTb[tt]], TT, 8, cst3[:, 0, :], cstb, lambda c: V[:, VB["n_mix1"] + c:VB["n_mix1"] + c + 1],
                    lambda c: bufA[:, c, sl], [bufAb[tt]], scr)
        P.fence(C.dummy[:, 0:1])
        NS = 4
        sqL = [(bufH[:, 0, i * 512:(i + 1) * 512], Buf("qk_sq%d" % i)) for i in range(NS)]
        f32v = [bufH[:, 1 + i, :].bitcast(F32) for i in range(4)]
        tl = [(f32v[i // 2][:, (i % 2) * 512:(i % 2 + 1) * 512], Buf("qk_t%d" % i)) for i in range(8)]
        rsL, qoL = tl[0:4], tl[4:8]
        qi = [0]
        P.op("dve", "tensor_scalar", PK(out=V[:, VB["da_qg"]:VB["da_qg"] + 1], in0=V[:, VB["da_qg"]:VB["da_qg"] + 1], scalar1=0.125, scalar2=None, op0=ALU.mult),
             reads=[vb], writes=[vb])
        for which, (dst, gcol) in enumerate(((qT_d, VB["da_qg"]), (kT_d, VB["da_kg"]))):
            def evac_qk(oc, tt, pt, pb, m, dst=dst, gcol=gcol):
                sl = slice(tt * TT, (tt + 1) * TT)
                k_ = qi[0] % NS; qi[0] += 1
                sq1, sq1b = sqL[k_]; rs, rsb = rsL[k_]; qo, qob = qoL[k_]
                P.op("act", "activation", PK(out=sq1, in_=pt[:, :], func=AF.Square), reads=[pb], writes=[sq1b])
                p2, p2b = C.psum()
                P.op("pe", "matmul", PK(p2[:, :], cst3[:, 2, :], sq1, start=True, stop=True), reads=[sq1b, cstb], writes=[p2b])
                rstd_from(C, rs, p2[:, :], p2b, rsb)
                P.op("dve", "scalar_tensor_tensor", PK(out=qo, in0=pt[:, :], scalar=V[:, gcol:gcol + 1], in1=rs,
                                                      op0=ALU.mult, op1=ALU.mult), reads=[pb, rsb, vb], writes=[qob])
                C.dma(dst[2 * oc:2 * oc + 2, :, sl].rearrange("c r n -> (c r) n"), qo, R=[qob])
            linear_fm(C, wqkv_d, 8, bufA, None, 1024, evac_qk, col0=which * 1024, xinbs=bufAb)
        vo, vob = scr["sg"]
        for half2 in range(2):
            wv2, wb2 = C.load_w(wqkv_d[:, 2048 + half2 * 512:2048 + (half2 + 1) * 512], 8, 512)
            for s in range(16):
                tt = s // 4
                pt, pb = C.psum()
                for k in range(8):
                    P.op("pe", "matmul", PK(pt[:, :], bufA[:, k, s * 128:(s + 1) * 128], wv2[:, k, :], start=(k == 0), stop=(k == 7)),
                         reads=[wb2, bufAb[tt]], writes=[pb])
                P.op("act", "activation", PK(out=vo[:, :], in_=pt[:, :], func=AF.Copy), reads=[pb], writes=[vob])
                C.dma(vtok_d[s * 128:(s + 1) * 128, half2 * 512:(half2 + 1) * 512], vo[:, :], R=[vob])


def body_C1(C, d, bufA, bufAb, bufs):
    P = C.P
    if True:
        qs_d = d["qT"]; ks_d = d["kT"]; vf_d = d["vtok"]; qa4_d = d["qaug4"]; ka4_d = d["kaug4"]
        bias_d = d["biasT"]; lamb_d = d["lamb"]; sgc_d = d["sgc"]; cst_d = d["cst"]; sel_d = d["sel"]
        h1b, qb_, kb_, vtb_ = bufs
        cst3, cstb = C.sb("cst_sb", [128, 5, 128], BF16)
        P.op("pool", "dma_start", PK(out=cst3[:, :, :], in_=cst_d.rearrange("p (a b) -> p a b", a=5)), writes=[cstb], is_dma=True)
        biasT, biasb = C.sb("bias_sb", [128, 8, 512], F32)
        C.dma(biasT[:, :, :], bias_d.rearrange("p (a b) -> p a b", a=8), W=[biasb])
        lamb, lambb = C.sb("lamb_sb", [128, 4, 64], F32)
        C.dma(lamb[:, :, :], lamb_d.rearrange("p (a b) -> p a b", a=4), W=[lambb])
        sgc, sgcb = C.sb("sgc_sb", [128, 1], F32)
        C.dma(sgc[:, :], sgc_d[:, :], W=[sgcb])
        scr = dict(sq=C.sb("sq", [128, 1, 512], BF16), rs=C.sb("rs", [128, 512], F32))
        lt, ltb = C.sb("lt", [128, 2, 64], F32)
        lc, lcb = C.sb("lc", [128, 4], F32)
        for i in range(2):
            P.op("dve", "tensor_tensor", PK(out=lt[:, i, :], in0=lamb[:, 2 * i, :], in1=lamb[:, 2 * i + 1, :], op=ALU.mult), reads=[lambb], writes=[ltb])
            P.op("dve", "reduce_sum", PK(out=lc[:, i:i + 1], in_=lt[:, i, :], axis=AX.X), reads=[ltb], writes=[lcb])
        P.op("act", "activation", PK(out=lc[:, 0:2], in_=lc[:, 0:2], func=AF.Exp), reads=[lcb], writes=[lcb])
        P.op("dve", "tensor_tensor", PK(out=lc[:, 2:3], in0=lc[:, 1:2], in1=lc[:, 0:1], op=ALU.subtract), reads=[lcb], writes=[lcb])
        P.op("dve", "tensor_scalar_add", PK(out=lc[:, 3:4], in0=lc[:, 2:3], scalar1=-LAMBDA_INIT), reads=[lcb], writes=[lcb])
        P.op("dve", "tensor_scalar", PK(out=sgc[:, :], in0=sgc[:, :], scalar1=float(1.0 - LAMBDA_INIT), scalar2=None, op0=ALU.mult), reads=[sgcb], writes=[sgcb])
        kA = [C.sb("kA%d" % i, [68, 2, 4096], BF16) for i in range(2)]
        qA = [C.sb("qA%d" % i, [68, 2, NTOK], BF16) for i in range(2)]
        vA = [C.sb("vA%d" % i, [128, 32, 128], BF16) for i in range(2)]
        qraw, qrawb = C.sb("qraw", [64, 2, 4096], BF16)
        sel, selb = C.sb("sel_sb", [128, 2], F32)
        C.dma(sel[:, :], sel_d[:, :], W=[selb])
        for i_ in range(2):
            for c_ in range(2):
                P.op("pool", "dma_start", PK(out=qA[i_][0][64:68, c_, :], in_=qa4_d[:, :]), writes=[qA[i_][1]], is_dma=True)
        et, _ = C.sb("et", [128, 4, 512], BF16); etb = [Buf("et%d" % i) for i in range(4)]
        tmp, _ = C.sb("tmp", [128, 2, 512], F32); tmpb = [Buf("tmp%d" % i) for i in range(2)]
        rd, rdb = C.sb("rd", [128, 512], F32)
        o0, o0b = C.sb("o0", [128, 512], F32)
        o1, o1b = C.sb("o1", [128, 512], F32)
        ot, otb = C.sb("ot", [128, 512], F32)
        ei = 0
        ti_ = 0
        for h in range(8):
            slope = float(2.0 ** (-(h + 1)))
            kt, ktb = kA[h % 2]; qt, qtb = qA[h % 2]; vt, vtb = vA[h % 2]
            P.op("pool", "dma_start", PK(out=kt[0:64, :, :], in_=ks_d[2 * h:2 * h + 2].rearrange("c r n -> r c n")), reads=[kb_], writes=[ktb], is_dma=True)
            P.op("pool", "dma_start", PK(out=kt[64:68, 0, :], in_=ka4_d[h]), writes=[ktb], is_dma=True)
            P.op("pool", "dma_start", PK(out=kt[64:68, 1, :], in_=ka4_d[h]), writes=[ktb], is_dma=True)
            P.op("pool", "dma_start", PK(out=qraw[:, :, :], in_=qs_d[2 * h:2 * h + 2].rearrange("c r n -> r c n")), reads=[qb_], writes=[qrawb], is_dma=True)
            for c in range(2):
                q5 = qraw[:, c, :].rearrange("p (m two n) -> p m two n", two=2, n=512)
                qo4 = qt[0:64, c, :].rearrange("p (m n) -> p m n", n=512)
                P.op("dve", "tensor_scalar", PK(out=qo4, in0=q5[:, :, 0, :], scalar1=sel[0:64, 0:1], scalar2=None, op0=ALU.mult), reads=[qrawb, selb], writes=[qtb])
                P.op("dve", "scalar_tensor_tensor", PK(out=qo4, in0=q5[:, :, 1, :], scalar=sel[0:64, 1:2], in1=qo4, op0=ALU.mult, op1=ALU.add),
                     reads=[qrawb, selb, qtb], writes=[qtb])
            P.op("pool", "dma_start", PK(out=vt[:, :, :], in_=vf_d[:, h * 128:(h + 1) * 128].rearrange("(j p) d -> p j d", p=128)), reads=[vtb_], writes=[vtb], is_dma=True)
            for m in range(4):
                nkb = 8 * (m + 1)
                qsl = slice(m * 512, (m + 1) * 512)
                N = [C.ps[0], C.ps[1]]
                Dn = [C.ps[2], C.ps[3]]
                def issue_scores(j):
                    for c in range(2):
                        sc, scb = C.ps[4 + ((2 * j + c) % 4)]
                        P.op("pe", "matmul", PK(sc[:, :], kt[:, c, j * 128:(j + 1) * 128], qt[:, c, qsl], start=True, stop=True),
                             reads=[ktb, qtb], writes=[scb])
                issue_scores(0)
                for j in range(nkb):
                    if j + 1 < nkb:
                        issue_scores(j + 1)
                    for c in range(2):
                        sc, scb = C.ps[4 + ((2 * j + c) % 4)]
                        e_slot = ei % 4; ei += 1
                        if j >= nkb - 8:
                            s = j - (nkb - 8)
                            tb = ti_ % 2; ti_ += 1
                            P.op("dve", "scalar_tensor_tensor", PK(out=tmp[:, tb, :], in0=biasT[:, s, :], scalar=slope, in1=sc[:, :], op0=ALU.mult, op1=ALU.add),
                                 reads=[biasb, scb], writes=[tmpb[tb]])
                            P.op("act", "activation", PK(out=et[:, e_slot, :], in_=tmp[:, tb, :], func=AF.Exp), reads=[tmpb[tb]], writes=[etb[e_slot]])
                        else:
                            P.op("act", "activation", PK(out=et[:, e_slot, :], in_=sc[:, :], func=AF.Exp), reads=[scb], writes=[etb[e_slot]])
                        P.op("pe", "matmul", PK(N[c][0][:, :], vt[:, j, :], et[:, e_slot, :], start=(j == 0), stop=(j == nkb - 1)),
                             reads=[vtb, etb[e_slot]], writes=[N[c][1]])
                        P.op("pe", "matmul", PK(Dn[c][0][:, :], cst3[:, 4, :], et[:, e_slot, :], start=(j == 0), stop=(j == nkb - 1)),
                             reads=[cstb, etb[e_slot]], writes=[Dn[c][1]])
                P.op("dve", "reciprocal", PK(out=rd[:, :], in_=Dn[0][0][:, :]), reads=[Dn[0][1]], writes=[rdb])
                P.op("dve", "tensor_tensor", PK(out=o0[:, :], in0=N[0][0][:, :], in1=rd[:, :], op=ALU.mult), reads=[N[0][1], rdb], writes=[o0b])
                P.op("dve", "reciprocal", PK(out=rd[:, :], in_=Dn[1][0][:, :]), reads=[Dn[1][1]], writes=[rdb])
                P.op("dve", "tensor_tensor", PK(out=o1[:, :], in0=N[1][0][:, :], in1=rd[:, :], op=ALU.mult), reads=[N[1][1], rdb], writes=[o1b])
                P.op("dve", "scalar_tensor_tensor", PK(out=o0[:, :], in0=o1[:, :], scalar=lc[:, 3:4], in1=o0[:, :], op0=ALU.mult, op1=ALU.add),
                     reads=[o1b, o0b, lcb], writes=[o0b])
                sq, sqb = scr["sq"]; rs, rsb = scr["rs"]
                P.op("act", "activation", PK(out=sq[:, 0, :], in_=o0[:, :], func=AF.Square), reads=[o0b], writes=[sqb])
                pn, pnb = C.ps[4]
                P.op("pe", "matmul", PK(pn[:, :], cst3[:, 3, :], sq[:, 0, :], start=True, stop=True), reads=[sqb, cstb], writes=[pnb])
                rstd_from(C, rs[:, :], pn[:, :], pnb, rsb)
                P.op("dve", "scalar_tensor_tensor", PK(out=bufA[:, h, qsl], in0=o0[:, :], scalar=sgc[:, 0:1], in1=rs[:, :], op0=ALU.mult, op1=ALU.mult),
                     reads=[o0b, rsb, sgcb], writes=[bufAb[m]])


def body_C2(C, d, bufA, bufAb, h1b):
    P = C.P
    if True:
        h1s_d = d["h1T"]; memT_d = d["memT"]; vec_d = d["vecsC"]; cst_d = d["cst"]; c32_d = d["c32"]; sel_d = d["sel"]
        wdo_d = d["da_w_o"]; wq_d = d["w_q1"]; wkv_d = d["w_kv1"]; wo_d = d["w_o1"]; wr_d = d["w_router"]
        wg_d = d["moe_g"]; wu_d = d["moe_u"]; wd_d = d["moe_d"]; outT_d = d["outT"]
        C.init_w(3, 4096)
        hT, _ = C.sb("hT", [128, 8, NTOK], F32); hTb = [Buf("hT%d" % i) for i in range(NTT)]
        bufH, _ = C.sb("bufH", [128, 8, NTOK], BF16); bufHb = [Buf("bH%d" % i) for i in range(NTT)]
        V, vb = C.sb("V_sb", [128, NVC], F32); C.vb = vb
        cst3, cstb = C.sb("cst_sb", [128, 5, 128], BF16)
        c32, c32b = C.sb("c32_sb", [128, 2, 128], F32)
        scr = dict(sq=C.sb("sq", [128, 8, 512], BF16), rs=C.sb("rs", [128, 512], F32), sg=C.sb("sg", [128, 512], F32),
                   rd=C.sb("rd", [128, 512], F32))
        sqq, _ = C.sb("sqq", [128, 2, NTT, 512], BF16)
        scr["sqq"] = (sqq, [Buf("sqq%d" % i) for i in range(NTT)])
        et, _ = C.sb("et", [128, 2, 512], BF16)
        scr["et"] = (et, [Buf("et0"), Buf("et1")])
        C.dma(V[:, :], vec_d[:, :], W=[vb])
        C.dma(c32[:, :, :], c32_d.rearrange("p (a b) -> p a b", a=2), W=[c32b])
        P.op("pool", "dma_start", PK(out=cst3[:, :, :], in_=cst_d.rearrange("p (a b) -> p a b", a=5)), writes=[cstb], is_dma=True)
        sel, selb = C.sb("sel_sb", [128, 2], F32)
        C.dma(sel[:, :], sel_d[:, :], W=[selb])
        htmp, _ = C.sb("htmp", [128, 2, 512], F32); htmpb = [Buf("htmp0"), Buf("htmp1")]
        hi_ = 0
        for m in range(NTT):
            sl = slice(m * TT, (m + 1) * TT)
            e0 = slice((2 * m) * TT, (2 * m + 1) * TT); e1 = slice((2 * m + 1) * TT, (2 * m + 2) * TT)
            C.dma(hT[:, :, sl], h1s_d[:, e0].rearrange("(c p) n -> p c n", p=128), R=[h1b], W=[hTb[m]])
            for c in range(8):
                hb = hi_ % 2; hi_ += 1
                C.dma(htmp[:, hb, :], h1s_d[c * 128:(c + 1) * 128, e1], R=[h1b], W=[htmpb[hb]])
                P.op("dve", "tensor_scalar", PK(out=hT[:, c, sl], in0=hT[:, c, sl], scalar1=sel[:, 0:1], scalar2=None, op0=ALU.mult), reads=[hTb[m], selb], writes=[hTb[m]])
                P.op("dve", "scalar_tensor_tensor", PK(out=hT[:, c, sl], in0=htmp[:, hb, :], scalar=sel[:, 1:2], in1=hT[:, c, sl], op0=ALU.mult, op1=ALU.add),
                     reads=[htmpb[hb], selb, hTb[m]], writes=[hTb[m]])
        def evac_res(oc, tt, pt, pb, m):
            sl = slice(tt * TT, (tt + 1) * TT)
            P.op("dve", "tensor_tensor", PK(out=hT[:, oc, sl], in0=pt[:, :], in1=hT[:, oc, sl], op=ALU.add), reads=[pb, hTb[tt]], writes=[hTb[tt]])
        linear_fm(C, wdo_d, 8, bufA, None, 1024, evac_res, xinbs=bufAb)
        _mem_off0 = C.off
        (kn, knb), (vt, vtb) = mem_kv(C, memT_d, wkv_d, V, vb, VC["n_mem"], VC["k_gain"], cst3, cstb, scr, "m1")
        _mem_off1 = C.off
        mem_xattn(C, hT, hTb, bufA, bufAb, bufH, bufHb, V, vb, VC["n_xattn"], VC["q_gain"], wq_d, wo_d, kn, knb, vt, vtb, cst3, cstb, scr)
        for tt in range(NTT):
            sl = slice(tt * TT, (tt + 1) * TT)
            norm_fm(C, lambda c: hT[:, c, sl], [hTb[tt]], TT, 8, cst3[:, 0, :], cstb, lambda c: V[:, VC["n_ffn"] + c:VC["n_ffn"] + c + 1],
                    lambda c: bufA[:, c, sl], [bufAb[tt]], scr)
        hn32, hn32b = C.sb("hn32", [128, 8, 128], F32)
        wr, wrb = C.sb("wr", [128, 8, 8], F32)
        G, Gb = C.sb("G", [128, 16, 8], F32)
        lg, lgb = C.sb("lg", [128, 8], F32)
        mx, mxb = C.sb("mx", [128, 8], F32)
        sm, smb = C.sb("sm", [128, 4], F32)
        C.dma(wr[:, :, :], wr_d.rearrange("(c p) n -> p c n", p=128), W=[wrb])
        for s in range(16):
            tt = s // 4
            sl = slice(s * 128, (s + 1) * 128)
            norm_fm(C, lambda c: hT[:, c, sl], [hTb[tt]], 128, 8, cst3[:, 0, :], cstb, lambda c: V[:, VC["n_ffn"] + c:VC["n_ffn"] + c + 1],
                    lambda c: hn32[:, c, :], [hn32b], scr)
            pt, pb = C.psum()
            for k in range(8):
                P.op("pe", "matmul", PK(pt[:, 0:8], hn32[:, k, :], wr[:, k, :], start=(k == 0), stop=(k == 7)), reads=[hn32b, wrb], writes=[pb])
            P.op("dve", "tensor_tensor", PK(out=lg[:, :], in0=pt[:, 0:8], in1=V[:, VC["b_router"]:VC["b_router"] + 8], op=ALU.add),
                 reads=[pb, vb], writes=[lgb])
            P.op("dve", "max", PK(out=mx[:, :], in_=lg[:, :]), reads=[lgb], writes=[mxb])
            P.op("dve", "tensor_scalar", PK(out=sm[:, 0:1], in0=mx[:, 0:1], scalar1=-1.0, scalar2=None, op0=ALU.mult), reads=[mxb], writes=[smb])
            ex, exb = scr["sg"]
            P.op("act", "activation", PK(out=ex[:, 0:8], in_=lg[:, :], func=AF.Exp, bias=sm[:, 0:1]), reads=[lgb, smb], writes=[exb])
            P.op("dve", "tensor_scalar", PK(out=lg[:, :], in0=lg[:, :], scalar1=mx[:, 1:2], scalar2=None, op0=ALU.is_ge), reads=[lgb, mxb], writes=[lgb])
            P.op("dve", "tensor_tensor", PK(out=ex[:, 0:8], in0=ex[:, 0:8], in1=lg[:, :], op=ALU.mult), reads=[exb, lgb], writes=[exb])
            P.op("dve", "reduce_sum", PK(out=sm[:, 1:2], in_=ex[:, 0:8], axis=AX.X), reads=[exb], writes=[smb])
            P.op("dve", "reciprocal", PK(out=sm[:, 2:3], in_=sm[:, 1:2]), reads=[smb], writes=[smb])
            P.op("dve", "tensor_scalar", PK(out=G[:, s, :], in0=ex[:, 0:8], scalar1=sm[:, 2:3], scalar2=None, op0=ALU.mult), reads=[exb, smb], writes=[Gb])
        P.fence(C.dummy[:, 0:1])
        gbc = sqq.rearrange("p a b c -> p (a b c)")[:, 0:NTOK]; gbcb = [Buf("gbc%d" % i) for i in range(NTT)]
        C.wbufs.append((scr["sq"][0].rearrange("p a b -> p (a b)"), Buf("wb_sq")))
        _o = _mem_off0
        while _o + 2048 <= _mem_off1:
            C.wbufs.append((C.arena[:, _o:_o + 2048].bitcast(BF16), Buf("wb_m%d" % _o)))
            _o += 2048
        dg, dgb = scr["rd"]
        for e_ in range(8):
            for tt in range(NTT):
                pt, pb = C.psum()
                for q in range(4):
                    s = tt * 4 + q
                    P.op("dve", "tensor_scalar", PK(out=dg[:, 0:128], in0=c32[:, 0, :], scalar1=G[:, s, e_:e_ + 1], scalar2=None, op0=ALU.mult),
                         reads=[c32b, Gb], writes=[dgb])
                    P.op("pe", "matmul", PK(pt[:, q * 128:(q + 1) * 128], c32[:, 1, :], dg[:, 0:128], start=True, stop=True), reads=[dgb, c32b], writes=[pb])
                P.op("act", "activation", PK(out=gbc[:, tt * TT:(tt + 1) * TT], in_=pt[:, :], func=AF.Copy), reads=[pb], writes=[gbcb[tt]])
            swiglu_ffn(C, hT, hTb, bufA, bufAb, bufH, bufHb, V, vb, None, wg_d[e_], wu_d[e_], wd_d[e_], 3584, cst3, cstb, scr, gate_bc=(gbc, gbcb))
        for tt in range(NTT):
            sl = slice(tt * TT, (tt + 1) * TT)
            C.dma(outT_d[:, sl].rearrange("(c p) n -> p c n", p=128), hT[:, :, sl], R=[hTb[tt]], is_out=True)


ARENA_WORDS = 53200


def build_fused():
    nc = bass.Bass("TRN2", target_bir_lowering=False)
    d = {}

    def I(n, s):
        d[n] = nc.dram_tensor(n, s, F32, kind="ExternalInput").ap()

    def S(n, s):
        d[n] = nc.dram_tensor(n, s, F32, kind="Internal").ap()
    I("xT", [D, T_SEQ]); I("memT", [D, 256]); I("cst", [128, 640]); I("c32A", [128, 384]); I("c32", [128, 256])
    I("mask5", [64, 320]); I("rmask", [128, SEG]); I("vecsB", [128, NVB]); I("vecsC", [128, NVC]); I("sel", [128, 2])
    I("qaug4", [4, NTOK]); I("kaug4", [8, 4, T_SEQ]); I("biasT", [128, 8 * 512]); I("lamb", [128, 256]); I("sgc", [128, 1])
    for hh in range(2):
        I("wc%d" % hh, [D, NCOLS]); I("vecsA%d" % hh, [128, NVA]); I("w_up%d" % hh, [64, 256]); I("a_up%d" % hh, [128, 256])
        I("g_upa%d" % hh, [128, 256]); I("g_upb%d" % hh, [32, 256])
        for n in ("breT", "bimT", "creT", "cimT"):
            I("%s%d" % (n, hh), [8, 128, 128])
    I("w_glu", [512, 512]); I("w_out", [D, D]); I("w_q0", [D, D]); I("w_kv0", [D, 2 * D]); I("w_o0", [D, D])
    I("ff_g", [D, 2816]); I("ff_u", [D, 2816]); I("ff_d", [2816, D]); I("w_qkv", [D, 3 * D])
    I("da_w_o", [D, D]); I("w_q1", [D, D]); I("w_kv1", [D, 2 * D]); I("w_o1", [D, D]); I("w_router", [D, 8])
    I("moe_g", [8, D, 3584]); I("moe_u", [8, D, 3584]); I("moe_d", [8, 3584, D])
    S("ymT", [D, T_SEQ]); S("h1T", [D, T_SEQ]); S("qT", [16, 64, T_SEQ]); S("kT", [16, 64, T_SEQ]); S("vtok", [T_SEQ, D])
    d["outT"] = nc.dram_tensor("outT", [D, NTOK], F32, kind="ExternalOutput").ap()
    with ExitStack() as st:
        P = Prog(nc)
        C = ACtx(nc, st, P, ARENA_WORDS)
        ymb, h1b, qb_, kb_, vtb_ = Buf("ymT"), Buf("h1T"), Buf("qT"), Buf("kT"), Buf("vtok")
        for hh in range(2):
            if hh:
                C.new_phase()
            body_A(C, d, hh, ymb)
        import os as _os
        if _os.environ.get("FSTOP") == "A":
            C.dma(d["outT"][:, 0:512], d["ymT"][:, 0:512], R=[ymb], is_out=True)
            P.emit(st)
            return nc
        for half in range(2):
            C.new_phase()
            body_B(C, d, half, (ymb, h1b, qb_, kb_, vtb_))
        C.new_phase()
        bufA, _ = C.sb("bufA_p", [128, 8, NTOK], BF16); bufAb = [Buf("bAp%d" % i) for i in range(NTT)]
        keep = C.off
        body_C1(C, d, bufA, bufAb, (h1b, qb_, kb_, vtb_))
        C.new_phase(keep=keep)
        body_C2(C, d, bufA, bufAb, h1b)
        P.emit(st)
    return nc


def pack_fused(inp, b, p):
    m = {}
    for hh in range(2):
        a = pack_A(inp, b, hh)
        if hh == 0:
            m["xT"] = a["xT"]; m["cst"] = a["cst"]; m["c32A"] = a["c32"]; m["mask5"] = a["mask5"]; m["rmask"] = a["rmask"]
        m["wc%d" % hh] = a["wc"]; m["vecsA%d" % hh] = a["vecs"]; m["w_up%d" % hh] = a["w_up"]; m["a_up%d" % hh] = a["a_up"]
        m["g_upa%d" % hh] = a["g_upa"]; m["g_upb%d" % hh] = a["g_upb"]
        for n in ("breT", "bimT", "creT", "cimT"):
            m["%s%d" % (n, hh)] = a[n]
    m["memT"] = np.ascontiguousarray(np.asarray(inp["mem"][b]).T)
    m["c32"] = c32_table(); m["vecsB"] = vecs_B(inp); m["vecsC"] = vecs_C(inp)
    sel = np.zeros((128, 2), np.float32); sel[:, p] = 1.0
    m["sel"] = sel
    m["qaug4"] = q_aug_rows(p)
    m["kaug4"] = np.stack([k_aug_rows(h) for h in range(8)])
    m["biasT"] = bias_table(p)
    m["lamb"] = np.ascontiguousarray(np.broadcast_to(np.stack([inp["da_lam_q1"][0], inp["da_lam_k1"][0], inp["da_lam_q2"][0],
                                                               inp["da_lam_k2"][0]]).reshape(1, 256), (128, 256))).astype(np.float32)
    m["sgc"] = np.asarray(inp["da_sub_gain"][0], np.float32).reshape(128, 1)
    for k_, src in (("w_glu", "s5_w_glu"), ("w_out", "hy_w_out"), ("ff_g", "ff_w_gate"), ("ff_u", "ff_w_up"), ("ff_d", "ff_w_down"),
                    ("w_qkv", "da_w_qkv"), ("da_w_o", "da_w_o"), ("w_router", "moe_w_router"), ("moe_g", "moe_w_gate"),
                    ("moe_u", "moe_w_up"), ("moe_d", "moe_w_down")):
        m[k_] = np.asarray(inp[src][0])
    for l in range(2):
        m["w_q%d" % l] = np.asarray(inp["xa_w_q"][l]); m["w_kv%d" % l] = np.asarray(inp["xa_w_kv"][l]); m["w_o%d" % l] = np.asarray(inp["xa_w_o"][l])
    return m


_CACHE = {}


def kernel(**inp):
    inp = {k: np.asarray(v) for k, v in inp.items()}
    B_, T_ = 4, 4096
    if "F" not in _CACHE:
        _CACHE["F"] = build_fused()
    cores = [(b, p) for b in range(B_) for p in range(2)]
    maps = [pack_fused(inp, b, p) for (b, p) in cores]
    res = run_bass_kernel_spmd(_CACHE["F"], maps, core_ids=list(range(8))).results
    out = np.zeros((B_, T_, 1024), np.float32)
    for i, (b, p) in enumerate(cores):
        out[b][tok_index(p)] = np.asarray(res[i]["outT"]).T
    return out
```

```python
from contextlib import ExitStack
import numpy as np
import concourse.bass as bass
import concourse.mybir as mybir
from concourse.bass_utils import run_bass_kernel_spmd

F32 = mybir.dt.float32
BF16 = mybir.dt.bfloat16
AF = mybir.ActivationFunctionType
ALU = mybir.AluOpType
AX = mybir.AxisListType

ENGS = ("pe", "act", "dve", "pool", "sp")


def PK(*a, **k):
    return (a, k)


class Buf:
    __slots__ = ("name", "w", "rs", "excl")

    def __init__(self, name, excl=False):
        self.name = name
        self.excl = excl
        self.w = None
        self.rs = []


class Op:
    __slots__ = ("eng", "fn", "deps", "is_dma", "needed", "idx", "semval", "dsem")

    def __init__(self, eng, fn, is_dma):
        self.eng = eng
        self.fn = fn
        self.deps = set()
        self.is_dma = is_dma
        self.needed = False
        self.semval = None
        self.dsem = None


class Prog:
    def __init__(self, nc, n_dma_sems=24):
        self.nc = nc
        self.ops = []
        self.n_dma_sems = n_dma_sems
        self.out_dma_ops = []
        self.fence_idx = None
        self.last = {}
        self.dmas_since = []

    def op(self, eng, fn, pack=None, reads=(), writes=(), is_dma=False, is_out=False, after=()):
        if isinstance(fn, str):
            _name, _a, _k = fn, pack[0], pack[1]
            fn = lambda e: getattr(e, _name)(*_a, **_k)
        o = Op(eng, fn, is_dma)
        o.idx = len(self.ops)
        ops = self.ops
        ex = [b for b in reads if b.excl and b not in writes]
        if ex:
            reads = [b for b in reads if not b.excl]
            writes = list(writes) + ex

        def add(d, raw):
            p = ops[d]
            if not (p.is_dma or is_dma) and p.eng == eng:
                if eng == "pe":
                    return
            o.deps.add(d)
        for b in reads:
            if b.w is not None:
                add(b.w, True)
        for b in writes:
            if b.w is not None:
                add(b.w, False)
            for r in b.rs:
                add(r, False)
        if self.fence_idx is not None:
            o.deps.add(self.fence_idx)
        for x in after:
            o.deps.add(x.idx)
        for b in reads:
            b.rs.append(o.idx)
        for b in writes:
            b.w = o.idx
            b.rs = []
        if is_dma:
            self.dmas_since.append(o.idx)
        else:
            self.last[eng] = o.idx
        self.ops.append(o)
        if is_out:
            self.out_dma_ops.append(o.idx)
        return o

    def fence(self, dummy_ap):
        deps = set(self.last.values()) | set(self.dmas_since)
        o = self.op("dve", "memset", PK(dummy_ap, 0.0))
        o.deps |= deps
        self.fence_idx = o.idx
        self.dmas_since = []
        return o

    def emit(self, stack):
        nc = self.nc
        ops = self.ops
        for o in ops:
            best = {}
            keep = set()
            for d in o.deps:
                p = ops[d]
                if p.is_dma:
                    keep.add(d)
                else:
                    if p.eng not in best or d > best[p.eng]:
                        best[p.eng] = d
            for e, d in best.items():
                keep.add(d)
            o.deps = keep
            for d in keep:
                ops[d].needed = True
        for i in self.out_dma_ops:
            ops[i].needed = True
        sems = {e: stack.enter_context(nc.semaphore("s_" + e)) for e in ENGS if e != "sp"}
        dsems = [stack.enter_context(nc.semaphore("d%d" % i)) for i in range(self.n_dma_sems)]
        cnt = {e: 0 for e in sems}
        dcnt = [0] * len(dsems)
        rr = {"sw": 0, "hw": 0}
        nsw = len(dsems) // 2
        pools = {"sw": list(range(0, nsw)), "hw": list(range(nsw, len(dsems)))}
        lastd = {}
        per_eng = {e: [] for e in ENGS}
        for o in ops:
            if o.is_dma:
                o.needed = True
                kind = "sw" if o.eng == "pool" else "hw"
                pl = pools[kind]
                k = pl[rr[kind] % len(pl)]
                rr[kind] += 1
                prev = lastd.get(k)
                if prev is not None:
                    o.deps.add(prev)
                lastd[k] = o.idx
                dcnt[k] += 16
                o.dsem = dsems[k]
                o.semval = dcnt[k]
            elif o.needed:
                cnt[o.eng] += 1
                o.semval = cnt[o.eng]
            per_eng[o.eng].append(o)
        self.stats = {e: len(per_eng[e]) for e in ENGS}
        self.stats["sem_max"] = dict(cnt)
        block = stack.enter_context(nc.Block())

        def run(engname, eng):
            seen = {}
            for o in per_eng[engname]:
                for d in sorted(o.deps):
                    p = ops[d]
                    s = p.dsem if p.is_dma else sems[p.eng]
                    key = id(s)
                    if seen.get(key, -1) >= p.semval:
                        continue
                    seen[key] = p.semval
                    eng.wait_ge(s, p.semval)
                ins = o.fn(eng)
                if o.is_dma:
                    ins.then_inc(o.dsem, 16)
                elif o.needed:
                    ins.then_inc(sems[o.eng], 1)
            if engname == "sp":
                for i in self.out_dma_ops:
                    p = ops[i]
                    eng.wait_ge(p.dsem, p.semval)

        @block.tensor
        def _(e):
            run("pe", e)

        @block.scalar
        def _(e):
            run("act", e)

        @block.vector
        def _(e):
            run("dve", e)

        @block.gpsimd
        def _(e):
            run("pool", e)

        @block.sync
        def _(e):
            run("sp", e)


D = 1024
NTOK = 2048
TT = 512
NTT = NTOK // TT
EPS = 1e-6


class Ctx:
    def __init__(self, nc, st, P):
        self.nc, self.st, self.P = nc, st, P
        self.ps = []
        for i in range(8):
            t = st.enter_context(nc.psum_tensor("ps%d" % i, [128, 512], F32))
            self.ps.append((t, Buf("ps%d" % i, excl=True)))
        self.psi = 0
        self.epsc, self.epsb = self.sb("epsc", [128, 2], F32)
        P.op("dve", "memset", PK(self.epsc[:, 0:1], EPS), writes=[self.epsb])
        P.op("dve", "memset", PK(self.epsc[:, 1:2], 64e-5), writes=[self.epsb])
        self.wbufs = []
        self.wi = 0
        self.dmaq = 0

    def sb(self, name, shape, dt):
        t = self.st.enter_context(self.nc.sbuf_tensor(name, shape, dt))
        return t, Buf(name)

    def psum(self):
        r = self.ps[self.psi % 8]
        self.psi += 1
        return r

    def init_w(self, n, cols):
        for i in range(n):
            self.wbufs.append(self.sb("wb%d" % i, [128, cols], BF16))

    def wbuf(self):
        r = self.wbufs[self.wi % len(self.wbufs)]
        self.wi += 1
        return r

    def load_w(self, src_ap, kc, ncols):
        t, b = self.wbuf()
        view = t[:, 0:kc * ncols].rearrange("p (c n) -> p c n", c=kc)
        src = src_ap.rearrange("(c p) n -> p c n", p=128)
        self.P.op("pool", "dma_start", PK(out=view, in_=src), writes=[b], is_dma=True)
        return view, b

    def dma(self, out, in_, R=(), W=(), is_out=False, q=None):
        if q is None:
            q = "sp"
        return self.P.op(q, "dma_start", PK(out=out, in_=in_), reads=R, writes=W, is_dma=True, is_out=is_out)


def rmsnorm_fm(C, xT, xb, gcol, outT, outb, tt, scr, dim_chunks=8, extra_scale=1.0):
    P = C.P
    sq, sqb = scr["sq"]
    rs, rsb = scr["rs"]
    ones, onesb = scr["ones"]
    sl = slice(tt * TT, (tt + 1) * TT)
    pt, pb = C.psum()
    for c in range(dim_chunks):
        P.op("act", "activation", PK(out=sq[:, c, :], in_=xT[:, c, sl], func=AF.Square), reads=[xb], writes=[sqb])
    for c in range(dim_chunks):
        P.op("pe", "matmul", PK(pt[:, :], ones[:, :], sq[:, c, :], start=(c == 0), stop=(c == dim_chunks - 1)),
             reads=[sqb, onesb], writes=[pb])
    dd = dim_chunks * 128
    P.op("dve", "tensor_scalar", PK(out=rs[:, :], in0=pt[:, :], scalar1=float(dd * EPS), scalar2=-0.5, op0=ALU.add, op1=ALU.pow),
         reads=[pb], writes=[rsb])
    sc = float(np.sqrt(dd) * extra_scale)
    for c in range(dim_chunks):
        eng = "dve"
        P.op(eng, "scalar_tensor_tensor", PK(out=outT[:, c, sl], in0=xT[:, c, sl], scalar=gcol[:, c:c + 1], in1=rs[:, :],
                                                       op0=ALU.mult, op1=ALU.mult),
             reads=[xb, rsb], writes=[outb])
    return sc


def linear_fm(C, W_dram, kc, xin, xinb, n_out, evac, tts=range(NTT), col0=0, blk=512, mcols=128, xinbs=None):
    P = C.P
    nblk = (n_out + blk - 1) // blk
    oc = 0
    for bi in range(nblk):
        c0 = col0 + bi * blk
        w = min(blk, n_out - bi * blk)
        wv, wb = C.load_w(W_dram[:, c0:c0 + w], kc, w)
        for j in range(0, w, mcols):
            m = min(mcols, w - j)
            for tt in tts:
                pt, pb = C.psum()
                sl = slice(tt * TT, (tt + 1) * TT)
                for k in range(kc):
                    P.op("pe", "matmul", PK(pt[0:m, :], wv[:, k, j:j + m], xin[:, k, sl],
                                                                              start=(k == 0), stop=(k == kc - 1)),
                         reads=[wb, (xinbs[tt] if xinbs is not None else xinb)], writes=[pb])
                evac(oc, tt, pt, pb, m)
            oc += 1


def rstd_from(C, rs_ap, ps_ap, pb, rsb, eps=EPS):
    np_ = rs_ap.shape[0]
    C.P.op("act", "activation", PK(out=rs_ap, in_=ps_ap, func=AF.Sqrt, bias=C.epsc[0:np_, 0:1] if eps == EPS else C.epsc[0:np_, 1:2]),
           reads=[pb, C.epsb], writes=[rsb])
    C.P.op("dve", "reciprocal", PK(out=rs_ap, in_=rs_ap), reads=[rsb], writes=[rsb])


def norm_fm(C, xin, xb, n, dim_chunks, onesm, onesb, gcol, out, outb, scr, sq_from_psum=False):
    P = C.P
    sq, sqb = scr["sq"]
    rs, rsb = scr["rs"]
    pt, pb = C.psum()
    for c in range(dim_chunks):
        P.op("act", "activation", PK(out=sq[:, c, 0:n], in_=xin(c), func=AF.Square), reads=xb, writes=[sqb])
    for c in range(dim_chunks):
        P.op("pe", "matmul", PK(pt[:, 0:n], onesm, sq[:, c, 0:n], start=(c == 0), stop=(c == dim_chunks - 1)),
             reads=[sqb, onesb], writes=[pb])
    rstd_from(C, rs[:, 0:n], pt[:, 0:n], pb, rsb)
    for c in range(dim_chunks):
        eng = "dve"
        P.op(eng, "scalar_tensor_tensor", PK(out=out(c), in0=xin(c), scalar=gcol(c), in1=rs[:, 0:n],
                                                       op0=ALU.mult, op1=ALU.mult),
             reads=list(xb) + [rsb] + ([C.vb] if getattr(C, 'vb', None) is not None else []), writes=outb)


def mem_kv(C, memT_d, wkv_d, V, vb, col_nm, col_kg, cst, cstb, scr, name):
    P = C.P
    mT, mTb = C.sb(name + "_mT", [128, 8, 256], BF16)
    mn, mnb = C.sb(name + "_mn", [128, 8, 256], BF16)
    kf, kfb = C.sb(name + "_kf", [128, 8, 256], BF16)
    kn, knb = C.sb(name + "_kn", [128, 8, 256], BF16)
    vt, vtb = C.sb(name + "_vt", [128, 2, 1024], BF16)
    P.op("pool", "dma_start", PK(out=mT[:, :, :], in_=memT_d.rearrange("(c p) n -> p c n", p=128)), writes=[mTb], is_dma=True)
    norm_fm(C, lambda c: mT[:, c, :], [mTb], 256, 8, cst[:, 0, :], cstb, lambda c: V[:, col_nm + c:col_nm + c + 1],
            lambda c: mn[:, c, :], [mnb], scr)
    for half in range(2):
        wv, wb = C.load_w(wkv_d[:, half * 512:(half + 1) * 512], 8, 512)
        for j in range(4):
            oc = half * 4 + j
            pt, pb = C.psum()
            for k in range(8):
                P.op("pe", "matmul", PK(pt[:, 0:256], wv[:, k, j * 128:(j + 1) * 128], mn[:, k, :],
                                                                      start=(k == 0), stop=(k == 7)), reads=[wb, mnb], writes=[pb])
            P.op("dve", "tensor_copy", PK(out=kf[:, oc, :], in_=pt[:, 0:256]), reads=[pb], writes=[kfb])
    for hh in range(4):
        norm_fm(C, lambda c, hh=hh: kf[:, hh * 2 + c, :], [kfb], 256, 2, cst[:, 1, :], cstb,
                lambda c: V[:, col_kg + c:col_kg + c + 1], lambda c, hh=hh: kn[:, hh * 2 + c, :], [knb], scr)
    for half in range(2):
        wv, wb = C.load_w(wkv_d[:, 1024 + half * 512:1024 + (half + 1) * 512], 8, 512)
        for j in range(2):
            pt, pb = C.psum()
            for k in range(8):
                P.op("pe", "matmul", PK(pt[:, :], mn[:, k, j * 128:(j + 1) * 128], wv[:, k, :],
                                                                      start=(k == 0), stop=(k == 7)), reads=[wb, mnb], writes=[pb])
            P.op("act", "activation", PK(out=vt[:, j, half * 512:(half + 1) * 512], in_=pt[:, :], func=AF.Copy),
                 reads=[pb], writes=[vtb])
    return (kn, knb), (vt, vtb)


def mem_xattn(C, hT, hTb, bufA, bufAb, bufQ, bufQb, V, vb, col_nx, col_qg, wq_d, wo_d, kn, knb, vt, vtb, cst, cstb, scr, stop=0):
    P = C.P
    for tt in range(NTT):
        sl = slice(tt * TT, (tt + 1) * TT)
        norm_fm(C, lambda c: hT[:, c, sl], [hTb[tt]], TT, 8, cst[:, 0, :], cstb, lambda c: V[:, col_nx + c:col_nx + c + 1],
                lambda c: bufA[:, c, sl], [bufAb[tt]], scr)
    if stop == 16:
        return
    sqq, sqqb = scr["sqq"]
    rs, rsb = scr["rs"]

    def evac_q(oc, tt, pt, pb, m):
        sl = slice(tt * TT, (tt + 1) * TT)
        P.op("act", "activation", PK(out=sqq[:, oc % 2, tt, :], in_=pt[:, :], func=AF.Square), reads=[pb], writes=[sqqb[tt]])
        P.op("dve", "tensor_copy", PK(out=bufQ[:, oc, sl], in_=pt[:, :]), reads=[pb], writes=[bufQb[tt]])
        import os
        V17 = os.environ.get('V17', '')
        if oc % 2 == 1 and V17 != 'a':
            p2, p2b = C.psum()
            for c in range(2):
                P.op("pe", "matmul", PK(p2[:, :], cst[:, 1, :], sqq[:, c, tt, :], start=(c == 0), stop=(c == 1)),
                     reads=[sqqb[tt], cstb], writes=[p2b])
            rstd_from(C, rs[:, :], p2[:, :], p2b, rsb)
            for c in range(2):
                o2 = oc - 1 + c
                P.op("dve", "tensor_tensor", PK(out=bufQ[:, o2, sl], in0=bufQ[:, o2, sl], in1=rs[:, :], op=ALU.mult),
                     reads=[bufQb[tt], rsb], writes=[bufQb[tt]])
                P.op("act", "activation", PK(out=bufQ[:, o2, sl], in_=bufQ[:, o2, sl], func=AF.Copy, scale=V[:, col_qg + c:col_qg + c + 1]),
                     reads=[bufQb[tt], vb], writes=[bufQb[tt]])
    linear_fm(C, wq_d, 8, bufA, None, 1024, evac_q, xinbs=bufAb)
    if stop == 17:
        return
    et, etb = scr["et"]
    rd, rdb = scr["rd"]
    for hh in range(4):
        for tt in range(NTT):
            sl = slice(tt * TT, (tt + 1) * TT)
            pden, pdenb = C.psum()
            pn = [C.psum(), C.psum()]
            for j in range(2):
                psc, pscb = C.psum()
                for dc in range(2):
                    P.op("pe", "matmul", PK(psc[:, :], kn[:, hh * 2 + dc, j * 128:(j + 1) * 128], bufQ[:, hh * 2 + dc, sl],
                                                                        start=(dc == 0), stop=(dc == 1)), reads=[knb, bufQb[tt]], writes=[pscb])
                P.op("act", "activation", PK(out=et[:, j, :], in_=psc[:, :], func=AF.Exp, scale=1.0 / 16.0),
                     reads=[pscb], writes=[etb[j]])
                P.op("pe", "matmul", PK(pden[:, :], cst[:, 4, :], et[:, j, :], start=(j == 0), stop=(j == 1)),
                     reads=[etb[j], cstb], writes=[pdenb])
                for c in range(2):
                    P.op("pe", "matmul", PK(pn[c][0][:, :], vt[:, j, hh * 256 + c * 128:hh * 256 + (c + 1) * 128], et[:, j, :],
                                                            start=(j == 0), stop=(j == 1)), reads=[etb[j], vtb], writes=[pn[c][1]])
            P.op("dve", "reciprocal", PK(out=rd[:, :], in_=pden[:, :]), reads=[pdenb], writes=[rdb])
            for c in range(2):
                P.op("dve", "tensor_tensor", PK(out=bufA[:, hh * 2 + c, sl], in0=pn[c][0][:, :], in1=rd[:, :], op=ALU.mult),
                     reads=[pn[c][1], rdb], writes=[bufAb[tt]])

    if stop == 18:
        return

    def evac_o(oc, tt, pt, pb, m):
        sl = slice(tt * TT, (tt + 1) * TT)
        P.op("dve", "tensor_tensor", PK(out=hT[:, oc, sl], in0=pt[:, :], in1=hT[:, oc, sl], op=ALU.add), reads=[pb, hTb[tt]], writes=[hTb[tt]])
    linear_fm(C, wo_d, 8, bufA, None, 1024, evac_o, xinbs=bufAb)


def swiglu_ffn(C, hT, hTb, bufA, bufAb, bufH, bufHb, V, vb, col_nf, wg_d, wu_d, wd_d, dff, cst, cstb, scr, gate_bc=None):
    P = C.P
    if col_nf is not None:
        for tt in range(NTT):
            sl = slice(tt * TT, (tt + 1) * TT)
            norm_fm(C, lambda c: hT[:, c, sl], [hTb[tt]], TT, 8, cst[:, 0, :], cstb, lambda c: V[:, col_nf + c:col_nf + c + 1],
                    lambda c: bufA[:, c, sl], [bufAb[tt]], scr)
    sg, sgb = scr["sg"]
    nch = dff // 128
    GRP = bufH.shape[1]
    g0 = 0
    while g0 < nch:
        gn = min(GRP, nch - g0)
        c = 0
        while c < gn:
            cb = min(4, gn - c)
            col = (g0 + c) * 128
            wgv, wgb = C.load_w(wg_d[:, col:col + cb * 128], 8, cb * 128)
            wuv, wub = C.load_w(wu_d[:, col:col + cb * 128], 8, cb * 128)
            for j in range(cb):
                for tt in range(NTT):
                    sl = slice(tt * TT, (tt + 1) * TT)
                    pg, pgb = C.psum()
                    pu, pub = C.psum()
                    for k in range(8):
                        P.op("pe", "matmul", PK(pg[:, :], wgv[:, k, j * 128:(j + 1) * 128], bufA[:, k, sl],
                                                                                       start=(k == 0), stop=(k == 7)), reads=[wgb, bufAb[tt]], writes=[pgb])
                    for k in range(8):
                        P.op("pe", "matmul", PK(pu[:, :], wuv[:, k, j * 128:(j + 1) * 128], bufA[:, k, sl],
                                                                                       start=(k == 0), stop=(k == 7)), reads=[wub, bufAb[tt]], writes=[pub])
                    P.op("act", "activation", PK(out=sg[:, :], in_=pg[:, :], func=AF.Silu), reads=[pgb], writes=[sgb])
                    if gate_bc is not None:
                        gt, gtb = gate_bc
                        P.op("dve", "tensor_tensor", PK(out=sg[:, :], in0=sg[:, :], in1=gt[:, sl], op=ALU.mult),
                             reads=[sgb, gtb[tt]], writes=[sgb])
                    P.op("dve", "tensor_tensor", PK(out=bufH[:, c + j, sl], in0=pu[:, :], in1=sg[:, :], op=ALU.mult),
                         reads=[pub, sgb], writes=[bufHb[tt]])
            c += cb
        wblks = []
        r = 0
        while r < gn:
            rb = min(4, gn - r)
            row = (g0 + r) * 128
            wblks.append((r, rb, C.load_w(wd_d[row:row + rb * 128, :], rb, 1024)))
            r += rb
        for oc in range(8):
            for tt in range(NTT):
                sl = slice(tt * TT, (tt + 1) * TT)
                pt, pb = C.psum()
                first = True
                for (r, rb, (wv, wb)) in wblks:
                    for q in range(rb):
                        last = (r + q == gn - 1)
                        P.op("pe", "matmul", PK(pt[:, :], wv[:, q, oc * 128:(oc + 1) * 128], bufH[:, r + q, sl], start=first, stop=last),
                             reads=[wb, bufHb[tt]], writes=[pb])
                        first = False
                P.op("dve", "tensor_tensor", PK(out=hT[:, oc, sl], in0=pt[:, :], in1=hT[:, oc, sl], op=ALU.add),
                     reads=[pb, hTb[tt]], writes=[hTb[tt]])
        g0 += gn


VB = dict(b_glu=0, n_xattn=4, n_mem=12, q_gain=20, k_gain=22, n_ffn=24, n_mix1=32, da_qg=40, da_kg=41)
NVB = 42


def build_B():
    nc = bass.Bass("TRN2", target_bir_lowering=False)
    dr = lambda n, s, k="ExternalInput", dt=F32: nc.dram_tensor(n, s, dt, kind=k).ap()
    xT_d = dr("xT", [D, NTOK]); ymT_d = dr("ymT", [D, NTOK]); memT_d = dr("memT", [D, 256])
    vec_d = dr("vecs", [128, NVB]); cst_d = dr("cst", [128, 5 * 128])
    wglu_d = dr("w_glu", [512, 512]); wout_d = dr("w_out", [D, D])
    wq_d = dr("w_q", [D, D]); wkv_d = dr("w_kv", [D, 2 * D]); wo_d = dr("w_o", [D, D])
    wg_d = dr("ff_g", [D, 2816]); wu_d = dr("ff_u", [D, 2816]); wd_d = dr("ff_d", [2816, D])
    wqkv_d = dr("w_qkv", [D, 3 * D])
    h1T_d = dr("h1T", [D, NTOK], "ExternalOutput")
    qT_d = dr("qT", [16, 64, NTOK], "ExternalOutput")
    kT_d = dr("kT", [16, 64, NTOK], "ExternalOutput")
    vT_d = dr("vT", [D, NTOK], "ExternalOutput")
    with ExitStack() as st:
        P = Prog(nc)
        C = Ctx(nc, st, P)
        C.init_w(4, 4096)
        hT, _ = C.sb("hT", [128, 8, NTOK], F32); hTb = [Buf("hT%d" % i) for i in range(NTT)]
        bufA, _ = C.sb("bufA", [128, 8, NTOK], BF16); bufAb = [Buf("bA%d" % i) for i in range(NTT)]
        bufH, _ = C.sb("bufH", [128, 8, NTOK], BF16); bufHb = [Buf("bH%d" % i) for i in range(NTT)]
        V, vb = C.sb("V_sb", [128, NVB], F32); C.vb = vb
        cst3, cstb = C.sb("cst_sb", [128, 5, 128], BF16)
        scr = dict(sq=C.sb("sq", [128, 8, 512], BF16), rs=C.sb("rs", [128, 512], F32), sg=C.sb("sg", [128, 512], F32),
                   rd=C.sb("rd", [128, 512], F32))
        sqq, _ = C.sb("sqq", [128, 2, NTT, 512], BF16)
        scr["sqq"] = (sqq, [Buf("sqq%d" % i) for i in range(NTT)])
        et, _ = C.sb("et", [128, 2, 512], BF16)
        scr["et"] = (et, [Buf("et0"), Buf("et1")])
        C.dma(V[:, :], vec_d[:, :], W=[vb])
        P.op("pool", "dma_start", PK(out=cst3[:, :, :], in_=cst_d.rearrange("p (a b) -> p a b", a=5)), writes=[cstb], is_dma=True)
        for tt in range(NTT):
            sl = slice(tt * TT, (tt + 1) * TT)
            C.dma(hT[:, :, sl], xT_d[:, sl].rearrange("(c p) n -> p c n", p=128), W=[hTb[tt]])
            P.op("pool", "dma_start", PK(out=bufA[:, :, sl], in_=ymT_d[:, sl].rearrange("(c p) n -> p c n", p=128)),
                 writes=[bufAb[tt]], is_dma=True)
        import os
        STAGE = int(os.environ.get("STAGE", "9"))
        def finish():
            for tt in range(NTT):
                sl = slice(tt * TT, (tt + 1) * TT)
                C.dma(h1T_d[:, sl].rearrange("(c p) n -> p c n", p=128), hT[:, :, sl], R=[hTb[tt]], is_out=True)
            P.emit(st)
            print("B stats", P.stats)
            return nc
        if STAGE == 0:
            return finish()
        wv, wb = C.load_w(wglu_d[:, :], 4, 512)
        sg, sgb = scr["sg"]
        for tt in range(NTT):
            sl = slice(tt * TT, (tt + 1) * TT)
            pts = []
            for oc in range(4):
                pt, pb = C.psum()
                for k in range(4):
                    P.op("pe", "matmul", PK(pt[:, :], wv[:, k, oc * 128:(oc + 1) * 128], bufA[:, 4 + k, sl],
                                                                            start=(k == 0), stop=(k == 3)), reads=[wb, bufAb[tt]], writes=[pb])
                pts.append((pt, pb))
            for oc in range(4):
                pt, pb = pts[oc]
                P.op("act", "activation", PK(out=sg[:, :], in_=pt[:, :], func=AF.Sigmoid,
                                                                bias=V[:, VB["b_glu"] + oc:VB["b_glu"] + oc + 1]), reads=[pb, vb], writes=[sgb])
                P.op("dve", "tensor_tensor", PK(out=bufA[:, 4 + oc, sl], in0=bufA[:, 4 + oc, sl], in1=sg[:, :], op=ALU.mult),
                     reads=[sgb, bufAb[tt]], writes=[bufAb[tt]])

        def evac_res(oc, tt, pt, pb, m):
            sl = slice(tt * TT, (tt + 1) * TT)
            P.op("dve", "tensor_tensor", PK(out=hT[:, oc, sl], in0=pt[:, :], in1=hT[:, oc, sl], op=ALU.add), reads=[pb, hTb[tt]], writes=[hTb[tt]])
        linear_fm(C, wout_d, 8, bufA, None, 1024, evac_res, xinbs=bufAb)
        import os
        STAGE = int(os.environ.get("STAGE", "9"))
        def finish():
            for tt in range(NTT):
                sl = slice(tt * TT, (tt + 1) * TT)
                C.dma(h1T_d[:, sl].rearrange("(c p) n -> p c n", p=128), hT[:, :, sl], R=[hTb[tt]], is_out=True)
            P.emit(st)
            return nc
        if STAGE == 1:
            return finish()
        (kn, knb), (vt, vtb) = mem_kv(C, memT_d, wkv_d, V, vb, VB["n_mem"], VB["k_gain"], cst3, cstb, scr, "m0")
        if STAGE == 15:
            return finish()
        mem_xattn(C, hT, hTb, bufA, bufAb, bufH, bufHb, V, vb, VB["n_xattn"], VB["q_gain"], wq_d, wo_d, kn, knb, vt, vtb, cst3, cstb, scr, stop=STAGE)
        if STAGE in (2, 16, 17, 18):
            return finish()
        swiglu_ffn(C, hT, hTb, bufA, bufAb, bufH, bufHb, V, vb, VB["n_ffn"], wg_d, wu_d, wd_d, 2816, cst3, cstb, scr)
        for tt in range(NTT):
            sl = slice(tt * TT, (tt + 1) * TT)
            C.dma(h1T_d[:, sl].rearrange("(c p) n -> p c n", p=128), hT[:, :, sl], R=[hTb[tt]], is_out=True)
        for tt in range(NTT):
            sl = slice(tt * TT, (tt + 1) * TT)
            norm_fm(C, lambda c: hT[:, c, sl], [hTb[tt]], TT, 8, cst3[:, 0, :], cstb, lambda c: V[:, VB["n_mix1"] + c:VB["n_mix1"] + c + 1],
                    lambda c: bufA[:, c, sl], [bufAb[tt]], scr)
        qs, qsb = scr["sg"]
        qo, qob = scr["rd"]
        sq1, sq1b = C.sb("sq1", [64, 512], BF16)
        rs, rsb = scr["rs"]
        for which, (dst, gcol, scl) in enumerate(((qT_d, VB["da_qg"], 0.125), (kT_d, VB["da_kg"], 1.0))):
            def evac_qk(oc, tt, pt, pb, m, dst=dst, gcol=gcol, scl=scl):
                sl = slice(tt * TT, (tt + 1) * TT)
                P.op("act", "activation", PK(out=sq1[:, :], in_=pt[0:64, :], func=AF.Square), reads=[pb], writes=[sq1b])
                p2, p2b = C.psum()
                P.op("pe", "matmul", PK(p2[0:64, :], cst3[0:64, 2, 0:64], sq1[:, :], start=True, stop=True), reads=[sq1b, cstb], writes=[p2b])
                rstd_from(C, rs[0:64, :], p2[0:64, :], p2b, rsb)
                P.op("dve", "scalar_tensor_tensor", PK(out=qs[0:64, :], in0=pt[0:64, :], scalar=V[0:64, gcol:gcol + 1], in1=rs[0:64, :],
                                                             op0=ALU.mult, op1=ALU.mult), reads=[pb, rsb, vb], writes=[qsb])
                P.op("act", "activation", PK(out=qo[0:64, :], in_=qs[0:64, :], func=AF.Copy, scale=float(scl)), reads=[qsb], writes=[qob])
                C.dma(dst[oc, :, sl], qo[0:64, :], R=[qob], is_out=True)
            linear_fm(C, wqkv_d, 8, bufA, None, 1024, evac_qk, col0=which * 1024, mcols=64, xinbs=bufAb)
        vo, vob = scr["sg"]

        def evac_v(oc, tt, pt, pb, m):
            sl = slice(tt * TT, (tt + 1) * TT)
            P.op("act", "activation", PK(out=vo[:, :], in_=pt[:, :], func=AF.Copy), reads=[pb], writes=[vob])
            C.dma(vT_d[oc * 128:(oc + 1) * 128, sl], vo[:, :], R=[vob], is_out=True)
        linear_fm(C, wqkv_d, 8, bufA, None, 1024, evac_v, col0=2048, xinbs=bufAb)
        P.emit(st)
        print("B stats", P.stats)
    return nc


def cst_table():
    c = np.zeros((128, 5, 128), np.float32)
    c[:, 0, :] = 1.0 / 1024
    c[:, 1, :] = 1.0 / 256
    c[0:64, 2, 0:64] = 1.0 / 64
    c[64:128, 2, 64:128] = 1.0 / 64
    c[:, 3, :] = 1.0 / 128
    c[:, 4, :] = 1.0
    return c.reshape(128, 640)


def col(v):
    v = np.asarray(v, np.float32).reshape(-1)
    if v.size < 128:
        o = np.zeros((128, 1), np.float32); o[:v.size, 0] = v
        return o
    return np.ascontiguousarray(v.reshape(-1, 128).T)


def vecs_B(inp):
    V = np.zeros((128, NVB), np.float32)
    def put(name, arr):
        a = col(arr); V[:, VB[name]:VB[name] + a.shape[1]] = a
    put("b_glu", inp["s5_b_glu"][0]); put("n_xattn", inp["norm_xattn"][0]); put("n_mem", inp["norm_mem"][0])
    put("q_gain", inp["xa_q_gain"][0]); put("k_gain", inp["xa_k_gain"][0]); put("n_ffn", inp["norm_ffn"][0])
    put("n_mix1", inp["norm_mix"][1])
    put("da_qg", np.tile(np.asarray(inp["da_q_gain"][0]), 2)); put("da_kg", np.tile(np.asarray(inp["da_k_gain"][0]), 2))
    return V


def tok_index(p):
    return np.concatenate([np.arange((2 * m + p) * 512, (2 * m + p + 1) * 512) for m in range(4)])


VC = dict(n_xattn=0, n_mem=8, q_gain=16, k_gain=18, n_ffn=20, b_router=28)
NVC = 36


def build_C2():
    nc = bass.Bass("TRN2", target_bir_lowering=False)
    dr = lambda n, s, k="ExternalInput", dt=F32: nc.dram_tensor(n, s, dt, kind=k).ap()
    h1T_d = dr("h1T", [D, NTOK]); atT_d = dr("attnT", [D, NTOK]); memT_d = dr("memT", [D, 256])
    vec_d = dr("vecs", [128, NVC]); cst_d = dr("cst", [128, 5 * 128]); c32_d = dr("c32", [128, 256])
    wdo_d = dr("da_w_o", [D, D])
    wq_d = dr("w_q", [D, D]); wkv_d = dr("w_kv", [D, 2 * D]); wo_d = dr("w_o", [D, D])
    wr_d = dr("w_router", [D, 8])
    wg_d = dr("moe_g", [8, D, 3584]); wu_d = dr("moe_u", [8, D, 3584]); wd_d = dr("moe_d", [8, 3584, D])
    outT_d = dr("outT", [D, NTOK], "ExternalOutput")
    with ExitStack() as st:
        P = Prog(nc)
        C = Ctx(nc, st, P)
        C.init_w(3, 4096)
        hT, _ = C.sb("hT", [128, 8, NTOK], F32); hTb = [Buf("hT%d" % i) for i in range(NTT)]
        bufA, _ = C.sb("bufA", [128, 8, NTOK], BF16); bufAb = [Buf("bA%d" % i) for i in range(NTT)]
        bufH, _ = C.sb("bufH", [128, 8, NTOK], BF16); bufHb = [Buf("bH%d" % i) for i in range(NTT)]
        V, vb = C.sb("V_sb", [128, NVC], F32); C.vb = vb
        cst3, cstb = C.sb("cst_sb", [128, 5, 128], BF16)
        c32, c32b = C.sb("c32_sb", [128, 2, 128], F32)
        scr = dict(sq=C.sb("sq", [128, 8, 512], BF16), rs=C.sb("rs", [128, 512], F32), sg=C.sb("sg", [128, 512], F32),
                   rd=C.sb("rd", [128, 512], F32))
        sqq, _ = C.sb("sqq", [128, 2, NTT, 512], BF16)
        scr["sqq"] = (sqq, [Buf("sqq%d" % i) for i in range(NTT)])
        et, _ = C.sb("et", [128, 2, 512], BF16)
        scr["et"] = (et, [Buf("et0"), Buf("et1")])
        C.dma(V[:, :], vec_d[:, :], W=[vb])
        C.dma(c32[:, :, :], c32_d.rearrange("p (a b) -> p a b", a=2), W=[c32b])
        P.op("pool", "dma_start", PK(out=cst3[:, :, :], in_=cst_d.rearrange("p (a b) -> p a b", a=5)), writes=[cstb], is_dma=True)
        for tt in range(NTT):
            sl = slice(tt * TT, (tt + 1) * TT)
            C.dma(hT[:, :, sl], h1T_d[:, sl].rearrange("(c p) n -> p c n", p=128), W=[hTb[tt]])
            P.op("pool", "dma_start", PK(out=bufA[:, :, sl], in_=atT_d[:, sl].rearrange("(c p) n -> p c n", p=128)),
                 writes=[bufAb[tt]], is_dma=True)

        def evac_res(oc, tt, pt, pb, m):
            sl = slice(tt * TT, (tt + 1) * TT)
            P.op("dve", "tensor_tensor", PK(out=hT[:, oc, sl], in0=pt[:, :], in1=hT[:, oc, sl], op=ALU.add), reads=[pb, hTb[tt]], writes=[hTb[tt]])
        linear_fm(C, wdo_d, 8, bufA, None, 1024, evac_res, xinbs=bufAb)
        (kn, knb), (vt, vtb) = mem_kv(C, memT_d, wkv_d, V, vb, VC["n_mem"], VC["k_gain"], cst3, cstb, scr, "m1")
        mem_xattn(C, hT, hTb, bufA, bufAb, bufH, bufHb, V, vb, VC["n_xattn"], VC["q_gain"], wq_d, wo_d, kn, knb, vt, vtb, cst3, cstb, scr)
        for tt in range(NTT):
            sl = slice(tt * TT, (tt + 1) * TT)
            norm_fm(C, lambda c: hT[:, c, sl], [hTb[tt]], TT, 8, cst3[:, 0, :], cstb, lambda c: V[:, VC["n_ffn"] + c:VC["n_ffn"] + c + 1],
                    lambda c: bufA[:, c, sl], [bufAb[tt]], scr)
        hn32, hn32b = C.sb("hn32", [128, 8, 128], F32)
        wr, wrb = C.sb("wr", [128, 8, 8], F32)
        G, Gb = C.sb("G", [128, 16, 8], F32)
        lg, lgb = C.sb("lg", [128, 8], F32)
        mx, mxb = C.sb("mx", [128, 8], F32)
        sm, smb = C.sb("sm", [128, 4], F32)
        C.dma(wr[:, :, :], wr_d.rearrange("(c p) n -> p c n", p=128), W=[wrb])
        for s in range(16):
            tt = s // 4
            sl = slice(s * 128, (s + 1) * 128)
            norm_fm(C, lambda c: hT[:, c, sl], [hTb[tt]], 128, 8, cst3[:, 0, :], cstb, lambda c: V[:, VC["n_ffn"] + c:VC["n_ffn"] + c + 1],
                    lambda c: hn32[:, c, :], [hn32b], scr)
            pt, pb = C.psum()
            for k in range(8):
                P.op("pe", "matmul", PK(pt[:, 0:8], hn32[:, k, :], wr[:, k, :], start=(k == 0), stop=(k == 7)), reads=[hn32b, wrb], writes=[pb])
            P.op("dve", "tensor_tensor", PK(out=lg[:, :], in0=pt[:, 0:8], in1=V[:, VC["b_router"]:VC["b_router"] + 8], op=ALU.add),
                 reads=[pb, vb], writes=[lgb])
            P.op("dve", "max", PK(out=mx[:, :], in_=lg[:, :]), reads=[lgb], writes=[mxb])
            P.op("dve", "tensor_scalar", PK(out=sm[:, 0:1], in0=mx[:, 0:1], scalar1=-1.0, scalar2=None, op0=ALU.mult), reads=[mxb], writes=[smb])
            ex, exb = scr["sg"]
            P.op("act", "activation", PK(out=ex[:, 0:8], in_=lg[:, :], func=AF.Exp, bias=sm[:, 0:1]), reads=[lgb, smb], writes=[exb])
            P.op("dve", "tensor_scalar", PK(out=lg[:, :], in0=lg[:, :], scalar1=mx[:, 1:2], scalar2=None, op0=ALU.is_ge), reads=[lgb, mxb], writes=[lgb])
            P.op("dve", "tensor_tensor", PK(out=ex[:, 0:8], in0=ex[:, 0:8], in1=lg[:, :], op=ALU.mult), reads=[exb, lgb], writes=[exb])
            P.op("dve", "reduce_sum", PK(out=sm[:, 1:2], in_=ex[:, 0:8], axis=AX.X), reads=[exb], writes=[smb])
            P.op("dve", "reciprocal", PK(out=sm[:, 2:3], in_=sm[:, 1:2]), reads=[smb], writes=[smb])
            P.op("dve", "tensor_scalar", PK(out=G[:, s, :], in0=ex[:, 0:8], scalar1=sm[:, 2:3], scalar2=None, op0=ALU.mult), reads=[exb, smb], writes=[Gb])
        gbc, _ = C.sb("gbc", [128, NTOK], BF16); gbcb = [Buf("gbc%d" % i) for i in range(NTT)]
        dg, dgb = C.sb("dg", [128, 128], F32)
        for e_ in range(8):
            for tt in range(NTT):
                pt, pb = C.psum()
                for q in range(4):
                    s = tt * 4 + q
                    P.op("dve", "tensor_scalar", PK(out=dg[:, :], in0=c32[:, 0, :], scalar1=G[:, s, e_:e_ + 1], scalar2=None, op0=ALU.mult),
                         reads=[c32b, Gb], writes=[dgb])
                    P.op("pe", "matmul", PK(pt[:, q * 128:(q + 1) * 128], c32[:, 1, :], dg[:, :], start=True, stop=True), reads=[dgb, c32b], writes=[pb])
                P.op("act", "activation", PK(out=gbc[:, tt * TT:(tt + 1) * TT], in_=pt[:, :], func=AF.Copy), reads=[pb], writes=[gbcb[tt]])
            swiglu_ffn(C, hT, hTb, bufA, bufAb, bufH, bufHb, V, vb, None, wg_d[e_], wu_d[e_], wd_d[e_], 3584, cst3, cstb, scr, gate_bc=(gbc, gbcb))
        for tt in range(NTT):
            sl = slice(tt * TT, (tt + 1) * TT)
            C.dma(outT_d[:, sl].rearrange("(c p) n -> p c n", p=128), hT[:, :, sl], R=[hTb[tt]], is_out=True)
        P.emit(st)
    return nc


def c32_table():
    c = np.zeros((128, 2, 128), np.float32)
    c[:, 0, :] = np.eye(128, dtype=np.float32)
    c[:, 1, :] = 1.0
    return c.reshape(128, 256)


def vecs_C(inp):
    V = np.zeros((128, NVC), np.float32)
    def put(name, arr):
        a = col(arr); V[:, VC[name]:VC[name] + a.shape[1]] = a
    put("n_xattn", inp["norm_xattn"][1]); put("n_mem", inp["norm_mem"][1])
    put("q_gain", inp["xa_q_gain"][1]); put("k_gain", inp["xa_k_gain"][1]); put("n_ffn", inp["norm_ffn"][1])
    V[:, VC["b_router"]:VC["b_router"] + 8] = np.asarray(inp["moe_b_router"][0], np.float32)[None, :]
    return V


LAMBDA_INIT = 0.8 - 0.6 * float(np.exp(-0.3 * 1))


def build_C1():
    nc = bass.Bass("TRN2", target_bir_lowering=False)
    dr = lambda n, s, k="ExternalInput", dt=F32: nc.dram_tensor(n, s, dt, kind=k).ap()
    qa_d = dr("qaug", [16, 68, NTOK]); ka_d = dr("kaug", [16, 68, 4096]); vf_d = dr("vfull", [4096, D])
    bias_d = dr("biasT", [128, 8 * 512]); lamb_d = dr("lamb", [128, 4 * 64]); sgc_d = dr("sgc", [128, 1]); cst_d = dr("cst", [128, 5 * 128])
    at_d = dr("attnT", [D, NTOK], "ExternalOutput")
    with ExitStack() as st:
        P = Prog(nc)
        C = Ctx(nc, st, P)
        cst3, cstb = C.sb("cst_sb", [128, 5, 128], BF16)
        P.op("pool", "dma_start", PK(out=cst3[:, :, :], in_=cst_d.rearrange("p (a b) -> p a b", a=5)), writes=[cstb], is_dma=True)
        biasT, biasb = C.sb("bias_sb", [128, 8, 512], F32)
        C.dma(biasT[:, :, :], bias_d.rearrange("p (a b) -> p a b", a=8), W=[biasb])
        lamb, lambb = C.sb("lamb_sb", [128, 4, 64], F32)
        C.dma(lamb[:, :, :], lamb_d.rearrange("p (a b) -> p a b", a=4), W=[lambb])
        sgc, sgcb = C.sb("sgc_sb", [128, 1], F32)
        C.dma(sgc[:, :], sgc_d[:, :], W=[sgcb])
        scr = dict(sq=C.sb("sq", [128, 1, 512], BF16), rs=C.sb("rs", [128, 512], F32))
        lt, ltb = C.sb("lt", [128, 2, 64], F32)
        lc, lcb = C.sb("lc", [128, 4], F32)
        for i in range(2):
            P.op("dve", "tensor_tensor", PK(out=lt[:, i, :], in0=lamb[:, 2 * i, :], in1=lamb[:, 2 * i + 1, :], op=ALU.mult), reads=[lambb], writes=[ltb])
            P.op("dve", "reduce_sum", PK(out=lc[:, i:i + 1], in_=lt[:, i, :], axis=AX.X), reads=[ltb], writes=[lcb])
        P.op("act", "activation", PK(out=lc[:, 0:2], in_=lc[:, 0:2], func=AF.Exp), reads=[lcb], writes=[lcb])
        P.op("dve", "tensor_tensor", PK(out=lc[:, 2:3], in0=lc[:, 1:2], in1=lc[:, 0:1], op=ALU.subtract), reads=[lcb], writes=[lcb])
        P.op("dve", "tensor_scalar_add", PK(out=lc[:, 3:4], in0=lc[:, 2:3], scalar1=-LAMBDA_INIT), reads=[lcb], writes=[lcb])
        P.op("dve", "tensor_scalar", PK(out=sgc[:, :], in0=sgc[:, :], scalar1=float(1.0 - LAMBDA_INIT), scalar2=None, op0=ALU.mult), reads=[sgcb], writes=[sgcb])
        kA = [C.sb("kA%d" % i, [68, 2, 4096], BF16) for i in range(2)]
        qA = [C.sb("qA%d" % i, [68, 2, NTOK], BF16) for i in range(2)]
        vA = [C.sb("vA%d" % i, [128, 32, 128], BF16) for i in range(2)]
        et, _ = C.sb("et", [128, 4, 512], BF16); etb = [Buf("et%d" % i) for i in range(4)]
        tmp, _ = C.sb("tmp", [128, 2, 512], F32); tmpb = [Buf("tmp%d" % i) for i in range(2)]
        rd, rdb = C.sb("rd", [128, 512], F32)
        o0, o0b = C.sb("o0", [128, 512], F32)
        o1, o1b = C.sb("o1", [128, 512], F32)
        ot, otb = C.sb("ot", [128, 512], F32)
        ei = 0
        ti_ = 0
        for h in range(8):
            slope = float(2.0 ** (-(h + 1)))
            kt, ktb = kA[h % 2]; qt, qtb = qA[h % 2]; vt, vtb = vA[h % 2]
            P.op("pool", "dma_start", PK(out=kt[:, :, :], in_=ka_d[2 * h:2 * h + 2].rearrange("c r n -> r c n")), writes=[ktb], is_dma=True)
            P.op("pool", "dma_start", PK(out=qt[:, :, :], in_=qa_d[2 * h:2 * h + 2].rearrange("c r n -> r c n")), writes=[qtb], is_dma=True)
            P.op("pool", "dma_start", PK(out=vt[:, :, :], in_=vf_d[:, h * 128:(h + 1) * 128].rearrange("(j p) d -> p j d", p=128)), writes=[vtb], is_dma=True)
            for m in range(4):
                nkb = 8 * (m + 1)
                qsl = slice(m * 512, (m + 1) * 512)
                N = [C.ps[0], C.ps[1]]
                Dn = [C.ps[2], C.ps[3]]
                for j in range(nkb):
                    for c in range(2):
                        sc, scb = C.ps[4 + ((2 * j + c) % 4)]
                        P.op("pe", "matmul", PK(sc[:, :], kt[:, c, j * 128:(j + 1) * 128], qt[:, c, qsl], start=True, stop=True),
                             reads=[ktb, qtb], writes=[scb])
                        e_slot = ei % 4; ei += 1
                        if j >= nkb - 8:
                            s = j - (nkb - 8)
                            tb = ti_ % 2; ti_ += 1
                            P.op("dve", "scalar_tensor_tensor", PK(out=tmp[:, tb, :], in0=biasT[:, s, :], scalar=slope, in1=sc[:, :], op0=ALU.mult, op1=ALU.add),
                                 reads=[biasb, scb], writes=[tmpb[tb]])
                            P.op("act", "activation", PK(out=et[:, e_slot, :], in_=tmp[:, tb, :], func=AF.Exp), reads=[tmpb[tb]], writes=[etb[e_slot]])
                        else:
                            P.op("act", "activation", PK(out=et[:, e_slot, :], in_=sc[:, :], func=AF.Exp), reads=[scb], writes=[etb[e_slot]])
                        P.op("pe", "matmul", PK(N[c][0][:, :], vt[:, j, :], et[:, e_slot, :], start=(j == 0), stop=(j == nkb - 1)),
                             reads=[vtb, etb[e_slot]], writes=[N[c][1]])
                        P.op("pe", "matmul", PK(Dn[c][0][:, :], cst3[:, 4, :], et[:, e_slot, :], start=(j == 0), stop=(j == nkb - 1)),
                             reads=[cstb, etb[e_slot]], writes=[Dn[c][1]])
                P.op("dve", "reciprocal", PK(out=rd[:, :], in_=Dn[0][0][:, :]), reads=[Dn[0][1]], writes=[rdb])
                P.op("dve", "tensor_tensor", PK(out=o0[:, :], in0=N[0][0][:, :], in1=rd[:, :], op=ALU.mult), reads=[N[0][1], rdb], writes=[o0b])
                P.op("dve", "reciprocal", PK(out=rd[:, :], in_=Dn[1][0][:, :]), reads=[Dn[1][1]], writes=[rdb])
                P.op("dve", "tensor_tensor", PK(out=o1[:, :], in0=N[1][0][:, :], in1=rd[:, :], op=ALU.mult), reads=[N[1][1], rdb], writes=[o1b])
                P.op("dve", "scalar_tensor_tensor", PK(out=o0[:, :], in0=o1[:, :], scalar=lc[:, 3:4], in1=o0[:, :], op0=ALU.mult, op1=ALU.add),
                     reads=[o1b, o0b, lcb], writes=[o0b])
                sq, sqb = scr["sq"]; rs, rsb = scr["rs"]
                P.op("act", "activation", PK(out=sq[:, 0, :], in_=o0[:, :], func=AF.Square), reads=[o0b], writes=[sqb])
                pn, pnb = C.ps[4]
                P.op("pe", "matmul", PK(pn[:, :], cst3[:, 3, :], sq[:, 0, :], start=True, stop=True), reads=[sqb, cstb], writes=[pnb])
                rstd_from(C, rs[:, :], pn[:, :], pnb, rsb)
                P.op("dve", "scalar_tensor_tensor", PK(out=ot[:, :], in0=o0[:, :], scalar=sgc[:, 0:1], in1=rs[:, :], op0=ALU.mult, op1=ALU.mult),
                     reads=[o0b, rsb, sgcb], writes=[otb])
                C.dma(at_d[h * 128:(h + 1) * 128, qsl], ot[:, :], R=[otb], is_out=True)
        P.emit(st)
    return nc


def bias_table(p):
    t = np.zeros((8, 128, 512), np.float32)
    kq = np.arange(128)[:, None]
    qq = np.arange(512)[None, :]
    for s in range(8):
        if p == 0:
            r = s if s < 4 else None
            full_mask = s >= 4
        else:
            r = s - 4 if s >= 4 else None
            full_mask = False
        if full_mask:
            t[s] = -1e30
        elif r is not None:
            kpos = 128 * r + kq
            kc = kpos // 64; qc = qq // 64
            d = -2.0 * np.maximum(kpos - qq, 0).astype(np.float32)
            t[s] = np.where(kc < qc, 0.0, np.where(kc == qc, d, -1e30))
    return np.ascontiguousarray(t.transpose(1, 0, 2).reshape(128, 8 * 512))


def q_aug_rows(p):
    pos = tok_index(p)
    return np.stack([pos // 64, pos % 64, np.ones_like(pos), np.ones_like(pos)]).astype(np.float32)


def k_aug_rows(h):
    s = np.arange(4096)
    sl = 2.0 ** (-(h + 1))
    return np.stack([np.full(4096, -64.0 * sl), np.full(4096, -sl), 64.0 * sl * (s // 64), sl * (s % 64)]).astype(np.float32)


T_SEQ = 4096
SEG = 512
NSEG_FULL = T_SEQ // SEG
NCOLS = 1312
ZCH = [(0, 128), (128, 128), (256, 128), (384, 128), (512, 128), (640, 128), (768, 128), (896, 128), (1024, 32), (1056, 128), (1184, 128)]
VA = dict(n_mix=0, mu=8, w0=17, a0=19, k_k=21, k_a=23, r_k=25, ln_w=27, ln_b=29, s5_d=31, lam_re=33, lam_im=41, log_dt=49, halfpi=57)
NVA = 58
C_ID, C_B64, C_ONES = 0, 1, 2
NEG_E05 = -float(np.exp(-0.5))


def build_A(nseg=NSEG_FULL):
    nc = bass.Bass("TRN2", target_bir_lowering=False)
    dr = lambda n, s, k="ExternalInput", dt=F32: nc.dram_tensor(n, s, dt, kind=k).ap()
    xT_d = dr("xT", [D, T_SEQ]); wc_d = dr("wc", [D, NCOLS]); vec_d = dr("vecs", [128, NVA])
    cst_d = dr("cst", [128, 5 * 128]); c32_d = dr("c32", [128, 3 * 128]); msk_d = dr("mask5", [64, 320]); rmask_d = dr("rmask", [128, SEG])
    wup_d = dr("w_up", [64, 256]); aup_d = dr("a_up", [128, 256]); gupa_d = dr("g_upa", [128, 256]); gupb_d = dr("g_upb", [32, 256])
    bre_d = dr("breT", [8, 128, 128]); bim_d = dr("bimT", [8, 128, 128]); cre_d = dr("creT", [8, 128, 128]); cim_d = dr("cimT", [8, 128, 128])
    ya_d = dr("yaT", [256, T_SEQ], "ExternalOutput"); yb_d = dr("ybT", [256, T_SEQ], "ExternalOutput")
    with ExitStack() as st:
        P = Prog(nc)
        C = Ctx(nc, st, P)
        rot = [0, 1, 2, 3, 4, 5, 7]
        rs_ = [0]

        def psum():
            r = C.ps[rot[rs_[0] % len(rot)]]
            rs_[0] += 1
            return r
        C.psum = psum
        T = lambda name, shape, dt=F32: C.sb(name, shape, dt)
        V, vb = T("V_sb", [128, NVA]); C.vb = vb
        C.dma(V[:, :], vec_d[:, :], W=[vb])
        cst3, cstb = T("cst_sb", [128, 5, 128], BF16)
        P.op("pool", "dma_start", PK(out=cst3[:, :, :], in_=cst_d.rearrange("p (a b) -> p a b", a=5)), writes=[cstb], is_dma=True)
        c32, c32b = T("c32_sb", [128, 3, 128]); C.dma(c32[:, :, :], c32_d.rearrange("p (a b) -> p a b", a=3), W=[c32b])
        msk, mskb = T("msk_sb", [64, 320]); C.dma(msk[:, :], msk_d[:, :], W=[mskb])
        rmask, rmaskb = T("rmask_sb", [128, SEG]); C.dma(rmask[:, :], rmask_d[:, :], W=[rmaskb])
        wup, wupb = T("wup_sb", [64, 256]); C.dma(wup[:, :], wup_d[:, :], W=[wupb])
        aup, aupb = T("aup_sb", [128, 256]); C.dma(aup[:, :], aup_d[:, :], W=[aupb])
        gupa, gupab = T("gupa_sb", [128, 256]); C.dma(gupa[:, :], gupa_d[:, :], W=[gupab])
        gupb, gupbb = T("gupb_sb", [32, 256]); C.dma(gupb[:, :], gupb_d[:, :], W=[gupbb])
        wc, wcb = T("wc_sb", [128, 8, NCOLS], BF16)
        for k in range(8):
            P.op("pool", "dma_start", PK(out=wc[:, k, :], in_=wc_d[k * 128:(k + 1) * 128, :]), writes=[wcb], is_dma=True)
        scr = dict(sq=T("sq", [128, 8, 512], BF16), rs=T("rs", [128, 512]))
        ident = c32[:, C_ID, :]

        breT, breb = T("breT_sb", [128, 8, 128], BF16); bimT, bimb = T("bimT_sb", [128, 8, 128], BF16)
        P.op("pool", "dma_start", PK(out=breT[:, :, :], in_=bre_d.rearrange("q k m -> k q m")), writes=[breb], is_dma=True)
        P.op("pool", "dma_start", PK(out=bimT[:, :, :], in_=bim_d.rearrange("q k m -> k q m")), writes=[bimb], is_dma=True)
        creT, creb = T("creT_sb", [128, 8, 128]); cimT, cimb = T("cimT_sb", [128, 8, 128])
        C.dma(creT[:, :, :], cre_d.rearrange("q k m -> k q m"), W=[creb])
        C.dma(cimT[:, :, :], cim_d.rearrange("q k m -> k q m"), W=[cimb])
        s5c, s5cb = T("s5c", [128, 12, 8])
        lre = V[:, VA["lam_re"]:VA["lam_re"] + 8]; lim = V[:, VA["lam_im"]:VA["lam_im"] + 8]
        S = lambda i: s5c[:, i, :]
        dv = lambda name, **kw: P.op("dve", name, PK(**kw), reads=[s5cb, vb], writes=[s5cb])
        P.op("act", "activation", PK(out=S(0), in_=V[:, VA["log_dt"]:VA["log_dt"] + 8], func=AF.Exp), reads=[vb], writes=[s5cb])
        dv("tensor_tensor", out=S(1), in0=lim, in1=S(0), op=ALU.mult)
        dv("tensor_tensor", out=S(9), in0=lre, in1=S(0), op=ALU.mult)
        P.op("act", "activation", PK(out=S(2), in_=S(9), func=AF.Exp), reads=[s5cb], writes=[s5cb])
        P.op("act", "activation", PK(out=S(4), in_=S(1), func=AF.Sin, scale=0.125), reads=[s5cb], writes=[s5cb])
        P.op("act", "activation", PK(out=S(3), in_=S(1), func=AF.Sin, scale=-0.125, bias=V[:, VA["halfpi"]:VA["halfpi"] + 1]), reads=[s5cb, vb], writes=[s5cb])
        for _ in range(3):
            dv("tensor_tensor", out=S(9), in0=S(3), in1=S(3), op=ALU.mult)
            dv("tensor_tensor", out=S(10), in0=S(4), in1=S(4), op=ALU.mult)
            dv("tensor_tensor", out=S(11), in0=S(3), in1=S(4), op=ALU.mult)
            dv("tensor_tensor", out=S(3), in0=S(9), in1=S(10), op=ALU.subtract)
            dv("tensor_scalar", out=S(4), in0=S(11), scalar1=2.0, scalar2=None, op0=ALU.mult)
        dv("tensor_tensor", out=S(5), in0=S(2), in1=S(3), op=ALU.mult)
        dv("tensor_tensor", out=S(6), in0=S(2), in1=S(4), op=ALU.mult)
        dv("tensor_scalar_add", out=S(5), in0=S(5), scalar1=-1.0)
        dv("tensor_tensor", out=S(9), in0=lre, in1=lre, op=ALU.mult)
        dv("tensor_tensor", out=S(10), in0=lim, in1=lim, op=ALU.mult)
        dv("tensor_tensor", out=S(9), in0=S(9), in1=S(10), op=ALU.add)
        dv("reciprocal", out=S(9), in_=S(9))
        dv("tensor_tensor", out=S(10), in0=S(5), in1=lre, op=ALU.mult)
        dv("tensor_tensor", out=S(11), in0=S(6), in1=lim, op=ALU.mult)
        dv("tensor_tensor", out=S(10), in0=S(10), in1=S(11), op=ALU.add)
        dv("tensor_tensor", out=S(7), in0=S(10), in1=S(9), op=ALU.mult)
        dv("tensor_tensor", out=S(10), in0=S(6), in1=lre, op=ALU.mult)
        dv("tensor_tensor", out=S(11), in0=S(5), in1=lim, op=ALU.mult)
        dv("tensor_tensor", out=S(10), in0=S(10), in1=S(11), op=ALU.subtract)
        dv("tensor_tensor", out=S(8), in0=S(10), in1=S(9), op=ALU.mult)
        cpre, cpreb = T("cpre", [128, 8, 128], BF16); cpim, cpimb = T("cpim", [128, 8, 128], BF16)
        ctmp, ctmpb = T("ctmp", [128, 128])
        for q in range(8):
            cr = s5c[:, 7, q:q + 1]; ci = s5c[:, 8, q:q + 1]
            P.op("dve", "tensor_scalar", PK(out=ctmp[:, :], in0=cimT[:, q, :], scalar1=ci, scalar2=None, op0=ALU.mult), reads=[cimb, s5cb], writes=[ctmpb])
            P.op("dve", "scalar_tensor_tensor", PK(out=cpre[:, q, :], in0=creT[:, q, :], scalar=cr, in1=ctmp[:, :], op0=ALU.mult, op1=ALU.subtract),
                 reads=[creb, ctmpb, s5cb], writes=[cpreb])
            P.op("dve", "tensor_scalar", PK(out=ctmp[:, :], in0=cimT[:, q, :], scalar1=cr, scalar2=-1.0, op0=ALU.mult, op1=ALU.mult), reads=[cimb, s5cb], writes=[ctmpb])
            P.op("dve", "scalar_tensor_tensor", PK(out=ctmp[:, :], in0=creT[:, q, :], scalar=ci, in1=ctmp[:, :], op0=ALU.mult, op1=ALU.subtract),
                 reads=[creb, ctmpb, s5cb], writes=[ctmpb])
            P.op("dve", "tensor_scalar", PK(out=cpim[:, q, :], in0=ctmp[:, :], scalar1=-1.0, scalar2=None, op0=ALU.mult), reads=[ctmpb], writes=[cpimb])
        Fc, Fcb = T("Fc", [128, 8, SEG]); Fs, Fsb = T("Fs", [128, 8, SEG])
        ncol, ncolb = T("ncol", [128, 2])
        ftmp, ftmpb = T("ftmp", [128, SEG // 2])
        for q in range(8):
            P.op("dve", "tensor_copy", PK(out=Fc[:, q, 0:1], in_=s5c[:, 3, q:q + 1]), reads=[s5cb], writes=[Fcb])
            P.op("dve", "tensor_copy", PK(out=Fs[:, q, 0:1], in_=s5c[:, 4, q:q + 1]), reads=[s5cb], writes=[Fsb])
            n = 1
            while n < SEG:
                cn = Fc[:, q, n - 1:n]; sn = Fs[:, q, n - 1:n]
                P.op("dve", "tensor_scalar", PK(out=ftmp[:, 0:n], in0=Fs[:, q, 0:n], scalar1=sn, scalar2=None, op0=ALU.mult), reads=[Fsb], writes=[ftmpb])
                P.op("dve", "scalar_tensor_tensor", PK(out=Fc[:, q, n:2 * n], in0=Fc[:, q, 0:n], scalar=cn, in1=ftmp[:, 0:n], op0=ALU.mult, op1=ALU.subtract),
                     reads=[Fcb, ftmpb], writes=[Fcb])
                P.op("dve", "tensor_scalar", PK(out=ftmp[:, 0:n], in0=Fc[:, q, 0:n], scalar1=sn, scalar2=None, op0=ALU.mult), reads=[Fcb, Fsb], writes=[ftmpb])
                P.op("dve", "scalar_tensor_tensor", PK(out=Fs[:, q, n:2 * n], in0=Fs[:, q, 0:n], scalar=cn, in1=ftmp[:, 0:n], op0=ALU.mult, op1=ALU.add),
                     reads=[Fsb, Fcb, ftmpb], writes=[Fsb])
                n *= 2
        xst_re, xreb = T("xst_re", [128, 8, 2]); xst_im, ximb = T("xst_im", [128, 8, 2])
        P.op("dve", "memset", PK(xst_re[:, :, :], 0.0), writes=[xreb]); P.op("dve", "memset", PK(xst_im[:, :, :], 0.0), writes=[ximb])

        zT, _ = T("zT", [128, 11, SEG + 1]); zTb = [Buf("zT%d" % c) for c in range(11)]
        for c in range(11):
            P.op("dve", "memset", PK(zT[:, c, 0:1], 0.0), writes=[zTb[c]])
        Sst = [[T("S%d_%d" % (hp, i), [128, 64]) for i in range(2)] for hp in range(2)]
        for hp in range(2):
            P.op("dve", "memset", PK(Sst[hp][0][0][:, :], 0.0), writes=[Sst[hp][0][1]])
        sidx = [0, 0]
        xT_t, xTb = T("xT_sb", [128, 8, SEG], BF16); hn, hnb = T("hn", [128, 8, SEG], BF16)
        names = "ld a g al be km cum Gi tA tB Rb Kb Ab Bb Kh Bh rk".split()
        W_ = {n: T("w_" + n, [128, SEG]) for n in names}
        gC, gCb = T("gC", [128, 8])
        AAs, AAsb = T("AAs", [64, 320]); TM, TMb = T("TM", [64, 4, 128])
        Asq = [T("Asq%d" % i, [64, 128]) for i in range(2)]
        Zt = [T("Zt%d" % i, [64, 64]) for i in range(2)]
        Wsb, Wsbb = T("Wsb", [64, 64]); Ut0, Ut0b = T("Ut0", [64, 64]); Ut, Utb = T("Ut", [64, 64])
        Phi, Phib = T("Phi", [128, 64])
        ubuf, ubufb = T("ubuf", [128, 2, SEG], BF16)
        s5t = {n: T("s5_" + n, [128, SEG]) for n in "t1 t2 cre cim zre zim xre xim".split()}
        tw, twb = s5t["t1"]; sg0, sg0b = s5t["t2"]; sg1, sg1b = s5t["zre"]
        xreb16, xreb16b = T("xre16", [128, 8, SEG], BF16); ximb16, ximb16b = T("xim16", [128, 8, SEG], BF16)
        yo, yob = s5t["cre"]

        for seg in range(nseg):
            tsl = slice(seg * SEG, (seg + 1) * SEG)
            P.op("pool", "dma_start", PK(out=xT_t[:, :, :], in_=xT_d[:, tsl].rearrange("(c p) n -> p c n", p=128)), writes=[xTb], is_dma=True)
            norm_fm(C, lambda c: xT_t[:, c, :], [xTb], SEG, 8, cst3[:, 0, :], cstb, lambda c: V[:, VA["n_mix"] + c:VA["n_mix"] + c + 1],
                    lambda c: hn[:, c, :], [hnb], scr)
            for c, (c0, wdt) in enumerate(ZCH):
                pt, pb = C.psum()
                for k in range(8):
                    P.op("pe", "matmul", PK(pt[0:wdt, :], wc[:, k, c0:c0 + wdt], hn[:, k, :], start=(k == 0), stop=(k == 7)), reads=[wcb, hnb], writes=[pb])
                P.op("act", "activation", PK(out=zT[0:wdt, c, 1:SEG + 1], in_=pt[0:wdt, :], func=AF.Copy), reads=[pb], writes=[zTb[c]])
            tA, tAb = W_["tA"]
            for c in range(9):
                wdt = ZCH[c][1]
                P.op("dve", "tensor_tensor", PK(out=tA[0:wdt, :], in0=zT[0:wdt, c, 0:SEG], in1=zT[0:wdt, c, 1:SEG + 1], op=ALU.subtract), reads=[zTb[c]], writes=[tAb])
                P.op("dve", "tensor_copy", PK(out=zT[0:wdt, c, 0:1], in_=zT[0:wdt, c, SEG:SEG + 1]), reads=[tAb], writes=[zTb[c]])
                P.op("dve", "scalar_tensor_tensor", PK(out=zT[0:wdt, c, 1:SEG + 1], in0=tA[0:wdt, :], scalar=V[0:wdt, VA["mu"] + c:VA["mu"] + c + 1],
                                                      in1=zT[0:wdt, c, 1:SEG + 1], op0=ALU.mult, op1=ALU.add), reads=[tAb, zTb[c], vb], writes=[zTb[c]])
            Z = lambda c, lo=0, hi=128: zT[lo:hi, c, 1:SEG + 1]
            P.op("act", "activation", PK(out=tw[0:64, :], in_=Z(6, 0, 64), func=AF.Tanh), reads=[zTb[6]], writes=[twb])
            P.op("act", "activation", PK(out=sg0[:, :], in_=Z(7), func=AF.Sigmoid), reads=[zTb[7]], writes=[sg0b])
            P.op("act", "activation", PK(out=sg1[0:32, :], in_=Z(8, 0, 32), func=AF.Sigmoid), reads=[zTb[8]], writes=[sg1b])
            for hp in range(2):
                cols = slice(hp * 128, (hp + 1) * 128)
                vcol = lambda nm: V[:, VA[nm] + hp:VA[nm] + hp + 1]
                r_ = Z(hp); k_ = Z(2 + hp); v_ = Z(4 + hp)
                rb_, kb_, vb_ = zTb[hp], zTb[2 + hp], zTb[4 + hp]
                X = lambda n: W_[n][0]
                B_ = lambda n: W_[n][1]
                DV = lambda name, R, Wn, **kw: P.op("dve", name, PK(**kw), reads=R, writes=[B_(Wn)])
                pt, pb = C.psum()
                P.op("pe", "matmul", PK(pt[:, :], wup[:, cols], tw[0:64, :], start=True, stop=True), reads=[wupb, twb], writes=[pb])
                P.op("act", "activation", PK(out=X("ld")[:, :], in_=pt[:, :], func=AF.Sigmoid, bias=vcol("w0")), reads=[pb, vb], writes=[B_("ld")])
                DV("tensor_scalar", [B_("ld")], "ld", out=X("ld")[:, :], in0=X("ld")[:, :], scalar1=NEG_E05, scalar2=None, op0=ALU.mult)
                pt, pb = C.psum()
                P.op("pe", "matmul", PK(pt[:, :], aup[64:128, cols], Z(6, 64, 128), start=True, stop=True), reads=[aupb, zTb[6]], writes=[pb])
                P.op("act", "activation", PK(out=X("a")[:, :], in_=pt[:, :], func=AF.Sigmoid, bias=vcol("a0")), reads=[pb, vb], writes=[B_("a")])
                pt, pb = C.psum()
                P.op("pe", "matmul", PK(pt[:, :], gupa[:, cols], sg0[:, :], start=True, stop=False), reads=[gupab, sg0b], writes=[pb])
                P.op("pe", "matmul", PK(pt[:, :], gupb[:, cols], sg1[0:32, :], start=False, stop=True), reads=[gupbb, sg1b], writes=[pb])
                P.op("act", "activation", PK(out=X("g")[:, :], in_=pt[:, :], func=AF.Copy), reads=[pb], writes=[B_("g")])
                DV("tensor_scalar", [kb_, vb], "tA", out=X("tA")[:, :], in0=k_, scalar1=vcol("k_k"), scalar2=None, op0=ALU.mult)
                P.op("act", "activation", PK(out=X("tB")[:, :], in_=X("tA")[:, :], func=AF.Square), reads=[B_("tA")], writes=[B_("tB")])
                pt, pb = C.psum()
                P.op("pe", "matmul", PK(pt[:, :], c32[:, C_B64, :], X("tB")[:, :], start=True, stop=True), reads=[c32b, B_("tB")], writes=[pb])
                rs, rsb = scr["rs"]
                rstd_from(C, rs[:, :], pt[:, :], pb, rsb)
                DV("scalar_tensor_tensor", [B_("tA"), rsb], "al", out=X("al")[:, :], in0=X("tA")[:, :], scalar=0.125, in1=rs[:, :], op0=ALU.mult, op1=ALU.mult)
                DV("scalar_tensor_tensor", [B_("al"), B_("a")], "be", out=X("be")[:, :], in0=X("al")[:, :], scalar=-1.0, in1=X("a")[:, :], op0=ALU.mult, op1=ALU.mult)
                DV("tensor_scalar", [B_("a"), vb], "tA", out=X("tA")[:, :], in0=X("a")[:, :], scalar1=-1.0, scalar2=vcol("k_a"), op0=ALU.add, op1=ALU.mult)
                DV("scalar_tensor_tensor", [B_("tA"), kb_], "km", out=X("km")[:, :], in0=X("tA")[:, :], scalar=1.0, in1=k_, op0=ALU.add, op1=ALU.mult)
                DV("scalar_tensor_tensor", [rb_, B_("km"), vb], "rk", out=X("rk")[:, :], in0=r_, scalar=vcol("r_k"), in1=X("km")[:, :], op0=ALU.mult, op1=ALU.mult)
                DV("tensor_tensor_scan", [rmaskb, B_("ld")], "cum", out=X("cum")[:, :], data0=rmask[:, :], data1=X("ld")[:, :], initial=0.0, op0=ALU.mult, op1=ALU.add)
                cum3 = X("cum")[:, :].rearrange("p (c t) -> p c t", t=64)
                P.op("act", "activation", PK(out=X("Gi")[:, :], in_=X("cum")[:, :], func=AF.Exp), reads=[B_("cum")], writes=[B_("Gi")])
                DV("tensor_tensor", [B_("Gi"), rb_], "Rb", out=X("Rb")[:, :], in0=r_, in1=X("Gi")[:, :], op=ALU.mult)
                DV("tensor_tensor", [B_("cum"), B_("ld")], "tA", out=X("tA")[:, :], in0=X("cum")[:, :], in1=X("ld")[:, :], op=ALU.subtract)
                P.op("act", "activation", PK(out=X("Gi")[:, :], in_=X("tA")[:, :], func=AF.Exp), reads=[B_("tA")], writes=[B_("Gi")])
                DV("tensor_tensor", [B_("Gi"), B_("al")], "Ab", out=X("Ab")[:, :], in0=X("al")[:, :], in1=X("Gi")[:, :], op=ALU.mult)
                P.op("act", "activation", PK(out=X("Gi")[:, :], in_=X("cum")[:, :], func=AF.Exp, scale=-1.0), reads=[B_("cum")], writes=[B_("Gi")])
                DV("tensor_tensor", [B_("Gi"), B_("km")], "Kb", out=X("Kb")[:, :], in0=X("km")[:, :], in1=X("Gi")[:, :], op=ALU.mult)
                DV("tensor_tensor", [B_("Gi"), B_("be")], "Bb", out=X("Bb")[:, :], in0=X("be")[:, :], in1=X("Gi")[:, :], op=ALU.mult)
                tA3 = X("tA")[:, :].rearrange("p (c t) -> p c t", t=64)
                DV("tensor_tensor", [B_("cum")], "tA", out=tA3, in0=cum3[:, :, 63:64].to_broadcast([128, 8, 64]), in1=cum3, op=ALU.subtract)
                P.op("act", "activation", PK(out=X("Gi")[:, :], in_=X("tA")[:, :], func=AF.Exp), reads=[B_("tA")], writes=[B_("Gi")])
                DV("tensor_tensor", [B_("Gi"), B_("km")], "Kh", out=X("Kh")[:, :], in0=X("km")[:, :], in1=X("Gi")[:, :], op=ALU.mult)
                DV("tensor_tensor", [B_("Gi"), B_("be")], "Bh", out=X("Bh")[:, :], in0=X("be")[:, :], in1=X("Gi")[:, :], op=ALU.mult)
                P.op("act", "activation", PK(out=gC[:, :], in_=cum3[:, :, 63], func=AF.Exp), reads=[B_("cum")], writes=[gCb])
                po, pob = C.ps[6]
                for c in range(SEG // 64):
                    cs = slice(c * 64, (c + 1) * 64)
                    pt, pb = C.psum()
                    for i, (src, sb_) in enumerate(((v_, vb_), (X("Ab")[:, :], B_("Ab")), (X("Bh")[:, :], B_("Bh")), (X("Kh")[:, :], B_("Kh")))):
                        P.op("pe", "transpose", PK(pt[0:64, i * 128:(i + 1) * 128], src[:, cs], ident), reads=[sb_, c32b], writes=[pb])
                    P.op("act", "activation", PK(out=TM[:, :, :], in_=pt[0:64, :].rearrange("p (i k) -> p i k", i=4), func=AF.Copy), reads=[pb], writes=[TMb])
                    for e in range(2):
                        pr = slice(e * 64, (e + 1) * 64)
                        es = slice(e * 64, (e + 1) * 64)
                        pt, pb = C.psum()
                        Bb_, Kb_, Ab_, Rb_ = X("Bb")[pr, cs], X("Kb")[pr, cs], X("Ab")[pr, cs], X("Rb")[pr, cs]
                        P.op("pe", "matmul", PK(pt[0:64, 0:64], Bb_, Ab_, start=True, stop=True), reads=[B_("Bb"), B_("Ab")], writes=[pb])
                        P.op("pe", "matmul", PK(pt[0:64, 64:128], Bb_, Rb_, start=True, stop=True), reads=[B_("Bb"), B_("Rb")], writes=[pb])
                        P.op("pe", "matmul", PK(pt[0:64, 128:192], Kb_, Ab_, start=True, stop=True), reads=[B_("Kb"), B_("Ab")], writes=[pb])
                        P.op("pe", "matmul", PK(pt[0:64, 192:256], Kb_, Rb_, start=True, stop=True), reads=[B_("Kb"), B_("Rb")], writes=[pb])
                        P.op("pe", "matmul", PK(pt[0:64, 256:320], Ab_, Bb_, start=True, stop=True), reads=[B_("Bb"), B_("Ab")], writes=[pb])
                        P.op("dve", "tensor_tensor", PK(out=AAs[:, :], in0=pt[0:64, 0:320], in1=msk[:, :], op=ALU.mult), reads=[pb, mskb], writes=[AAsb])
                        A_ab, A_rb, A_ak, A_rk, A_abT = (AAs[:, 0:64], AAs[:, 64:128], AAs[:, 128:192], AAs[:, 192:256], AAs[:, 256:320])
                        Vt = TM[:, 0, es]; AbT = TM[:, 1, es]; BhT = TM[:, 2, es]; KhT = TM[:, 3, es]
                        zi = 0
                        P.op("dve", "tensor_tensor", PK(out=Zt[0][0][:, :], in0=A_ab, in1=ident[0:64, 0:64], op=ALU.add), reads=[AAsb, c32b], writes=[Zt[0][1]])
                        curA, curAT, curb = A_ab, A_abT, AAsb
                        for lvl in range(1, 6):
                            psq, psqb = C.psum()
                            if lvl < 5:
                                P.op("pe", "matmul", PK(psq[0:64, 0:64], curAT, curA, start=True, stop=True), reads=[curb], writes=[psqb])
                            P.op("pe", "matmul", PK(psq[0:64, 64:128], curA, curAT, start=True, stop=True), reads=[curb], writes=[psqb])
                            at, atb = Asq[lvl % 2]
                            lo = 0 if lvl < 5 else 64
                            P.op("act", "activation", PK(out=at[:, lo:128], in_=psq[0:64, lo:128], func=AF.Copy), reads=[psqb], writes=[atb])
                            curA, curAT, curb = at[:, 0:64], at[:, 64:128], atb
                            pz, pzb = C.psum()
                            P.op("pe", "matmul", PK(pz[0:64, 0:64], curAT, Zt[zi][0][:, :], start=True, stop=True), reads=[curb, Zt[zi][1]], writes=[pzb])
                            P.op("dve", "tensor_tensor", PK(out=Zt[1 - zi][0][:, :], in0=pz[0:64, 0:64], in1=Zt[zi][0][:, :], op=ALU.add),
                                 reads=[pzb, Zt[zi][1]], writes=[Zt[1 - zi][1]])
                            zi = 1 - zi
                        Tm, Tmb = Zt[zi]
                        pt, pb = C.psum()
                        P.op("pe", "matmul", PK(pt[0:64, 0:64], A_ak, Vt, start=True, stop=True), reads=[AAsb, TMb], writes=[pb])
                        P.op("act", "activation", PK(out=Wsb[:, :], in_=pt[0:64, 0:64], func=AF.Copy), reads=[pb], writes=[Wsbb])
                        pt, pb = C.psum()
                        P.op("pe", "matmul", PK(pt[0:64, 0:64], Tm[:, :], Wsb[:, :], start=True, stop=True), reads=[Tmb, Wsbb], writes=[pb])
                        P.op("pe", "matmul", PK(pt[pr, 64:128], AbT, Tm[:, :], start=True, stop=True), reads=[Tmb, TMb], writes=[pb])
                        P.op("act", "activation", PK(out=Ut0[:, :], in_=pt[0:64, 0:64], func=AF.Copy), reads=[pb], writes=[Ut0b])
                        P.op("act", "activation", PK(out=Phi[pr, :], in_=pt[pr, 64:128], func=AF.Copy), reads=[pb], writes=[Phib])
                        Sc, Scb = Sst[hp][sidx[hp] % 2] if e == 0 else Sst[hp][sidx[hp] % 2]
                        Sn, Snb = Sst[hp][(sidx[hp] + 1) % 2]
                        pt, pb = C.psum()
                        P.op("pe", "matmul", PK(pt[0:64, 0:64], Phi[pr, :], Sc[pr, :], start=True, stop=True), reads=[Phib, Scb], writes=[pb])
                        P.op("dve", "tensor_tensor", PK(out=Ut[:, :], in0=pt[0:64, 0:64], in1=Ut0[:, :], op=ALU.add), reads=[pb, Ut0b], writes=[Utb])
                        P.op("pe", "matmul", PK(po[pr, cs], Sc[pr, :], Rb_, start=True, stop=False), reads=[Scb, B_("Rb")], writes=[pob])
                        P.op("pe", "matmul", PK(po[pr, cs], Ut[:, :], A_rb, start=False, stop=False), reads=[Utb, AAsb], writes=[pob])
                        P.op("pe", "matmul", PK(po[pr, cs], Vt, A_rk, start=False, stop=True), reads=[TMb, AAsb], writes=[pob])
                        pt, pb = C.psum()
                        P.op("pe", "matmul", PK(pt[pr, 0:64], BhT, Ut[:, :], start=True, stop=False), reads=[TMb, Utb], writes=[pb])
                        P.op("pe", "matmul", PK(pt[pr, 0:64], KhT, Vt, start=False, stop=True), reads=[TMb], writes=[pb])
                        P.op("dve", "scalar_tensor_tensor", PK(out=Sn[pr, :], in0=Sc[pr, :], scalar=gC[pr, c:c + 1], in1=pt[pr, 0:64], op0=ALU.mult, op1=ALU.add),
                             reads=[Scb, gCb, pb], writes=[Snb])
                    sidx[hp] += 1
                Osb, Osbb = W_["Gi"]
                P.op("act", "activation", PK(out=Osb[:, :], in_=po[:, :], func=AF.Copy), reads=[pob], writes=[Osbb])
                pm, pmb = C.psum()
                P.op("pe", "matmul", PK(pm[:, :], c32[:, C_B64, :], Osb[:, :], start=True, stop=True), reads=[c32b, Osbb], writes=[pmb])
                DV("tensor_tensor", [Osbb, pmb], "tA", out=X("tA")[:, :], in0=Osb[:, :], in1=pm[:, :], op=ALU.subtract)
                P.op("act", "activation", PK(out=X("tB")[:, :], in_=X("tA")[:, :], func=AF.Square), reads=[B_("tA")], writes=[B_("tB")])
                pv, pvb = C.psum()
                P.op("pe", "matmul", PK(pv[:, :], c32[:, C_B64, :], X("tB")[:, :], start=True, stop=True), reads=[c32b, B_("tB")], writes=[pvb])
                rstd_from(C, rs[:, :], pv[:, :], pvb, rsb, eps=64e-5)
                DV("tensor_tensor", [B_("tA"), rsb], "tA", out=X("tA")[:, :], in0=X("tA")[:, :], in1=rs[:, :], op=ALU.mult)
                P.op("act", "activation", PK(out=X("tB")[:, :], in_=X("tA")[:, :], func=AF.Identity, scale=vcol("ln_w"), bias=vcol("ln_b")),
                     reads=[B_("tA"), vb], writes=[B_("tB")])
                pk, pkb = C.psum()
                P.op("pe", "matmul", PK(pk[:, :], c32[:, C_B64, :], X("rk")[:, :], start=True, stop=True), reads=[c32b, B_("rk")], writes=[pkb])
                DV("scalar_tensor_tensor", [pkb, vb_], "tA", out=X("tA")[:, :], in0=pk[:, :], scalar=64.0, in1=v_, op0=ALU.mult, op1=ALU.mult)
                DV("tensor_tensor", [B_("tA"), B_("tB")], "tB", out=X("tB")[:, :], in0=X("tB")[:, :], in1=X("tA")[:, :], op=ALU.add)
                DV("tensor_tensor", [B_("tB"), B_("g")], "Gi", out=Osb[:, :], in0=X("tB")[:, :], in1=X("g")[:, :], op=ALU.mult)
                C.dma(ya_d[hp * 128:(hp + 1) * 128, tsl], Osb[:, :], R=[Osbb], is_out=True)

            for uc in range(2):
                P.op("act", "activation", PK(out=ubuf[:, uc, :], in_=Z(9 + uc), func=AF.Copy), reads=[zTb[9 + uc]], writes=[ubufb])
            for q in range(8):
                uc = q // 4
                t1, t1b = s5t["t1"]; t2, t2b = s5t["t2"]
                cre_, creb_ = s5t["cre"]; cim_, cimb_ = s5t["cim"]
                zre, zreb = s5t["zre"]; zim, zimb = s5t["zim"]
                pr_, prb_ = C.psum(); pi_, pib_ = C.psum()
                P.op("pe", "matmul", PK(pr_[:, :], breT[:, q, :], ubuf[:, uc, :], start=True, stop=True), reads=[breb, ubufb], writes=[prb_])
                P.op("pe", "matmul", PK(pi_[:, :], bimT[:, q, :], ubuf[:, uc, :], start=True, stop=True), reads=[bimb, ubufb], writes=[pib_])
                fc = Fc[:, q, :]; fs = Fs[:, q, :]
                P.op("dve", "tensor_tensor", PK(out=t1[:, :], in0=pr_[:, :], in1=fc, op=ALU.mult), reads=[prb_, Fcb], writes=[t1b])
                P.op("dve", "tensor_tensor", PK(out=t2[:, :], in0=pi_[:, :], in1=fs, op=ALU.mult), reads=[pib_, Fsb], writes=[t2b])
                P.op("dve", "tensor_tensor", PK(out=cre_[:, :], in0=t1[:, :], in1=t2[:, :], op=ALU.add), reads=[t1b, t2b], writes=[creb_])
                P.op("dve", "tensor_tensor", PK(out=t1[:, :], in0=pi_[:, :], in1=fc, op=ALU.mult), reads=[pib_, Fcb], writes=[t1b])
                P.op("dve", "tensor_tensor", PK(out=t2[:, :], in0=pr_[:, :], in1=fs, op=ALU.mult), reads=[prb_, Fsb], writes=[t2b])
                P.op("dve", "tensor_tensor", PK(out=cim_[:, :], in0=t1[:, :], in1=t2[:, :], op=ALU.subtract), reads=[t1b, t2b], writes=[cimb_])
                P.op("dve", "tensor_tensor_scan", PK(out=zre[:, :], data0=s5c[:, 2, q:q + 1].to_broadcast([128, SEG]), data1=cre_[:, :], initial=xst_re[:, q, 0:1], op0=ALU.mult, op1=ALU.add),
                     reads=[s5cb, creb_, xreb], writes=[zreb])
                P.op("dve", "tensor_tensor_scan", PK(out=zim[:, :], data0=s5c[:, 2, q:q + 1].to_broadcast([128, SEG]), data1=cim_[:, :], initial=xst_im[:, q, 0:1], op0=ALU.mult, op1=ALU.add),
                     reads=[s5cb, cimb_, ximb], writes=[zimb])
                xre_, xreb_ = s5t["xre"]; xim_, ximb_ = s5t["xim"]
                P.op("dve", "tensor_tensor", PK(out=t1[:, :], in0=zre[:, :], in1=fc, op=ALU.mult), reads=[zreb, Fcb], writes=[t1b])
                P.op("dve", "tensor_tensor", PK(out=t2[:, :], in0=zim[:, :], in1=fs, op=ALU.mult), reads=[zimb, Fsb], writes=[t2b])
                P.op("dve", "tensor_tensor", PK(out=xre_[:, :], in0=t1[:, :], in1=t2[:, :], op=ALU.subtract), reads=[t1b, t2b], writes=[xreb_])
                P.op("dve", "tensor_tensor", PK(out=t1[:, :], in0=zim[:, :], in1=fc, op=ALU.mult), reads=[zimb, Fcb], writes=[t1b])
                P.op("dve", "tensor_tensor", PK(out=t2[:, :], in0=zre[:, :], in1=fs, op=ALU.mult), reads=[zreb, Fsb], writes=[t2b])
                P.op("dve", "tensor_tensor", PK(out=xim_[:, :], in0=t1[:, :], in1=t2[:, :], op=ALU.add), reads=[t1b, t2b], writes=[ximb_])
                P.op("dve", "tensor_copy", PK(out=xst_re[:, q, 0:1], in_=xre_[:, SEG - 1:SEG]), reads=[xreb_], writes=[xreb])
                P.op("dve", "tensor_copy", PK(out=xst_im[:, q, 0:1], in_=xim_[:, SEG - 1:SEG]), reads=[ximb_], writes=[ximb])
                P.op("act", "activation", PK(out=xreb16[:, q, :], in_=xre_[:, :], func=AF.Copy), reads=[xreb_], writes=[xreb16b])
                P.op("act", "activation", PK(out=ximb16[:, q, :], in_=xim_[:, :], func=AF.Copy), reads=[ximb_], writes=[ximb16b])
            for yc in range(2):
                py, pyb = C.psum()
                for qq in range(4):
                    q = yc * 4 + qq
                    P.op("pe", "matmul", PK(py[:, :], cpre[:, q, :], xreb16[:, q, :], start=(qq == 0), stop=False), reads=[cpreb, xreb16b], writes=[pyb])
                    P.op("pe", "matmul", PK(py[:, :], cpim[:, q, :], ximb16[:, q, :], start=False, stop=(qq == 3)), reads=[cpimb, ximb16b], writes=[pyb])
                t1, t1b = s5t["t1"]; t2, t2b = s5t["t2"]
                P.op("dve", "scalar_tensor_tensor", PK(out=yo[:, :], in0=Z(9 + yc), scalar=V[:, VA["s5_d"] + yc:VA["s5_d"] + yc + 1], in1=py[:, :], op0=ALU.mult, op1=ALU.add),
                     reads=[zTb[9 + yc], vb, pyb], writes=[yob])
                P.op("act", "activation", PK(out=t1[:, :], in_=yo[:, :], func=AF.Square), reads=[yob], writes=[t1b])
                P.op("dve", "tensor_scalar", PK(out=t1[:, :], in0=t1[:, :], scalar1=0.044715, scalar2=1.0, op0=ALU.mult, op1=ALU.add), reads=[t1b], writes=[t1b])
                P.op("dve", "tensor_tensor", PK(out=t1[:, :], in0=t1[:, :], in1=yo[:, :], op=ALU.mult), reads=[t1b, yob], writes=[t1b])
                P.op("act", "activation", PK(out=t2[:, :], in_=t1[:, :], func=AF.Sigmoid, scale=1.5957691216057308), reads=[t1b], writes=[t2b])
                P.op("dve", "tensor_tensor", PK(out=t2[:, :], in0=t2[:, :], in1=yo[:, :], op=ALU.mult), reads=[t2b, yob], writes=[t2b])
                C.dma(yb_d[yc * 128:(yc + 1) * 128, tsl], t2[:, :], R=[t2b], is_out=True)
        P.emit(st)
    return nc


def pack_A(inp, b, hh):
    W = np.asarray(inp["hy_w_in"][0]); mu = np.asarray(inp["rw_mu"][0])
    hc = slice(hh * 256, (hh + 1) * 256)
    idx = np.concatenate([np.arange(0, 512)[hc], 512 + np.arange(0, 512)[hc], 1024 + np.arange(0, 512)[hc],
                          np.arange(1536, 1824), 1824 + np.arange(0, 512)[hc]])
    wc = np.ascontiguousarray(W[:, idx])
    muc = np.concatenate([mu[idx[:1056]], np.zeros(96, np.float32)])
    Vt = np.zeros((128, NVA), np.float32)

    def put(name, arr):
        a = col(arr); Vt[:, VA[name]:VA[name] + a.shape[1]] = a
    put("n_mix", inp["norm_mix"][0])
    put("mu", muc)
    for nm, key in (("w0", "rw_w0"), ("a0", "rw_a0"), ("k_k", "rw_k_k"), ("k_a", "rw_k_a"), ("ln_w", "rw_ln_w"), ("ln_b", "rw_ln_b")):
        put(nm, np.asarray(inp[key][0])[hc])
    put("r_k", np.asarray(inp["rw_r_k"][0]).reshape(-1)[hc])
    put("s5_d", np.asarray(inp["s5_d"][0])[hc])
    gs = slice(hh * 16, (hh + 1) * 16)
    lre = np.asarray(inp["s5_lam_re"][0])[gs]; lim = np.asarray(inp["s5_lam_im"][0])[gs]; ldt = np.asarray(inp["s5_log_dt"][0])[gs]
    Vt[:, VA["lam_re"]:VA["lam_re"] + 8] = lre.reshape(8, 128).T
    Vt[:, VA["lam_im"]:VA["lam_im"] + 8] = lim.reshape(8, 128).T
    Vt[:, VA["log_dt"]:VA["log_dt"] + 8] = np.repeat(ldt, 64).reshape(8, 128).T
    Vt[:, VA["halfpi"]] = np.float32(np.pi / 2)
    bre = np.asarray(inp["s5_b_re"][0])[gs]; bim = np.asarray(inp["s5_b_im"][0])[gs]
    cre = np.asarray(inp["s5_c_re"][0])[gs]; cim = np.asarray(inp["s5_c_im"][0])[gs]
    breT = np.zeros((8, 128, 128), np.float32); bimT = np.zeros_like(breT); creT = np.zeros_like(breT); cimT = np.zeros_like(breT)
    for q in range(8):
        for e in range(2):
            gl = 2 * q + e
            rows = slice((gl % 8) * 16, (gl % 8) * 16 + 16)
            breT[q, rows, e * 64:(e + 1) * 64] = bre[gl].T
            bimT[q, rows, e * 64:(e + 1) * 64] = bim[gl].T
            creT[q, e * 64:(e + 1) * 64, rows] = cre[gl].T
            cimT[q, e * 64:(e + 1) * 64, rows] = cim[gl].T
    aup = np.zeros((128, 256), np.float32); aup[64:] = np.asarray(inp["rw_a_up"][0])[:, hc]
    gup = np.asarray(inp["rw_g_up"][0])[:, hc]
    c32 = np.zeros((128, 3, 128), np.float32)
    c32[:, 0, :] = np.eye(128, dtype=np.float32)
    c32[0:64, 1, 0:64] = 1.0 / 64; c32[64:, 1, 64:] = 1.0 / 64
    c32[:, 2, :] = 1.0
    s = np.arange(64)[:, None]; t = np.arange(64)[None, :]
    su = (s < t).astype(np.float32); ui = (s <= t).astype(np.float32)
    mask5 = np.concatenate([su, ui, su, ui, su.T], 1)
    rmask = np.ones((128, SEG), np.float32); rmask[:, ::64] = 0.0
    return dict(xT=np.ascontiguousarray(np.asarray(inp["x"][b]).T), wc=wc, vecs=Vt, cst=cst_table(), c32=c32.reshape(128, 384), mask5=mask5, rmask=rmask,
                w_up=np.ascontiguousarray(np.asarray(inp["rw_w_up"][0])[:, hc]), a_up=aup, g_upa=np.ascontiguousarray(gup[:128]),
                g_upb=np.ascontiguousarray(gup[128:]), breT=breT, bimT=bimT, creT=creT, cimT=cimT)


class ACtx(Ctx):
    def __init__(self, nc, st, P, words):
        self.nc, self.st, self.P = nc, st, P
        self.arena = st.enter_context(nc.sbuf_tensor("arena", [128, words], F32))
        self.words = words
        self.off = 0
        self.ps = []
        for i in range(8):
            t = st.enter_context(nc.psum_tensor("ps%d" % i, [128, 512], F32))
            self.ps.append((t, Buf("ps%d" % i, excl=True)))
        self.psi = 0
        self.wbufs = []
        self.wi = 0
        self.dmaq = 0
        self.vb = None
        self.nphase = 0
        self.epsc, self.epsb = self.sb("epsc", [128, 2], F32)
        self.dummy, _ = self.sb("dummy", [128, 2], F32)
        P.op("dve", "memset", PK(self.epsc[:, 0:1], EPS), writes=[self.epsb])
        P.op("dve", "memset", PK(self.epsc[:, 1:2], 64e-5), writes=[self.epsb])
        self.mark = self.off
        self.psum_default = self.psum

    def sb(self, name, shape, dt):
        n = 1
        for s_ in shape[1:]:
            n *= s_
        nbytes = n * (2 if dt == BF16 else 4)
        nw = (nbytes + 31) // 32 * 8
        assert self.off + nw <= self.words, ("arena overflow", name, self.off, nw, self.words)
        v = self.arena[0:shape[0], self.off:self.off + nw]
        self.off += nw
        if dt == BF16:
            v = v.bitcast(BF16)
        v = v[:, 0:n]
        if len(shape) == 3:
            v = v.rearrange("p (a b) -> p a b", a=shape[1])
        elif len(shape) == 4:
            v = v.rearrange("p (a b c) -> p a b c", a=shape[1], b=shape[2])
        return v, Buf("%s_%d" % (name, self.nphase))

    def new_phase(self, keep=None):
        self.P.fence(self.dummy[:, 0:1])
        self.off = self.mark if keep is None else keep
        self.wbufs = []
        self.wi = 0
        self.nphase += 1
        self.vb = None

    def init_w(self, n, cols):
        self.wbufs = []
        self.wi = 0
        for i in range(n):
            self.wbufs.append(self.sb("wb%d" % i, [128, cols], BF16))


def body_A(C, d, hh, ymb, nseg=NSEG_FULL):
    P = C.P
    if True:
        xT_d = d["xT"]; wc_d = d["wc%d" % hh]; vec_d = d["vecsA%d" % hh]
        cst_d = d["cst"]; c32_d = d["c32A"]; msk_d = d["mask5"]; rmask_d = d["rmask"]
        wup_d = d["w_up%d" % hh]; aup_d = d["a_up%d" % hh]; gupa_d = d["g_upa%d" % hh]; gupb_d = d["g_upb%d" % hh]
        bre_d = d["breT%d" % hh]; bim_d = d["bimT%d" % hh]; cre_d = d["creT%d" % hh]; cim_d = d["cimT%d" % hh]
        ya_d = d["ymT"][hh * 256:(hh + 1) * 256, :]; yb_d = d["ymT"][512 + hh * 256:512 + (hh + 1) * 256, :]
        rot = [0, 1, 2, 3, 4, 7]
        rs_ = [0]

        def psum():
            r = C.ps[rot[rs_[0] % len(rot)]]
            rs_[0] += 1
            return r
        C.psum = psum
        T = lambda name, shape, dt=F32: C.sb(name, shape, dt)
        V, vb = T("V_sb", [128, NVA]); C.vb = vb
        C.dma(V[:, :], vec_d[:, :], W=[vb])
        cst3, cstb = T("cst_sb", [128, 5, 128], BF16)
        P.op("pool", "dma_start", PK(out=cst3[:, :, :], in_=cst_d.rearrange("p (a b) -> p a b", a=5)), writes=[cstb], is_dma=True)
        c32, c32b = T("c32_sb", [128, 3, 128]); C.dma(c32[:, :, :], c32_d.rearrange("p (a b) -> p a b", a=3), W=[c32b])
        msk, mskb = T("msk_sb", [64, 320]); C.dma(msk[:, :], msk_d[:, :], W=[mskb])
        rmask, rmaskb = T("rmask_sb", [128, SEG]); C.dma(rmask[:, :], rmask_d[:, :], W=[rmaskb])
        wup, wupb = T("wup_sb", [64, 256]); C.dma(wup[:, :], wup_d[:, :], W=[wupb])
        aup, aupb = T("aup_sb", [128, 256]); C.dma(aup[:, :], aup_d[:, :], W=[aupb])
        gupa, gupab = T("gupa_sb", [128, 256]); C.dma(gupa[:, :], gupa_d[:, :], W=[gupab])
        gupb, gupbb = T("gupb_sb", [32, 256]); C.dma(gupb[:, :], gupb_d[:, :], W=[gupbb])
        wcL = [T("wc_sb%d" % i, [128, 8, 128], BF16) for i in range(2)]
        wci = [0]
        scr = dict(sq=T("sq", [128, 8, 512], BF16), rs=T("rs", [128, 512]))
        ident = c32[:, C_ID, :]
        names = "ld a g al be km cum Gi tA tB Rb Kb Ab Bb Kh Bh rk".split()
        W_ = {n: T("w_" + n, [128, SEG]) for n in names}

        breT, breb = T("breT_sb", [128, 8, 128], BF16); bimT, bimb = T("bimT_sb", [128, 8, 128], BF16)
        P.op("pool", "dma_start", PK(out=breT[:, :, :], in_=bre_d.rearrange("q k m -> k q m")), writes=[breb], is_dma=True)
        P.op("pool", "dma_start", PK(out=bimT[:, :, :], in_=bim_d.rearrange("q k m -> k q m")), writes=[bimb], is_dma=True)
        creT, creb = T("creT_sb", [128, 8, 128]); cimT, cimb = T("cimT_sb", [128, 8, 128])
        C.dma(creT[:, :, :], cre_d.rearrange("q k m -> k q m"), W=[creb])
        C.dma(cimT[:, :, :], cim_d.rearrange("q k m -> k q m"), W=[cimb])
        s5c, s5cb = T("s5c", [128, 12, 8])
        lre = V[:, VA["lam_re"]:VA["lam_re"] + 8]; lim = V[:, VA["lam_im"]:VA["lam_im"] + 8]
        S = lambda i: s5c[:, i, :]
        dv = lambda name, **kw: P.op("dve", name, PK(**kw), reads=[s5cb, vb], writes=[s5cb])
        P.op("act", "activation", PK(out=S(0), in_=V[:, VA["log_dt"]:VA["log_dt"] + 8], func=AF.Exp), reads=[vb], writes=[s5cb])
        dv("tensor_tensor", out=S(1), in0=lim, in1=S(0), op=ALU.mult)
        dv("tensor_tensor", out=S(9), in0=lre, in1=S(0), op=ALU.mult)
        P.op("act", "activation", PK(out=S(2), in_=S(9), func=AF.Exp), reads=[s5cb], writes=[s5cb])
        P.op("act", "activation", PK(out=S(4), in_=S(1), func=AF.Sin, scale=0.125), reads=[s5cb], writes=[s5cb])
        P.op("act", "activation", PK(out=S(3), in_=S(1), func=AF.Sin, scale=-0.125, bias=V[:, VA["halfpi"]:VA["halfpi"] + 1]), reads=[s5cb, vb], writes=[s5cb])
        for _ in range(3):
            dv("tensor_tensor", out=S(9), in0=S(3), in1=S(3), op=ALU.mult)
            dv("tensor_tensor", out=S(10), in0=S(4), in1=S(4), op=ALU.mult)
            dv("tensor_tensor", out=S(11), in0=S(3), in1=S(4), op=ALU.mult)
            dv("tensor_tensor", out=S(3), in0=S(9), in1=S(10), op=ALU.subtract)
            dv("tensor_scalar", out=S(4), in0=S(11), scalar1=2.0, scalar2=None, op0=ALU.mult)
        dv("tensor_tensor", out=S(5), in0=S(2), in1=S(3), op=ALU.mult)
        dv("tensor_tensor", out=S(6), in0=S(2), in1=S(4), op=ALU.mult)
        dv("tensor_scalar_add", out=S(5), in0=S(5), scalar1=-1.0)
        dv("tensor_tensor", out=S(9), in0=lre, in1=lre, op=ALU.mult)
        dv("tensor_tensor", out=S(10), in0=lim, in1=lim, op=ALU.mult)
        dv("tensor_tensor", out=S(9), in0=S(9), in1=S(10), op=ALU.add)
        dv("reciprocal", out=S(9), in_=S(9))
        dv("tensor_tensor", out=S(10), in0=S(5), in1=lre, op=ALU.mult)
        dv("tensor_tensor", out=S(11), in0=S(6), in1=lim, op=ALU.mult)
        dv("tensor_tensor", out=S(10), in0=S(10), in1=S(11), op=ALU.add)
        dv("tensor_tensor", out=S(7), in0=S(10), in1=S(9), op=ALU.mult)
        dv("tensor_tensor", out=S(10), in0=S(6), in1=lre, op=ALU.mult)
        dv("tensor_tensor", out=S(11), in0=S(5), in1=lim, op=ALU.mult)
        dv("tensor_tensor", out=S(10), in0=S(10), in1=S(11), op=ALU.subtract)
        dv("tensor_tensor", out=S(8), in0=S(10), in1=S(9), op=ALU.mult)
        cpre, cpreb = T("cpre", [128, 8, 128], BF16); cpim, cpimb = T("cpim", [128, 8, 128], BF16)
        ctmp = W_["tA"][0][:, 0:128]; ctmpb = W_["tA"][1]
        for q in range(8):
            cr = s5c[:, 7, q:q + 1]; ci = s5c[:, 8, q:q + 1]
            P.op("dve", "tensor_scalar", PK(out=ctmp[:, :], in0=cimT[:, q, :], scalar1=ci, scalar2=None, op0=ALU.mult), reads=[cimb, s5cb], writes=[ctmpb])
            P.op("dve", "scalar_tensor_tensor", PK(out=cpre[:, q, :], in0=creT[:, q, :], scalar=cr, in1=ctmp[:, :], op0=ALU.mult, op1=ALU.subtract),
                 reads=[creb, ctmpb, s5cb], writes=[cpreb])
            P.op("dve", "tensor_scalar", PK(out=ctmp[:, :], in0=cimT[:, q, :], scalar1=cr, scalar2=-1.0, op0=ALU.mult, op1=ALU.mult), reads=[cimb, s5cb], writes=[ctmpb])
            P.op("dve", "scalar_tensor_tensor", PK(out=ctmp[:, :], in0=creT[:, q, :], scalar=ci, in1=ctmp[:, :], op0=ALU.mult, op1=ALU.subtract),
                 reads=[creb, ctmpb, s5cb], writes=[ctmpb])
            P.op("dve", "tensor_scalar", PK(out=cpim[:, q, :], in0=ctmp[:, :], scalar1=-1.0, scalar2=None, op0=ALU.mult), reads=[ctmpb], writes=[cpimb])
        Fc, Fcb = T("Fc", [128, 8, SEG]); Fs, Fsb = T("Fs", [128, 8, SEG])
        ncol, ncolb = T("ncol", [128, 2])
        ftmp = W_["tB"][0][:, 0:SEG // 2]; ftmpb = W_["tB"][1]
        for q in range(8):
            P.op("dve", "tensor_copy", PK(out=Fc[:, q, 0:1], in_=s5c[:, 3, q:q + 1]), reads=[s5cb], writes=[Fcb])
            P.op("dve", "tensor_copy", PK(out=Fs[:, q, 0:1], in_=s5c[:, 4, q:q + 1]), reads=[s5cb], writes=[Fsb])
            n = 1
            while n < SEG:
                cn = Fc[:, q, n - 1:n]; sn = Fs[:, q, n - 1:n]
                P.op("dve", "tensor_scalar", PK(out=ftmp[:, 0:n], in0=Fs[:, q, 0:n], scalar1=sn, scalar2=None, op0=ALU.mult), reads=[Fsb], writes=[ftmpb])
                P.op("dve", "scalar_tensor_tensor", PK(out=Fc[:, q, n:2 * n], in0=Fc[:, q, 0:n], scalar=cn, in1=ftmp[:, 0:n], op0=ALU.mult, op1=ALU.subtract),
                     reads=[Fcb, ftmpb], writes=[Fcb])
                P.op("dve", "tensor_scalar", PK(out=ftmp[:, 0:n], in0=Fc[:, q, 0:n], scalar1=sn, scalar2=None, op0=ALU.mult), reads=[Fcb, Fsb], writes=[ftmpb])
                P.op("dve", "scalar_tensor_tensor", PK(out=Fs[:, q, n:2 * n], in0=Fs[:, q, 0:n], scalar=cn, in1=ftmp[:, 0:n], op0=ALU.mult, op1=ALU.add),
                     reads=[Fsb, Fcb, ftmpb], writes=[Fsb])
                n *= 2
        xst_re, xreb = T("xst_re", [128, 8, 2]); xst_im, ximb = T("xst_im", [128, 8, 2])
        P.op("dve", "memset", PK(xst_re[:, :, :], 0.0), writes=[xreb]); P.op("dve", "memset", PK(xst_im[:, :, :], 0.0), writes=[ximb])

        zT, _ = T("zT", [128, 11, SEG + 1]); zTb = [Buf("zT%d" % c) for c in range(11)]
        for c in range(11):
            P.op("dve", "memset", PK(zT[:, c, 0:1], 0.0), writes=[zTb[c]])
        Sst = [[T("S%d_%d" % (hp, i), [128, 64]) for i in range(2)] for hp in range(2)]
        for hp in range(2):
            P.op("dve", "memset", PK(Sst[hp][0][0][:, :], 0.0), writes=[Sst[hp][0][1]])
        sidx = [0, 0]
        xT_t, xTb = T("xT_sb", [128, 8, SEG], BF16); hn, hnb = T("hn", [128, 8, SEG], BF16)
        tw = xT_t[:, 0:2, :].rearrange("p a b -> p (a b)").bitcast(F32); sg0 = xT_t[:, 2:4, :].rearrange("p a b -> p (a b)").bitcast(F32)
        sg1 = xT_t[:, 4:6, :].rearrange("p a b -> p (a b)").bitcast(F32); twb = sg0b = sg1b = xTb
        gC, gCb = T("gC", [128, 8])
        NG = 4
        NU = 2 * NG
        AAsL = [T("AAs%d" % u, [64, 320]) for u in range(NU)]
        TML = [T("TM%d" % g, [64, 4, 128]) for g in range(NG)]
        AsqL = [[T("Asq%d_%d" % (u, i), [64, 128]) for i in range(2)] for u in range(NU)]
        ZtL = [[T("Zt%d_%d" % (u, i), [64, 64]) for i in range(2)] for u in range(NU)]
        WsbL = [T("Wsb%d" % u, [64, 64]) for u in range(NU)]
        Ut0L = [T("Ut0%d" % u, [64, 64]) for u in range(NU)]
        UtL = [T("Ut%d" % u, [64, 64]) for u in range(NU)]
        PhiL = [T("Phi%d" % g, [128, 64]) for g in range(NG)]
        ubuf, ubufb = T("ubuf", [128, 2, SEG], BF16)
        s5t = {n: T("s5_" + n, [128, SEG]) for n in "t1 t2 cre cim zre zim xre xim".split()}
        xreb16, _ = T("xre16", [128, 2, SEG], BF16); ximb16, _ = T("xim16", [128, 2, SEG], BF16)
        yacc, yaccb = T("yacc", [128, SEG])
        xre16bL = [Buf("xre16_0"), Buf("xre16_1")]; xim16bL = [Buf("xim16_0"), Buf("xim16_1")]
        yo, yob = s5t["cre"]

        for seg in range(nseg):
            tsl = slice(seg * SEG, (seg + 1) * SEG)
            P.op("pool", "dma_start", PK(out=xT_t[:, :, :], in_=xT_d[:, tsl].rearrange("(c p) n -> p c n", p=128)), writes=[xTb], is_dma=True)
            norm_fm(C, lambda c: xT_t[:, c, :], [xTb], SEG, 8, cst3[:, 0, :], cstb, lambda c: V[:, VA["n_mix"] + c:VA["n_mix"] + c + 1],
                    lambda c: hn[:, c, :], [hnb], scr)
            for c, (c0, wdt) in enumerate(ZCH):
                wc, wcb = wcL[wci[0] % 2]; wci[0] += 1
                P.op("pool", "dma_start", PK(out=wc[:, :, 0:wdt], in_=wc_d[:, c0:c0 + wdt].rearrange("(k p) n -> p k n", p=128)), writes=[wcb], is_dma=True)
                pt, pb = C.psum()
                for k in range(8):
                    P.op("pe", "matmul", PK(pt[0:wdt, :], wc[:, k, 0:wdt], hn[:, k, :], start=(k == 0), stop=(k == 7)), reads=[wcb, hnb], writes=[pb])
                P.op("act", "activation", PK(out=zT[0:wdt, c, 1:SEG + 1], in_=pt[0:wdt, :], func=AF.Copy), reads=[pb], writes=[zTb[c]])
            tA, tAb = W_["tA"]
            for c in range(9):
                wdt = ZCH[c][1]
                P.op("dve", "tensor_tensor", PK(out=tA[0:wdt, :], in0=zT[0:wdt, c, 0:SEG], in1=zT[0:wdt, c, 1:SEG + 1], op=ALU.subtract), reads=[zTb[c]], writes=[tAb])
                P.op("dve", "tensor_copy", PK(out=zT[0:wdt, c, 0:1], in_=zT[0:wdt, c, SEG:SEG + 1]), reads=[tAb], writes=[zTb[c]])
                P.op("dve", "scalar_tensor_tensor", PK(out=zT[0:wdt, c, 1:SEG + 1], in0=tA[0:wdt, :], scalar=V[0:wdt, VA["mu"] + c:VA["mu"] + c + 1],
                                                      in1=zT[0:wdt, c, 1:SEG + 1], op0=ALU.mult, op1=ALU.add), reads=[tAb, zTb[c], vb], writes=[zTb[c]])
            Z = lambda c, lo=0, hi=128: zT[lo:hi, c, 1:SEG + 1]
            P.op("act", "activation", PK(out=tw[0:64, :], in_=Z(6, 0, 64), func=AF.Tanh), reads=[zTb[6]], writes=[twb])
            P.op("act", "activation", PK(out=sg0[:, :], in_=Z(7), func=AF.Sigmoid), reads=[zTb[7]], writes=[sg0b])
            P.op("act", "activation", PK(out=sg1[0:32, :], in_=Z(8, 0, 32), func=AF.Sigmoid), reads=[zTb[8]], writes=[sg1b])
            for uc in range(2):
                P.op("act", "activation", PK(out=ubuf[:, uc, :], in_=Z(9 + uc), func=AF.Copy), reads=[zTb[9 + uc]], writes=[ubufb])
            def s5_pair(q):
                    uc = q // 4
                    t1, t1b = s5t["t1"]; t2, t2b = s5t["t2"]
                    cre_, creb_ = s5t["cre"]; cim_, cimb_ = s5t["cim"]
                    zre, zreb = s5t["zre"]; zim, zimb = s5t["zim"]
                    pr_, prb_ = C.psum(); pi_, pib_ = C.psum()
                    P.op("pe", "matmul", PK(pr_[:, :], breT[:, q, :], ubuf[:, uc, :], start=True, stop=True), reads=[breb, ubufb], writes=[prb_])
                    P.op("pe", "matmul", PK(pi_[:, :], bimT[:, q, :], ubuf[:, uc, :], start=True, stop=True), reads=[bimb, ubufb], writes=[pib_])
                    fc = Fc[:, q, :]; fs = Fs[:, q, :]
                    P.op("dve", "tensor_tensor", PK(out=t1[:, :], in0=pr_[:, :], in1=fc, op=ALU.mult), reads=[prb_, Fcb], writes=[t1b])
                    P.op("dve", "tensor_tensor", PK(out=t2[:, :], in0=pi_[:, :], in1=fs, op=ALU.mult), reads=[pib_, Fsb], writes=[t2b])
                    P.op("dve", "tensor_tensor", PK(out=cre_[:, :], in0=t1[:, :], in1=t2[:, :], op=ALU.add), reads=[t1b, t2b], writes=[creb_])
                    P.op("dve", "tensor_tensor", PK(out=t1[:, :], in0=pi_[:, :], in1=fc, op=ALU.mult), reads=[pib_, Fcb], writes=[t1b])
                    P.op("dve", "tensor_tensor", PK(out=t2[:, :], in0=pr_[:, :], in1=fs, op=ALU.mult), reads=[prb_, Fsb], writes=[t2b])
                    P.op("dve", "tensor_tensor", PK(out=cim_[:, :], in0=t1[:, :], in1=t2[:, :], op=ALU.subtract), reads=[t1b, t2b], writes=[cimb_])
                    P.op("dve", "tensor_tensor_scan", PK(out=zre[:, :], data0=s5c[:, 2, q:q + 1].to_broadcast([128, SEG]), data1=cre_[:, :], initial=xst_re[:, q, 0:1], op0=ALU.mult, op1=ALU.add),
                         reads=[s5cb, creb_, xreb], writes=[zreb])
                    P.op("dve", "tensor_tensor_scan", PK(out=zim[:, :], data0=s5c[:, 2, q:q + 1].to_broadcast([128, SEG]), data1=cim_[:, :], initial=xst_im[:, q, 0:1], op0=ALU.mult, op1=ALU.add),
                         reads=[s5cb, cimb_, ximb], writes=[zimb])
                    xre_, xreb_ = s5t["xre"]; xim_, ximb_ = s5t["xim"]
                    P.op("dve", "tensor_tensor", PK(out=t1[:, :], in0=zre[:, :], in1=fc, op=ALU.mult), reads=[zreb, Fcb], writes=[t1b])
                    P.op("dve", "tensor_tensor", PK(out=t2[:, :], in0=zim[:, :], in1=fs, op=ALU.mult), reads=[zimb, Fsb], writes=[t2b])
                    P.op("dve", "tensor_tensor", PK(out=xre_[:, :], in0=t1[:, :], in1=t2[:, :], op=ALU.subtract), reads=[t1b, t2b], writes=[xreb_])
                    P.op("dve", "tensor_tensor", PK(out=t1[:, :], in0=zim[:, :], in1=fc, op=ALU.mult), reads=[zimb, Fcb], writes=[t1b])
                    P.op("dve", "tensor_tensor", PK(out=t2[:, :], in0=zre[:, :], in1=fs, op=ALU.mult), reads=[zreb, Fsb], writes=[t2b])
                    P.op("dve", "tensor_tensor", PK(out=xim_[:, :], in0=t1[:, :], in1=t2[:, :], op=ALU.add), reads=[t1b, t2b], writes=[ximb_])
                    P.op("dve", "tensor_copy", PK(out=xst_re[:, q, 0:1], in_=xre_[:, SEG - 1:SEG]), reads=[xreb_], writes=[xreb])
                    P.op("dve", "tensor_copy", PK(out=xst_im[:, q, 0:1], in_=xim_[:, SEG - 1:SEG]), reads=[ximb_], writes=[ximb])
                    xb_ = q % 2
                    P.op("act", "activation", PK(out=xreb16[:, xb_, :], in_=xre_[:, :], func=AF.Copy), reads=[xreb_], writes=[xre16bL[xb_]])
                    P.op("act", "activation", PK(out=ximb16[:, xb_, :], in_=xim_[:, :], func=AF.Copy), reads=[ximb_], writes=[xim16bL[xb_]])
                    py, pyb = C.psum()
                    qq = q % 4
                    P.op("pe", "matmul", PK(py[:, :], cpre[:, q, :], xreb16[:, xb_, :], start=True, stop=False), reads=[cpreb, xre16bL[xb_]], writes=[pyb])
                    P.op("pe", "matmul", PK(py[:, :], cpim[:, q, :], ximb16[:, xb_, :], start=False, stop=True), reads=[cpimb, xim16bL[xb_]], writes=[pyb])
                    if qq == 0:
                        P.op("act", "activation", PK(out=yacc[:, :], in_=py[:, :], func=AF.Copy), reads=[pyb], writes=[yaccb])
                    else:
                        P.op("dve", "tensor_tensor", PK(out=yacc[:, :], in0=py[:, :], in1=yacc[:, :], op=ALU.add), reads=[pyb, yaccb], writes=[yaccb])
                    if qq != 3:
                        return
                    yc = q // 4
                    t1, t1b = s5t["t1"]; t2, t2b = s5t["t2"]
                    P.op("dve", "scalar_tensor_tensor", PK(out=yo[:, :], in0=Z(9 + yc), scalar=V[:, VA["s5_d"] + yc:VA["s5_d"] + yc + 1], in1=yacc[:, :], op0=ALU.mult, op1=ALU.add),
                         reads=[zTb[9 + yc], vb, yaccb], writes=[yob])
                    P.op("act", "activation", PK(out=t1[:, :], in_=yo[:, :], func=AF.Square), reads=[yob], writes=[t1b])
                    P.op("dve", "tensor_scalar", PK(out=t1[:, :], in0=t1[:, :], scalar1=0.044715, scalar2=1.0, op0=ALU.mult, op1=ALU.add), reads=[t1b], writes=[t1b])
                    P.op("dve", "tensor_tensor", PK(out=t1[:, :], in0=t1[:, :], in1=yo[:, :], op=ALU.mult), reads=[t1b, yob], writes=[t1b])
                    P.op("act", "activation", PK(out=t2[:, :], in_=t1[:, :], func=AF.Sigmoid, scale=1.5957691216057308), reads=[t1b], writes=[t2b])
                    P.op("dve", "tensor_tensor", PK(out=t2[:, :], in0=t2[:, :], in1=yo[:, :], op=ALU.mult), reads=[t2b, yob], writes=[t2b])
                    C.dma(yb_d[yc * 128:(yc + 1) * 128, tsl], t2[:, :], R=[t2b])
            s5_next = [0]
            for hp in range(2):
                cols = slice(hp * 128, (hp + 1) * 128)
                vcol = lambda nm: V[:, VA[nm] + hp:VA[nm] + hp + 1]
                r_ = Z(hp); k_ = Z(2 + hp); v_ = Z(4 + hp)
                rb_, kb_, vb_ = zTb[hp], zTb[2 + hp], zTb[4 + hp]
                X = lambda n: W_[n][0]
                B_ = lambda n: W_[n][1]
                DV = lambda name, R, Wn, **kw: P.op("dve", name, PK(**kw), reads=R, writes=[B_(Wn)])
                pt, pb = C.psum()
                P.op("pe", "matmul", PK(pt[:, :], wup[:, cols], tw[0:64, :], start=True, stop=True), reads=[wupb, twb], writes=[pb])
                P.op("act", "activation", PK(out=X("ld")[:, :], in_=pt[:, :], func=AF.Sigmoid, bias=vcol("w0")), reads=[pb, vb], writes=[B_("ld")])
                DV("tensor_scalar", [B_("ld")], "ld", out=X("ld")[:, :], in0=X("ld")[:, :], scalar1=NEG_E05, scalar2=None, op0=ALU.mult)
                pt, pb = C.psum()
                P.op("pe", "matmul", PK(pt[:, :], aup[64:128, cols], Z(6, 64, 128), start=True, stop=True), reads=[aupb, zTb[6]], writes=[pb])
                P.op("act", "activation", PK(out=X("a")[:, :], in_=pt[:, :], func=AF.Sigmoid, bias=vcol("a0")), reads=[pb, vb], writes=[B_("a")])
                pt, pb = C.psum()
                P.op("pe", "matmul", PK(pt[:, :], gupa[:, cols], sg0[:, :], start=True, stop=False), reads=[gupab, sg0b], writes=[pb])
                P.op("pe", "matmul", PK(pt[:, :], gupb[:, cols], sg1[0:32, :], start=False, stop=True), reads=[gupbb, sg1b], writes=[pb])
                P.op("act", "activation", PK(out=X("g")[:, :], in_=pt[:, :], func=AF.Copy), reads=[pb], writes=[B_("g")])
                DV("tensor_scalar", [kb_, vb], "tA", out=X("tA")[:, :], in0=k_, scalar1=vcol("k_k"), scalar2=None, op0=ALU.mult)
                P.op("act", "activation", PK(out=X("tB")[:, :], in_=X("tA")[:, :], func=AF.Square), reads=[B_("tA")], writes=[B_("tB")])
                pt, pb = C.psum()
                P.op("pe", "matmul", PK(pt[:, :], c32[:, C_B64, :], X("tB")[:, :], start=True, stop=True), reads=[c32b, B_("tB")], writes=[pb])
                rs, rsb = scr["rs"]
                rstd_from(C, rs[:, :], pt[:, :], pb, rsb)
                DV("scalar_tensor_tensor", [B_("tA"), rsb], "al", out=X("al")[:, :], in0=X("tA")[:, :], scalar=0.125, in1=rs[:, :], op0=ALU.mult, op1=ALU.mult)
                DV("scalar_tensor_tensor", [B_("al"), B_("a")], "be", out=X("be")[:, :], in0=X("al")[:, :], scalar=-1.0, in1=X("a")[:, :], op0=ALU.mult, op1=ALU.mult)
                DV("tensor_scalar", [B_("a"), vb], "tA", out=X("tA")[:, :], in0=X("a")[:, :], scalar1=-1.0, scalar2=vcol("k_a"), op0=ALU.add, op1=ALU.mult)
                DV("scalar_tensor_tensor", [B_("tA"), kb_], "km", out=X("km")[:, :], in0=X("tA")[:, :], scalar=1.0, in1=k_, op0=ALU.add, op1=ALU.mult)
                DV("scalar_tensor_tensor", [rb_, B_("km"), vb], "rk", out=X("rk")[:, :], in0=r_, scalar=vcol("r_k"), in1=X("km")[:, :], op0=ALU.mult, op1=ALU.mult)
                DV("tensor_tensor_scan", [rmaskb, B_("ld")], "cum", out=X("cum")[:, :], data0=rmask[:, :], data1=X("ld")[:, :], initial=0.0, op0=ALU.mult, op1=ALU.add)
                cum3 = X("cum")[:, :].rearrange("p (c t) -> p c t", t=64)
                P.op("act", "activation", PK(out=X("Gi")[:, :], in_=X("cum")[:, :], func=AF.Exp), reads=[B_("cum")], writes=[B_("Gi")])
                DV("tensor_tensor", [B_("Gi"), rb_], "Rb", out=X("Rb")[:, :], in0=r_, in1=X("Gi")[:, :], op=ALU.mult)
                DV("tensor_tensor", [B_("cum"), B_("ld")], "tA", out=X("tA")[:, :], in0=X("cum")[:, :], in1=X("ld")[:, :], op=ALU.subtract)
                P.op("act", "activation", PK(out=X("Gi")[:, :], in_=X("tA")[:, :], func=AF.Exp), reads=[B_("tA")], writes=[B_("Gi")])
                DV("tensor_tensor", [B_("Gi"), B_("al")], "Ab", out=X("Ab")[:, :], in0=X("al")[:, :], in1=X("Gi")[:, :], op=ALU.mult)
                P.op("act", "activation", PK(out=X("Gi")[:, :], in_=X("cum")[:, :], func=AF.Exp, scale=-1.0), reads=[B_("cum")], writes=[B_("Gi")])
                DV("tensor_tensor", [B_("Gi"), B_("km")], "Kb", out=X("Kb")[:, :], in0=X("km")[:, :], in1=X("Gi")[:, :], op=ALU.mult)
                DV("tensor_tensor", [B_("Gi"), B_("be")], "Bb", out=X("Bb")[:, :], in0=X("be")[:, :], in1=X("Gi")[:, :], op=ALU.mult)
                tA3 = X("tA")[:, :].rearrange("p (c t) -> p c t", t=64)
                DV("tensor_tensor", [B_("cum")], "tA", out=tA3, in0=cum3[:, :, 63:64].to_broadcast([128, 8, 64]), in1=cum3, op=ALU.subtract)
                P.op("act", "activation", PK(out=X("Gi")[:, :], in_=X("tA")[:, :], func=AF.Exp), reads=[B_("tA")], writes=[B_("Gi")])
                DV("tensor_tensor", [B_("Gi"), B_("km")], "Kh", out=X("Kh")[:, :], in0=X("km")[:, :], in1=X("Gi")[:, :], op=ALU.mult)
                DV("tensor_tensor", [B_("Gi"), B_("be")], "Bh", out=X("Bh")[:, :], in0=X("be")[:, :], in1=X("Gi")[:, :], op=ALU.mult)
                P.op("act", "activation", PK(out=gC[:, :], in_=cum3[:, :, 63], func=AF.Exp), reads=[B_("cum")], writes=[gCb])
                poL = [C.ps[6], C.ps[5]]
                vsrcs = ((v_, vb_), (X("Ab")[:, :], B_("Ab")), (X("Bh")[:, :], B_("Bh")), (X("Kh")[:, :], B_("Kh")))
                for g0 in range(0, SEG // 64, NG):
                    units = [(gi, e) for gi in range(NG) for e in range(2)]
                    CS = [slice((g0 + gi) * 64, (g0 + gi + 1) * 64) for gi in range(NG)]
                    PR = [slice(0, 64), slice(64, 128)]
                    for gi in range(NG):
                        pt, pb = C.psum()
                        for i, (src, sb_) in enumerate(vsrcs):
                            P.op("pe", "transpose", PK(pt[0:64, i * 128:(i + 1) * 128], src[:, CS[gi]], ident), reads=[sb_, c32b], writes=[pb])
                        P.op("act", "activation", PK(out=TML[gi][0][:, :, :], in_=pt[0:64, :].rearrange("p (i k) -> p i k", i=4), func=AF.Copy), reads=[pb], writes=[TML[gi][1]])
                    SB = 4
                    UB = [range(b0, min(NU, b0 + SB)) for b0 in range(0, NU, SB)]
                    for ub in UB:
                        pts = {}
                        for u in ub:
                            gi, e = units[u]
                            pr, cs = PR[e], CS[gi]
                            pt, pb = C.psum()
                            Bb_, Kb_, Ab_, Rb_ = X("Bb")[pr, cs], X("Kb")[pr, cs], X("Ab")[pr, cs], X("Rb")[pr, cs]
                            P.op("pe", "matmul", PK(pt[0:64, 0:64], Bb_, Ab_, start=True, stop=True), reads=[B_("Bb"), B_("Ab")], writes=[pb])
                            P.op("pe", "matmul", PK(pt[0:64, 64:128], Bb_, Rb_, start=True, stop=True), reads=[B_("Bb"), B_("Rb")], writes=[pb])
                            P.op("pe", "matmul", PK(pt[0:64, 128:192], Kb_, Ab_, start=True, stop=True), reads=[B_("Kb"), B_("Ab")], writes=[pb])
                            P.op("pe", "matmul", PK(pt[0:64, 192:256], Kb_, Rb_, start=True, stop=True), reads=[B_("Kb"), B_("Rb")], writes=[pb])
                            P.op("pe", "matmul", PK(pt[0:64, 256:320], Ab_, Bb_, start=True, stop=True), reads=[B_("Bb"), B_("Ab")], writes=[pb])
                            pts[u] = (pt, pb)
                        for u in ub:
                            pt, pb = pts[u]
                            P.op("dve", "tensor_tensor", PK(out=AAsL[u][0][:, :], in0=pt[0:64, 0:320], in1=msk[:, :], op=ALU.mult), reads=[pb, mskb], writes=[AAsL[u][1]])
                    for u in range(NU):
                        AAs, AAsb = AAsL[u]
                        P.op("dve", "tensor_tensor", PK(out=ZtL[u][0][0][:, :], in0=AAs[:, 0:64], in1=ident[0:64, 0:64], op=ALU.add), reads=[AAsb, c32b], writes=[ZtL[u][0][1]])
                    cur = [(AAsL[u][0][:, 0:64], AAsL[u][0][:, 256:320], AAsL[u][1]) for u in range(NU)]
                    zi = 0
                    for lvl in range(1, 6):
                        lo = 0 if lvl < 5 else 64
                        for ub in UB:
                            pts = {}
                            for u in ub:
                                curA, curAT, curb = cur[u]
                                psq, psqb = C.psum()
                                if lvl < 5:
                                    P.op("pe", "matmul", PK(psq[0:64, 0:64], curAT, curA, start=True, stop=True), reads=[curb], writes=[psqb])
                                P.op("pe", "matmul", PK(psq[0:64, 64:128], curA, curAT, start=True, stop=True), reads=[curb], writes=[psqb])
                                pts[u] = (psq, psqb)
                            for u in ub:
                                psq, psqb = pts[u]
                                at, atb = AsqL[u][lvl % 2]
                                P.op("act", "activation", PK(out=at[:, lo:128], in_=psq[0:64, lo:128], func=AF.Copy), reads=[psqb], writes=[atb])
                                cur[u] = (at[:, 0:64], at[:, 64:128], atb)
                        for ub in UB:
                            pts = {}
                            for u in ub:
                                pz, pzb = C.psum()
                                P.op("pe", "matmul", PK(pz[0:64, 0:64], cur[u][1], ZtL[u][zi][0][:, :], start=True, stop=True), reads=[cur[u][2], ZtL[u][zi][1]], writes=[pzb])
                                pts[u] = (pz, pzb)
                            for u in ub:
                                pz, pzb = pts[u]
                                P.op("dve", "tensor_tensor", PK(out=ZtL[u][1 - zi][0][:, :], in0=pz[0:64, 0:64], in1=ZtL[u][zi][0][:, :], op=ALU.add),
                                     reads=[pzb, ZtL[u][zi][1]], writes=[ZtL[u][1 - zi][1]])
                        zi = 1 - zi
                    for ub in UB:
                        pts = {}
                        for u in ub:
                            gi, e = units[u]
                            es = PR[e]
                            pt, pb = C.psum()
                            P.op("pe", "matmul", PK(pt[0:64, 0:64], AAsL[u][0][:, 128:192], TML[gi][0][:, 0, es], start=True, stop=True), reads=[AAsL[u][1], TML[gi][1]], writes=[pb])
                            pts[u] = (pt, pb)
                        for u in ub:
                            pt, pb = pts[u]
                            P.op("act", "activation", PK(out=WsbL[u][0][:, :], in_=pt[0:64, 0:64], func=AF.Copy), reads=[pb], writes=[WsbL[u][1]])
                    for ub in UB:
                        pts = {}
                        for u in ub:
                            gi, e = units[u]
                            pr, es = PR[e], PR[e]
                            Tm, Tmb = ZtL[u][zi]
                            pt, pb = C.psum()
                            P.op("pe", "matmul", PK(pt[0:64, 0:64], Tm[:, :], WsbL[u][0][:, :], start=True, stop=True), reads=[Tmb, WsbL[u][1]], writes=[pb])
                            P.op("pe", "matmul", PK(pt[pr, 64:128], TML[gi][0][:, 1, es], Tm[:, :], start=True, stop=True), reads=[Tmb, TML[gi][1]], writes=[pb])
                            pts[u] = (pt, pb)
                        for u in ub:
                            gi, e = units[u]
                            pt, pb = pts[u]
                            pr = PR[e]
                            P.op("act", "activation", PK(out=Ut0L[u][0][:, :], in_=pt[0:64, 0:64], func=AF.Copy), reads=[pb], writes=[Ut0L[u][1]])
                            P.op("act", "activation", PK(out=PhiL[gi][0][pr, :], in_=pt[pr, 64:128], func=AF.Copy), reads=[pb], writes=[PhiL[gi][1]])
                    for gi in range(NG):
                        c = g0 + gi
                        cs = CS[gi]
                        Sc, Scb = Sst[hp][sidx[hp] % 2]
                        Sn, Snb = Sst[hp][(sidx[hp] + 1) % 2]
                        TM, TMb = TML[gi]
                        Phi, Phib = PhiL[gi]
                        pts = []
                        for e in range(2):
                            pr = PR[e]
                            pt, pb = C.psum()
                            P.op("pe", "matmul", PK(pt[0:64, 0:64], Phi[pr, :], Sc[pr, :], start=True, stop=True), reads=[Phib, Scb], writes=[pb])
                            pts.append((pt, pb))
                        for e in range(2):
                            u = gi * 2 + e
                            pt, pb = pts[e]
                            P.op("dve", "tensor_tensor", PK(out=UtL[u][0][:, :], in0=pt[0:64, 0:64], in1=Ut0L[u][0][:, :], op=ALU.add), reads=[pb, Ut0L[u][1]], writes=[UtL[u][1]])
                        for e in range(2):
                            u = gi * 2 + e
                            pr = PR[e]
                            AAs, AAsb = AAsL[u]
                            Ut, Utb = UtL[u]
                            po, pob = poL[e]
                            mm1 = P.op("pe", "matmul", PK(po[pr, cs], Sc[pr, :], X("Rb")[pr, cs], start=True, stop=False), reads=[Scb, B_("Rb")], writes=[pob])
                            P.op("pe", "matmul", PK(po[pr, cs], Ut[:, :], AAs[:, 64:128], start=False, stop=False), reads=[Utb, AAsb], writes=[pob],
                                 after=([mm1] if e == 1 else []))
                            P.op("pe", "matmul", PK(po[pr, cs], TM[:, 0, pr], AAs[:, 192:256], start=False, stop=True), reads=[TMb, AAsb], writes=[pob])
                        pts = []
                        for e in range(2):
                            u = gi * 2 + e
                            pr = PR[e]
                            Ut, Utb = UtL[u]
                            pt, pb = C.psum()
                            P.op("pe", "matmul", PK(pt[pr, 0:64], TM[:, 2, pr], Ut[:, :], start=True, stop=False), reads=[TMb, Utb], writes=[pb])
                            P.op("pe", "matmul", PK(pt[pr, 0:64], TM[:, 3, pr], TM[:, 0, pr], start=False, stop=True), reads=[TMb], writes=[pb])
                            pts.append((pt, pb))
                        for e in range(2):
                            pr = PR[e]
                            pt, pb = pts[e]
                            P.op("dve", "scalar_tensor_tensor", PK(out=Sn[pr, :], in0=Sc[pr, :], scalar=gC[pr, c:c + 1], in1=pt[pr, 0:64], op0=ALU.mult, op1=ALU.add),
                                 reads=[Scb, gCb, pb], writes=[Snb])
                        sidx[hp] += 1
                    for _ in range(NG // 2):
                        s5_pair(s5_next[0]); s5_next[0] += 1
                Osb, Osbb = W_["Gi"]
                P.op("act", "activation", PK(out=Osb[0:64, :], in_=poL[0][0][0:64, :], func=AF.Copy), reads=[poL[0][1]], writes=[Osbb])
                P.op("act", "activation", PK(out=Osb[64:128, :], in_=poL[1][0][64:128, :], func=AF.Copy), reads=[poL[1][1]], writes=[Osbb])
                pm, pmb = C.psum()
                P.op("pe", "matmul", PK(pm[:, :], c32[:, C_B64, :], Osb[:, :], start=True, stop=True), reads=[c32b, Osbb], writes=[pmb])
                DV("tensor_tensor", [Osbb, pmb], "tA", out=X("tA")[:, :], in0=Osb[:, :], in1=pm[:, :], op=ALU.subtract)
                P.op("act", "activation", PK(out=X("tB")[:, :], in_=X("tA")[:, :], func=AF.Square), reads=[B_("tA")], writes=[B_("tB")])
                pv, pvb = C.psum()
                P.op("pe", "matmul", PK(pv[:, :], c32[:, C_B64, :], X("tB")[:, :], start=True, stop=True), reads=[c32b, B_("tB")], writes=[pvb])
                rstd_from(C, rs[:, :], pv[:, :], pvb, rsb, eps=64e-5)
                DV("tensor_tensor", [B_("tA"), rsb], "tA", out=X("tA")[:, :], in0=X("tA")[:, :], in1=rs[:, :], op=ALU.mult)
                P.op("act", "activation", PK(out=X("tB")[:, :], in_=X("tA")[:, :], func=AF.Identity, scale=vcol("ln_w"), bias=vcol("ln_b")),
                     reads=[B_("tA"), vb], writes=[B_("tB")])
                pk, pkb = C.psum()
                P.op("pe", "matmul", PK(pk[:, :], c32[:, C_B64, :], X("rk")[:, :], start=True, stop=True), reads=[c32b, B_("rk")], writes=[pkb])
                DV("scalar_tensor_tensor", [pkb, vb_], "tA", out=X("tA")[:, :], in0=pk[:, :], scalar=64.0, in1=v_, op0=ALU.mult, op1=ALU.mult)
                DV("tensor_tensor", [B_("tA"), B_("tB")], "tB", out=X("tB")[:, :], in0=X("tB")[:, :], in1=X("tA")[:, :], op=ALU.add)
                DV("tensor_tensor", [B_("tB"), B_("g")], "Gi", out=Osb[:, :], in0=X("tB")[:, :], in1=X("g")[:, :], op=ALU.mult)
                C.dma(ya_d[hp * 128:(hp + 1) * 128, tsl], Osb[:, :], R=[Osbb])

        C.psum = C.psum_default


def body_B(C, d, half, bufs):
    P = C.P
    t0 = half * NTOK
    if True:
        xT_d = d["xT"][:, t0:t0 + NTOK]; ymT_d = d["ymT"][:, t0:t0 + NTOK]; memT_d = d["memT"]
        vec_d = d["vecsB"]; cst_d = d["cst"]
        wglu_d = d["w_glu"]; wout_d = d["w_out"]; wq_d = d["w_q0"]; wkv_d = d["w_kv0"]; wo_d = d["w_o0"]
        wg_d = d["ff_g"]; wu_d = d["ff_u"]; wd_d = d["ff_d"]; wqkv_d = d["w_qkv"]
        h1T_d = d["h1T"][:, t0:t0 + NTOK]; qT_d = d["qT"][:, :, t0:t0 + NTOK]; kT_d = d["kT"][:, :, t0:t0 + NTOK]
        vtok_d = d["vtok"][t0:t0 + NTOK, :]
        ymb, h1b, qb_, kb_, vtb_ = bufs
        C.init_w(4, 4096)
        hT, _ = C.sb("hT", [128, 8, NTOK], F32); hTb = [Buf("hT%d" % i) for i in range(NTT)]
        bufA, _ = C.sb("bufA", [128, 8, NTOK], BF16); bufAb = [Buf("bA%d" % i) for i in range(NTT)]
        bufH, _ = C.sb("bufH", [128, 8, NTOK], BF16); bufHb = [Buf("bH%d" % i) for i in range(NTT)]
        V, vb = C.sb("V_sb", [128, NVB], F32); C.vb = vb
        cst3, cstb = C.sb("cst_sb", [128, 5, 128], BF16)
        scr = dict(sq=C.sb("sq", [128, 8, 512], BF16), rs=C.sb("rs", [128, 512], F32), sg=C.sb("sg", [128, 512], F32),
                   rd=C.sb("rd", [128, 512], F32))
        sqq, _ = C.sb("sqq", [128, 2, NTT, 512], BF16)
        scr["sqq"] = (sqq, [Buf("sqq%d" % i) for i in range(NTT)])
        et, _ = C.sb("et", [128, 2, 512], BF16)
        scr["et"] = (et, [Buf("et0"), Buf("et1")])
        C.dma(V[:, :], vec_d[:, :], W=[vb])
        P.op("pool", "dma_start", PK(out=cst3[:, :, :], in_=cst_d.rearrange("p (a b) -> p a b", a=5)), writes=[cstb], is_dma=True)
        for tt in range(NTT):
            sl = slice(tt * TT, (tt + 1) * TT)
            C.dma(hT[:, :, sl], xT_d[:, sl].rearrange("(c p) n -> p c n", p=128), W=[hTb[tt]])
            P.op("pool", "dma_start", PK(out=bufA[:, :, sl], in_=ymT_d[:, sl].rearrange("(c p) n -> p c n", p=128)),
                 reads=[ymb], writes=[bufAb[tt]], is_dma=True)
        wv, wb = C.load_w(wglu_d[:, :], 4, 512)
        sg, sgb = scr["sg"]
        for tt in range(NTT):
            sl = slice(tt * TT, (tt + 1) * TT)
            pts = []
            for oc in range(4):
                pt, pb = C.psum()
                for k in range(4):
                    P.op("pe", "matmul", PK(pt[:, :], wv[:, k, oc * 128:(oc + 1) * 128], bufA[:, 4 + k, sl],
                                                                            start=(k == 0), stop=(k == 3)), reads=[wb, bufAb[tt]], writes=[pb])
                pts.append((pt, pb))
            for oc in range(4):
                pt, pb = pts[oc]
                P.op("act", "activation", PK(out=sg[:, :], in_=pt[:, :], func=AF.Sigmoid,
                                                                bias=V[:, VB["b_glu"] + oc:VB["b_glu"] + oc + 1]), reads=[pb, vb], writes=[sgb])
                P.op("dve", "tensor_tensor", PK(out=bufA[:, 4 + oc, sl], in0=bufA[:, 4 + oc, sl], in1=sg[:, :], op=ALU.mult),
                     reads=[sgb, bufAb[tt]], writes=[bufAb[tt]])

        def evac_res(oc, tt, pt, pb, m):
            sl = slice(tt * TT, (tt + 1) * TT)
            P.op("dve", "tensor_tensor", PK(out=hT[:, oc, sl], in0=pt[:, :], in1=hT[:, oc, sl], op=ALU.add), reads=[pb, hTb[tt]], writes=[hTb[tt]])
        linear_fm(C, wout_d, 8, bufA, None, 1024, evac_res, xinbs=bufAb)
        (kn, knb), (vt, vtb) = mem_kv(C, memT_d, wkv_d, V, vb, VB["n_mem"], VB["k_gain"], cst3, cstb, scr, "m0")
        mem_xattn(C, hT, hTb, bufA, bufAb, bufH, bufHb, V, vb, VB["n_xattn"], VB["q_gain"], wq_d, wo_d, kn, knb, vt, vtb, cst3, cstb, scr)
        swiglu_ffn(C, hT, hTb, bufA, bufAb, bufH, bufHb, V, vb, VB["n_ffn"], wg_d, wu_d, wd_d, 2816, cst3, cstb, scr)
        for tt in range(NTT):
            sl = slice(tt * TT, (tt + 1) * TT)
            C.dma(h1T_d[:, sl].rearrange("(c p) n -> p c n", p=128), hT[:, :, sl], R=[hTb[tt]])
        for tt in range(NTT):
            sl = slice(tt * TT, (tt + 1) * TT)
            norm_fm(C, lambda c: hT[:, c, sl], [hTb[tt]], TT, 8, cst3[:, 0, :], cstb, lambda c: V[:, VB["n_mix1"] + c:VB["n_mix1"] + c + 1],
                    lambda c: bufA[:, c, sl], [bufAb[tt]], scr)
        P.fence(C.dummy[:, 0:1])
        NS = 4
        sqL = [(bufH[:, 0, i * 512:(i + 1) * 512], Buf("qk_sq%d" % i)) for i in range(NS)]
        f32v = [bufH[:, 1 + i, :].bitcast(F32) for i in range(4)]
        tl = [(f32v[i // 2][:, (i % 2) * 512:(i % 2 + 1) * 512], Buf("qk_t%d" % i)) for i in range(8)]
        rsL, qoL = tl[0:4], tl[4:8]
        qi = [0]
        P.op("dve", "tensor_scalar", PK(out=V[:, VB["da_qg"]:VB["da_qg"] + 1], in0=V[:, VB["da_qg"]:VB["da_qg"] + 1], scalar1=0.125, scalar2=None, op0=ALU.mult),
             reads=[vb], writes=[vb])
        for which, (dst, gcol) in enumerate(((qT_d, VB["da_qg"]), (kT_d, VB["da_kg"]))):
            def evac_qk(oc, tt, pt, pb, m, dst=dst, gcol=gcol):
                sl = slice(tt * TT, (tt + 1) * TT)
                k_ = qi[0] % NS; qi[0] += 1
                sq1, sq1b = sqL[k_]; rs, rsb = rsL[k_]; qo, qob = qoL[k_]
                P.op("act", "activation", PK(out=sq1, in_=pt[:, :], func=AF.Square), reads=[pb], writes=[sq1b])
                p2, p2b = C.psum()
                P.op("pe", "matmul", PK(p2[:, :], cst3[:, 2, :], sq1, start=True, stop=True), reads=[sq1b, cstb], writes=[p2b])
                rstd_from(C, rs, p2[:, :], p2b, rsb)
                P.op("dve", "scalar_tensor_tensor", PK(out=qo, in0=pt[:, :], scalar=V[:, gcol:gcol + 1], in1=rs,
                                                      op0=ALU.mult, op1=ALU.mult), reads=[pb, rsb, vb], writes=[qob])
                C.dma(dst[2 * oc:2 * oc + 2, :, sl].rearrange("c r n -> (c r) n"), qo, R=[qob])
            linear_fm(C, wqkv_d, 8, bufA, None, 1024, evac_qk, col0=which * 1024, xinbs=bufAb)
        vo, vob = scr["sg"]
        for half2 in range(2):
            wv2, wb2 = C.load_w(wqkv_d[:, 2048 + half2 * 512:2048 + (half2 + 1) * 512], 8, 512)
            for s in range(16):
                tt = s // 4
                pt, pb = C.psum()
                for k in range(8):
                    P.op("pe", "matmul", PK(pt[:, :], bufA[:, k, s * 128:(s + 1) * 128], wv2[:, k, :], start=(k == 0), stop=(k == 7)),
                         reads=[wb2, bufAb[tt]], writes=[pb])
                P.op("act", "activation", PK(out=vo[:, :], in_=pt[:, :], func=AF.Copy), reads=[pb], writes=[vob])
                C.dma(vtok_d[s * 128:(s + 1) * 128, half2 * 512:(half2 + 1) * 512], vo[:, :], R=[vob])


def body_C1(C, d, bufA, bufAb, bufs):
    P = C.P
    if True:
        qs_d = d["qT"]; ks_d = d["kT"]; vf_d = d["vtok"]; qa4_d = d["qaug4"]; ka4_d = d["kaug4"]
        bias_d = d["biasT"]; lamb_d = d["lamb"]; sgc_d = d["sgc"]; cst_d = d["cst"]; sel_d = d["sel"]
        h1b, qb_, kb_, vtb_ = bufs
        cst3, cstb = C.sb("cst_sb", [128, 5, 128], BF16)
        P.op("pool", "dma_start", PK(out=cst3[:, :, :], in_=cst_d.rearrange("p (a b) -> p a b", a=5)), writes=[cstb], is_dma=True)
        biasT, biasb = C.sb("bias_sb", [128, 8, 512], F32)
        C.dma(biasT[:, :, :], bias_d.rearrange("p (a b) -> p a b", a=8), W=[biasb])
        lamb, lambb = C.sb("lamb_sb", [128, 4, 64], F32)
        C.dma(lamb[:, :, :], lamb_d.rearrange("p (a b) -> p a b", a=4), W=[lambb])
        sgc, sgcb = C.sb("sgc_sb", [128, 1], F32)
        C.dma(sgc[:, :], sgc_d[:, :], W=[sgcb])
        scr = dict(sq=C.sb("sq", [128, 1, 512], BF16), rs=C.sb("rs", [128, 512], F32))
        lt, ltb = C.sb("lt", [128, 2, 64], F32)
        lc, lcb = C.sb("lc", [128, 4], F32)
        for i in range(2):
            P.op("dve", "tensor_tensor", PK(out=lt[:, i, :], in0=lamb[:, 2 * i, :], in1=lamb[:, 2 * i + 1, :], op=ALU.mult), reads=[lambb], writes=[ltb])
            P.op("dve", "reduce_sum", PK(out=lc[:, i:i + 1], in_=lt[:, i, :], axis=AX.X), reads=[ltb], writes=[lcb])
        P.op("act", "activation", PK(out=lc[:, 0:2], in_=lc[:, 0:2], func=AF.Exp), reads=[lcb], writes=[lcb])
        P.op("dve", "tensor_tensor", PK(out=lc[:, 2:3], in0=lc[:, 1:2], in1=lc[:, 0:1], op=ALU.subtract), reads=[lcb], writes=[lcb])
        P.op("dve", "tensor_scalar_add", PK(out=lc[:, 3:4], in0=lc[:, 2:3], scalar1=-LAMBDA_INIT), reads=[lcb], writes=[lcb])
        P.op("dve", "tensor_scalar", PK(out=sgc[:, :], in0=sgc[:, :], scalar1=float(1.0 - LAMBDA_INIT), scalar2=None, op0=ALU.mult), reads=[sgcb], writes=[sgcb])
        kA = [C.sb("kA%d" % i, [68, 2, 4096], BF16) for i in range(2)]
        qA = [C.sb("qA%d" % i, [68, 2, NTOK], BF16) for i in range(2)]
        vA = [C.sb("vA%d" % i, [128, 32, 128], BF16) for i in range(2)]
        qraw, qrawb = C.sb("qraw", [64, 2, 4096], BF16)
        sel, selb = C.sb("sel_sb", [128, 2], F32)
        C.dma(sel[:, :], sel_d[:, :], W=[selb])
        for i_ in range(2):
            for c_ in range(2):
                P.op("pool", "dma_start", PK(out=qA[i_][0][64:68, c_, :], in_=qa4_d[:, :]), writes=[qA[i_][1]], is_dma=True)
        et, _ = C.sb("et", [128, 4, 512], BF16); etb = [Buf("et%d" % i) for i in range(4)]
        tmp, _ = C.sb("tmp", [128, 2, 512], F32); tmpb = [Buf("tmp%d" % i) for i in range(2)]
        rd, rdb = C.sb("rd", [128, 512], F32)
        o0, o0b = C.sb("o0", [128, 512], F32)
        o1, o1b = C.sb("o1", [128, 512], F32)
        ot, otb = C.sb("ot", [128, 512], F32)
        ei = 0
        ti_ = 0
        for h in range(8):
            slope = float(2.0 ** (-(h + 1)))
            kt, ktb = kA[h % 2]; qt, qtb = qA[h % 2]; vt, vtb = vA[h % 2]
            P.op("pool", "dma_start", PK(out=kt[0:64, :, :], in_=ks_d[2 * h:2 * h + 2].rearrange("c r n -> r c n")), reads=[kb_], writes=[ktb], is_dma=True)
            P.op("pool", "dma_start", PK(out=kt[64:68, 0, :], in_=ka4_d[h]), writes=[ktb], is_dma=True)
            P.op("pool", "dma_start", PK(out=kt[64:68, 1, :], in_=ka4_d[h]), writes=[ktb], is_dma=True)
            P.op("pool", "dma_start", PK(out=qraw[:, :, :], in_=qs_d[2 * h:2 * h + 2].rearrange("c r n -> r c n")), reads=[qb_], writes=[qrawb], is_dma=True)
            for c in range(2):
                q5 = qraw[:, c, :].rearrange("p (m two n) -> p m two n", two=2, n=512)
                qo4 = qt[0:64, c, :].rearrange("p (m n) -> p m n", n=512)
                P.op("dve", "tensor_scalar", PK(out=qo4, in0=q5[:, :, 0, :], scalar1=sel[0:64, 0:1], scalar2=None, op0=ALU.mult), reads=[qrawb, selb], writes=[qtb])
                P.op("dve", "scalar_tensor_tensor", PK(out=qo4, in0=q5[:, :, 1, :], scalar=sel[0:64, 1:2], in1=qo4, op0=ALU.mult, op1=ALU.add),
                     reads=[qrawb, selb, qtb], writes=[qtb])
            P.op("pool", "dma_start", PK(out=vt[:, :, :], in_=vf_d[:, h * 128:(h + 1) * 128].rearrange("(j p) d -> p j d", p=128)), reads=[vtb_], writes=[vtb], is_dma=True)
            for m in range(4):
                nkb = 8 * (m + 1)
                qsl = slice(m * 512, (m + 1) * 512)
                N = [C.ps[0], C.ps[1]]
                Dn = [C.ps[2], C.ps[3]]
                def issue_scores(j):
                    for c in range(2):
                        sc, scb = C.ps[4 + ((2 * j + c) % 4)]
                        P.op("pe", "matmul", PK(sc[:, :], kt[:, c, j * 128:(j + 1) * 128], qt[:, c, qsl], start=True, stop=True),
                             reads=[ktb, qtb], writes=[scb])
                issue_scores(0)
                for j in range(nkb):
                    if j + 1 < nkb:
                        issue_scores(j + 1)
                    for c in range(2):
                        sc, scb = C.ps[4 + ((2 * j + c) % 4)]
                        e_slot = ei % 4; ei += 1
                        if j >= nkb - 8:
                            s = j - (nkb - 8)
                            tb = ti_ % 2; ti_ += 1
                            P.op("dve", "scalar_tensor_tensor", PK(out=tmp[:, tb, :], in0=biasT[:, s, :], scalar=slope, in1=sc[:, :], op0=ALU.mult, op1=ALU.add),
                                 reads=[biasb, scb], writes=[tmpb[tb]])
                            P.op("act", "activation", PK(out=et[:, e_slot, :], in_=tmp[:, tb, :], func=AF.Exp), reads=[tmpb[tb]], writes=[etb[e_slot]])
                        else:
                            P.op("act", "activation", PK(out=et[:, e_slot, :], in_=sc[:, :], func=AF.Exp), reads=[scb], writes=[etb[e_slot]])
                        P.op("pe", "matmul", PK(N[c][0][:, :], vt[:, j, :], et[:, e_slot, :], start=(j == 0), stop=(j == nkb - 1)),
                             reads=[vtb, etb[e_slot]], writes=[N[c][1]])
                        P.op("pe", "matmul", PK(Dn[c][0][:, :], cst3[:, 4, :], et[:, e_slot, :], start=(j == 0), stop=(j == nkb - 1)),
                             reads=[cstb, etb[e_slot]], writes=[Dn[c][1]])
                P.op("dve", "reciprocal", PK(out=rd[:, :], in_=Dn[0][0][:, :]), reads=[Dn[0][1]], writes=[rdb])
                P.op("dve", "tensor_tensor", PK(out=o0[:, :], in0=N[0][0][:, :], in1=rd[:, :], op=ALU.mult), reads=[N[0][1], rdb], writes=[o0b])
                P.op("dve", "reciprocal", PK(out=rd[:, :], in_=Dn[1][0][:, :]), reads=[Dn[1][1]], writes=[rdb])
                P.op("dve", "tensor_tensor", PK(out=o1[:, :], in0=N[1][0][:, :], in1=rd[:, :], op=ALU.mult), reads=[N[1][1], rdb], writes=[o1b])
                P.op("dve", "scalar_tensor_tensor", PK(out=o0[:, :], in0=o1[:, :], scalar=lc[:, 3:4], in1=o0[:, :], op0=ALU.mult, op1=ALU.add),
                     reads=[o1b, o0b, lcb], writes=[o0b])
                sq, sqb = scr["sq"]; rs, rsb = scr["rs"]
                P.op("act", "activation", PK(out=sq[:, 0, :], in_=o0[:, :], func=AF.Square), reads=[o0b], writes=[sqb])
                pn, pnb = C.ps[4]
                P.op("pe", "matmul", PK(pn[:, :], cst3[:, 3, :], sq[:, 0, :], start=True, stop=True), reads=[sqb, cstb], writes=[pnb])
                rstd_from(C, rs[:, :], pn[:, :], pnb, rsb)
                P.op("dve", "scalar_tensor_tensor", PK(out=bufA[:, h, qsl], in0=o0[:, :], scalar=sgc[:, 0:1], in1=rs[:, :], op0=ALU.mult, op1=ALU.mult),
                     reads=[o0b, rsb, sgcb], writes=[bufAb[m]])


def body_C2(C, d, bufA, bufAb, h1b):
    P = C.P
    if True:
        h1s_d = d["h1T"]; memT_d = d["memT"]; vec_d = d["vecsC"]; cst_d = d["cst"]; c32_d = d["c32"]; sel_d = d["sel"]
        wdo_d = d["da_w_o"]; wq_d = d["w_q1"]; wkv_d = d["w_kv1"]; wo_d = d["w_o1"]; wr_d = d["w_router"]
        wg_d = d["moe_g"]; wu_d = d["moe_u"]; wd_d = d["moe_d"]; outT_d = d["outT"]
        C.init_w(3, 4096)
        hT, _ = C.sb("hT", [128, 8, NTOK], F32); hTb = [Buf("hT%d" % i) for i in range(NTT)]
        bufH, _ = C.sb("bufH", [128, 8, NTOK], BF16); bufHb = [Buf("bH%d" % i) for i in range(NTT)]
        V, vb = C.sb("V_sb", [128, NVC], F32); C.vb = vb
        cst3, cstb = C.sb("cst_sb", [128, 5, 128], BF16)
        c32, c32b = C.sb("c32_sb", [128, 2, 128], F32)
        scr = dict(sq=C.sb("sq", [128, 8, 512], BF16), rs=C.sb("rs", [128, 512], F32), sg=C.sb("sg", [128, 512], F32),
                   rd=C.sb("rd", [128, 512], F32))
        sqq, _ = C.sb("sqq", [128, 2, NTT, 512], BF16)
        scr["sqq"] = (sqq, [Buf("sqq%d" % i) for i in range(NTT)])
        et, _ = C.sb("et", [128, 2, 512], BF16)
        scr["et"] = (et, [Buf("et0"), Buf("et1")])
        C.dma(V[:, :], vec_d[:, :], W=[vb])
        C.dma(c32[:, :, :], c32_d.rearrange("p (a b) -> p a b", a=2), W=[c32b])
        P.op("pool", "dma_start", PK(out=cst3[:, :, :], in_=cst_d.rearrange("p (a b) -> p a b", a=5)), writes=[cstb], is_dma=True)
        sel, selb = C.sb("sel_sb", [128, 2], F32)
        C.dma(sel[:, :], sel_d[:, :], W=[selb])
        htmp, _ = C.sb("htmp", [128, 2, 512], F32); htmpb = [Buf("htmp0"), Buf("htmp1")]
        hi_ = 0
        for m in range(NTT):
            sl = slice(m * TT, (m + 1) * TT)
            e0 = slice((2 * m) * TT, (2 * m + 1) * TT); e1 = slice((2 * m + 1) * TT, (2 * m + 2) * TT)
            C.dma(hT[:, :, sl], h1s_d[:, e0].rearrange("(c p) n -> p c n", p=128), R=[h1b], W=[hTb[m]])
            for c in range(8):
                hb = hi_ % 2; hi_ += 1
                C.dma(htmp[:, hb, :], h1s_d[c * 128:(c + 1) * 128, e1], R=[h1b], W=[htmpb[hb]])
                P.op("dve", "tensor_scalar", PK(out=hT[:, c, sl], in0=hT[:, c, sl], scalar1=sel[:, 0:1], scalar2=None, op0=ALU.mult), reads=[hTb[m], selb], writes=[hTb[m]])
                P.op("dve", "scalar_tensor_tensor", PK(out=hT[:, c, sl], in0=htmp[:, hb, :], scalar=sel[:, 1:2], in1=hT[:, c, sl], op0=ALU.mult, op1=ALU.add),
                     reads=[htmpb[hb], selb, hTb[m]], writes=[hTb[m]])
        def evac_res(oc, tt, pt, pb, m):
            sl = slice(tt * TT, (tt + 1) * TT)
            P.op("dve", "tensor_tensor", PK(out=hT[:, oc, sl], in0=pt[:, :], in1=hT[:, oc, sl], op=ALU.add), reads=[pb, hTb[tt]], writes=[hTb[tt]])
        linear_fm(C, wdo_d, 8, bufA, None, 1024, evac_res, xinbs=bufAb)
        _mem_off0 = C.off
        (kn, knb), (vt, vtb) = mem_kv(C, memT_d, wkv_d, V, vb, VC["n_mem"], VC["k_gain"], cst3, cstb, scr, "m1")
        _mem_off1 = C.off
        mem_xattn(C, hT, hTb, bufA, bufAb, bufH, bufHb, V, vb, VC["n_xattn"], VC["q_gain"], wq_d, wo_d, kn, knb, vt, vtb, cst3, cstb, scr)
        for tt in range(NTT):
            sl = slice(tt * TT, (tt + 1) * TT)
            norm_fm(C, lambda c: hT[:, c, sl], [hTb[tt]], TT, 8, cst3[:, 0, :], cstb, lambda c: V[:, VC["n_ffn"] + c:VC["n_ffn"] + c + 1],
                    lambda c: bufA[:, c, sl], [bufAb[tt]], scr)
        hn32, hn32b = C.sb("hn32", [128, 8, 128], F32)
        wr, wrb = C.sb("wr", [128, 8, 8], F32)
        G, Gb = C.sb("G", [128, 16, 8], F32)
        lg, lgb = C.sb("lg", [128, 8], F32)
        mx, mxb = C.sb("mx", [128, 8], F32)
        sm, smb = C.sb("sm", [128, 4], F32)
        C.dma(wr[:, :, :], wr_d.rearrange("(c p) n -> p c n", p=128), W=[wrb])
        for s in range(16):
            tt = s // 4
            sl = slice(s * 128, (s + 1) * 128)
            norm_fm(C, lambda c: hT[:, c, sl], [hTb[tt]], 128, 8, cst3[:, 0, :], cstb, lambda c: V[:, VC["n_ffn"] + c:VC["n_ffn"] + c + 1],
                    lambda c: hn32[:, c, :], [hn32b], scr)
            pt, pb = C.psum()
            for k in range(8):
                P.op("pe", "matmul", PK(pt[:, 0:8], hn32[:, k, :], wr[:, k, :], start=(k == 0), stop=(k == 7)), reads=[hn32b, wrb], writes=[pb])
            P.op("dve", "tensor_tensor", PK(out=lg[:, :], in0=pt[:, 0:8], in1=V[:, VC["b_router"]:VC["b_router"] + 8], op=ALU.add),
                 reads=[pb, vb], writes=[lgb])
            P.op("dve", "max", PK(out=mx[:, :], in_=lg[:, :]), reads=[lgb], writes=[mxb])
            P.op("dve", "tensor_scalar", PK(out=sm[:, 0:1], in0=mx[:, 0:1], scalar1=-1.0, scalar2=None, op0=ALU.mult), reads=[mxb], writes=[smb])
            ex, exb = scr["sg"]
            P.op("act", "activation", PK(out=ex[:, 0:8], in_=lg[:, :], func=AF.Exp, bias=sm[:, 0:1]), reads=[lgb, smb], writes=[exb])
            P.op("dve", "tensor_scalar", PK(out=lg[:, :], in0=lg[:, :], scalar1=mx[:, 1:2], scalar2=None, op0=ALU.is_ge), reads=[lgb, mxb], writes=[lgb])
            P.op("dve", "tensor_tensor", PK(out=ex[:, 0:8], in0=ex[:, 0:8], in1=lg[:, :], op=ALU.mult), reads=[exb, lgb], writes=[exb])
            P.op("dve", "reduce_sum", PK(out=sm[:, 1:2], in_=ex[:, 0:8], axis=AX.X), reads=[exb], writes=[smb])
            P.op("dve", "reciprocal", PK(out=sm[:, 2:3], in_=sm[:, 1:2]), reads=[smb], writes=[smb])
            P.op("dve", "tensor_scalar", PK(out=G[:, s, :], in0=ex[:, 0:8], scalar1=sm[:, 2:3], scalar2=None, op0=ALU.mult), reads=[exb, smb], writes=[Gb])
        P.fence(C.dummy[:, 0:1])
        gbc = sqq.rearrange("p a b c -> p (a b c)")[:, 0:NTOK]; gbcb = [Buf("gbc%d" % i) for i in range(NTT)]
        C.wbufs.append((scr["sq"][0].rearrange("p a b -> p (a b)"), Buf("wb_sq")))
        _o = _mem_off0
        while _o + 2048 <= _mem_off1:
            C.wbufs.append((C.arena[:, _o:_o + 2048].bitcast(BF16), Buf("wb_m%d" % _o)))
            _o += 2048
        dg, dgb = scr["rd"]
        for e_ in range(8):
            for tt in range(NTT):
                pt, pb = C.psum()
                for q in range(4):
                    s = tt * 4 + q
                    P.op("dve", "tensor_scalar", PK(out=dg[:, 0:128], in0=c32[:, 0, :], scalar1=G[:, s, e_:e_ + 1], scalar2=None, op0=ALU.mult),
                         reads=[c32b, Gb], writes=[dgb])
                    P.op("pe", "matmul", PK(pt[:, q * 128:(q + 1) * 128], c32[:, 1, :], dg[:, 0:128], start=True, stop=True), reads=[dgb, c32b], writes=[pb])
                P.op("act", "activation", PK(out=gbc[:, tt * TT:(tt + 1) * TT], in_=pt[:, :], func=AF.Copy), reads=[pb], writes=[gbcb[tt]])
            swiglu_ffn(C, hT, hTb, bufA, bufAb, bufH, bufHb, V, vb, None, wg_d[e_], wu_d[e_], wd_d[e_], 3584, cst3, cstb, scr, gate_bc=(gbc, gbcb))
        for tt in range(NTT):
            sl = slice(tt * TT, (tt + 1) * TT)
            C.dma(outT_d[:, sl].rearrange("(c p) n -> p c n", p=128), hT[:, :, sl], R=[hTb[tt]], is_out=True)


ARENA_WORDS = 53200


def build_fused():
    nc = bass.Bass("TRN2", target_bir_lowering=False)
    d = {}

    def I(n, s):
        d[n] = nc.dram_tensor(n, s, F32, kind="ExternalInput").ap()

    def S(n, s):
        d[n] = nc.dram_tensor(n, s, F32, kind="Internal").ap()
    I("xT", [D, T_SEQ]); I("memT", [D, 256]); I("cst", [128, 640]); I("c32A", [128, 384]); I("c32", [128, 256])
    I("mask5", [64, 320]); I("rmask", [128, SEG]); I("vecsB", [128, NVB]); I("vecsC", [128, NVC]); I("sel", [128, 2])
    I("qaug4", [4, NTOK]); I("kaug4", [8, 4, T_SEQ]); I("biasT", [128, 8 * 512]); I("lamb", [128, 256]); I("sgc", [128, 1])
    for hh in range(2):
        I("wc%d" % hh, [D, NCOLS]); I("vecsA%d" % hh, [128, NVA]); I("w_up%d" % hh, [64, 256]); I("a_up%d" % hh, [128, 256])
        I("g_upa%d" % hh, [128, 256]); I("g_upb%d" % hh, [32, 256])
        for n in ("breT", "bimT", "creT", "cimT"):
            I("%s%d" % (n, hh), [8, 128, 128])
    I("w_glu", [512, 512]); I("w_out", [D, D]); I("w_q0", [D, D]); I("w_kv0", [D, 2 * D]); I("w_o0", [D, D])
    I("ff_g", [D, 2816]); I("ff_u", [D, 2816]); I("ff_d", [2816, D]); I("w_qkv", [D, 3 * D])
    I("da_w_o", [D, D]); I("w_q1", [D, D]); I("w_kv1", [D, 2 * D]); I("w_o1", [D, D]); I("w_router", [D, 8])
    I("moe_g", [8, D, 3584]); I("moe_u", [8, D, 3584]); I("moe_d", [8, 3584, D])
    S("ymT", [D, T_SEQ]); S("h1T", [D, T_SEQ]); S("qT", [16, 64, T_SEQ]); S("kT", [16, 64, T_SEQ]); S("vtok", [T_SEQ, D])
    d["outT"] = nc.dram_tensor("outT", [D, NTOK], F32, kind="ExternalOutput").ap()
    with ExitStack() as st:
        P = Prog(nc)
        C = ACtx(nc, st, P, ARENA_WORDS)
        ymb, h1b, qb_, kb_, vtb_ = Buf("ymT"), Buf("h1T"), Buf("qT"), Buf("kT"), Buf("vtok")
        for hh in range(2):
            if hh:
                C.new_phase()
            body_A(C, d, hh, ymb)
        import os as _os
        if _os.environ.get("FSTOP") == "A":
            C.dma(d["outT"][:, 0:512], d["ymT"][:, 0:512], R=[ymb], is_out=True)
            P.emit(st)
            return nc
        for half in range(2):
            C.new_phase()
            body_B(C, d, half, (ymb, h1b, qb_, kb_, vtb_))
        C.new_phase()
        bufA, _ = C.sb("bufA_p", [128, 8, NTOK], BF16); bufAb = [Buf("bAp%d" % i) for i in range(NTT)]
        keep = C.off
        body_C1(C, d, bufA, bufAb, (h1b, qb_, kb_, vtb_))
        C.new_phase(keep=keep)
        body_C2(C, d, bufA, bufAb, h1b)
        P.emit(st)
    return nc


def pack_fused(inp, b, p):
    m = {}
    for hh in range(2):
        a = pack_A(inp, b, hh)
        if hh == 0:
            m["xT"] = a["xT"]; m["cst"] = a["cst"]; m["c32A"] = a["c32"]; m["mask5"] = a["mask5"]; m["rmask"] = a["rmask"]
        m["wc%d" % hh] = a["wc"]; m["vecsA%d" % hh] = a["vecs"]; m["w_up%d" % hh] = a["w_up"]; m["a_up%d" % hh] = a["a_up"]
        m["g_upa%d" % hh] = a["g_upa"]; m["g_upb%d" % hh] = a["g_upb"]
        for n in ("breT", "bimT", "creT", "cimT"):
            m["%s%d" % (n, hh)] = a[n]
    m["memT"] = np.ascontiguousarray(np.asarray(inp["mem"][b]).T)
    m["c32"] = c32_table(); m["vecsB"] = vecs_B(inp); m["vecsC"] = vecs_C(inp)
    sel = np.zeros((128, 2), np.float32); sel[:, p] = 1.0
    m["sel"] = sel
    m["qaug4"] = q_aug_rows(p)
    m["kaug4"] = np.stack([k_aug_rows(h) for h in range(8)])
    m["biasT"] = bias_table(p)
    m["lamb"] = np.ascontiguousarray(np.broadcast_to(np.stack([inp["da_lam_q1"][0], inp["da_lam_k1"][0], inp["da_lam_q2"][0],
                                                               inp["da_lam_k2"][0]]).reshape(1, 256), (128, 256))).astype(np.float32)
    m["sgc"] = np.asarray(inp["da_sub_gain"][0], np.float32).reshape(128, 1)
    for k_, src in (("w_glu", "s5_w_glu"), ("w_out", "hy_w_out"), ("ff_g", "ff_w_gate"), ("ff_u", "ff_w_up"), ("ff_d", "ff_w_down"),
                    ("w_qkv", "da_w_qkv"), ("da_w_o", "da_w_o"), ("w_router", "moe_w_router"), ("moe_g", "moe_w_gate"),
                    ("moe_u", "moe_w_up"), ("moe_d", "moe_w_down")):
        m[k_] = np.asarray(inp[src][0])
    for l in range(2):
        m["w_q%d" % l] = np.asarray(inp["xa_w_q"][l]); m["w_kv%d" % l] = np.asarray(inp["xa_w_kv"][l]); m["w_o%d" % l] = np.asarray(inp["xa_w_o"][l])
    return m


_CACHE = {}


def kernel(**inp):
    inp = {k: np.asarray(v) for k, v in inp.items()}
    B_, T_ = 4, 4096
    if "F" not in _CACHE:
        _CACHE["F"] = build_fused()
    cores = [(b, p) for b in range(B_) for p in range(2)]
    maps = [pack_fused(inp, b, p) for (b, p) in cores]
    res = run_bass_kernel_spmd(_CACHE["F"], maps, core_ids=list(range(8))).results
    out = np.zeros((B_, T_, 1024), np.float32)
    for i, (b, p) in enumerate(cores):
        out[b][tok_index(p)] = np.asarray(res[i]["outT"]).T
    return out
```

```python
from contextlib import ExitStack
import numpy as np
import concourse.bass as bass
import concourse.mybir as mybir
from concourse.bass_utils import run_bass_kernel_spmd

F32 = mybir.dt.float32
BF16 = mybir.dt.bfloat16
AF = mybir.ActivationFunctionType
ALU = mybir.AluOpType
AX = mybir.AxisListType

ENGS = ("pe", "act", "dve", "pool", "sp")


def PK(*a, **k):
    return (a, k)


class Buf:
    __slots__ = ("name", "w", "rs", "excl")

    def __init__(self, name, excl=False):
        self.name = name
        self.excl = excl
        self.w = None
        self.rs = []


class Op:
    __slots__ = ("eng", "fn", "deps", "is_dma", "needed", "idx", "semval", "dsem")

    def __init__(self, eng, fn, is_dma):
        self.eng = eng
        self.fn = fn
        self.deps = set()
        self.is_dma = is_dma
        self.needed = False
        self.semval = None
        self.dsem = None


class Prog:
    def __init__(self, nc, n_dma_sems=24):
        self.nc = nc
        self.ops = []
        self.n_dma_sems = n_dma_sems
        self.out_dma_ops = []
        self.fence_idx = None
        self.last = {}
        self.dmas_since = []

    def op(self, eng, fn, pack=None, reads=(), writes=(), is_dma=False, is_out=False, after=()):
        if isinstance(fn, str):
            _name, _a, _k = fn, pack[0], pack[1]
            fn = lambda e: getattr(e, _name)(*_a, **_k)
        o = Op(eng, fn, is_dma)
        o.idx = len(self.ops)
        ops = self.ops
        ex = [b for b in reads if b.excl and b not in writes]
        if ex:
            reads = [b for b in reads if not b.excl]
            writes = list(writes) + ex

        def add(d, raw):
            p = ops[d]
            if not (p.is_dma or is_dma) and p.eng == eng:
                if eng == "pe":
                    return
            o.deps.add(d)
        for b in reads:
            if b.w is not None:
                add(b.w, True)
        for b in writes:
            if b.w is not None:
                add(b.w, False)
            for r in b.rs:
                add(r, False)
        if self.fence_idx is not None:
            o.deps.add(self.fence_idx)
        for x in after:
            o.deps.add(x.idx)
        for b in reads:
            b.rs.append(o.idx)
        for b in writes:
            b.w = o.idx
            b.rs = []
        if is_dma:
            self.dmas_since.append(o.idx)
        else:
            self.last[eng] = o.idx
        self.ops.append(o)
        if is_out:
            self.out_dma_ops.append(o.idx)
        return o

    def fence(self, dummy_ap):
        deps = set(self.last.values()) | set(self.dmas_since)
        o = self.op("dve", "memset", PK(dummy_ap, 0.0))
        o.deps |= deps
        self.fence_idx = o.idx
        self.dmas_since = []
        return o

    def emit(self, stack):
        nc = self.nc
        ops = self.ops
        for o in ops:
            best = {}
            keep = set()
            for d in o.deps:
                p = ops[d]
                if p.is_dma:
                    keep.add(d)
                else:
                    if p.eng not in best or d > best[p.eng]:
                        best[p.eng] = d
            for e, d in best.items():
                keep.add(d)
            o.deps = keep
            for d in keep:
                ops[d].needed = True
        for i in self.out_dma_ops:
            ops[i].needed = True
        sems = {e: stack.enter_context(nc.semaphore("s_" + e)) for e in ENGS if e != "sp"}
        dsems = [stack.enter_context(nc.semaphore("d%d" % i)) for i in range(self.n_dma_sems)]
        cnt = {e: 0 for e in sems}
        dcnt = [0] * len(dsems)
        rr = {"sw": 0, "hw": 0}
        nsw = len(dsems) // 2
        pools = {"sw": list(range(0, nsw)), "hw": list(range(nsw, len(dsems)))}
        lastd = {}
        per_eng = {e: [] for e in ENGS}
        for o in ops:
            if o.is_dma:
                o.needed = True
                kind = "sw" if o.eng == "pool" else "hw"
                pl = pools[kind]
                k = pl[rr[kind] % len(pl)]
                rr[kind] += 1
                prev = lastd.get(k)
                if prev is not None:
                    o.deps.add(prev)
                lastd[k] = o.idx
                dcnt[k] += 16
                o.dsem = dsems[k]
                o.semval = dcnt[k]
            elif o.needed:
                cnt[o.eng] += 1
                o.semval = cnt[o.eng]
            per_eng[o.eng].append(o)
        self.stats = {e: len(per_eng[e]) for e in ENGS}
        self.stats["sem_max"] = dict(cnt)
        block = stack.enter_context(nc.Block())

        def run(engname, eng):
            seen = {}
            for o in per_eng[engname]:
                for d in sorted(o.deps):
                    p = ops[d]
                    s = p.dsem if p.is_dma else sems[p.eng]
                    key = id(s)
                    if seen.get(key, -1) >= p.semval:
                        continue
                    seen[key] = p.semval
                    eng.wait_ge(s, p.semval)
                ins = o.fn(eng)
                if o.is_dma:
                    ins.then_inc(o.dsem, 16)
                elif o.needed:
                    ins.then_inc(sems[o.eng], 1)
            if engname == "sp":
                for i in self.out_dma_ops:
                    p = ops[i]
                    eng.wait_ge(p.dsem, p.semval)

        @block.tensor
        def _(e):
            run("pe", e)

        @block.scalar
        def _(e):
            run("act", e)

        @block.vector
        def _(e):
            run("dve", e)

        @block.gpsimd
        def _(e):
            run("pool", e)

        @block.sync
        def _(e):
            run("sp", e)


D = 1024
NTOK = 2048
TT = 512
NTT = NTOK // TT
EPS = 1e-6


class Ctx:
    def __init__(self, nc, st, P):
        self.nc, self.st, self.P = nc, st, P
        self.ps = []
        for i in range(8):
            t = st.enter_context(nc.psum_tensor("ps%d" % i, [128, 512], F32))
            self.ps.append((t, Buf("ps%d" % i, excl=True)))
        self.psi = 0
        self.epsc, self.epsb = self.sb("epsc", [128, 2], F32)
        P.op("dve", "memset", PK(self.epsc[:, 0:1], EPS), writes=[self.epsb])
        P.op("dve", "memset", PK(self.epsc[:, 1:2], 64e-5), writes=[self.epsb])
        self.wbufs = []
        self.wi = 0
        self.dmaq = 0

    def sb(self, name, shape, dt):
        t = self.st.enter_context(self.nc.sbuf_tensor(name, shape, dt))
        return t, Buf(name)

    def psum(self):
        r = self.ps[self.psi % 8]
        self.psi += 1
        return r

    def init_w(self, n, cols):
        for i in range(n):
            self.wbufs.append(self.sb("wb%d" % i, [128, cols], BF16))

    def wbuf(self):
        r = self.wbufs[self.wi % len(self.wbufs)]
        self.wi += 1
        return r

    def load_w(self, src_ap, kc, ncols):
        t, b = self.wbuf()
        view = t[:, 0:kc * ncols].rearrange("p (c n) -> p c n", c=kc)
        src = src_ap.rearrange("(c p) n -> p c n", p=128)
        self.P.op("pool", "dma_start", PK(out=view, in_=src), writes=[b], is_dma=True)
        return view, b

    def dma(self, out, in_, R=(), W=(), is_out=False, q=None):
        if q is None:
            q = "sp"
        return self.P.op(q, "dma_start", PK(out=out, in_=in_), reads=R, writes=W, is_dma=True, is_out=is_out)


def rmsnorm_fm(C, xT, xb, gcol, outT, outb, tt, scr, dim_chunks=8, extra_scale=1.0):
    P = C.P
    sq, sqb = scr["sq"]
    rs, rsb = scr["rs"]
    ones, onesb = scr["ones"]
    sl = slice(tt * TT, (tt + 1) * TT)
    pt, pb = C.psum()
    for c in range(dim_chunks):
        P.op("act", "activation", PK(out=sq[:, c, :], in_=xT[:, c, sl], func=AF.Square), reads=[xb], writes=[sqb])
    for c in range(dim_chunks):
        P.op("pe", "matmul", PK(pt[:, :], ones[:, :], sq[:, c, :], start=(c == 0), stop=(c == dim_chunks - 1)),
             reads=[sqb, onesb], writes=[pb])
    dd = dim_chunks * 128
    P.op("dve", "tensor_scalar", PK(out=rs[:, :], in0=pt[:, :], scalar1=float(dd * EPS), scalar2=-0.5, op0=ALU.add, op1=ALU.pow),
         reads=[pb], writes=[rsb])
    sc = float(np.sqrt(dd) * extra_scale)
    for c in range(dim_chunks):
        eng = "dve"
        P.op(eng, "scalar_tensor_tensor", PK(out=outT[:, c, sl], in0=xT[:, c, sl], scalar=gcol[:, c:c + 1], in1=rs[:, :],
                                                       op0=ALU.mult, op1=ALU.mult),
             reads=[xb, rsb], writes=[outb])
    return sc


def linear_fm(C, W_dram, kc, xin, xinb, n_out, evac, tts=range(NTT), col0=0, blk=512, mcols=128, xinbs=None):
    P = C.P
    nblk = (n_out + blk - 1) // blk
    oc = 0
    for bi in range(nblk):
        c0 = col0 + bi * blk
        w = min(blk, n_out - bi * blk)
        wv, wb = C.load_w(W_dram[:, c0:c0 + w], kc, w)
        for j in range(0, w, mcols):
            m = min(mcols, w - j)
            for tt in tts:
                pt, pb = C.psum()
                sl = slice(tt * TT, (tt + 1) * TT)
                for k in range(kc):
                    P.op("pe", "matmul", PK(pt[0:m, :], wv[:, k, j:j + m], xin[:, k, sl],
                                                                              start=(k == 0), stop=(k == kc - 1)),
                         reads=[wb, (xinbs[tt] if xinbs is not None else xinb)], writes=[pb])
                evac(oc, tt, pt, pb, m)
            oc += 1


def rstd_from(C, rs_ap, ps_ap, pb, rsb, eps=EPS):
    np_ = rs_ap.shape[0]
    C.P.op("act", "activation", PK(out=rs_ap, in_=ps_ap, func=AF.Sqrt, bias=C.epsc[0:np_, 0:1] if eps == EPS else C.epsc[0:np_, 1:2]),
           reads=[pb, C.epsb], writes=[rsb])
    C.P.op("dve", "reciprocal", PK(out=rs_ap, in_=rs_ap), reads=[rsb], writes=[rsb])


def norm_fm(C, xin, xb, n, dim_chunks, onesm, onesb, gcol, out, outb, scr, sq_from_psum=False):
    P = C.P
    sq, sqb = scr["sq"]
    rs, rsb = scr["rs"]
    pt, pb = C.psum()
    for c in range(dim_chunks):
        P.op("act", "activation", PK(out=sq[:, c, 0:n], in_=xin(c), func=AF.Square), reads=xb, writes=[sqb])
    for c in range(dim_chunks):
        P.op("pe", "matmul", PK(pt[:, 0:n], onesm, sq[:, c, 0:n], start=(c == 0), stop=(c == dim_chunks - 1)),
             reads=[sqb, onesb], writes=[pb])
    rstd_from(C, rs[:, 0:n], pt[:, 0:n], pb, rsb)
    for c in range(dim_chunks):
        eng = "dve"
        P.op(eng, "scalar_tensor_tensor", PK(out=out(c), in0=xin(c), scalar=gcol(c), in1=rs[:, 0:n],
                                                       op0=ALU.mult, op1=ALU.mult),
             reads=list(xb) + [rsb] + ([C.vb] if getattr(C, 'vb', None) is not None else []), writes=outb)


def mem_kv(C, memT_d, wkv_d, V, vb, col_nm, col_kg, cst, cstb, scr, name):
    P = C.P
    mT, mTb = C.sb(name + "_mT", [128, 8, 256], BF16)
    mn, mnb = C.sb(name + "_mn", [128, 8, 256], BF16)
    kf, kfb = C.sb(name + "_kf", [128, 8, 256], BF16)
    kn, knb = C.sb(name + "_kn", [128, 8, 256], BF16)
    vt, vtb = C.sb(name + "_vt", [128, 2, 1024], BF16)
    P.op("pool", "dma_start", PK(out=mT[:, :, :], in_=memT_d.rearrange("(c p) n -> p c n", p=128)), writes=[mTb], is_dma=True)
    norm_fm(C, lambda c: mT[:, c, :], [mTb], 256, 8, cst[:, 0, :], cstb, lambda c: V[:, col_nm + c:col_nm + c + 1],
            lambda c: mn[:, c, :], [mnb], scr)
    for half in range(2):
        wv, wb = C.load_w(wkv_d[:, half * 512:(half + 1) * 512], 8, 512)
        for j in range(4):
            oc = half * 4 + j
            pt, pb = C.psum()
            for k in range(8):
                P.op("pe", "matmul", PK(pt[:, 0:256], wv[:, k, j * 128:(j + 1) * 128], mn[:, k, :],
                                                                      start=(k == 0), stop=(k == 7)), reads=[wb, mnb], writes=[pb])
            P.op("dve", "tensor_copy", PK(out=kf[:, oc, :], in_=pt[:, 0:256]), reads=[pb], writes=[kfb])
    for hh in range(4):
        norm_fm(C, lambda c, hh=hh: kf[:, hh * 2 + c, :], [kfb], 256, 2, cst[:, 1, :], cstb,
                lambda c: V[:, col_kg + c:col_kg + c + 1], lambda c, hh=hh: kn[:, hh * 2 + c, :], [knb], scr)
    for half in range(2):
        wv, wb = C.load_w(wkv_d[:, 1024 + half * 512:1024 + (half + 1) * 512], 8, 512)
        for j in range(2):
            pt, pb = C.psum()
            for k in range(8):
                P.op("pe", "matmul", PK(pt[:, :], mn[:, k, j * 128:(j + 1) * 128], wv[:, k, :],
                                                                      start=(k == 0), stop=(k == 7)), reads=[wb, mnb], writes=[pb])
            P.op("act", "activation", PK(out=vt[:, j, half * 512:(half + 1) * 512], in_=pt[:, :], func=AF.Copy),
                 reads=[pb], writes=[vtb])
    return (kn, knb), (vt, vtb)


def mem_xattn(C, hT, hTb, bufA, bufAb, bufQ, bufQb, V, vb, col_nx, col_qg, wq_d, wo_d, kn, knb, vt, vtb, cst, cstb, scr, stop=0):
    P = C.P
    for tt in range(NTT):
        sl = slice(tt * TT, (tt + 1) * TT)
        norm_fm(C, lambda c: hT[:, c, sl], [hTb[tt]], TT, 8, cst[:, 0, :], cstb, lambda c: V[:, col_nx + c:col_nx + c + 1],
                lambda c: bufA[:, c, sl], [bufAb[tt]], scr)
    if stop == 16:
        return
    sqq, sqqb = scr["sqq"]
    rs, rsb = scr["rs"]

    def evac_q(oc, tt, pt, pb, m):
        sl = slice(tt * TT, (tt + 1) * TT)
        P.op("act", "activation", PK(out=sqq[:, oc % 2, tt, :], in_=pt[:, :], func=AF.Square), reads=[pb], writes=[sqqb[tt]])
        P.op("dve", "tensor_copy", PK(out=bufQ[:, oc, sl], in_=pt[:, :]), reads=[pb], writes=[bufQb[tt]])
        import os
        V17 = os.environ.get('V17', '')
        if oc % 2 == 1 and V17 != 'a':
            p2, p2b = C.psum()
            for c in range(2):
                P.op("pe", "matmul", PK(p2[:, :], cst[:, 1, :], sqq[:, c, tt, :], start=(c == 0), stop=(c == 1)),
                     reads=[sqqb[tt], cstb], writes=[p2b])
            rstd_from(C, rs[:, :], p2[:, :], p2b, rsb)
            for c in range(2):
                o2 = oc - 1 + c
                P.op("dve", "tensor_tensor", PK(out=bufQ[:, o2, sl], in0=bufQ[:, o2, sl], in1=rs[:, :], op=ALU.mult),
                     reads=[bufQb[tt], rsb], writes=[bufQb[tt]])
                P.op("act", "activation", PK(out=bufQ[:, o2, sl], in_=bufQ[:, o2, sl], func=AF.Copy, scale=V[:, col_qg + c:col_qg + c + 1]),
                     reads=[bufQb[tt], vb], writes=[bufQb[tt]])
    linear_fm(C, wq_d, 8, bufA, None, 1024, evac_q, xinbs=bufAb)
    if stop == 17:
        return
    et, etb = scr["et"]
    rd, rdb = scr["rd"]
    for hh in range(4):
        for tt in range(NTT):
            sl = slice(tt * TT, (tt + 1) * TT)
            pden, pdenb = C.psum()
            pn = [C.psum(), C.psum()]
            for j in range(2):
                psc, pscb = C.psum()
                for dc in range(2):
                    P.op("pe", "matmul", PK(psc[:, :], kn[:, hh * 2 + dc, j * 128:(j + 1) * 128], bufQ[:, hh * 2 + dc, sl],
                                                                        start=(dc == 0), stop=(dc == 1)), reads=[knb, bufQb[tt]], writes=[pscb])
                P.op("act", "activation", PK(out=et[:, j, :], in_=psc[:, :], func=AF.Exp, scale=1.0 / 16.0),
                     reads=[pscb], writes=[etb[j]])
                P.op("pe", "matmul", PK(pden[:, :], cst[:, 4, :], et[:, j, :], start=(j == 0), stop=(j == 1)),
                     reads=[etb[j], cstb], writes=[pdenb])
                for c in range(2):
                    P.op("pe", "matmul", PK(pn[c][0][:, :], vt[:, j, hh * 256 + c * 128:hh * 256 + (c + 1) * 128], et[:, j, :],
                                                            start=(j == 0), stop=(j == 1)), reads=[etb[j], vtb], writes=[pn[c][1]])
            P.op("dve", "reciprocal", PK(out=rd[:, :], in_=pden[:, :]), reads=[pdenb], writes=[rdb])
            for c in range(2):
                P.op("dve", "tensor_tensor", PK(out=bufA[:, hh * 2 + c, sl], in0=pn[c][0][:, :], in1=rd[:, :], op=ALU.mult),
                     reads=[pn[c][1], rdb], writes=[bufAb[tt]])

    if stop == 18:
        return

    def evac_o(oc, tt, pt, pb, m):
        sl = slice(tt * TT, (tt + 1) * TT)
        P.op("dve", "tensor_tensor", PK(out=hT[:, oc, sl], in0=pt[:, :], in1=hT[:, oc, sl], op=ALU.add), reads=[pb, hTb[tt]], writes=[hTb[tt]])
    linear_fm(C, wo_d, 8, bufA, None, 1024, evac_o, xinbs=bufAb)


def swiglu_ffn(C, hT, hTb, bufA, bufAb, bufH, bufHb, V, vb, col_nf, wg_d, wu_d, wd_d, dff, cst, cstb, scr, gate_bc=None):
    P = C.P
    if col_nf is not None:
        for tt in range(NTT):
            sl = slice(tt * TT, (tt + 1) * TT)
            norm_fm(C, lambda c: hT[:, c, sl], [hTb[tt]], TT, 8, cst[:, 0, :], cstb, lambda c: V[:, col_nf + c:col_nf + c + 1],
                    lambda c: bufA[:, c, sl], [bufAb[tt]], scr)
    sg, sgb = scr["sg"]
    nch = dff // 128
    GRP = bufH.shape[1]
    g0 = 0
    while g0 < nch:
        gn = min(GRP, nch - g0)
        c = 0
        while c < gn:
            cb = min(4, gn - c)
            col = (g0 + c) * 128
            wgv, wgb = C.load_w(wg_d[:, col:col + cb * 128], 8, cb * 128)
            wuv, wub = C.load_w(wu_d[:, col:col + cb * 128], 8, cb * 128)
            for j in range(cb):
                for tt in range(NTT):
                    sl = slice(tt * TT, (tt + 1) * TT)
                    pg, pgb = C.psum()
                    pu, pub = C.psum()
                    for k in range(8):
                        P.op("pe", "matmul", PK(pg[:, :], wgv[:, k, j * 128:(j + 1) * 128], bufA[:, k, sl],
                                                                                       start=(k == 0), stop=(k == 7)), reads=[wgb, bufAb[tt]], writes=[pgb])
                    for k in range(8):
                        P.op("pe", "matmul", PK(pu[:, :], wuv[:, k, j * 128:(j + 1) * 128], bufA[:, k, sl],
                                                                                       start=(k == 0), stop=(k == 7)), reads=[wub, bufAb[tt]], writes=[pub])
                    P.op("act", "activation", PK(out=sg[:, :], in_=pg[:, :], func=AF.Silu), reads=[pgb], writes=[sgb])
                    if gate_bc is not None:
                        gt, gtb = gate_bc
                        P.op("dve", "tensor_tensor", PK(out=sg[:, :], in0=sg[:, :], in1=gt[:, sl], op=ALU.mult),
                             reads=[sgb, gtb[tt]], writes=[sgb])
                    P.op("dve", "tensor_tensor", PK(out=bufH[:, c + j, sl], in0=pu[:, :], in1=sg[:, :], op=ALU.mult),
                         reads=[pub, sgb], writes=[bufHb[tt]])
            c += cb
        wblks = []
        r = 0
        while r < gn:
            rb = min(4, gn - r)
            row = (g0 + r) * 128
            wblks.append((r, rb, C.load_w(wd_d[row:row + rb * 128, :], rb, 1024)))
            r += rb
        for oc in range(8):
            for tt in range(NTT):
                sl = slice(tt * TT, (tt + 1) * TT)
                pt, pb = C.psum()
                first = True
                for (r, rb, (wv, wb)) in wblks:
                    for q in range(rb):
                        last = (r + q == gn - 1)
                        P.op("pe", "matmul", PK(pt[:, :], wv[:, q, oc * 128:(oc + 1) * 128], bufH[:, r + q, sl], start=first, stop=last),
                             reads=[wb, bufHb[tt]], writes=[pb])
                        first = False
                P.op("dve", "tensor_tensor", PK(out=hT[:, oc, sl], in0=pt[:, :], in1=hT[:, oc, sl], op=ALU.add),
                     reads=[pb, hTb[tt]], writes=[hTb[tt]])
        g0 += gn


VB = dict(b_glu=0, n_xattn=4, n_mem=12, q_gain=20, k_gain=22, n_ffn=24, n_mix1=32, da_qg=40, da_kg=41)
NVB = 42


def build_B():
    nc = bass.Bass("TRN2", target_bir_lowering=False)
    dr = lambda n, s, k="ExternalInput", dt=F32: nc.dram_tensor(n, s, dt, kind=k).ap()
    xT_d = dr("xT", [D, NTOK]); ymT_d = dr("ymT", [D, NTOK]); memT_d = dr("memT", [D, 256])
    vec_d = dr("vecs", [128, NVB]); cst_d = dr("cst", [128, 5 * 128])
    wglu_d = dr("w_glu", [512, 512]); wout_d = dr("w_out", [D, D])
    wq_d = dr("w_q", [D, D]); wkv_d = dr("w_kv", [D, 2 * D]); wo_d = dr("w_o", [D, D])
    wg_d = dr("ff_g", [D, 2816]); wu_d = dr("ff_u", [D, 2816]); wd_d = dr("ff_d", [2816, D])
    wqkv_d = dr("w_qkv", [D, 3 * D])
    h1T_d = dr("h1T", [D, NTOK], "ExternalOutput")
    qT_d = dr("qT", [16, 64, NTOK], "ExternalOutput")
    kT_d = dr("kT", [16, 64, NTOK], "ExternalOutput")
    vT_d = dr("vT", [D, NTOK], "ExternalOutput")
    with ExitStack() as st:
        P = Prog(nc)
        C = Ctx(nc, st, P)
        C.init_w(4, 4096)
        hT, _ = C.sb("hT", [128, 8, NTOK], F32); hTb = [Buf("hT%d" % i) for i in range(NTT)]
        bufA, _ = C.sb("bufA", [128, 8, NTOK], BF16); bufAb = [Buf("bA%d" % i) for i in range(NTT)]
        bufH, _ = C.sb("bufH", [128, 8, NTOK], BF16); bufHb = [Buf("bH%d" % i) for i in range(NTT)]
        V, vb = C.sb("V_sb", [128, NVB], F32); C.vb = vb
        cst3, cstb = C.sb("cst_sb", [128, 5, 128], BF16)
        scr = dict(sq=C.sb("sq", [128, 8, 512], BF16), rs=C.sb("rs", [128, 512], F32), sg=C.sb("sg", [128, 512], F32),
                   rd=C.sb("rd", [128, 512], F32))
        sqq, _ = C.sb("sqq", [128, 2, NTT, 512], BF16)
        scr["sqq"] = (sqq, [Buf("sqq%d" % i) for i in range(NTT)])
        et, _ = C.sb("et", [128, 2, 512], BF16)
        scr["et"] = (et, [Buf("et0"), Buf("et1")])
        C.dma(V[:, :], vec_d[:, :], W=[vb])
        P.op("pool", "dma_start", PK(out=cst3[:, :, :], in_=cst_d.rearrange("p (a b) -> p a b", a=5)), writes=[cstb], is_dma=True)
        for tt in range(NTT):
            sl = slice(tt * TT, (tt + 1) * TT)
            C.dma(hT[:, :, sl], xT_d[:, sl].rearrange("(c p) n -> p c n", p=128), W=[hTb[tt]])
            P.op("pool", "dma_start", PK(out=bufA[:, :, sl], in_=ymT_d[:, sl].rearrange("(c p) n -> p c n", p=128)),
                 writes=[bufAb[tt]], is_dma=True)
        import os
        STAGE = int(os.environ.get("STAGE", "9"))
        def finish():
            for tt in range(NTT):
                sl = slice(tt * TT, (tt + 1) * TT)
                C.dma(h1T_d[:, sl].rearrange("(c p) n -> p c n", p=128), hT[:, :, sl], R=[hTb[tt]], is_out=True)
            P.emit(st)
            print("B stats", P.stats)
            return nc
        if STAGE == 0:
            return finish()
        wv, wb = C.load_w(wglu_d[:, :], 4, 512)
        sg, sgb = scr["sg"]
        for tt in range(NTT):
            sl = slice(tt * TT, (tt + 1) * TT)
            pts = []
            for oc in range(4):
                pt, pb = C.psum()
                for k in range(4):
                    P.op("pe", "matmul", PK(pt[:, :], wv[:, k, oc * 128:(oc + 1) * 128], bufA[:, 4 + k, sl],
                                                                            start=(k == 0), stop=(k == 3)), reads=[wb, bufAb[tt]], writes=[pb])
                pts.append((pt, pb))
            for oc in range(4):
                pt, pb = pts[oc]
                P.op("act", "activation", PK(out=sg[:, :], in_=pt[:, :], func=AF.Sigmoid,
                                                                bias=V[:, VB["b_glu"] + oc:VB["b_glu"] + oc + 1]), reads=[pb, vb], writes=[sgb])
                P.op("dve", "tensor_tensor", PK(out=bufA[:, 4 + oc, sl], in0=bufA[:, 4 + oc, sl], in1=sg[:, :], op=ALU.mult),
                     reads=[sgb, bufAb[tt]], writes=[bufAb[tt]])

        def evac_res(oc, tt, pt, pb, m):
            sl = slice(tt * TT, (tt + 1) * TT)
            P.op("dve", "tensor_tensor", PK(out=hT[:, oc, sl], in0=pt[:, :], in1=hT[:, oc, sl], op=ALU.add), reads=[pb, hTb[tt]], writes=[hTb[tt]])
        linear_fm(C, wout_d, 8, bufA, None, 1024, evac_res, xinbs=bufAb)
        import os
        STAGE = int(os.environ.get("STAGE", "9"))
        def finish():
            for tt in range(NTT):
                sl = slice(tt * TT, (tt + 1) * TT)
                C.dma(h1T_d[:, sl].rearrange("(c p) n -> p c n", p=128), hT[:, :, sl], R=[hTb[tt]], is_out=True)
            P.emit(st)
            return nc
        if STAGE == 1:
            return finish()
        (kn, knb), (vt, vtb) = mem_kv(C, memT_d, wkv_d, V, vb, VB["n_mem"], VB["k_gain"], cst3, cstb, scr, "m0")
        if STAGE == 15:
            return finish()
        mem_xattn(C, hT, hTb, bufA, bufAb, bufH, bufHb, V, vb, VB["n_xattn"], VB["q_gain"], wq_d, wo_d, kn, knb, vt, vtb, cst3, cstb, scr, stop=STAGE)
        if STAGE in (2, 16, 17, 18):
            return finish()
        swiglu_ffn(C, hT, hTb, bufA, bufAb, bufH, bufHb, V, vb, VB["n_ffn"], wg_d, wu_d, wd_d, 2816, cst3, cstb, scr)
        for tt in range(NTT):
            sl = slice(tt * TT, (tt + 1) * TT)
            C.dma(h1T_d[:, sl].rearrange("(c p) n -> p c n", p=128), hT[:, :, sl], R=[hTb[tt]], is_out=True)
        for tt in range(NTT):
            sl = slice(tt * TT, (tt + 1) * TT)
            norm_fm(C, lambda c: hT[:, c, sl], [hTb[tt]], TT, 8, cst3[:, 0, :], cstb, lambda c: V[:, VB["n_mix1"] + c:VB["n_mix1"] + c + 1],
                    lambda c: bufA[:, c, sl], [bufAb[tt]], scr)
        qs, qsb = scr["sg"]
        qo, qob = scr["rd"]
        sq1, sq1b = C.sb("sq1", [64, 512], BF16)
        rs, rsb = scr["rs"]
        for which, (dst, gcol, scl) in enumerate(((qT_d, VB["da_qg"], 0.125), (kT_d, VB["da_kg"], 1.0))):
            def evac_qk(oc, tt, pt, pb, m, dst=dst, gcol=gcol, scl=scl):
                sl = slice(tt * TT, (tt + 1) * TT)
                P.op("act", "activation", PK(out=sq1[:, :], in_=pt[0:64, :], func=AF.Square), reads=[pb], writes=[sq1b])
                p2, p2b = C.psum()
                P.op("pe", "matmul", PK(p2[0:64, :], cst3[0:64, 2, 0:64], sq1[:, :], start=True, stop=True), reads=[sq1b, cstb], writes=[p2b])
                rstd_from(C, rs[0:64, :], p2[0:64, :], p2b, rsb)
                P.op("dve", "scalar_tensor_tensor", PK(out=qs[0:64, :], in0=pt[0:64, :], scalar=V[0:64, gcol:gcol + 1], in1=rs[0:64, :],
                                                             op0=ALU.mult, op1=ALU.mult), reads=[pb, rsb, vb], writes=[qsb])
                P.op("act", "activation", PK(out=qo[0:64, :], in_=qs[0:64, :], func=AF.Copy, scale=float(scl)), reads=[qsb], writes=[qob])
                C.dma(dst[oc, :, sl], qo[0:64, :], R=[qob], is_out=True)
            linear_fm(C, wqkv_d, 8, bufA, None, 1024, evac_qk, col0=which * 1024, mcols=64, xinbs=bufAb)
        vo, vob = scr["sg"]

        def evac_v(oc, tt, pt, pb, m):
            sl = slice(tt * TT, (tt + 1) * TT)
            P.op("act", "activation", PK(out=vo[:, :], in_=pt[:, :], func=AF.Copy), reads=[pb], writes=[vob])
            C.dma(vT_d[oc * 128:(oc + 1) * 128, sl], vo[:, :], R=[vob], is_out=True)
        linear_fm(C, wqkv_d, 8, bufA, None, 1024, evac_v, col0=2048, xinbs=bufAb)
        P.emit(st)
        print("B stats", P.stats)
    return nc


def cst_table():
    c = np.zeros((128, 5, 128), np.float32)
    c[:, 0, :] = 1.0 / 1024
    c[:, 1, :] = 1.0 / 256
    c[0:64, 2, 0:64] = 1.0 / 64
    c[64:128, 2, 64:128] = 1.0 / 64
    c[:, 3, :] = 1.0 / 128
    c[:, 4, :] = 1.0
    return c.reshape(128, 640)


def col(v):
    v = np.asarray(v, np.float32).reshape(-1)
    if v.size < 128:
        o = np.zeros((128, 1), np.float32); o[:v.size, 0] = v
        return o
    return np.ascontiguousarray(v.reshape(-1, 128).T)


def vecs_B(inp):
    V = np.zeros((128, NVB), np.float32)
    def put(name, arr):
        a = col(arr); V[:, VB[name]:VB[name] + a.shape[1]] = a
    put("b_glu", inp["s5_b_glu"][0]); put("n_xattn", inp["norm_xattn"][0]); put("n_mem", inp["norm_mem"][0])
    put("q_gain", inp["xa_q_gain"][0]); put("k_gain", inp["xa_k_gain"][0]); put("n_ffn", inp["norm_ffn"][0])
    put("n_mix1", inp["norm_mix"][1])
    put("da_qg", np.tile(np.asarray(inp["da_q_gain"][0]), 2)); put("da_kg", np.tile(np.asarray(inp["da_k_gain"][0]), 2))
    return V


def tok_index(p):
    return np.concatenate([np.arange((2 * m + p) * 512, (2 * m + p + 1) * 512) for m in range(4)])


VC = dict(n_xattn=0, n_mem=8, q_gain=16, k_gain=18, n_ffn=20, b_router=28)
NVC = 36


def build_C2():
    nc = bass.Bass("TRN2", target_bir_lowering=False)
    dr = lambda n, s, k="ExternalInput", dt=F32: nc.dram_tensor(n, s, dt, kind=k).ap()
    h1T_d = dr("h1T", [D, NTOK]); atT_d = dr("attnT", [D, NTOK]); memT_d = dr("memT", [D, 256])
    vec_d = dr("vecs", [128, NVC]); cst_d = dr("cst", [128, 5 * 128]); c32_d = dr("c32", [128, 256])
    wdo_d = dr("da_w_o", [D, D])
    wq_d = dr("w_q", [D, D]); wkv_d = dr("w_kv", [D, 2 * D]); wo_d = dr("w_o", [D, D])
    wr_d = dr("w_router", [D, 8])
    wg_d = dr("moe_g", [8, D, 3584]); wu_d = dr("moe_u", [8, D, 3584]); wd_d = dr("moe_d", [8, 3584, D])
    outT_d = dr("outT", [D, NTOK], "ExternalOutput")
    with ExitStack() as st:
        P = Prog(nc)
        C = Ctx(nc, st, P)
        C.init_w(3, 4096)
        hT, _ = C.sb("hT", [128, 8, NTOK], F32); hTb = [Buf("hT%d" % i) for i in range(NTT)]
        bufA, _ = C.sb("bufA", [128, 8, NTOK], BF16); bufAb = [Buf("bA%d" % i) for i in range(NTT)]
        bufH, _ = C.sb("bufH", [128, 8, NTOK], BF16); bufHb = [Buf("bH%d" % i) for i in range(NTT)]
        V, vb = C.sb("V_sb", [128, NVC], F32); C.vb = vb
        cst3, cstb = C.sb("cst_sb", [128, 5, 128], BF16)
        c32, c32b = C.sb("c32_sb", [128, 2, 128], F32)
        scr = dict(sq=C.sb("sq", [128, 8, 512], BF16), rs=C.sb("rs", [128, 512], F32), sg=C.sb("sg", [128, 512], F32),
                   rd=C.sb("rd", [128, 512], F32))
        sqq, _ = C.sb("sqq", [128, 2, NTT, 512], BF16)
        scr["sqq"] = (sqq, [Buf("sqq%d" % i) for i in range(NTT)])
        et, _ = C.sb("et", [128, 2, 512], BF16)
        scr["et"] = (et, [Buf("et0"), Buf("et1")])
        C.dma(V[:, :], vec_d[:, :], W=[vb])
        C.dma(c32[:, :, :], c32_d.rearrange("p (a b) -> p a b", a=2), W=[c32b])
        P.op("pool", "dma_start", PK(out=cst3[:, :, :], in_=cst_d.rearrange("p (a b) -> p a b", a=5)), writes=[cstb], is_dma=True)
        for tt in range(NTT):
            sl = slice(tt * TT, (tt + 1) * TT)
            C.dma(hT[:, :, sl], h1T_d[:, sl].rearrange("(c p) n -> p c n", p=128), W=[hTb[tt]])
            P.op("pool", "dma_start", PK(out=bufA[:, :, sl], in_=atT_d[:, sl].rearrange("(c p) n -> p c n", p=128)),
                 writes=[bufAb[tt]], is_dma=True)

        def evac_res(oc, tt, pt, pb, m):
            sl = slice(tt * TT, (tt + 1) * TT)
            P.op("dve", "tensor_tensor", PK(out=hT[:, oc, sl], in0=pt[:, :], in1=hT[:, oc, sl], op=ALU.add), reads=[pb, hTb[tt]], writes=[hTb[tt]])
        linear_fm(C, wdo_d, 8, bufA, None, 1024, evac_res, xinbs=bufAb)
        (kn, knb), (vt, vtb) = mem_kv(C, memT_d, wkv_d, V, vb, VC["n_mem"], VC["k_gain"], cst3, cstb, scr, "m1")
        mem_xattn(C, hT, hTb, bufA, bufAb, bufH, bufHb, V, vb, VC["n_xattn"], VC["q_gain"], wq_d, wo_d, kn, knb, vt, vtb, cst3, cstb, scr)
        for tt in range(NTT):
            sl = slice(tt * TT, (tt + 1) * TT)
            norm_fm(C, lambda c: hT[:, c, sl], [hTb[tt]], TT, 8, cst3[:, 0, :], cstb, lambda c: V[:, VC["n_ffn"] + c:VC["n_ffn"] + c + 1],
                    lambda c: bufA[:, c, sl], [bufAb[tt]], scr)
        hn32, hn32b = C.sb("hn32", [128, 8, 128], F32)
        wr, wrb = C.sb("wr", [128, 8, 8], F32)
        G, Gb = C.sb("G", [128, 16, 8], F32)
        lg, lgb = C.sb("lg", [128, 8], F32)
        mx, mxb = C.sb("mx", [128, 8], F32)
        sm, smb = C.sb("sm", [128, 4], F32)
        C.dma(wr[:, :, :], wr_d.rearrange("(c p) n -> p c n", p=128), W=[wrb])
        for s in range(16):
            tt = s // 4
            sl = slice(s * 128, (s + 1) * 128)
            norm_fm(C, lambda c: hT[:, c, sl], [hTb[tt]], 128, 8, cst3[:, 0, :], cstb, lambda c: V[:, VC["n_ffn"] + c:VC["n_ffn"] + c + 1],
                    lambda c: hn32[:, c, :], [hn32b], scr)
            pt, pb = C.psum()
            for k in range(8):
                P.op("pe", "matmul", PK(pt[:, 0:8], hn32[:, k, :], wr[:, k, :], start=(k == 0), stop=(k == 7)), reads=[hn32b, wrb], writes=[pb])
            P.op("dve", "tensor_tensor", PK(out=lg[:, :], in0=pt[:, 0:8], in1=V[:, VC["b_router"]:VC["b_router"] + 8], op=ALU.add),
                 reads=[pb, vb], writes=[lgb])
            P.op("dve", "max", PK(out=mx[:, :], in_=lg[:, :]), reads=[lgb], writes=[mxb])
            P.op("dve", "tensor_scalar", PK(out=sm[:, 0:1], in0=mx[:, 0:1], scalar1=-1.0, scalar2=None, op0=ALU.mult), reads=[mxb], writes=[smb])
            ex, exb = scr["sg"]
            P.op("act", "activation", PK(out=ex[:, 0:8], in_=lg[:, :], func=AF.Exp, bias=sm[:, 0:1]), reads=[lgb, smb], writes=[exb])
            P.op("dve", "tensor_scalar", PK(out=lg[:, :], in0=lg[:, :], scalar1=mx[:, 1:2], scalar2=None, op0=ALU.is_ge), reads=[lgb, mxb], writes=[lgb])
            P.op("dve", "tensor_tensor", PK(out=ex[:, 0:8], in0=ex[:, 0:8], in1=lg[:, :], op=ALU.mult), reads=[exb, lgb], writes=[exb])
            P.op("dve", "reduce_sum", PK(out=sm[:, 1:2], in_=ex[:, 0:8], axis=AX.X), reads=[exb], writes=[smb])
            P.op("dve", "reciprocal", PK(out=sm[:, 2:3], in_=sm[:, 1:2]), reads=[smb], writes=[smb])
            P.op("dve", "tensor_scalar", PK(out=G[:, s, :], in0=ex[:, 0:8], scalar1=sm[:, 2:3], scalar2=None, op0=ALU.mult), reads=[exb, smb], writes=[Gb])
        gbc, _ = C.sb("gbc", [128, NTOK], BF16); gbcb = [Buf("gbc%d" % i) for i in range(NTT)]
        dg, dgb = C.sb("dg", [128, 128], F32)
        for e_ in range(8):
            for tt in range(NTT):
                pt, pb = C.psum()
                for q in range(4):
                    s = tt * 4 + q
                    P.op("dve", "tensor_scalar", PK(out=dg[:, :], in0=c32[:, 0, :], scalar1=G[:, s, e_:e_ + 1], scalar2=None, op0=ALU.mult),
                         reads=[c32b, Gb], writes=[dgb])
                    P.op("pe", "matmul", PK(pt[:, q * 128:(q + 1) * 128], c32[:, 1, :], dg[:, :], start=True, stop=True), reads=[dgb, c32b], writes=[pb])
                P.op("act", "activation", PK(out=gbc[:, tt * TT:(tt + 1) * TT], in_=pt[:, :], func=AF.Copy), reads=[pb], writes=[gbcb[tt]])
            swiglu_ffn(C, hT, hTb, bufA, bufAb, bufH, bufHb, V, vb, None, wg_d[e_], wu_d[e_], wd_d[e_], 3584, cst3, cstb, scr, gate_bc=(gbc, gbcb))
        for tt in range(NTT):
            sl = slice(tt * TT, (tt + 1) * TT)
            C.dma(outT_d[:, sl].rearrange("(c p) n -> p c n", p=128), hT[:, :, sl], R=[hTb[tt]], is_out=True)
        P.emit(st)
    return nc


def c32_table():
    c = np.zeros((128, 2, 128), np.float32)
    c[:, 0, :] = np.eye(128, dtype=np.float32)
    c[:, 1, :] = 1.0
    return c.reshape(128, 256)


def vecs_C(inp):
    V = np.zeros((128, NVC), np.float32)
    def put(name, arr):
        a = col(arr); V[:, VC[name]:VC[name] + a.shape[1]] = a
    put("n_xattn", inp["norm_xattn"][1]); put("n_mem", inp["norm_mem"][1])
    put("q_gain", inp["xa_q_gain"][1]); put("k_gain", inp["xa_k_gain"][1]); put("n_ffn", inp["norm_ffn"][1])
    V[:, VC["b_router"]:VC["b_router"] + 8] = np.asarray(inp["moe_b_router"][0], np.float32)[None, :]
    return V


LAMBDA_INIT = 0.8 - 0.6 * float(np.exp(-0.3 * 1))


def build_C1():
    nc = bass.Bass("TRN2", target_bir_lowering=False)
    dr = lambda n, s, k="ExternalInput", dt=F32: nc.dram_tensor(n, s, dt, kind=k).ap()
    qa_d = dr("qaug", [16, 68, NTOK]); ka_d = dr("kaug", [16, 68, 4096]); vf_d = dr("vfull", [4096, D])
    bias_d = dr("biasT", [128, 8 * 512]); lamb_d = dr("lamb", [128, 4 * 64]); sgc_d = dr("sgc", [128, 1]); cst_d = dr("cst", [128, 5 * 128])
    at_d = dr("attnT", [D, NTOK], "ExternalOutput")
    with ExitStack() as st:
        P = Prog(nc)
        C = Ctx(nc, st, P)
        cst3, cstb = C.sb("cst_sb", [128, 5, 128], BF16)
        P.op("pool", "dma_start", PK(out=cst3[:, :, :], in_=cst_d.rearrange("p (a b) -> p a b", a=5)), writes=[cstb], is_dma=True)
        biasT, biasb = C.sb("bias_sb", [128, 8, 512], F32)
        C.dma(biasT[:, :, :], bias_d.rearrange("p (a b) -> p a b", a=8), W=[biasb])
        lamb, lambb = C.sb("lamb_sb", [128, 4, 64], F32)
        C.dma(lamb[:, :, :], lamb_d.rearrange("p (a b) -> p a b", a=4), W=[lambb])
        sgc, sgcb = C.sb("sgc_sb", [128, 1], F32)
        C.dma(sgc[:, :], sgc_d[:, :], W=[sgcb])
        scr = dict(sq=C.sb("sq", [128, 1, 512], BF16), rs=C.sb("rs", [128, 512], F32))
        lt, ltb = C.sb("lt", [128, 2, 64], F32)
        lc, lcb = C.sb("lc", [128, 4], F32)
        for i in range(2):
            P.op("dve", "tensor_tensor", PK(out=lt[:, i, :], in0=lamb[:, 2 * i, :], in1=lamb[:, 2 * i + 1, :], op=ALU.mult), reads=[lambb], writes=[ltb])
            P.op("dve", "reduce_sum", PK(out=lc[:, i:i + 1], in_=lt[:, i, :], axis=AX.X), reads=[ltb], writes=[lcb])
        P.op("act", "activation", PK(out=lc[:, 0:2], in_=lc[:, 0:2], func=AF.Exp), reads=[lcb], writes=[lcb])
        P.op("dve", "tensor_tensor", PK(out=lc[:, 2:3], in0=lc[:, 1:2], in1=lc[:, 0:1], op=ALU.subtract), reads=[lcb], writes=[lcb])
        P.op("dve", "tensor_scalar_add", PK(out=lc[:, 3:4], in0=lc[:, 2:3], scalar1=-LAMBDA_INIT), reads=[lcb], writes=[lcb])
        P.op("dve", "tensor_scalar", PK(out=sgc[:, :], in0=sgc[:, :], scalar1=float(1.0 - LAMBDA_INIT), scalar2=None, op0=ALU.mult), reads=[sgcb], writes=[sgcb])
        kA = [C.sb("kA%d" % i, [68, 2, 4096], BF16) for i in range(2)]
        qA = [C.sb("qA%d" % i, [68, 2, NTOK], BF16) for i in range(2)]
        vA = [C.sb("vA%d" % i, [128, 32, 128], BF16) for i in range(2)]
        et, _ = C.sb("et", [128, 4, 512], BF16); etb = [Buf("et%d" % i) for i in range(4)]
        tmp, _ = C.sb("tmp", [128, 2, 512], F32); tmpb = [Buf("tmp%d" % i) for i in range(2)]
        rd, rdb = C.sb("rd", [128, 512], F32)
        o0, o0b = C.sb("o0", [128, 512], F32)
        o1, o1b = C.sb("o1", [128, 512], F32)
        ot, otb = C.sb("ot", [128, 512], F32)
        ei = 0
        ti_ = 0
        for h in range(8):
            slope = float(2.0 ** (-(h + 1)))
            kt, ktb = kA[h % 2]; qt, qtb = qA[h % 2]; vt, vtb = vA[h % 2]
            P.op("pool", "dma_start", PK(out=kt[:, :, :], in_=ka_d[2 * h:2 * h + 2].rearrange("c r n -> r c n")), writes=[ktb], is_dma=True)
            P.op("pool", "dma_start", PK(out=qt[:, :, :], in_=qa_d[2 * h:2 * h + 2].rearrange("c r n -> r c n")), writes=[qtb], is_dma=True)
            P.op("pool", "dma_start", PK(out=vt[:, :, :], in_=vf_d[:, h * 128:(h + 1) * 128].rearrange("(j p) d -> p j d", p=128)), writes=[vtb], is_dma=True)
            for m in range(4):
                nkb = 8 * (m + 1)
                qsl = slice(m * 512, (m + 1) * 512)
                N = [C.ps[0], C.ps[1]]
                Dn = [C.ps[2], C.ps[3]]
                for j in range(nkb):
                    for c in range(2):
                        sc, scb = C.ps[4 + ((2 * j + c) % 4)]
                        P.op("pe", "matmul", PK(sc[:, :], kt[:, c, j * 128:(j + 1) * 128], qt[:, c, qsl], start=True, stop=True),
                             reads=[ktb, qtb], writes=[scb])
                        e_slot = ei % 4; ei += 1
                        if j >= nkb - 8:
                            s = j - (nkb - 8)
                            tb = ti_ % 2; ti_ += 1
                            P.op("dve", "scalar_tensor_tensor", PK(out=tmp[:, tb, :], in0=biasT[:, s, :], scalar=slope, in1=sc[:, :], op0=ALU.mult, op1=ALU.add),
                                 reads=[biasb, scb], writes=[tmpb[tb]])
                            P.op("act", "activation", PK(out=et[:, e_slot, :], in_=tmp[:, tb, :], func=AF.Exp), reads=[tmpb[tb]], writes=[etb[e_slot]])
                        else:
                            P.op("act", "activation", PK(out=et[:, e_slot, :], in_=sc[:, :], func=AF.Exp), reads=[scb], writes=[etb[e_slot]])
                        P.op("pe", "matmul", PK(N[c][0][:, :], vt[:, j, :], et[:, e_slot, :], start=(j == 0), stop=(j == nkb - 1)),
                             reads=[vtb, etb[e_slot]], writes=[N[c][1]])
                        P.op("pe", "matmul", PK(Dn[c][0][:, :], cst3[:, 4, :], et[:, e_slot, :], start=(j == 0), stop=(j == nkb - 1)),
                             reads=[cstb, etb[e_slot]], writes=[Dn[c][1]])
                P.op("dve", "reciprocal", PK(out=rd[:, :], in_=Dn[0][0][:, :]), reads=[Dn[0][1]], writes=[rdb])
                P.op("dve", "tensor_tensor", PK(out=o0[:, :], in0=N[0][0][:, :], in1=rd[:, :], op=ALU.mult), reads=[N[0][1], rdb], writes=[o0b])
                P.op("dve", "reciprocal", PK(out=rd[:, :], in_=Dn[1][0][:, :]), reads=[Dn[1][1]], writes=[rdb])
                P.op("dve", "tensor_tensor", PK(out=o1[:, :], in0=N[1][0][:, :], in1=rd[:, :], op=ALU.mult), reads=[N[1][1], rdb], writes=[o1b])
                P.op("dve", "scalar_tensor_tensor", PK(out=o0[:, :], in0=o1[:, :], scalar=lc[:, 3:4], in1=o0[:, :], op0=ALU.mult, op1=ALU.add),
                     reads=[o1b, o0b, lcb], writes=[o0b])
                sq, sqb = scr["sq"]; rs, rsb = scr["rs"]
                P.op("act", "activation", PK(out=sq[:, 0, :], in_=o0[:, :], func=AF.Square), reads=[o0b], writes=[sqb])
                pn, pnb = C.ps[4]
                P.op("pe", "matmul", PK(pn[:, :], cst3[:, 3, :], sq[:, 0, :], start=True, stop=True), reads=[sqb, cstb], writes=[pnb])
                rstd_from(C, rs[:, :], pn[:, :], pnb, rsb)
                P.op("dve", "scalar_tensor_tensor", PK(out=ot[:, :], in0=o0[:, :], scalar=sgc[:, 0:1], in1=rs[:, :], op0=ALU.mult, op1=ALU.mult),
                     reads=[o0b, rsb, sgcb], writes=[otb])
                C.dma(at_d[h * 128:(h + 1) * 128, qsl], ot[:, :], R=[otb], is_out=True)
        P.emit(st)
    return nc


def bias_table(p):
    t = np.zeros((8, 128, 512), np.float32)
    kq = np.arange(128)[:, None]
    qq = np.arange(512)[None, :]
    for s in range(8):
        if p == 0:
            r = s if s < 4 else None
            full_mask = s >= 4
        else:
            r = s - 4 if s >= 4 else None
            full_mask = False
        if full_mask:
            t[s] = -1e30
        elif r is not None:
            kpos = 128 * r + kq
            kc = kpos // 64; qc = qq // 64
            d = -2.0 * np.maximum(kpos - qq, 0).astype(np.float32)
            t[s] = np.where(kc < qc, 0.0, np.where(kc == qc, d, -1e30))
    return np.ascontiguousarray(t.transpose(1, 0, 2).reshape(128, 8 * 512))


def q_aug_rows(p):
    pos = tok_index(p)
    return np.stack([pos // 64, pos % 64, np.ones_like(pos), np.ones_like(pos)]).astype(np.float32)


def k_aug_rows(h):
    s = np.arange(4096)
    sl = 2.0 ** (-(h + 1))
    return np.stack([np.full(4096, -64.0 * sl), np.full(4096, -sl), 64.0 * sl * (s // 64), sl * (s % 64)]).astype(np.float32)


T_SEQ = 4096
SEG = 512
NSEG_FULL = T_SEQ // SEG
NCOLS = 1312
ZCH = [(0, 128), (128, 128), (256, 128), (384, 128), (512, 128), (640, 128), (768, 128), (896, 128), (1024, 32), (1056, 128), (1184, 128)]
VA = dict(n_mix=0, mu=8, w0=17, a0=19, k_k=21, k_a=23, r_k=25, ln_w=27, ln_b=29, s5_d=31, lam_re=33, lam_im=41, log_dt=49, halfpi=57)
NVA = 58
C_ID, C_B64, C_ONES = 0, 1, 2
NEG_E05 = -float(np.exp(-0.5))


def build_A(nseg=NSEG_FULL):
    nc = bass.Bass("TRN2", target_bir_lowering=False)
    dr = lambda n, s, k="ExternalInput", dt=F32: nc.dram_tensor(n, s, dt, kind=k).ap()
    xT_d = dr("xT", [D, T_SEQ]); wc_d = dr("wc", [D, NCOLS]); vec_d = dr("vecs", [128, NVA])
    cst_d = dr("cst", [128, 5 * 128]); c32_d = dr("c32", [128, 3 * 128]); msk_d = dr("mask5", [64, 320]); rmask_d = dr("rmask", [128, SEG])
    wup_d = dr("w_up", [64, 256]); aup_d = dr("a_up", [128, 256]); gupa_d = dr("g_upa", [128, 256]); gupb_d = dr("g_upb", [32, 256])
    bre_d = dr("breT", [8, 128, 128]); bim_d = dr("bimT", [8, 128, 128]); cre_d = dr("creT", [8, 128, 128]); cim_d = dr("cimT", [8, 128, 128])
    ya_d = dr("yaT", [256, T_SEQ], "ExternalOutput"); yb_d = dr("ybT", [256, T_SEQ], "ExternalOutput")
    with ExitStack() as st:
        P = Prog(nc)
        C = Ctx(nc, st, P)
        rot = [0, 1, 2, 3, 4, 5, 7]
        rs_ = [0]

        def psum():
            r = C.ps[rot[rs_[0] % len(rot)]]
            rs_[0] += 1
            return r
        C.psum = psum
        T = lambda name, shape, dt=F32: C.sb(name, shape, dt)
        V, vb = T("V_sb", [128, NVA]); C.vb = vb
        C.dma(V[:, :], vec_d[:, :], W=[vb])
        cst3, cstb = T("cst_sb", [128, 5, 128], BF16)
        P.op("pool", "dma_start", PK(out=cst3[:, :, :], in_=cst_d.rearrange("p (a b) -> p a b", a=5)), writes=[cstb], is_dma=True)
        c32, c32b = T("c32_sb", [128, 3, 128]); C.dma(c32[:, :, :], c32_d.rearrange("p (a b) -> p a b", a=3), W=[c32b])
        msk, mskb = T("msk_sb", [64, 320]); C.dma(msk[:, :], msk_d[:, :], W=[mskb])
        rmask, rmaskb = T("rmask_sb", [128, SEG]); C.dma(rmask[:, :], rmask_d[:, :], W=[rmaskb])
        wup, wupb = T("wup_sb", [64, 256]); C.dma(wup[:, :], wup_d[:, :], W=[wupb])
        aup, aupb = T("aup_sb", [128, 256]); C.dma(aup[:, :], aup_d[:, :], W=[aupb])
        gupa, gupab = T("gupa_sb", [128, 256]); C.dma(gupa[:, :], gupa_d[:, :], W=[gupab])
        gupb, gupbb = T("gupb_sb", [32, 256]); C.dma(gupb[:, :], gupb_d[:, :], W=[gupbb])
        wc, wcb = T("wc_sb", [128, 8, NCOLS], BF16)
        for k in range(8):
            P.op("pool", "dma_start", PK(out=wc[:, k, :], in_=wc_d[k * 128:(k + 1) * 128, :]), writes=[wcb], is_dma=True)
        scr = dict(sq=T("sq", [128, 8, 512], BF16), rs=T("rs", [128, 512]))
        ident = c32[:, C_ID, :]

        breT, breb = T("breT_sb", [128, 8, 128], BF16); bimT, bimb = T("bimT_sb", [128, 8, 128], BF16)
        P.op("pool", "dma_start", PK(out=breT[:, :, :], in_=bre_d.rearrange("q k m -> k q m")), writes=[breb], is_dma=True)
        P.op("pool", "dma_start", PK(out=bimT[:, :, :], in_=bim_d.rearrange("q k m -> k q m")), writes=[bimb], is_dma=True)
        creT, creb = T("creT_sb", [128, 8, 128]); cimT, cimb = T("cimT_sb", [128, 8, 128])
        C.dma(creT[:, :, :], cre_d.rearrange("q k m -> k q m"), W=[creb])
        C.dma(cimT[:, :, :], cim_d.rearrange("q k m -> k q m"), W=[cimb])
        s5c, s5cb = T("s5c", [128, 12, 8])
        lre = V[:, VA["lam_re"]:VA["lam_re"] + 8]; lim = V[:, VA["lam_im"]:VA["lam_im"] + 8]
        S = lambda i: s5c[:, i, :]
        dv = lambda name, **kw: P.op("dve", name, PK(**kw), reads=[s5cb, vb], writes=[s5cb])
        P.op("act", "activation", PK(out=S(0), in_=V[:, VA["log_dt"]:VA["log_dt"] + 8], func=AF.Exp), reads=[vb], writes=[s5cb])
        dv("tensor_tensor", out=S(1), in0=lim, in1=S(0), op=ALU.mult)
        dv("tensor_tensor", out=S(9), in0=lre, in1=S(0), op=ALU.mult)
        P.op("act", "activation", PK(out=S(2), in_=S(9), func=AF.Exp), reads=[s5cb], writes=[s5cb])
        P.op("act", "activation", PK(out=S(4), in_=S(1), func=AF.Sin, scale=0.125), reads=[s5cb], writes=[s5cb])
        P.op("act", "activation", PK(out=S(3), in_=S(1), func=AF.Sin, scale=-0.125, bias=V[:, VA["halfpi"]:VA["halfpi"] + 1]), reads=[s5cb, vb], writes=[s5cb])
        for _ in range(3):
            dv("tensor_tensor", out=S(9), in0=S(3), in1=S(3), op=ALU.mult)
            dv("tensor_tensor", out=S(10), in0=S(4), in1=S(4), op=ALU.mult)
            dv("tensor_tensor", out=S(11), in0=S(3), in1=S(4), op=ALU.mult)
            dv("tensor_tensor", out=S(3), in0=S(9), in1=S(10), op=ALU.subtract)
            dv("tensor_scalar", out=S(4), in0=S(11), scalar1=2.0, scalar2=None, op0=ALU.mult)
        dv("tensor_tensor", out=S(5), in0=S(2), in1=S(3), op=ALU.mult)
        dv("tensor_tensor", out=S(6), in0=S(2), in1=S(4), op=ALU.mult)
        dv("tensor_scalar_add", out=S(5), in0=S(5), scalar1=-1.0)
        dv("tensor_tensor", out=S(9), in0=lre, in1=lre, op=ALU.mult)
        dv("tensor_tensor", out=S(10), in0=lim, in1=lim, op=ALU.mult)
        dv("tensor_tensor", out=S(9), in0=S(9), in1=S(10), op=ALU.add)
        dv("reciprocal", out=S(9), in_=S(9))
        dv("tensor_tensor", out=S(10), in0=S(5), in1=lre, op=ALU.mult)
        dv("tensor_tensor", out=S(11), in0=S(6), in1=lim, op=ALU.mult)
        dv("tensor_tensor", out=S(10), in0=S(10), in1=S(11), op=ALU.add)
        dv("tensor_tensor", out=S(7), in0=S(10), in1=S(9), op=ALU.mult)
        dv("tensor_tensor", out=S(10), in0=S(6), in1=lre, op=ALU.mult)
        dv("tensor_tensor", out=S(11), in0=S(5), in1=lim, op=ALU.mult)
        dv("tensor_tensor", out=S(10), in0=S(10), in1=S(11), op=ALU.subtract)
        dv("tensor_tensor", out=S(8), in0=S(10), in1=S(9), op=ALU.mult)
        cpre, cpreb = T("cpre", [128, 8, 128], BF16); cpim, cpimb = T("cpim", [128, 8, 128], BF16)
        ctmp, ctmpb = T("ctmp", [128, 128])
        for q in range(8):
            cr = s5c[:, 7, q:q + 1]; ci = s5c[:, 8, q:q + 1]
            P.op("dve", "tensor_scalar", PK(out=ctmp[:, :], in0=cimT[:, q, :], scalar1=ci, scalar2=None, op0=ALU.mult), reads=[cimb, s5cb], writes=[ctmpb])
            P.op("dve", "scalar_tensor_tensor", PK(out=cpre[:, q, :], in0=creT[:, q, :], scalar=cr, in1=ctmp[:, :], op0=ALU.mult, op1=ALU.subtract),
                 reads=[creb, ctmpb, s5cb], writes=[cpreb])
            P.op("dve", "tensor_scalar", PK(out=ctmp[:, :], in0=cimT[:, q, :], scalar1=cr, scalar2=-1.0, op0=ALU.mult, op1=ALU.mult), reads=[cimb, s5cb], writes=[ctmpb])
            P.op("dve", "scalar_tensor_tensor", PK(out=ctmp[:, :], in0=creT[:, q, :], scalar=ci, in1=ctmp[:, :], op0=ALU.mult, op1=ALU.subtract),
                 reads=[creb, ctmpb, s5cb], writes=[ctmpb])
            P.op("dve", "tensor_scalar", PK(out=cpim[:, q, :], in0=ctmp[:, :], scalar1=-1.0, scalar2=None, op0=ALU.mult), reads=[ctmpb], writes=[cpimb])
        Fc, Fcb = T("Fc", [128, 8, SEG]); Fs, Fsb = T("Fs", [128, 8, SEG])
        ncol, ncolb = T("ncol", [128, 2])
        ftmp, ftmpb = T("ftmp", [128, SEG // 2])
        for q in range(8):
            P.op("dve", "tensor_copy", PK(out=Fc[:, q, 0:1], in_=s5c[:, 3, q:q + 1]), reads=[s5cb], writes=[Fcb])
            P.op("dve", "tensor_copy", PK(out=Fs[:, q, 0:1], in_=s5c[:, 4, q:q + 1]), reads=[s5cb], writes=[Fsb])
            n = 1
            while n < SEG:
                cn = Fc[:, q, n - 1:n]; sn = Fs[:, q, n - 1:n]
                P.op("dve", "tensor_scalar", PK(out=ftmp[:, 0:n], in0=Fs[:, q, 0:n], scalar1=sn, scalar2=None, op0=ALU.mult), reads=[Fsb], writes=[ftmpb])
                P.op("dve", "scalar_tensor_tensor", PK(out=Fc[:, q, n:2 * n], in0=Fc[:, q, 0:n], scalar=cn, in1=ftmp[:, 0:n], op0=ALU.mult, op1=ALU.subtract),
                     reads=[Fcb, ftmpb], writes=[Fcb])
                P.op("dve", "tensor_scalar", PK(out=ftmp[:, 0:n], in0=Fc[:, q, 0:n], scalar1=sn, scalar2=None, op0=ALU.mult), reads=[Fcb, Fsb], writes=[ftmpb])
                P.op("dve", "scalar_tensor_tensor", PK(out=Fs[:, q, n:2 * n], in0=Fs[:, q, 0:n], scalar=cn, in1=ftmp[:, 0:n], op0=ALU.mult, op1=ALU.add),
                     reads=[Fsb, Fcb, ftmpb], writes=[Fsb])
                n *= 2
        xst_re, xreb = T("xst_re", [128, 8, 2]); xst_im, ximb = T("xst_im", [128, 8, 2])
        P.op("dve", "memset", PK(xst_re[:, :, :], 0.0), writes=[xreb]); P.op("dve", "memset", PK(xst_im[:, :, :], 0.0), writes=[ximb])

        zT, _ = T("zT", [128, 11, SEG + 1]); zTb = [Buf("zT%d" % c) for c in range(11)]
        for c in range(11):
            P.op("dve", "memset", PK(zT[:, c, 0:1], 0.0), writes=[zTb[c]])
        Sst = [[T("S%d_%d" % (hp, i), [128, 64]) for i in range(2)] for hp in range(2)]
        for hp in range(2):
            P.op("dve", "memset", PK(Sst[hp][0][0][:, :], 0.0), writes=[Sst[hp][0][1]])
        sidx = [0, 0]
        xT_t, xTb = T("xT_sb", [128, 8, SEG], BF16); hn, hnb = T("hn", [128, 8, SEG], BF16)
        names = "ld a g al be km cum Gi tA tB Rb Kb Ab Bb Kh Bh rk".split()
        W_ = {n: T("w_" + n, [128, SEG]) for n in names}
        gC, gCb = T("gC", [128, 8])
        AAs, AAsb = T("AAs", [64, 320]); TM, TMb = T("TM", [64, 4, 128])
        Asq = [T("Asq%d" % i, [64, 128]) for i in range(2)]
        Zt = [T("Zt%d" % i, [64, 64]) for i in range(2)]
        Wsb, Wsbb = T("Wsb", [64, 64]); Ut0, Ut0b = T("Ut0", [64, 64]); Ut, Utb = T("Ut", [64, 64])
        Phi, Phib = T("Phi", [128, 64])
        ubuf, ubufb = T("ubuf", [128, 2, SEG], BF16)
        s5t = {n: T("s5_" + n, [128, SEG]) for n in "t1 t2 cre cim zre zim xre xim".split()}
        tw, twb = s5t["t1"]; sg0, sg0b = s5t["t2"]; sg1, sg1b = s5t["zre"]
        xreb16, xreb16b = T("xre16", [128, 8, SEG], BF16); ximb16, ximb16b = T("xim16", [128, 8, SEG], BF16)
        yo, yob = s5t["cre"]

        for seg in range(nseg):
            tsl = slice(seg * SEG, (seg + 1) * SEG)
            P.op("pool", "dma_start", PK(out=xT_t[:, :, :], in_=xT_d[:, tsl].rearrange("(c p) n -> p c n", p=128)), writes=[xTb], is_dma=True)
            norm_fm(C, lambda c: xT_t[:, c, :], [xTb], SEG, 8, cst3[:, 0, :], cstb, lambda c: V[:, VA["n_mix"] + c:VA["n_mix"] + c + 1],
                    lambda c: hn[:, c, :], [hnb], scr)
            for c, (c0, wdt) in enumerate(ZCH):
                pt, pb = C.psum()
                for k in range(8):
                    P.op("pe", "matmul", PK(pt[0:wdt, :], wc[:, k, c0:c0 + wdt], hn[:, k, :], start=(k == 0), stop=(k == 7)), reads=[wcb, hnb], writes=[pb])
                P.op("act", "activation", PK(out=zT[0:wdt, c, 1:SEG + 1], in_=pt[0:wdt, :], func=AF.Copy), reads=[pb], writes=[zTb[c]])
            tA, tAb = W_["tA"]
            for c in range(9):
                wdt = ZCH[c][1]
                P.op("dve", "tensor_tensor", PK(out=tA[0:wdt, :], in0=zT[0:wdt, c, 0:SEG], in1=zT[0:wdt, c, 1:SEG + 1], op=ALU.subtract), reads=[zTb[c]], writes=[tAb])
                P.op("dve", "tensor_copy", PK(out=zT[0:wdt, c, 0:1], in_=zT[0:wdt, c, SEG:SEG + 1]), reads=[tAb], writes=[zTb[c]])
                P.op("dve", "scalar_tensor_tensor", PK(out=zT[0:wdt, c, 1:SEG + 1], in0=tA[0:wdt, :], scalar=V[0:wdt, VA["mu"] + c:VA["mu"] + c + 1],
                                                      in1=zT[0:wdt, c, 1:SEG + 1], op0=ALU.mult, op1=ALU.add), reads=[tAb, zTb[c], vb], writes=[zTb[c]])
            Z = lambda c, lo=0, hi=128: zT[lo:hi, c, 1:SEG + 1]
            P.op("act", "activation", PK(out=tw[0:64, :], in_=Z(6, 0, 64), func=AF.Tanh), reads=[zTb[6]], writes=[twb])
            P.op("act", "activation", PK(out=sg0[:, :], in_=Z(7), func=AF.Sigmoid), reads=[zTb[7]], writes=[sg0b])
            P.op("act", "activation", PK(out=sg1[0:32, :], in_=Z(8, 0, 32), func=AF.Sigmoid), reads=[zTb[8]], writes=[sg1b])
            for hp in range(2):
                cols = slice(hp * 128, (hp + 1) * 128)
                vcol = lambda nm: V[:, VA[nm] + hp:VA[nm] + hp + 1]
                r_ = Z(hp); k_ = Z(2 + hp); v_ = Z(4 + hp)
                rb_, kb_, vb_ = zTb[hp], zTb[2 + hp], zTb[4 + hp]
                X = lambda n: W_[n][0]
                B_ = lambda n: W_[n][1]
                DV = lambda name, R, Wn, **kw: P.op("dve", name, PK(**kw), reads=R, writes=[B_(Wn)])
                pt, pb = C.psum()
                P.op("pe", "matmul", PK(pt[:, :], wup[:, cols], tw[0:64, :], start=True, stop=True), reads=[wupb, twb], writes=[pb])
                P.op("act", "activation", PK(out=X("ld")[:, :], in_=pt[:, :], func=AF.Sigmoid, bias=vcol("w0")), reads=[pb, vb], writes=[B_("ld")])
                DV("tensor_scalar", [B_("ld")], "ld", out=X("ld")[:, :], in0=X("ld")[:, :], scalar1=NEG_E05, scalar2=None, op0=ALU.mult)
                pt, pb = C.psum()
                P.op("pe", "matmul", PK(pt[:, :], aup[64:128, cols], Z(6, 64, 128), start=True, stop=True), reads=[aupb, zTb[6]], writes=[pb])
                P.op("act", "activation", PK(out=X("a")[:, :], in_=pt[:, :], func=AF.Sigmoid, bias=vcol("a0")), reads=[pb, vb], writes=[B_("a")])
                pt, pb = C.psum()
                P.op("pe", "matmul", PK(pt[:, :], gupa[:, cols], sg0[:, :], start=True, stop=False), reads=[gupab, sg0b], writes=[pb])
                P.op("pe", "matmul", PK(pt[:, :], gupb[:, cols], sg1[0:32, :], start=False, stop=True), reads=[gupbb, sg1b], writes=[pb])
                P.op("act", "activation", PK(out=X("g")[:, :], in_=pt[:, :], func=AF.Copy), reads=[pb], writes=[B_("g")])
                DV("tensor_scalar", [kb_, vb], "tA", out=X("tA")[:, :], in0=k_, scalar1=vcol("k_k"), scalar2=None, op0=ALU.mult)
                P.op("act", "activation", PK(out=X("tB")[:, :], in_=X("tA")[:, :], func=AF.Square), reads=[B_("tA")], writes=[B_("tB")])
                pt, pb = C.psum()
                P.op("pe", "matmul", PK(pt[:, :], c32[:, C_B64, :], X("tB")[:, :], start=True, stop=True), reads=[c32b, B_("tB")], writes=[pb])
                rs, rsb = scr["rs"]
                rstd_from(C, rs[:, :], pt[:, :], pb, rsb)
                DV("scalar_tensor_tensor", [B_("tA"), rsb], "al", out=X("al")[:, :], in0=X("tA")[:, :], scalar=0.125, in1=rs[:, :], op0=ALU.mult, op1=ALU.mult)
                DV("scalar_tensor_tensor", [B_("al"), B_("a")], "be", out=X("be")[:, :], in0=X("al")[:, :], scalar=-1.0, in1=X("a")[:, :], op0=ALU.mult, op1=ALU.mult)
                DV("tensor_scalar", [B_("a"), vb], "tA", out=X("tA")[:, :], in0=X("a")[:, :], scalar1=-1.0, scalar2=vcol("k_a"), op0=ALU.add, op1=ALU.mult)
                DV("scalar_tensor_tensor", [B_("tA"), kb_], "km", out=X("km")[:, :], in0=X("tA")[:, :], scalar=1.0, in1=k_, op0=ALU.add, op1=ALU.mult)
                DV("scalar_tensor_tensor", [rb_, B_("km"), vb], "rk", out=X("rk")[:, :], in0=r_, scalar=vcol("r_k"), in1=X("km")[:, :], op0=ALU.mult, op1=ALU.mult)
                DV("tensor_tensor_scan", [rmaskb, B_("ld")], "cum", out=X("cum")[:, :], data0=rmask[:, :], data1=X("ld")[:, :], initial=0.0, op0=ALU.mult, op1=ALU.add)
                cum3 = X("cum")[:, :].rearrange("p (c t) -> p c t", t=64)
                P.op("act", "activation", PK(out=X("Gi")[:, :], in_=X("cum")[:, :], func=AF.Exp), reads=[B_("cum")], writes=[B_("Gi")])
                DV("tensor_tensor", [B_("Gi"), rb_], "Rb", out=X("Rb")[:, :], in0=r_, in1=X("Gi")[:, :], op=ALU.mult)
                DV("tensor_tensor", [B_("cum"), B_("ld")], "tA", out=X("tA")[:, :], in0=X("cum")[:, :], in1=X("ld")[:, :], op=ALU.subtract)
                P.op("act", "activation", PK(out=X("Gi")[:, :], in_=X("tA")[:, :], func=AF.Exp), reads=[B_("tA")], writes=[B_("Gi")])
                DV("tensor_tensor", [B_("Gi"), B_("al")], "Ab", out=X("Ab")[:, :], in0=X("al")[:, :], in1=X("Gi")[:, :], op=ALU.mult)
                P.op("act", "activation", PK(out=X("Gi")[:, :], in_=X("cum")[:, :], func=AF.Exp, scale=-1.0), reads=[B_("cum")], writes=[B_("Gi")])
                DV("tensor_tensor", [B_("Gi"), B_("km")], "Kb", out=X("Kb")[:, :], in0=X("km")[:, :], in1=X("Gi")[:, :], op=ALU.mult)
                DV("tensor_tensor", [B_("Gi"), B_("be")], "Bb", out=X("Bb")[:, :], in0=X("be")[:, :], in1=X("Gi")[:, :], op=ALU.mult)
                tA3 = X("tA")[:, :].rearrange("p (c t) -> p c t", t=64)
                DV("tensor_tensor", [B_("cum")], "tA", out=tA3, in0=cum3[:, :, 63:64].to_broadcast([128, 8, 64]), in1=cum3, op=ALU.subtract)
                P.op("act", "activation", PK(out=X("Gi")[:, :], in_=X("tA")[:, :], func=AF.Exp), reads=[B_("tA")], writes=[B_("Gi")])
                DV("tensor_tensor", [B_("Gi"), B_("km")], "Kh", out=X("Kh")[:, :], in0=X("km")[:, :], in1=X("Gi")[:, :], op=ALU.mult)
                DV("tensor_tensor", [B_("Gi"), B_("be")], "Bh", out=X("Bh")[:, :], in0=X("be")[:, :], in1=X("Gi")[:, :], op=ALU.mult)
                P.op("act", "activation", PK(out=gC[:, :], in_=cum3[:, :, 63], func=AF.Exp), reads=[B_("cum")], writes=[gCb])
                po, pob = C.ps[6]
                for c in range(SEG // 64):
                    cs = slice(c * 64, (c + 1) * 64)
                    pt, pb = C.psum()
                    for i, (src, sb_) in enumerate(((v_, vb_), (X("Ab")[:, :], B_("Ab")), (X("Bh")[:, :], B_("Bh")), (X("Kh")[:, :], B_("Kh")))):
                        P.op("pe", "transpose", PK(pt[0:64, i * 128:(i + 1) * 128], src[:, cs], ident), reads=[sb_, c32b], writes=[pb])
                    P.op("act", "activation", PK(out=TM[:, :, :], in_=pt[0:64, :].rearrange("p (i k) -> p i k", i=4), func=AF.Copy), reads=[pb], writes=[TMb])
                    for e in range(2):
                        pr = slice(e * 64, (e + 1) * 64)
                        es = slice(e * 64, (e + 1) * 64)
                        pt, pb = C.psum()
                        Bb_, Kb_, Ab_, Rb_ = X("Bb")[pr, cs], X("Kb")[pr, cs], X("Ab")[pr, cs], X("Rb")[pr, cs]
                        P.op("pe", "matmul", PK(pt[0:64, 0:64], Bb_, Ab_, start=True, stop=True), reads=[B_("Bb"), B_("Ab")], writes=[pb])
                        P.op("pe", "matmul", PK(pt[0:64, 64:128], Bb_, Rb_, start=True, stop=True), reads=[B_("Bb"), B_("Rb")], writes=[pb])
                        P.op("pe", "matmul", PK(pt[0:64, 128:192], Kb_, Ab_, start=True, stop=True), reads=[B_("Kb"), B_("Ab")], writes=[pb])
                        P.op("pe", "matmul", PK(pt[0:64, 192:256], Kb_, Rb_, start=True, stop=True), reads=[B_("Kb"), B_("Rb")], writes=[pb])
                        P.op("pe", "matmul", PK(pt[0:64, 256:320], Ab_, Bb_, start=True, stop=True), reads=[B_("Bb"), B_("Ab")], writes=[pb])
                        P.op("dve", "tensor_tensor", PK(out=AAs[:, :], in0=pt[0:64, 0:320], in1=msk[:, :], op=ALU.mult), reads=[pb, mskb], writes=[AAsb])
                        A_ab, A_rb, A_ak, A_rk, A_abT = (AAs[:, 0:64], AAs[:, 64:128], AAs[:, 128:192], AAs[:, 192:256], AAs[:, 256:320])
                        Vt = TM[:, 0, es]; AbT = TM[:, 1, es]; BhT = TM[:, 2, es]; KhT = TM[:, 3, es]
                        zi = 0
                        P.op("dve", "tensor_tensor", PK(out=Zt[0][0][:, :], in0=A_ab, in1=ident[0:64, 0:64], op=ALU.add), reads=[AAsb, c32b], writes=[Zt[0][1]])
                        curA, curAT, curb = A_ab, A_abT, AAsb
                        for lvl in range(1, 6):
                            psq, psqb = C.psum()
                            if lvl < 5:
                                P.op("pe", "matmul", PK(psq[0:64, 0:64], curAT, curA, start=True, stop=True), reads=[curb], writes=[psqb])
                            P.op("pe", "matmul", PK(psq[0:64, 64:128], curA, curAT, start=True, stop=True), reads=[curb], writes=[psqb])
                            at, atb = Asq[lvl % 2]
                            lo = 0 if lvl < 5 else 64
                            P.op("act", "activation", PK(out=at[:, lo:128], in_=psq[0:64, lo:128], func=AF.Copy), reads=[psqb], writes=[atb])
                            curA, curAT, curb = at[:, 0:64], at[:, 64:128], atb
                            pz, pzb = C.psum()
                            P.op("pe", "matmul", PK(pz[0:64, 0:64], curAT, Zt[zi][0][:, :], start=True, stop=True), reads=[curb, Zt[zi][1]], writes=[pzb])
                            P.op("dve", "tensor_tensor", PK(out=Zt[1 - zi][0][:, :], in0=pz[0:64, 0:64], in1=Zt[zi][0][:, :], op=ALU.add),
                                 reads=[pzb, Zt[zi][1]], writes=[Zt[1 - zi][1]])
                            zi = 1 - zi
                        Tm, Tmb = Zt[zi]
                        pt, pb = C.psum()
                        P.op("pe", "matmul", PK(pt[0:64, 0:64], A_ak, Vt, start=True, stop=True), reads=[AAsb, TMb], writes=[pb])
                        P.op("act", "activation", PK(out=Wsb[:, :], in_=pt[0:64, 0:64], func=AF.Copy), reads=[pb], writes=[Wsbb])
                        pt, pb = C.psum()
                        P.op("pe", "matmul", PK(pt[0:64, 0:64], Tm[:, :], Wsb[:, :], start=True, stop=True), reads=[Tmb, Wsbb], writes=[pb])
                        P.op("pe", "matmul", PK(pt[pr, 64:128], AbT, Tm[:, :], start=True, stop=True), reads=[Tmb, TMb], writes=[pb])
                        P.op("act", "activation", PK(out=Ut0[:, :], in_=pt[0:64, 0:64], func=AF.Copy), reads=[pb], writes=[Ut0b])
                        P.op("act", "activation", PK(out=Phi[pr, :], in_=pt[pr, 64:128], func=AF.Copy), reads=[pb], writes=[Phib])
                        Sc, Scb = Sst[hp][sidx[hp] % 2] if e == 0 else Sst[hp][sidx[hp] % 2]
                        Sn, Snb = Sst[hp][(sidx[hp] + 1) % 2]
                        pt, pb = C.psum()
                        P.op("pe", "matmul", PK(pt[0:64, 0:64], Phi[pr, :], Sc[pr, :], start=True, stop=True), reads=[Phib, Scb], writes=[pb])
                        P.op("dve", "tensor_tensor", PK(out=Ut[:, :], in0=pt[0:64, 0:64], in1=Ut0[:, :], op=ALU.add), reads=[pb, Ut0b], writes=[Utb])
                        P.op("pe", "matmul", PK(po[pr, cs], Sc[pr, :], Rb_, start=True, stop=False), reads=[Scb, B_("Rb")], writes=[pob])
                        P.op("pe", "matmul", PK(po[pr, cs], Ut[:, :], A_rb, start=False, stop=False), reads=[Utb, AAsb], writes=[pob])
                        P.op("pe", "matmul", PK(po[pr, cs], Vt, A_rk, start=False, stop=True), reads=[TMb, AAsb], writes=[pob])
                        pt, pb = C.psum()
                        P.op("pe", "matmul", PK(pt[pr, 0:64], BhT, Ut[:, :], start=True, stop=False), reads=[TMb, Utb], writes=[pb])
                        P.op("pe", "matmul", PK(pt[pr, 0:64], KhT, Vt, start=False, stop=True), reads=[TMb], writes=[pb])
                        P.op("dve", "scalar_tensor_tensor", PK(out=Sn[pr, :], in0=Sc[pr, :], scalar=gC[pr, c:c + 1], in1=pt[pr, 0:64], op0=ALU.mult, op1=ALU.add),
                             reads=[Scb, gCb, pb], writes=[Snb])
                    sidx[hp] += 1
                Osb, Osbb = W_["Gi"]
                P.op("act", "activation", PK(out=Osb[:, :], in_=po[:, :], func=AF.Copy), reads=[pob], writes=[Osbb])
                pm, pmb = C.psum()
                P.op("pe", "matmul", PK(pm[:, :], c32[:, C_B64, :], Osb[:, :], start=True, stop=True), reads=[c32b, Osbb], writes=[pmb])
                DV("tensor_tensor", [Osbb, pmb], "tA", out=X("tA")[:, :], in0=Osb[:, :], in1=pm[:, :], op=ALU.subtract)
                P.op("act", "activation", PK(out=X("tB")[:, :], in_=X("tA")[:, :], func=AF.Square), reads=[B_("tA")], writes=[B_("tB")])
                pv, pvb = C.psum()
                P.op("pe", "matmul", PK(pv[:, :], c32[:, C_B64, :], X("tB")[:, :], start=True, stop=True), reads=[c32b, B_("tB")], writes=[pvb])
                rstd_from(C, rs[:, :], pv[:, :], pvb, rsb, eps=64e-5)
                DV("tensor_tensor", [B_("tA"), rsb], "tA", out=X("tA")[:, :], in0=X("tA")[:, :], in1=rs[:, :], op=ALU.mult)
                P.op("act", "activation", PK(out=X("tB")[:, :], in_=X("tA")[:, :], func=AF.Identity, scale=vcol("ln_w"), bias=vcol("ln_b")),
                     reads=[B_("tA"), vb], writes=[B_("tB")])
                pk, pkb = C.psum()
                P.op("pe", "matmul", PK(pk[:, :], c32[:, C_B64, :], X("rk")[:, :], start=True, stop=True), reads=[c32b, B_("rk")], writes=[pkb])
                DV("scalar_tensor_tensor", [pkb, vb_], "tA", out=X("tA")[:, :], in0=pk[:, :], scalar=64.0, in1=v_, op0=ALU.mult, op1=ALU.mult)
                DV("tensor_tensor", [B_("tA"), B_("tB")], "tB", out=X("tB")[:, :], in0=X("tB")[:, :], in1=X("tA")[:, :], op=ALU.add)
                DV("tensor_tensor", [B_("tB"), B_("g")], "Gi", out=Osb[:, :], in0=X("tB")[:, :], in1=X("g")[:, :], op=ALU.mult)
                C.dma(ya_d[hp * 128:(hp + 1) * 128, tsl], Osb[:, :], R=[Osbb], is_out=True)

            for uc in range(2):
                P.op("act", "activation", PK(out=ubuf[:, uc, :], in_=Z(9 + uc), func=AF.Copy), reads=[zTb[9 + uc]], writes=[ubufb])
            for q in range(8):
                uc = q // 4
                t1, t1b = s5t["t1"]; t2, t2b = s5t["t2"]
                cre_, creb_ = s5t["cre"]; cim_, cimb_ = s5t["cim"]
                zre, zreb = s5t["zre"]; zim, zimb = s5t["zim"]
                pr_, prb_ = C.psum(); pi_, pib_ = C.psum()
                P.op("pe", "matmul", PK(pr_[:, :], breT[:, q, :], ubuf[:, uc, :], start=True, stop=True), reads=[breb, ubufb], writes=[prb_])
                P.op("pe", "matmul", PK(pi_[:, :], bimT[:, q, :], ubuf[:, uc, :], start=True, stop=True), reads=[bimb, ubufb], writes=[pib_])
                fc = Fc[:, q, :]; fs = Fs[:, q, :]
                P.op("dve", "tensor_tensor", PK(out=t1[:, :], in0=pr_[:, :], in1=fc, op=ALU.mult), reads=[prb_, Fcb], writes=[t1b])
                P.op("dve", "tensor_tensor", PK(out=t2[:, :], in0=pi_[:, :], in1=fs, op=ALU.mult), reads=[pib_, Fsb], writes=[t2b])
                P.op("dve", "tensor_tensor", PK(out=cre_[:, :], in0=t1[:, :], in1=t2[:, :], op=ALU.add), reads=[t1b, t2b], writes=[creb_])
                P.op("dve", "tensor_tensor", PK(out=t1[:, :], in0=pi_[:, :], in1=fc, op=ALU.mult), reads=[pib_, Fcb], writes=[t1b])
                P.op("dve", "tensor_tensor", PK(out=t2[:, :], in0=pr_[:, :], in1=fs, op=ALU.mult), reads=[prb_, Fsb], writes=[t2b])
                P.op("dve", "tensor_tensor", PK(out=cim_[:, :], in0=t1[:, :], in1=t2[:, :], op=ALU.subtract), reads=[t1b, t2b], writes=[cimb_])
                P.op("dve", "tensor_tensor_scan", PK(out=zre[:, :], data0=s5c[:, 2, q:q + 1].to_broadcast([128, SEG]), data1=cre_[:, :], initial=xst_re[:, q, 0:1], op0=ALU.mult, op1=ALU.add),
                     reads=[s5cb, creb_, xreb], writes=[zreb])
                P.op("dve", "tensor_tensor_scan", PK(out=zim[:, :], data0=s5c[:, 2, q:q + 1].to_broadcast([128, SEG]), data1=cim_[:, :], initial=xst_im[:, q, 0:1], op0=ALU.mult, op1=ALU.add),
                     reads=[s5cb, cimb_, ximb], writes=[zimb])
                xre_, xreb_ = s5t["xre"]; xim_, ximb_ = s5t["xim"]
                P.op("dve", "tensor_tensor", PK(out=t1[:, :], in0=zre[:, :], in1=fc, op=ALU.mult), reads=[zreb, Fcb], writes=[t1b])
                P.op("dve", "tensor_tensor", PK(out=t2[:, :], in0=zim[:, :], in1=fs, op=ALU.mult), reads=[zimb, Fsb], writes=[t2b])
                P.op("dve", "tensor_tensor", PK(out=xre_[:, :], in0=t1[:, :], in1=t2[:, :], op=ALU.subtract), reads=[t1b, t2b], writes=[xreb_])
                P.op("dve", "tensor_tensor", PK(out=t1[:, :], in0=zim[:, :], in1=fc, op=ALU.mult), reads=[zimb, Fcb], writes=[t1b])
                P.op("dve", "tensor_tensor", PK(out=t2[:, :], in0=zre[:, :], in1=fs, op=ALU.mult), reads=[zreb, Fsb], writes=[t2b])
                P.op("dve", "tensor_tensor", PK(out=xim_[:, :], in0=t1[:, :], in1=t2[:, :], op=ALU.add), reads=[t1b, t2b], writes=[ximb_])
                P.op("dve", "tensor_copy", PK(out=xst_re[:, q, 0:1], in_=xre_[:, SEG - 1:SEG]), reads=[xreb_], writes=[xreb])
                P.op("dve", "tensor_copy", PK(out=xst_im[:, q, 0:1], in_=xim_[:, SEG - 1:SEG]), reads=[ximb_], writes=[ximb])
                P.op("act", "activation", PK(out=xreb16[:, q, :], in_=xre_[:, :], func=AF.Copy), reads=[xreb_], writes=[xreb16b])
                P.op("act", "activation", PK(out=ximb16[:, q, :], in_=xim_[:, :], func=AF.Copy), reads=[ximb_], writes=[ximb16b])
            for yc in range(2):
                py, pyb = C.psum()
                for qq in range(4):
                    q = yc * 4 + qq
                    P.op("pe", "matmul", PK(py[:, :], cpre[:, q, :], xreb16[:, q, :], start=(qq == 0), stop=False), reads=[cpreb, xreb16b], writes=[pyb])
                    P.op("pe", "matmul", PK(py[:, :], cpim[:, q, :], ximb16[:, q, :], start=False, stop=(qq == 3)), reads=[cpimb, ximb16b], writes=[pyb])
                t1, t1b = s5t["t1"]; t2, t2b = s5t["t2"]
                P.op("dve", "scalar_tensor_tensor", PK(out=yo[:, :], in0=Z(9 + yc), scalar=V[:, VA["s5_d"] + yc:VA["s5_d"] + yc + 1], in1=py[:, :], op0=ALU.mult, op1=ALU.add),
                     reads=[zTb[9 + yc], vb, pyb], writes=[yob])
                P.op("act", "activation", PK(out=t1[:, :], in_=yo[:, :], func=AF.Square), reads=[yob], writes=[t1b])
                P.op("dve", "tensor_scalar", PK(out=t1[:, :], in0=t1[:, :], scalar1=0.044715, scalar2=1.0, op0=ALU.mult, op1=ALU.add), reads=[t1b], writes=[t1b])
                P.op("dve", "tensor_tensor", PK(out=t1[:, :], in0=t1[:, :], in1=yo[:, :], op=ALU.mult), reads=[t1b, yob], writes=[t1b])
                P.op("act", "activation", PK(out=t2[:, :], in_=t1[:, :], func=AF.Sigmoid, scale=1.5957691216057308), reads=[t1b], writes=[t2b])
                P.op("dve", "tensor_tensor", PK(out=t2[:, :], in0=t2[:, :], in1=yo[:, :], op=ALU.mult), reads=[t2b, yob], writes=[t2b])
                C.dma(yb_d[yc * 128:(yc + 1) * 128, tsl], t2[:, :], R=[t2b], is_out=True)
        P.emit(st)
    return nc


def pack_A(inp, b, hh):
    W = np.asarray(inp["hy_w_in"][0]); mu = np.asarray(inp["rw_mu"][0])
    hc = slice(hh * 256, (hh + 1) * 256)
    idx = np.concatenate([np.arange(0, 512)[hc], 512 + np.arange(0, 512)[hc], 1024 + np.arange(0, 512)[hc],
                          np.arange(1536, 1824), 1824 + np.arange(0, 512)[hc]])
    wc = np.ascontiguousarray(W[:, idx])
    muc = np.concatenate([mu[idx[:1056]], np.zeros(96, np.float32)])
    Vt = np.zeros((128, NVA), np.float32)

    def put(name, arr):
        a = col(arr); Vt[:, VA[name]:VA[name] + a.shape[1]] = a
    put("n_mix", inp["norm_mix"][0])
    put("mu", muc)
    for nm, key in (("w0", "rw_w0"), ("a0", "rw_a0"), ("k_k", "rw_k_k"), ("k_a", "rw_k_a"), ("ln_w", "rw_ln_w"), ("ln_b", "rw_ln_b")):
        put(nm, np.asarray(inp[key][0])[hc])
    put("r_k", np.asarray(inp["rw_r_k"][0]).reshape(-1)[hc])
    put("s5_d", np.asarray(inp["s5_d"][0])[hc])
    gs = slice(hh * 16, (hh + 1) * 16)
    lre = np.asarray(inp["s5_lam_re"][0])[gs]; lim = np.asarray(inp["s5_lam_im"][0])[gs]; ldt = np.asarray(inp["s5_log_dt"][0])[gs]
    Vt[:, VA["lam_re"]:VA["lam_re"] + 8] = lre.reshape(8, 128).T
    Vt[:, VA["lam_im"]:VA["lam_im"] + 8] = lim.reshape(8, 128).T
    Vt[:, VA["log_dt"]:VA["log_dt"] + 8] = np.repeat(ldt, 64).reshape(8, 128).T
    Vt[:, VA["halfpi"]] = np.float32(np.pi / 2)
    bre = np.asarray(inp["s5_b_re"][0])[gs]; bim = np.asarray(inp["s5_b_im"][0])[gs]
    cre = np.asarray(inp["s5_c_re"][0])[gs]; cim = np.asarray(inp["s5_c_im"][0])[gs]
    breT = np.zeros((8, 128, 128), np.float32); bimT = np.zeros_like(breT); creT = np.zeros_like(breT); cimT = np.zeros_like(breT)
    for q in range(8):
        for e in range(2):
            gl = 2 * q + e
            rows = slice((gl % 8) * 16, (gl % 8) * 16 + 16)
            breT[q, rows, e * 64:(e + 1) * 64] = bre[gl].T
            bimT[q, rows, e * 64:(e + 1) * 64] = bim[gl].T
            creT[q, e * 64:(e + 1) * 64, rows] = cre[gl].T
            cimT[q, e * 64:(e + 1) * 64, rows] = cim[gl].T
    aup = np.zeros((128, 256), np.float32); aup[64:] = np.asarray(inp["rw_a_up"][0])[:, hc]
    gup = np.asarray(inp["rw_g_up"][0])[:, hc]
    c32 = np.zeros((128, 3, 128), np.float32)
    c32[:, 0, :] = np.eye(128, dtype=np.float32)
    c32[0:64, 1, 0:64] = 1.0 / 64; c32[64:, 1, 64:] = 1.0 / 64
    c32[:, 2, :] = 1.0
    s = np.arange(64)[:, None]; t = np.arange(64)[None, :]
    su = (s < t).astype(np.float32); ui = (s <= t).astype(np.float32)
    mask5 = np.concatenate([su, ui, su, ui, su.T], 1)
    rmask = np.ones((128, SEG), np.float32); rmask[:, ::64] = 0.0
    return dict(xT=np.ascontiguousarray(np.asarray(inp["x"][b]).T), wc=wc, vecs=Vt, cst=cst_table(), c32=c32.reshape(128, 384), mask5=mask5, rmask=rmask,
                w_up=np.ascontiguousarray(np.asarray(inp["rw_w_up"][0])[:, hc]), a_up=aup, g_upa=np.ascontiguousarray(gup[:128]),
                g_upb=np.ascontiguousarray(gup[128:]), breT=breT, bimT=bimT, creT=creT, cimT=cimT)


class ACtx(Ctx):
    def __init__(self, nc, st, P, words):
        self.nc, self.st, self.P = nc, st, P
        self.arena = st.enter_context(nc.sbuf_tensor("arena", [128, words], F32))
        self.words = words
        self.off = 0
        self.ps = []
        for i in range(8):
            t = st.enter_context(nc.psum_tensor("ps%d" % i, [128, 512], F32))
            self.ps.append((t, Buf("ps%d" % i, excl=True)))
        self.psi = 0
        self.wbufs = []
        self.wi = 0
        self.dmaq = 0
        self.vb = None
        self.nphase = 0
        self.epsc, self.epsb = self.sb("epsc", [128, 2], F32)
        self.dummy, _ = self.sb("dummy", [128, 2], F32)
        P.op("dve", "memset", PK(self.epsc[:, 0:1], EPS), writes=[self.epsb])
        P.op("dve", "memset", PK(self.epsc[:, 1:2], 64e-5), writes=[self.epsb])
        self.mark = self.off
        self.psum_default = self.psum

    def sb(self, name, shape, dt):
        n = 1
        for s_ in shape[1:]:
            n *= s_
        nbytes = n * (2 if dt == BF16 else 4)
        nw = (nbytes + 31) // 32 * 8
        assert self.off + nw <= self.words, ("arena overflow", name, self.off, nw, self.words)
        v = self.arena[0:shape[0], self.off:self.off + nw]
        self.off += nw
        if dt == BF16:
            v = v.bitcast(BF16)
        v = v[:, 0:n]
        if len(shape) == 3:
            v = v.rearrange("p (a b) -> p a b", a=shape[1])
        elif len(shape) == 4:
            v = v.rearrange("p (a b c) -> p a b c", a=shape[1], b=shape[2])
        return v, Buf("%s_%d" % (name, self.nphase))

    def new_phase(self, keep=None):
        self.P.fence(self.dummy[:, 0:1])
        self.off = self.mark if keep is None else keep
        self.wbufs = []
        self.wi = 0
        self.nphase += 1
        self.vb = None

    def init_w(self, n, cols):
        self.wbufs = []
        self.wi = 0
        for i in range(n):
            self.wbufs.append(self.sb("wb%d" % i, [128, cols], BF16))


def body_A(C, d, hh, ymb, nseg=NSEG_FULL):
    P = C.P
    if True:
        xT_d = d["xT"]; wc_d = d["wc%d" % hh]; vec_d = d["vecsA%d" % hh]
        cst_d = d["cst"]; c32_d = d["c32A"]; msk_d = d["mask5"]; rmask_d = d["rmask"]
        wup_d = d["w_up%d" % hh]; aup_d = d["a_up%d" % hh]; gupa_d = d["g_upa%d" % hh]; gupb_d = d["g_upb%d" % hh]
        bre_d = d["breT%d" % hh]; bim_d = d["bimT%d" % hh]; cre_d = d["creT%d" % hh]; cim_d = d["cimT%d" % hh]
        ya_d = d["ymT"][hh * 256:(hh + 1) * 256, :]; yb_d = d["ymT"][512 + hh * 256:512 + (hh + 1) * 256, :]
        rot = [0, 1, 2, 3, 4, 7]
        rs_ = [0]

        def psum():
            r = C.ps[rot[rs_[0] % len(rot)]]
            rs_[0] += 1
            return r
        C.psum = psum
        T = lambda name, shape, dt=F32: C.sb(name, shape, dt)
        V, vb = T("V_sb", [128, NVA]); C.vb = vb
        C.dma(V[:, :], vec_d[:, :], W=[vb])
        cst3, cstb = T("cst_sb", [128, 5, 128], BF16)
        P.op("pool", "dma_start", PK(out=cst3[:, :, :], in_=cst_d.rearrange("p (a b) -> p a b", a=5)), writes=[cstb], is_dma=True)
        c32, c32b = T("c32_sb", [128, 3, 128]); C.dma(c32[:, :, :], c32_d.rearrange("p (a b) -> p a b", a=3), W=[c32b])
        msk, mskb = T("msk_sb", [64, 320]); C.dma(msk[:, :], msk_d[:, :], W=[mskb])
        rmask, rmaskb = T("rmask_sb", [128, SEG]); C.dma(rmask[:, :], rmask_d[:, :], W=[rmaskb])
        wup, wupb = T("wup_sb", [64, 256]); C.dma(wup[:, :], wup_d[:, :], W=[wupb])
        aup, aupb = T("aup_sb", [128, 256]); C.dma(aup[:, :], aup_d[:, :], W=[aupb])
        gupa, gupab = T("gupa_sb", [128, 256]); C.dma(gupa[:, :], gupa_d[:, :], W=[gupab])
        gupb, gupbb = T("gupb_sb", [32, 256]); C.dma(gupb[:, :], gupb_d[:, :], W=[gupbb])
        wcL = [T("wc_sb%d" % i, [128, 8, 128], BF16) for i in range(2)]
        wci = [0]
        scr = dict(sq=T("sq", [128, 8, 512], BF16), rs=T("rs", [128, 512]))
        ident = c32[:, C_ID, :]
        names = "ld a g al be km cum Gi tA tB Rb Kb Ab Bb Kh Bh rk".split()
        W_ = {n: T("w_" + n, [128, SEG]) for n in names}

        breT, breb = T("breT_sb", [128, 8, 128], BF16); bimT, bimb = T("bimT_sb", [128, 8, 128], BF16)
        P.op("pool", "dma_start", PK(out=breT[:, :, :], in_=bre_d.rearrange("q k m -> k q m")), writes=[breb], is_dma=True)
        P.op("pool", "dma_start", PK(out=bimT[:, :, :], in_=bim_d.rearrange("q k m -> k q m")), writes=[bimb], is_dma=True)
        creT, creb = T("creT_sb", [128, 8, 128]); cimT, cimb = T("cimT_sb", [128, 8, 128])
        C.dma(creT[:, :, :], cre_d.rearrange("q k m -> k q m"), W=[creb])
        C.dma(cimT[:, :, :], cim_d.rearrange("q k m -> k q m"), W=[cimb])
        s5c, s5cb = T("s5c", [128, 12, 8])
        lre = V[:, VA["lam_re"]:VA["lam_re"] + 8]; lim = V[:, VA["lam_im"]:VA["lam_im"] + 8]
        S = lambda i: s5c[:, i, :]
        dv = lambda name, **kw: P.op("dve", name, PK(**kw), reads=[s5cb, vb], writes=[s5cb])
        P.op("act", "activation", PK(out=S(0), in_=V[:, VA["log_dt"]:VA["log_dt"] + 8], func=AF.Exp), reads=[vb], writes=[s5cb])
        dv("tensor_tensor", out=S(1), in0=lim, in1=S(0), op=ALU.mult)
        dv("tensor_tensor", out=S(9), in0=lre, in1=S(0), op=ALU.mult)
        P.op("act", "activation", PK(out=S(2), in_=S(9), func=AF.Exp), reads=[s5cb], writes=[s5cb])
        P.op("act", "activation", PK(out=S(4), in_=S(1), func=AF.Sin, scale=0.125), reads=[s5cb], writes=[s5cb])
        P.op("act", "activation", PK(out=S(3), in_=S(1), func=AF.Sin, scale=-0.125, bias=V[:, VA["halfpi"]:VA["halfpi"] + 1]), reads=[s5cb, vb], writes=[s5cb])
        for _ in range(3):
            dv("tensor_tensor", out=S(9), in0=S(3), in1=S(3), op=ALU.mult)
            dv("tensor_tensor", out=S(10), in0=S(4), in1=S(4), op=ALU.mult)
            dv("tensor_tensor", out=S(11), in0=S(3), in1=S(4), op=ALU.mult)
            dv("tensor_tensor", out=S(3), in0=S(9), in1=S(10), op=ALU.subtract)
            dv("tensor_scalar", out=S(4), in0=S(11), scalar1=2.0, scalar2=None, op0=ALU.mult)
        dv("tensor_tensor", out=S(5), in0=S(2), in1=S(3), op=ALU.mult)
        dv("tensor_tensor", out=S(6), in0=S(2), in1=S(4), op=ALU.mult)
        dv("tensor_scalar_add", out=S(5), in0=S(5), scalar1=-1.0)
        dv("tensor_tensor", out=S(9), in0=lre, in1=lre, op=ALU.mult)
        dv("tensor_tensor", out=S(10), in0=lim, in1=lim, op=ALU.mult)
        dv("tensor_tensor", out=S(9), in0=S(9), in1=S(10), op=ALU.add)
        dv("reciprocal", out=S(9), in_=S(9))
        dv("tensor_tensor", out=S(10), in0=S(5), in1=lre, op=ALU.mult)
        dv("tensor_tensor", out=S(11), in0=S(6), in1=lim, op=ALU.mult)
        dv("tensor_tensor", out=S(10), in0=S(10), in1=S(11), op=ALU.add)
        dv("tensor_tensor", out=S(7), in0=S(10), in1=S(9), op=ALU.mult)
        dv("tensor_tensor", out=S(10), in0=S(6), in1=lre, op=ALU.mult)
        dv("tensor_tensor", out=S(11), in0=S(5), in1=lim, op=ALU.mult)
        dv("tensor_tensor", out=S(10), in0=S(10), in1=S(11), op=ALU.subtract)
        dv("tensor_tensor", out=S(8), in0=S(10), in1=S(9), op=ALU.mult)
        cpre, cpreb = T("cpre", [128, 8, 128], BF16); cpim, cpimb = T("cpim", [128, 8, 128], BF16)
        ctmp = W_["tA"][0][:, 0:128]; ctmpb = W_["tA"][1]
        for q in range(8):
            cr = s5c[:, 7, q:q + 1]; ci = s5c[:, 8, q:q + 1]
            P.op("dve", "tensor_scalar", PK(out=ctmp[:, :], in0=cimT[:, q, :], scalar1=ci, scalar2=None, op0=ALU.mult), reads=[cimb, s5cb], writes=[ctmpb])
            P.op("dve", "scalar_tensor_tensor", PK(out=cpre[:, q, :], in0=creT[:, q, :], scalar=cr, in1=ctmp[:, :], op0=ALU.mult, op1=ALU.subtract),
                 reads=[creb, ctmpb, s5cb], writes=[cpreb])
            P.op("dve", "tensor_scalar", PK(out=ctmp[:, :], in0=cimT[:, q, :], scalar1=cr, scalar2=-1.0, op0=ALU.mult, op1=ALU.mult), reads=[cimb, s5cb], writes=[ctmpb])
            P.op("dve", "scalar_tensor_tensor", PK(out=ctmp[:, :], in0=creT[:, q, :], scalar=ci, in1=ctmp[:, :], op0=ALU.mult, op1=ALU.subtract),
                 reads=[creb, ctmpb, s5cb], writes=[ctmpb])
            P.op("dve", "tensor_scalar", PK(out=cpim[:, q, :], in0=ctmp[:, :], scalar1=-1.0, scalar2=None, op0=ALU.mult), reads=[ctmpb], writes=[cpimb])
        Fc, Fcb = T("Fc", [128, 8, SEG]); Fs, Fsb = T("Fs", [128, 8, SEG])
        ncol, ncolb = T("ncol", [128, 2])
        ftmp = W_["tB"][0][:, 0:SEG // 2]; ftmpb = W_["tB"][1]
        for q in range(8):
            P.op("dve", "tensor_copy", PK(out=Fc[:, q, 0:1], in_=s5c[:, 3, q:q + 1]), reads=[s5cb], writes=[Fcb])
            P.op("dve", "tensor_copy", PK(out=Fs[:, q, 0:1], in_=s5c[:, 4, q:q + 1]), reads=[s5cb], writes=[Fsb])
            n = 1
            while n < SEG:
                cn = Fc[:, q, n - 1:n]; sn = Fs[:, q, n - 1:n]
                P.op("dve", "tensor_scalar", PK(out=ftmp[:, 0:n], in0=Fs[:, q, 0:n], scalar1=sn, scalar2=None, op0=ALU.mult), reads=[Fsb], writes=[ftmpb])
                P.op("dve", "scalar_tensor_tensor", PK(out=Fc[:, q, n:2 * n], in0=Fc[:, q, 0:n], scalar=cn, in1=ftmp[:, 0:n], op0=ALU.mult, op1=ALU.subtract),
                     reads=[Fcb, ftmpb], writes=[Fcb])
                P.op("dve", "tensor_scalar", PK(out=ftmp[:, 0:n], in0=Fc[:, q, 0:n], scalar1=sn, scalar2=None, op0=ALU.mult), reads=[Fcb, Fsb], writes=[ftmpb])
                P.op("dve", "scalar_tensor_tensor", PK(out=Fs[:, q, n:2 * n], in0=Fs[:, q, 0:n], scalar=cn, in1=ftmp[:, 0:n], op0=ALU.mult, op1=ALU.add),
                     reads=[Fsb, Fcb, ftmpb], writes=[Fsb])
                n *= 2
        xst_re, xreb = T("xst_re", [128, 8, 2]); xst_im, ximb = T("xst_im", [128, 8, 2])
        P.op("dve", "memset", PK(xst_re[:, :, :], 0.0), writes=[xreb]); P.op("dve", "memset", PK(xst_im[:, :, :], 0.0), writes=[ximb])

        zT, _ = T("zT", [128, 11, SEG + 1]); zTb = [Buf("zT%d" % c) for c in range(11)]
        for c in range(11):
            P.op("dve", "memset", PK(zT[:, c, 0:1], 0.0), writes=[zTb[c]])
        Sst = [[T("S%d_%d" % (hp, i), [128, 64]) for i in range(2)] for hp in range(2)]
        for hp in range(2):
            P.op("dve", "memset", PK(Sst[hp][0][0][:, :], 0.0), writes=[Sst[hp][0][1]])
        sidx = [0, 0]
        xT_t, xTb = T("xT_sb", [128, 8, SEG], BF16); hn, hnb = T("hn", [128, 8, SEG], BF16)
        tw = xT_t[:, 0:2, :].rearrange("p a b -> p (a b)").bitcast(F32); sg0 = xT_t[:, 2:4, :].rearrange("p a b -> p (a b)").bitcast(F32)
        sg1 = xT_t[:, 4:6, :].rearrange("p a b -> p (a b)").bitcast(F32); twb = sg0b = sg1b = xTb
        gC, gCb = T("gC", [128, 8])
        NG = 4
        NU = 2 * NG
        AAsL = [T("AAs%d" % u, [64, 320]) for u in range(NU)]
        TML = [T("TM%d" % g, [64, 4, 128]) for g in range(NG)]
        AsqL = [[T("Asq%d_%d" % (u, i), [64, 128]) for i in range(2)] for u in range(NU)]
        ZtL = [[T("Zt%d_%d" % (u, i), [64, 64]) for i in range(2)] for u in range(NU)]
        WsbL = [T("Wsb%d" % u, [64, 64]) for u in range(NU)]
        Ut0L = [T("Ut0%d" % u, [64, 64]) for u in range(NU)]
        UtL = [T("Ut%d" % u, [64, 64]) for u in range(NU)]
        PhiL = [T("Phi%d" % g, [128, 64]) for g in range(NG)]
        ubuf, ubufb = T("ubuf", [128, 2, SEG], BF16)
        s5t = {n: T("s5_" + n, [128, SEG]) for n in "t1 t2 cre cim zre zim xre xim".split()}
        xreb16, _ = T("xre16", [128, 2, SEG], BF16); ximb16, _ = T("xim16", [128, 2, SEG], BF16)
        yacc, yaccb = T("yacc", [128, SEG])
        xre16bL = [Buf("xre16_0"), Buf("xre16_1")]; xim16bL = [Buf("xim16_0"), Buf("xim16_1")]
        yo, yob = s5t["cre"]

        for seg in range(nseg):
            tsl = slice(seg * SEG, (seg + 1) * SEG)
            P.op("pool", "dma_start", PK(out=xT_t[:, :, :], in_=xT_d[:, tsl].rearrange("(c p) n -> p c n", p=128)), writes=[xTb], is_dma=True)
            norm_fm(C, lambda c: xT_t[:, c, :], [xTb], SEG, 8, cst3[:, 0, :], cstb, lambda c: V[:, VA["n_mix"] + c:VA["n_mix"] + c + 1],
                    lambda c: hn[:, c, :], [hnb], scr)
            for c, (c0, wdt) in enumerate(ZCH):
                wc, wcb = wcL[wci[0] % 2]; wci[0] += 1
                P.op("pool", "dma_start", PK(out=wc[:, :, 0:wdt], in_=wc_d[:, c0:c0 + wdt].rearrange("(k p) n -> p k n", p=128)), writes=[wcb], is_dma=True)
                pt, pb = C.psum()
                for k in range(8):
                    P.op("pe", "matmul", PK(pt[0:wdt, :], wc[:, k, 0:wdt], hn[:, k, :], start=(k == 0), stop=(k == 7)), reads=[wcb, hnb], writes=[pb])
                P.op("act", "activation", PK(out=zT[0:wdt, c, 1:SEG + 1], in_=pt[0:wdt, :], func=AF.Copy), reads=[pb], writes=[zTb[c]])
            tA, tAb = W_["tA"]
            for c in range(9):
                wdt = ZCH[c][1]
                P.op("dve", "tensor_tensor", PK(out=tA[0:wdt, :], in0=zT[0:wdt, c, 0:SEG], in1=zT[0:wdt, c, 1:SEG + 1], op=ALU.subtract), reads=[zTb[c]], writes=[tAb])
                P.op("dve", "tensor_copy", PK(out=zT[0:wdt, c, 0:1], in_=zT[0:wdt, c, SEG:SEG + 1]), reads=[tAb], writes=[zTb[c]])
                P.op("dve", "scalar_tensor_tensor", PK(out=zT[0:wdt, c, 1:SEG + 1], in0=tA[0:wdt, :], scalar=V[0:wdt, VA["mu"] + c:VA["mu"] + c + 1],
                                                      in1=zT[0:wdt, c, 1:SEG + 1], op0=ALU.mult, op1=ALU.add), reads=[tAb, zTb[c], vb], writes=[zTb[c]])
            Z = lambda c, lo=0, hi=128: zT[lo:hi, c, 1:SEG + 1]
            P.op("act", "activation", PK(out=tw[0:64, :], in_=Z(6, 0, 64), func=AF.Tanh), reads=[zTb[6]], writes=[twb])
            P.op("act", "activation", PK(out=sg0[:, :], in_=Z(7), func=AF.Sigmoid), reads=[zTb[7]], writes=[sg0b])
            P.op("act", "activation", PK(out=sg1[0:32, :], in_=Z(8, 0, 32), func=AF.Sigmoid), reads=[zTb[8]], writes=[sg1b])
            for uc in range(2):
                P.op("act", "activation", PK(out=ubuf[:, uc, :], in_=Z(9 + uc), func=AF.Copy), reads=[zTb[9 + uc]], writes=[ubufb])
            def s5_pair(q):
                    uc = q // 4
                    t1, t1b = s5t["t1"]; t2, t2b = s5t["t2"]
                    cre_, creb_ = s5t["cre"]; cim_, cimb_ = s5t["cim"]
                    zre, zreb = s5t["zre"]; zim, zimb = s5t["zim"]
                    pr_, prb_ = C.psum(); pi_, pib_ = C.psum()
                    P.op("pe", "matmul", PK(pr_[:, :], breT[:, q, :], ubuf[:, uc, :], start=True, stop=True), reads=[breb, ubufb], writes=[prb_])
                    P.op("pe", "matmul", PK(pi_[:, :], bimT[:, q, :], ubuf[:, uc, :], start=True, stop=True), reads=[bimb, ubufb], writes=[pib_])
                    fc = Fc[:, q, :]; fs = Fs[:, q, :]
                    P.op("dve", "tensor_tensor", PK(out=t1[:, :], in0=pr_[:, :], in1=fc, op=ALU.mult), reads=[prb_, Fcb], writes=[t1b])
                    P.op("dve", "tensor_tensor", PK(out=t2[:, :], in0=pi_[:, :], in1=fs, op=ALU.mult), reads=[pib_, Fsb], writes=[t2b])
                    P.op("dve", "tensor_tensor", PK(out=cre_[:, :], in0=t1[:, :], in1=t2[:, :], op=ALU.add), reads=[t1b, t2b], writes=[creb_])
                    P.op("dve", "tensor_tensor", PK(out=t1[:, :], in0=pi_[:, :], in1=fc, op=ALU.mult), reads=[pib_, Fcb], writes=[t1b])
                    P.op("dve", "tensor_tensor", PK(out=t2[:, :], in0=pr_[:, :], in1=fs, op=ALU.mult), reads=[prb_, Fsb], writes=[t2b])
                    P.op("dve", "tensor_tensor", PK(out=cim_[:, :], in0=t1[:, :], in1=t2[:, :], op=ALU.subtract), reads=[t1b, t2b], writes=[cimb_])
                    P.op("dve", "tensor_tensor_scan", PK(out=zre[:, :], data0=s5c[:, 2, q:q + 1].to_broadcast([128, SEG]), data1=cre_[:, :], initial=xst_re[:, q, 0:1], op0=ALU.mult, op1=ALU.add),
                         reads=[s5cb, creb_, xreb], writes=[zreb])
                    P.op("dve", "tensor_tensor_scan", PK(out=zim[:, :], data0=s5c[:, 2, q:q + 1].to_broadcast([128, SEG]), data1=cim_[:, :], initial=xst_im[:, q, 0:1], op0=ALU.mult, op1=ALU.add),
                         reads=[s5cb, cimb_, ximb], writes=[zimb])
                    xre_, xreb_ = s5t["xre"]; xim_, ximb_ = s5t["xim"]
                    P.op("dve", "tensor_tensor", PK(out=t1[:, :], in0=zre[:, :], in1=fc, op=ALU.mult), reads=[zreb, Fcb], writes=[t1b])
                    P.op("dve", "tensor_tensor", PK(out=t2[:, :], in0=zim[:, :], in1=fs, op=ALU.mult), reads=[zimb, Fsb], writes=[t2b])
                    P.op("dve", "tensor_tensor", PK(out=xre_[:, :], in0=t1[:, :], in1=t2[:, :], op=ALU.subtract), reads=[t1b, t2b], writes=[xreb_])
                    P.op("dve", "tensor_tensor", PK(out=t1[:, :], in0=zim[:, :], in1=fc, op=ALU.mult), reads=[zimb, Fcb], writes=[t1b])
                    P.op("dve", "tensor_tensor", PK(out=t2[:, :], in0=zre[:, :], in1=fs, op=ALU.mult), reads=[zreb, Fsb], writes=[t2b])
                    P.op("dve", "tensor_tensor", PK(out=xim_[:, :], in0=t1[:, :], in1=t2[:, :], op=ALU.add), reads=[t1b, t2b], writes=[ximb_])
                    P.op("dve", "tensor_copy", PK(out=xst_re[:, q, 0:1], in_=xre_[:, SEG - 1:SEG]), reads=[xreb_], writes=[xreb])
                    P.op("dve", "tensor_copy", PK(out=xst_im[:, q, 0:1], in_=xim_[:, SEG - 1:SEG]), reads=[ximb_], writes=[ximb])
                    xb_ = q % 2
                    P.op("act", "activation", PK(out=xreb16[:, xb_, :], in_=xre_[:, :], func=AF.Copy), reads=[xreb_], writes=[xre16bL[xb_]])
                    P.op("act", "activation", PK(out=ximb16[:, xb_, :], in_=xim_[:, :], func=AF.Copy), reads=[ximb_], writes=[xim16bL[xb_]])
                    py, pyb = C.psum()
                    qq = q % 4
                    P.op("pe", "matmul", PK(py[:, :], cpre[:, q, :], xreb16[:, xb_, :], start=True, stop=False), reads=[cpreb, xre16bL[xb_]], writes=[pyb])
                    P.op("pe", "matmul", PK(py[:, :], cpim[:, q, :], ximb16[:, xb_, :], start=False, stop=True), reads=[cpimb, xim16bL[xb_]], writes=[pyb])
                    if qq == 0:
                        P.op("act", "activation", PK(out=yacc[:, :], in_=py[:, :], func=AF.Copy), reads=[pyb], writes=[yaccb])
                    else:
                        P.op("dve", "tensor_tensor", PK(out=yacc[:, :], in0=py[:, :], in1=yacc[:, :], op=ALU.add), reads=[pyb, yaccb], writes=[yaccb])
                    if qq != 3:
                        return
                    yc = q // 4
                    t1, t1b = s5t["t1"]; t2, t2b = s5t["t2"]
                    P.op("dve", "scalar_tensor_tensor", PK(out=yo[:, :], in0=Z(9 + yc), scalar=V[:, VA["s5_d"] + yc:VA["s5_d"] + yc + 1], in1=yacc[:, :], op0=ALU.mult, op1=ALU.add),
                         reads=[zTb[9 + yc], vb, yaccb], writes=[yob])
                    P.op("act", "activation", PK(out=t1[:, :], in_=yo[:, :], func=AF.Square), reads=[yob], writes=[t1b])
                    P.op("dve", "tensor_scalar", PK(out=t1[:, :], in0=t1[:, :], scalar1=0.044715, scalar2=1.0, op0=ALU.mult, op1=ALU.add), reads=[t1b], writes=[t1b])
                    P.op("dve", "tensor_tensor", PK(out=t1[:, :], in0=t1[:, :], in1=yo[:, :], op=ALU.mult), reads=[t1b, yob], writes=[t1b])
                    P.op("act", "activation", PK(out=t2[:, :], in_=t1[:, :], func=AF.Sigmoid, scale=1.5957691216057308), reads=[t1b], writes=[t2b])
                    P.op("dve", "tensor_tensor", PK(out=t2[:, :], in0=t2[:, :], in1=yo[:, :], op=ALU.mult), reads=[t2b, yob], writes=[t2b])
                    C.dma(yb_d[yc * 128:(yc + 1) * 128, tsl], t2[:, :], R=[t2b])
            s5_next = [0]
            for hp in range(2):
                cols = slice(hp * 128, (hp + 1) * 128)
                vcol = lambda nm: V[:, VA[nm] + hp:VA[nm] + hp + 1]
                r_ = Z(hp); k_ = Z(2 + hp); v_ = Z(4 + hp)
                rb_, kb_, vb_ = zTb[hp], zTb[2 + hp], zTb[4 + hp]
                X = lambda n: W_[n][0]
                B_ = lambda n: W_[n][1]
                DV = lambda name, R, Wn, **kw: P.op("dve", name, PK(**kw), reads=R, writes=[B_(Wn)])
                pt, pb = C.psum()
                P.op("pe", "matmul", PK(pt[:, :], wup[:, cols], tw[0:64, :], start=True, stop=True), reads=[wupb, twb], writes=[pb])
                P.op("act", "activation", PK(out=X("ld")[:, :], in_=pt[:, :], func=AF.Sigmoid, bias=vcol("w0")), reads=[pb, vb], writes=[B_("ld")])
                DV("tensor_scalar", [B_("ld")], "ld", out=X("ld")[:, :], in0=X("ld")[:, :], scalar1=NEG_E05, scalar2=None, op0=ALU.mult)
                pt, pb = C.psum()
                P.op("pe", "matmul", PK(pt[:, :], aup[64:128, cols], Z(6, 64, 128), start=True, stop=True), reads=[aupb, zTb[6]], writes=[pb])
                P.op("act", "activation", PK(out=X("a")[:, :], in_=pt[:, :], func=AF.Sigmoid, bias=vcol("a0")), reads=[pb, vb], writes=[B_("a")])
                pt, pb = C.psum()
                P.op("pe", "matmul", PK(pt[:, :], gupa[:, cols], sg0[:, :], start=True, stop=False), reads=[gupab, sg0b], writes=[pb])
                P.op("pe", "matmul", PK(pt[:, :], gupb[:, cols], sg1[0:32, :], start=False, stop=True), reads=[gupbb, sg1b], writes=[pb])
                P.op("act", "activation", PK(out=X("g")[:, :], in_=pt[:, :], func=AF.Copy), reads=[pb], writes=[B_("g")])
                DV("tensor_scalar", [kb_, vb], "tA", out=X("tA")[:, :], in0=k_, scalar1=vcol("k_k"), scalar2=None, op0=ALU.mult)
                P.op("act", "activation", PK(out=X("tB")[:, :], in_=X("tA")[:, :], func=AF.Square), reads=[B_("tA")], writes=[B_("tB")])
                pt, pb = C.psum()
                P.op("pe", "matmul", PK(pt[:, :], c32[:, C_B64, :], X("tB")[:, :], start=True, stop=True), reads=[c32b, B_("tB")], writes=[pb])
                rs, rsb = scr["rs"]
                rstd_from(C, rs[:, :], pt[:, :], pb, rsb)
                DV("scalar_tensor_tensor", [B_("tA"), rsb], "al", out=X("al")[:, :], in0=X("tA")[:, :], scalar=0.125, in1=rs[:, :], op0=ALU.mult, op1=ALU.mult)
                DV("scalar_tensor_tensor", [B_("al"), B_("a")], "be", out=X("be")[:, :], in0=X("al")[:, :], scalar=-1.0, in1=X("a")[:, :], op0=ALU.mult, op1=ALU.mult)
                DV("tensor_scalar", [B_("a"), vb], "tA", out=X("tA")[:, :], in0=X("a")[:, :], scalar1=-1.0, scalar2=vcol("k_a"), op0=ALU.add, op1=ALU.mult)
                DV("scalar_tensor_tensor", [B_("tA"), kb_], "km", out=X("km")[:, :], in0=X("tA")[:, :], scalar=1.0, in1=k_, op0=ALU.add, op1=ALU.mult)
                DV("scalar_tensor_tensor", [rb_, B_("km"), vb], "rk", out=X("rk")[:, :], in0=r_, scalar=vcol("r_k"), in1=X("km")[:, :], op0=ALU.mult, op1=ALU.mult)
                DV("tensor_tensor_scan", [rmaskb, B_("ld")], "cum", out=X("cum")[:, :], data0=rmask[:, :], data1=X("ld")[:, :], initial=0.0, op0=ALU.mult, op1=ALU.add)
                cum3 = X("cum")[:, :].rearrange("p (c t) -> p c t", t=64)
                P.op("act", "activation", PK(out=X("Gi")[:, :], in_=X("cum")[:, :], func=AF.Exp), reads=[B_("cum")], writes=[B_("Gi")])
                DV("tensor_tensor", [B_("Gi"), rb_], "Rb", out=X("Rb")[:, :], in0=r_, in1=X("Gi")[:, :], op=ALU.mult)
                DV("tensor_tensor", [B_("cum"), B_("ld")], "tA", out=X("tA")[:, :], in0=X("cum")[:, :], in1=X("ld")[:, :], op=ALU.subtract)
                P.op("act", "activation", PK(out=X("Gi")[:, :], in_=X("tA")[:, :], func=AF.Exp), reads=[B_("tA")], writes=[B_("Gi")])
                DV("tensor_tensor", [B_("Gi"), B_("al")], "Ab", out=X("Ab")[:, :], in0=X("al")[:, :], in1=X("Gi")[:, :], op=ALU.mult)
                P.op("act", "activation", PK(out=X("Gi")[:, :], in_=X("cum")[:, :], func=AF.Exp, scale=-1.0), reads=[B_("cum")], writes=[B_("Gi")])
                DV("tensor_tensor", [B_("Gi"), B_("km")], "Kb", out=X("Kb")[:, :], in0=X("km")[:, :], in1=X("Gi")[:, :], op=ALU.mult)
                DV("tensor_tensor", [B_("Gi"), B_("be")], "Bb", out=X("Bb")[:, :], in0=X("be")[:, :], in1=X("Gi")[:, :], op=ALU.mult)
                tA3 = X("tA")[:, :].rearrange("p (c t) -> p c t", t=64)
                DV("tensor_tensor", [B_("cum")], "tA", out=tA3, in0=cum3[:, :, 63:64].to_broadcast([128, 8, 64]), in1=cum3, op=ALU.subtract)
                P.op("act", "activation", PK(out=X("Gi")[:, :], in_=X("tA")[:, :], func=AF.Exp), reads=[B_("tA")], writes=[B_("Gi")])
                DV("tensor_tensor", [B_("Gi"), B_("km")], "Kh", out=X("Kh")[:, :], in0=X("km")[:, :], in1=X("Gi")[:, :], op=ALU.mult)
                DV("tensor_tensor", [B_("Gi"), B_("be")], "Bh", out=X("Bh")[:, :], in0=X("be")[:, :], in1=X("Gi")[:, :], op=ALU.mult)
                P.op("act", "activation", PK(out=gC[:, :], in_=cum3[:, :, 63], func=AF.Exp), reads=[B_("cum")], writes=[gCb])
                poL = [C.ps[6], C.ps[5]]
                vsrcs = ((v_, vb_), (X("Ab")[:, :], B_("Ab")), (X("Bh")[:, :], B_("Bh")), (X("Kh")[:, :], B_("Kh")))
                for g0 in range(0, SEG // 64, NG):
                    units = [(gi, e) for gi in range(NG) for e in range(2)]
                    CS = [slice((g0 + gi) * 64, (g0 + gi + 1) * 64) for gi in range(NG)]
                    PR = [slice(0, 64), slice(64, 128)]
                    for gi in range(NG):
                        pt, pb = C.psum()
                        for i, (src, sb_) in enumerate(vsrcs):
                            P.op("pe", "transpose", PK(pt[0:64, i * 128:(i + 1) * 128], src[:, CS[gi]], ident), reads=[sb_, c32b], writes=[pb])
                        P.op("act", "activation", PK(out=TML[gi][0][:, :, :], in_=pt[0:64, :].rearrange("p (i k) -> p i k", i=4), func=AF.Copy), reads=[pb], writes=[TML[gi][1]])
                    SB = 4
                    UB = [range(b0, min(NU, b0 + SB)) for b0 in range(0, NU, SB)]
                    for ub in UB:
                        pts = {}
                        for u in ub:
                            gi, e = units[u]
                            pr, cs = PR[e], CS[gi]
                            pt, pb = C.psum()
                            Bb_, Kb_, Ab_, Rb_ = X("Bb")[pr, cs], X("Kb")[pr, cs], X("Ab")[pr, cs], X("Rb")[pr, cs]
                            P.op("pe", "matmul", PK(pt[0:64, 0:64], Bb_, Ab_, start=True, stop=True), reads=[B_("Bb"), B_("Ab")], writes=[pb])
                            P.op("pe", "matmul", PK(pt[0:64, 64:128], Bb_, Rb_, start=True, stop=True), reads=[B_("Bb"), B_("Rb")], writes=[pb])
                            P.op("pe", "matmul", PK(pt[0:64, 128:192], Kb_, Ab_, start=True, stop=True), reads=[B_("Kb"), B_("Ab")], writes=[pb])
                            P.op("pe", "matmul", PK(pt[0:64, 192:256], Kb_, Rb_, start=True, stop=True), reads=[B_("Kb"), B_("Rb")], writes=[pb])
                            P.op("pe", "matmul", PK(pt[0:64, 256:320], Ab_, Bb_, start=True, stop=True), reads=[B_("Bb"), B_("Ab")], writes=[pb])
                            pts[u] = (pt, pb)
                        for u in ub:
                            pt, pb = pts[u]
                            P.op("dve", "tensor_tensor", PK(out=AAsL[u][0][:, :], in0=pt[0:64, 0:320], in1=msk[:, :], op=ALU.mult), reads=[pb, mskb], writes=[AAsL[u][1]])
                    for u in range(NU):
                        AAs, AAsb = AAsL[u]
                        P.op("dve", "tensor_tensor", PK(out=ZtL[u][0][0][:, :], in0=AAs[:, 0:64], in1=ident[0:64, 0:64], op=ALU.add), reads=[AAsb, c32b], writes=[ZtL[u][0][1]])
                    cur = [(AAsL[u][0][:, 0:64], AAsL[u][0][:, 256:320], AAsL[u][1]) for u in range(NU)]
                    zi = 0
                    for lvl in range(1, 6):
                        lo = 0 if lvl < 5 else 64
                        for ub in UB:
                            pts = {}
                            for u in ub:
                                curA, curAT, curb = cur[u]
                                psq, psqb = C.psum()
                                if lvl < 5:
                                    P.op("pe", "matmul", PK(psq[0:64, 0:64], curAT, curA, start=True, stop=True), reads=[curb], writes=[psqb])
                                P.op("pe", "matmul", PK(psq[0:64, 64:128], curA, curAT, start=True, stop=True), reads=[curb], writes=[psqb])
                                pts[u] = (psq, psqb)
                            for u in ub:
                                psq, psqb = pts[u]
                                at, atb = AsqL[u][lvl % 2]
                                P.op("act", "activation", PK(out=at[:, lo:128], in_=psq[0:64, lo:128], func=AF.Copy), reads=[psqb], writes=[atb])
                                cur[u] = (at[:, 0:64], at[:, 64:128], atb)
                        for ub in UB:
                            pts = {}
                            for u in ub:
                                pz, pzb = C.psum()
                                P.op("pe", "matmul", PK(pz[0:64, 0:64], cur[u][1], ZtL[u][zi][0][:, :], start=True, stop=True), reads=[cur[u][2], ZtL[u][zi][1]], writes=[pzb])
                                pts[u] = (pz, pzb)
                            for u in ub:
                                pz, pzb = pts[u]
                                P.op("dve", "tensor_tensor", PK(out=ZtL[u][1 - zi][0][:, :], in0=pz[0:64, 0:64], in1=ZtL[u][zi][0][:, :], op=ALU.add),
                                     reads=[pzb, ZtL[u][zi][1]], writes=[ZtL[u][1 - zi][1]])
                        zi = 1 - zi
                    for ub in UB:
                        pts = {}
                        for u in ub:
                            gi, e = units[u]
                            es = PR[e]
                            pt, pb = C.psum()
                            P.op("pe", "matmul", PK(pt[0:64, 0:64], AAsL[u][0][:, 128:192], TML[gi][0][:, 0, es], start=True, stop=True), reads=[AAsL[u][1], TML[gi][1]], writes=[pb])
                            pts[u] = (pt, pb)
                        for u in ub:
                            pt, pb = pts[u]
                            P.op("act", "activation", PK(out=WsbL[u][0][:, :], in_=pt[0:64, 0:64], func=AF.Copy), reads=[pb], writes=[WsbL[u][1]])
                    for ub in UB:
                        pts = {}
                        for u in ub:
                            gi, e = units[u]
                            pr, es = PR[e], PR[e]
                            Tm, Tmb = ZtL[u][zi]
                            pt, pb = C.psum()
                            P.op("pe", "matmul", PK(pt[0:64, 0:64], Tm[:, :], WsbL[u][0][:, :], start=True, stop=True), reads=[Tmb, WsbL[u][1]], writes=[pb])
                            P.op("pe", "matmul", PK(pt[pr, 64:128], TML[gi][0][:, 1, es], Tm[:, :], start=True, stop=True), reads=[Tmb, TML[gi][1]], writes=[pb])
                            pts[u] = (pt, pb)
                        for u in ub:
                            gi, e = units[u]
                            pt, pb = pts[u]
                            pr = PR[e]
                            P.op("act", "activation", PK(out=Ut0L[u][0][:, :], in_=pt[0:64, 0:64], func=AF.Copy), reads=[pb], writes=[Ut0L[u][1]])
                            P.op("act", "activation", PK(out=PhiL[gi][0][pr, :], in_=pt[pr, 64:128], func=AF.Copy), reads=[pb], writes=[PhiL[gi][1]])
                    for gi in range(NG):
                        c = g0 + gi
                        cs = CS[gi]
                        Sc, Scb = Sst[hp][sidx[hp] % 2]
                        Sn, Snb = Sst[hp][(sidx[hp] + 1) % 2]
                        TM, TMb = TML[gi]
                        Phi, Phib = PhiL[gi]
                        pts = []
                        for e in range(2):
                            pr = PR[e]
                            pt, pb = C.psum()
                            P.op("pe", "matmul", PK(pt[0:64, 0:64], Phi[pr, :], Sc[pr, :], start=True, stop=True), reads=[Phib, Scb], writes=[pb])
                            pts.append((pt, pb))
                        for e in range(2):
                            u = gi * 2 + e
                            pt, pb = pts[e]
                            P.op("dve", "tensor_tensor", PK(out=UtL[u][0][:, :], in0=pt[0:64, 0:64], in1=Ut0L[u][0][:, :], op=ALU.add), reads=[pb, Ut0L[u][1]], writes=[UtL[u][1]])
                        for e in range(2):
                            u = gi * 2 + e
                            pr = PR[e]
                            AAs, AAsb = AAsL[u]
                            Ut, Utb = UtL[u]
                            po, pob = poL[e]
                            mm1 = P.op("pe", "matmul", PK(po[pr, cs], Sc[pr, :], X("Rb")[pr, cs], start=True, stop=False), reads=[Scb, B_("Rb")], writes=[pob])
                            P.op("pe", "matmul", PK(po[pr, cs], Ut[:, :], AAs[:, 64:128], start=False, stop=False), reads=[Utb, AAsb], writes=[pob],
                                 after=([mm1] if e == 1 else []))
                            P.op("pe", "matmul", PK(po[pr, cs], TM[:, 0, pr], AAs[:, 192:256], start=False, stop=True), reads=[TMb, AAsb], writes=[pob])
                        pts = []
                        for e in range(2):
                            u = gi * 2 + e
                            pr = PR[e]
                            Ut, Utb = UtL[u]
                            pt, pb = C.psum()
                            P.op("pe", "matmul", PK(pt[pr, 0:64], TM[:, 2, pr], Ut[:, :], start=True, stop=False), reads=[TMb, Utb], writes=[pb])
                            P.op("pe", "matmul", PK(pt[pr, 0:64], TM[:, 3, pr], TM[:, 0, pr], start=False, stop=True), reads=[TMb], writes=[pb])
                            pts.append((pt, pb))
                        for e in range(2):
                            pr = PR[e]
                            pt, pb = pts[e]
                            P.op("dve", "scalar_tensor_tensor", PK(out=Sn[pr, :], in0=Sc[pr, :], scalar=gC[pr, c:c + 1], in1=pt[pr, 0:64], op0=ALU.mult, op1=ALU.add),
                                 reads=[Scb, gCb, pb], writes=[Snb])
                        sidx[hp] += 1
                    for _ in range(NG // 2):
                        s5_pair(s5_next[0]); s5_next[0] += 1
                Osb, Osbb = W_["Gi"]
                P.op("act", "activation", PK(out=Osb[0:64, :], in_=poL[0][0][0:64, :], func=AF.Copy), reads=[poL[0][1]], writes=[Osbb])
                P.op("act", "activation", PK(out=Osb[64:128, :], in_=poL[1][0][64:128, :], func=AF.Copy), reads=[poL[1][1]], writes=[Osbb])
                pm, pmb = C.psum()
                P.op("pe", "matmul", PK(pm[:, :], c32[:, C_B64, :], Osb[:, :], start=True, stop=True), reads=[c32b, Osbb], writes=[pmb])
                DV("tensor_tensor", [Osbb, pmb], "tA", out=X("tA")[:, :], in0=Osb[:, :], in1=pm[:, :], op=ALU.subtract)
                P.op("act", "activation", PK(out=X("tB")[:, :], in_=X("tA")[:, :], func=AF.Square), reads=[B_("tA")], writes=[B_("tB")])
                pv, pvb = C.psum()
                P.op("pe", "matmul", PK(pv[:, :], c32[:, C_B64, :], X("tB")[:, :], start=True, stop=True), reads=[c32b, B_("tB")], writes=[pvb])
                rstd_from(C, rs[:, :], pv[:, :], pvb, rsb, eps=64e-5)
                DV("tensor_tensor", [B_("tA"), rsb], "tA", out=X("tA")[:, :], in0=X("tA")[:, :], in1=rs[:, :], op=ALU.mult)
                P.op("act", "activation", PK(out=X("tB")[:, :], in_=X("tA")[:, :], func=AF.Identity, scale=vcol("ln_w"), bias=vcol("ln_b")),
                     reads=[B_("tA"), vb], writes=[B_("tB")])
                pk, pkb = C.psum()
                P.op("pe", "matmul", PK(pk[:, :], c32[:, C_B64, :], X("rk")[:, :], start=True, stop=True), reads=[c32b, B_("rk")], writes=[pkb])
                DV("scalar_tensor_tensor", [pkb, vb_], "tA", out=X("tA")[:, :], in0=pk[:, :], scalar=64.0, in1=v_, op0=ALU.mult, op1=ALU.mult)
                DV("tensor_tensor", [B_("tA"), B_("tB")], "tB", out=X("tB")[:, :], in0=X("tB")[:, :], in1=X("tA")[:, :], op=ALU.add)
                DV("tensor_tensor", [B_("tB"), B_("g")], "Gi", out=Osb[:, :], in0=X("tB")[:, :], in1=X("g")[:, :], op=ALU.mult)
                C.dma(ya_d[hp * 128:(hp + 1) * 128, tsl], Osb[:, :], R=[Osbb])

        C.psum = C.psum_default


def body_B(C, d, half, bufs):
    P = C.P
    t0 = half * NTOK
    if True:
        xT_d = d["xT"][:, t0:t0 + NTOK]; ymT_d = d["ymT"][:, t0:t0 + NTOK]; memT_d = d["memT"]
        vec_d = d["vecsB"]; cst_d = d["cst"]
        wglu_d = d["w_glu"]; wout_d = d["w_out"]; wq_d = d["w_q0"]; wkv_d = d["w_kv0"]; wo_d = d["w_o0"]
        wg_d = d["ff_g"]; wu_d = d["ff_u"]; wd_d = d["ff_d"]; wqkv_d = d["w_qkv"]
        h1T_d = d["h1T"][:, t0:t0 + NTOK]; qT_d = d["qT"][:, :, t0:t0 + NTOK]; kT_d = d["kT"][:, :, t0:t0 + NTOK]
        vtok_d = d["vtok"][t0:t0 + NTOK, :]
        ymb, h1b, qb_, kb_, vtb_ = bufs
        C.init_w(4, 4096)
        hT, _ = C.sb("hT", [128, 8, NTOK], F32); hTb = [Buf("hT%d" % i) for i in range(NTT)]
        bufA, _ = C.sb("bufA", [128, 8, NTOK], BF16); bufAb = [Buf("bA%d" % i) for i in range(NTT)]
        bufH, _ = C.sb("bufH", [128, 8, NTOK], BF16); bufHb = [Buf("bH%d" % i) for i in range(NTT)]
        V, vb = C.sb("V_sb", [128, NVB], F32); C.vb = vb
        cst3, cstb = C.sb("cst_sb", [128, 5, 128], BF16)
        scr = dict(sq=C.sb("sq", [128, 8, 512], BF16), rs=C.sb("rs", [128, 512], F32), sg=C.sb("sg", [128, 512], F32),
                   rd=C.sb("rd", [128, 512], F32))
        sqq, _ = C.sb("sqq", [128, 2, NTT, 512], BF16)
        scr["sqq"] = (sqq, [Buf("sqq%d" % i) for i in range(NTT)])
        et, _ = C.sb("et", [128, 2, 512], BF16)
        scr["et"] = (et, [Buf("et0"), Buf("et1")])
        C.dma(V[:, :], vec_d[:, :], W=[vb])
        P.op("pool", "dma_start", PK(out=cst3[:, :, :], in_=cst_d.rearrange("p (a b) -> p a b", a=5)), writes=[cstb], is_dma=True)
        for tt in range(NTT):
            sl = slice(tt * TT, (tt + 1) * TT)
            C.dma(hT[:, :, sl], xT_d[:, sl].rearrange("(c p) n -> p c n", p=128), W=[hTb[tt]])
            P.op("pool", "dma_start", PK(out=bufA[:, :, sl], in_=ymT_d[:, sl].rearrange("(c p) n -> p c n", p=128)),
                 reads=[ymb], writes=[bufAb[tt]], is_dma=True)
        wv, wb = C.load_w(wglu_d[:, :], 4, 512)
        sg, sgb = scr["sg"]
        for tt in range(NTT):
            sl = slice(tt * TT, (tt + 1) * TT)
            pts = []
            for oc in range(4):
                pt, pb = C.psum()
                for k in range(4):
                    P.op("pe", "matmul", PK(pt[:, :], wv[:, k, oc * 128:(oc + 1) * 128], bufA[:, 4 + k, sl],
                                                                            start=(k == 0), stop=(k == 3)), reads=[wb, bufAb[tt]], writes=[pb])
                pts.append((pt, pb))
            for oc in range(4):
                pt, pb = pts[oc]
                P.op("act", "activation", PK(out=sg[:, :], in_=pt[:, :], func=AF.Sigmoid,
                                                                bias=V[:, VB["b_glu"] + oc:VB["b_glu"] + oc + 1]), reads=[pb, vb], writes=[sgb])
                P.op("dve", "tensor_tensor", PK(out=bufA[:, 4 + oc, sl], in0=bufA[:, 4 + oc, sl], in1=sg[:, :], op=ALU.mult),
                     reads=[sgb, bufAb[tt]], writes=[bufAb[tt]])

        def evac_res(oc, tt, pt, pb, m):
            sl = slice(tt * TT, (tt + 1) * TT)
            P.op("dve", "tensor_tensor", PK(out=hT[:, oc, sl], in0=pt[:, :], in1=hT[:, oc, sl], op=ALU.add), reads=[pb, hTb[tt]], writes=[hTb[tt]])
        linear_fm(C, wout_d, 8, bufA, None, 1024, evac_res, xinbs=bufAb)
        (kn, knb), (vt, vtb) = mem_kv(C, memT_d, wkv_d, V, vb, VB["n_mem"], VB["k_gain"], cst3, cstb, scr, "m0")
        mem_xattn(C, hT, hTb, bufA, bufAb, bufH, bufHb, V, vb, VB["n_xattn"], VB["q_gain"], wq_d, wo_d, kn, knb, vt, vtb, cst3, cstb, scr)
        swiglu_ffn(C, hT, hTb, bufA, bufAb, bufH, bufHb, V, vb, VB["n_ffn"], wg_d, wu_d, wd_d, 2816, cst3, cstb, scr)
        for tt in range(NTT):
            sl = slice(tt * TT, (tt + 1) * TT)
            C.dma(h1T_d[:, sl].rearrange("(c p) n -> p c n", p=128), hT[:, :, sl], R=[hTb[tt]])
        for tt in range(NTT):
            sl = slice(tt * TT, (tt + 1) * TT)
            norm_fm(C, lambda c: hT[:, c, sl], [hTb[tt]], TT, 8, cst3[:, 0, :], cstb, lambda c: V[:, VB["n_mix1"] + c:VB["n_mix1"] + c + 1],
                    lambda c: bufA[:, c, sl], [bufAb[tt]], scr)
        P.fence(C.dummy[:, 0:1])
        NS = 4
        sqL = [(bufH[:, 0, i * 512:(i + 1) * 512], Buf("qk_sq%d" % i)) for i in range(NS)]
        f32v = [bufH[:, 1 + i, :].bitcast(F32) for i in range(4)]
        tl = [(f32v[i // 2][:, (i % 2) * 512:(i % 2 + 1) * 512], Buf("qk_t%d" % i)) for i in range(8)]
        rsL, qoL = tl[0:4], tl[4:8]
        qi = [0]
        P.op("dve", "tensor_scalar", PK(out=V[:, VB["da_qg"]:VB["da_qg"] + 1], in0=V[:, VB["da_qg"]:VB["da_qg"] + 1], scalar1=0.125, scalar2=None, op0=ALU.mult),
             reads=[vb], writes=[vb])
        for which, (dst, gcol) in enumerate(((qT_d, VB["da_qg"]), (kT_d, VB["da_kg"]))):
            def evac_qk(oc, tt, pt, pb, m, dst=dst, gcol=gcol):
                sl = slice(tt * TT, (tt + 1) * TT)
                k_ = qi[0] % NS; qi[0] += 1
                sq1, sq1b = sqL[k_]; rs, rsb = rsL[k_]; qo, qob = qoL[k_]
                P.op("act", "activation", PK(out=sq1, in_=pt[:, :], func=AF.Square), reads=[pb], writes=[sq1b])
                p2, p2b = C.psum()
                P.op("pe", "matmul", PK(p2[:, :], cst3[:, 2, :], sq1, start=True, stop=True), reads=[sq1b, cstb], writes=[p2b])
                rstd_from(C, rs, p2[:, :], p2b, rsb)
                P.op("dve", "scalar_tensor_tensor", PK(out=qo, in0=pt[:, :], scalar=V[:, gcol:gcol + 1], in1=rs,
                                                      op0=ALU.mult, op1=ALU.mult), reads=[pb, rsb, vb], writes=[qob])
                C.dma(dst[2 * oc:2 * oc + 2, :, sl].rearrange("c r n -> (c r) n"), qo, R=[qob])
            linear_fm(C, wqkv_d, 8, bufA, None, 1024, evac_qk, col0=which * 1024, xinbs=bufAb)
        vo, vob = scr["sg"]
        for half2 in range(2):
            wv2, wb2 = C.load_w(wqkv_d[:, 2048 + half2 * 512:2048 + (half2 + 1) * 512], 8, 512)
            for s in range(16):
                tt = s // 4
                pt, pb = C.psum()
                for k in range(8):
                    P.op("pe", "matmul", PK(pt[:, :], bufA[:, k, s * 128:(s + 1) * 128], wv2[:, k, :], start=(k == 0), stop=(k == 7)),
                         reads=[wb2, bufAb[tt]], writes=[pb])
                P.op("act", "activation", PK(out=vo[:, :], in_=pt[:, :], func=AF.Copy), reads=[pb], writes=[vob])
                C.dma(vtok_d[s * 128:(s + 1) * 128, half2 * 512:(half2 + 1) * 512], vo[:, :], R=[vob])


def body_C1(C, d, bufA, bufAb, bufs):
    P = C.P
    if True:
        qs_d = d["qT"]; ks_d = d["kT"]; vf_d = d["vtok"]; qa4_d = d["qaug4"]; ka4_d = d["kaug4"]
        bias_d = d["biasT"]; lamb_d = d["lamb"]; sgc_d = d["sgc"]; cst_d = d["cst"]; sel_d = d["sel"]
        h1b, qb_, kb_, vtb_ = bufs
        cst3, cstb = C.sb("cst_sb", [128, 5, 128], BF16)
        P.op("pool", "dma_start", PK(out=cst3[:, :, :], in_=cst_d.rearrange("p (a b) -> p a b", a=5)), writes=[cstb], is_dma=True)
        biasT, biasb = C.sb("bias_sb", [128, 8, 512], F32)
        C.dma(biasT[:, :, :], bias_d.rearrange("p (a b) -> p a b", a=8), W=[biasb])
        lamb, lambb = C.sb("lamb_sb", [128, 4, 64], F32)
        C.dma(lamb[:, :, :], lamb_d.rearrange("p (a b) -> p a b", a=4), W=[lambb])
        sgc, sgcb = C.sb("sgc_sb", [128, 1], F32)
        C.dma(sgc[:, :], sgc_d[:, :], W=[sgcb])
        scr = dict(sq=C.sb("sq", [128, 1, 512], BF16), rs=C.sb("rs", [128, 512], F32))
        lt, ltb = C.sb("lt", [128, 2, 64], F32)
        lc, lcb = C.sb("lc", [128, 4], F32)
        for i in range(2):
            P.op("dve", "tensor_tensor", PK(out=lt[:, i, :], in0=lamb[:, 2 * i, :], in1=lamb[:, 2 * i + 1, :], op=ALU.mult), reads=[lambb], writes=[ltb])
            P.op("dve", "reduce_sum", PK(out=lc[:, i:i + 1], in_=lt[:, i, :], axis=AX.X), reads=[ltb], writes=[lcb])
        P.op("act", "activation", PK(out=lc[:, 0:2], in_=lc[:, 0:2], func=AF.Exp), reads=[lcb], writes=[lcb])
        P.op("dve", "tensor_tensor", PK(out=lc[:, 2:3], in0=lc[:, 1:2], in1=lc[:, 0:1], op=ALU.subtract), reads=[lcb], writes=[lcb])
        P.op("dve", "tensor_scalar_add", PK(out=lc[:, 3:4], in0=lc[:, 2:3], scalar1=-LAMBDA_INIT), reads=[lcb], writes=[lcb])
        P.op("dve", "tensor_scalar", PK(out=sgc[:, :], in0=sgc[:, :], scalar1=float(1.0 - LAMBDA_INIT), scalar2=None, op0=ALU.mult), reads=[sgcb], writes=[sgcb])
        kA = [C.sb("kA%d" % i, [68, 2, 4096], BF16) for i in range(2)]
        qA = [C.sb("qA%d" % i, [68, 2, NTOK], BF16) for i in range(2)]
        vA = [C.sb("vA%d" % i, [128, 32, 128], BF16) for i in range(2)]
        qraw, qrawb = C.sb("qraw", [64, 2, 4096], BF16)
        sel, selb = C.sb("sel_sb", [128, 2], F32)
        C.dma(sel[:, :], sel_d[:, :], W=[selb])
        for i_ in range(2):
            for c_ in range(2):
                P.op("pool", "dma_start", PK(out=qA[i_][0][64:68, c_, :], in_=qa4_d[:, :]), writes=[qA[i_][1]], is_dma=True)
        NE_, NTMP = 8, 8
        et, _ = C.sb("et", [128, NE_, 512], BF16); etb = [Buf("et%d" % i) for i in range(NE_)]
        tmp, _ = C.sb("tmp", [128, NTMP, 512], F32); tmpb = [Buf("tmp%d" % i) for i in range(NTMP)]
        rd, rdb = C.sb("rd", [128, 512], F32)
        o0, o0b = C.sb("o0", [128, 512], F32)
        o1, o1b = C.sb("o1", [128, 512], F32)
        ot, otb = C.sb("ot", [128, 512], F32)
        ei = 0
        ti_ = [0]
        for h in range(8):
            slope = float(2.0 ** (-(h + 1)))
            kt, ktb = kA[h % 2]; qt, qtb = qA[h % 2]; vt, vtb = vA[h % 2]
            P.op("pool", "dma_start", PK(out=kt[0:64, :, :], in_=ks_d[2 * h:2 * h + 2].rearrange("c r n -> r c n")), reads=[kb_], writes=[ktb], is_dma=True)
            P.op("pool", "dma_start", PK(out=kt[64:68, 0, :], in_=ka4_d[h]), writes=[ktb], is_dma=True)
            P.op("pool", "dma_start", PK(out=kt[64:68, 1, :], in_=ka4_d[h]), writes=[ktb], is_dma=True)
            P.op("pool", "dma_start", PK(out=qraw[:, :, :], in_=qs_d[2 * h:2 * h + 2].rearrange("c r n -> r c n")), reads=[qb_], writes=[qrawb], is_dma=True)
            for c in range(2):
                q5 = qraw[:, c, :].rearrange("p (m two n) -> p m two n", two=2, n=512)
                qo4 = qt[0:64, c, :].rearrange("p (m n) -> p m n", n=512)
                P.op("dve", "tensor_scalar", PK(out=qo4, in0=q5[:, :, 0, :], scalar1=sel[0:64, 0:1], scalar2=None, op0=ALU.mult), reads=[qrawb, selb], writes=[qtb])
                P.op("dve", "scalar_tensor_tensor", PK(out=qo4, in0=q5[:, :, 1, :], scalar=sel[0:64, 1:2], in1=qo4, op0=ALU.mult, op1=ALU.add),
                     reads=[qrawb, selb, qtb], writes=[qtb])
            P.op("pool", "dma_start", PK(out=vt[:, :, :], in_=vf_d[:, h * 128:(h + 1) * 128].rearrange("(j p) d -> p j d", p=128)), reads=[vtb_], writes=[vtb], is_dma=True)
            for m in range(4):
                nkb = 8 * (m + 1)
                qsl = slice(m * 512, (m + 1) * 512)
                N = [C.ps[0], C.ps[1]]
                Dn = [C.ps[2], C.ps[3]]
                LOOK = 3
                slot_of = {}

                def stage1(j):
                    for c in range(2):
                        sc, scb = C.ps[4 + ((2 * j + c) % 4)]
                        P.op("pe", "matmul", PK(sc[:, :], kt[:, c, j * 128:(j + 1) * 128], qt[:, c, qsl], start=True, stop=True),
                             reads=[ktb, qtb], writes=[scb])
                        tb = ti_[0] % NTMP; ti_[0] += 1
                        slot_of[(j, c)] = tb
                        if j >= nkb - 8:
                            s = j - (nkb - 8)
                            P.op("dve", "scalar_tensor_tensor", PK(out=tmp[:, tb, :], in0=biasT[:, s, :], scalar=slope, in1=sc[:, :], op0=ALU.mult, op1=ALU.add),
                                 reads=[biasb, scb], writes=[tmpb[tb]])
                        else:
                            P.op("dve", "tensor_copy", PK(out=tmp[:, tb, :], in_=sc[:, :]), reads=[scb], writes=[tmpb[tb]])
                for j0 in range(min(LOOK, nkb)):
                    stage1(j0)
                for j in range(nkb):
                    if j + LOOK < nkb:
                        stage1(j + LOOK)
                    for c in range(2):
                        e_slot = ei % NE_; ei += 1
                        tb = slot_of[(j, c)]
                        P.op("act", "activation", PK(out=et[:, e_slot, :], in_=tmp[:, tb, :], func=AF.Exp), reads=[tmpb[tb]], writes=[etb[e_slot]])
                        P.op("pe", "matmul", PK(N[c][0][:, :], vt[:, j, :], et[:, e_slot, :], start=(j == 0), stop=(j == nkb - 1)),
                             reads=[vtb, etb[e_slot]], writes=[N[c][1]])
                        P.op("pe", "matmul", PK(Dn[c][0][:, :], cst3[:, 4, :], et[:, e_slot, :], start=(j == 0), stop=(j == nkb - 1)),
                             reads=[cstb, etb[e_slot]], writes=[Dn[c][1]])
                P.op("dve", "reciprocal", PK(out=rd[:, :], in_=Dn[0][0][:, :]), reads=[Dn[0][1]], writes=[rdb])
                P.op("dve", "tensor_tensor", PK(out=o0[:, :], in0=N[0][0][:, :], in1=rd[:, :], op=ALU.mult), reads=[N[0][1], rdb], writes=[o0b])
                P.op("dve", "reciprocal", PK(out=rd[:, :], in_=Dn[1][0][:, :]), reads=[Dn[1][1]], writes=[rdb])
                P.op("dve", "tensor_tensor", PK(out=o1[:, :], in0=N[1][0][:, :], in1=rd[:, :], op=ALU.mult), reads=[N[1][1], rdb], writes=[o1b])
                P.op("dve", "scalar_tensor_tensor", PK(out=o0[:, :], in0=o1[:, :], scalar=lc[:, 3:4], in1=o0[:, :], op0=ALU.mult, op1=ALU.add),
                     reads=[o1b, o0b, lcb], writes=[o0b])
                sq, sqb = scr["sq"]; rs, rsb = scr["rs"]
                P.op("act", "activation", PK(out=sq[:, 0, :], in_=o0[:, :], func=AF.Square), reads=[o0b], writes=[sqb])
                pn, pnb = C.ps[4]
                P.op("pe", "matmul", PK(pn[:, :], cst3[:, 3, :], sq[:, 0, :], start=True, stop=True), reads=[sqb, cstb], writes=[pnb])
                rstd_from(C, rs[:, :], pn[:, :], pnb, rsb)
                P.op("dve", "scalar_tensor_tensor", PK(out=bufA[:, h, qsl], in0=o0[:, :], scalar=sgc[:, 0:1], in1=rs[:, :], op0=ALU.mult, op1=ALU.mult),
                     reads=[o0b, rsb, sgcb], writes=[bufAb[m]])


def body_C2(C, d, bufA, bufAb, h1b):
    P = C.P
    if True:
        h1s_d = d["h1T"]; memT_d = d["memT"]; vec_d = d["vecsC"]; cst_d = d["cst"]; c32_d = d["c32"]; sel_d = d["sel"]
        wdo_d = d["da_w_o"]; wq_d = d["w_q1"]; wkv_d = d["w_kv1"]; wo_d = d["w_o1"]; wr_d = d["w_router"]
        wg_d = d["moe_g"]; wu_d = d["moe_u"]; wd_d = d["moe_d"]; outT_d = d["outT"]
        C.init_w(3, 4096)
        hT, _ = C.sb("hT", [128, 8, NTOK], F32); hTb = [Buf("hT%d" % i) for i in range(NTT)]
        bufH, _ = C.sb("bufH", [128, 8, NTOK], BF16); bufHb = [Buf("bH%d" % i) for i in range(NTT)]
        V, vb = C.sb("V_sb", [128, NVC], F32); C.vb = vb
        cst3, cstb = C.sb("cst_sb", [128, 5, 128], BF16)
        c32, c32b = C.sb("c32_sb", [128, 2, 128], F32)
        scr = dict(sq=C.sb("sq", [128, 8, 512], BF16), rs=C.sb("rs", [128, 512], F32), sg=C.sb("sg", [128, 512], F32),
                   rd=C.sb("rd", [128, 512], F32))
        sqq, _ = C.sb("sqq", [128, 2, NTT, 512], BF16)
        scr["sqq"] = (sqq, [Buf("sqq%d" % i) for i in range(NTT)])
        et, _ = C.sb("et", [128, 2, 512], BF16)
        scr["et"] = (et, [Buf("et0"), Buf("et1")])
        C.dma(V[:, :], vec_d[:, :], W=[vb])
        C.dma(c32[:, :, :], c32_d.rearrange("p (a b) -> p a b", a=2), W=[c32b])
        P.op("pool", "dma_start", PK(out=cst3[:, :, :], in_=cst_d.rearrange("p (a b) -> p a b", a=5)), writes=[cstb], is_dma=True)
        sel, selb = C.sb("sel_sb", [128, 2], F32)
        C.dma(sel[:, :], sel_d[:, :], W=[selb])
        htmp, _ = C.sb("htmp", [128, 2, 512], F32); htmpb = [Buf("htmp0"), Buf("htmp1")]
        hi_ = 0
        for m in range(NTT):
            sl = slice(m * TT, (m + 1) * TT)
            e0 = slice((2 * m) * TT, (2 * m + 1) * TT); e1 = slice((2 * m + 1) * TT, (2 * m + 2) * TT)
            C.dma(hT[:, :, sl], h1s_d[:, e0].rearrange("(c p) n -> p c n", p=128), R=[h1b], W=[hTb[m]])
            for c in range(8):
                hb = hi_ % 2; hi_ += 1
                C.dma(htmp[:, hb, :], h1s_d[c * 128:(c + 1) * 128, e1], R=[h1b], W=[htmpb[hb]])
                P.op("dve", "tensor_scalar", PK(out=hT[:, c, sl], in0=hT[:, c, sl], scalar1=sel[:, 0:1], scalar2=None, op0=ALU.mult), reads=[hTb[m], selb], writes=[hTb[m]])
                P.op("dve", "scalar_tensor_tensor", PK(out=hT[:, c, sl], in0=htmp[:, hb, :], scalar=sel[:, 1:2], in1=hT[:, c, sl], op0=ALU.mult, op1=ALU.add),
                     reads=[htmpb[hb], selb, hTb[m]], writes=[hTb[m]])
        def evac_res(oc, tt, pt, pb, m):
            sl = slice(tt * TT, (tt + 1) * TT)
            P.op("dve", "tensor_tensor", PK(out=hT[:, oc, sl], in0=pt[:, :], in1=hT[:, oc, sl], op=ALU.add), reads=[pb, hTb[tt]], writes=[hTb[tt]])
        linear_fm(C, wdo_d, 8, bufA, None, 1024, evac_res, xinbs=bufAb)
        _mem_off0 = C.off
        (kn, knb), (vt, vtb) = mem_kv(C, memT_d, wkv_d, V, vb, VC["n_mem"], VC["k_gain"], cst3, cstb, scr, "m1")
        _mem_off1 = C.off
        mem_xattn(C, hT, hTb, bufA, bufAb, bufH, bufHb, V, vb, VC["n_xattn"], VC["q_gain"], wq_d, wo_d, kn, knb, vt, vtb, cst3, cstb, scr)
        for tt in range(NTT):
            sl = slice(tt * TT, (tt + 1) * TT)
            norm_fm(C, lambda c: hT[:, c, sl], [hTb[tt]], TT, 8, cst3[:, 0, :], cstb, lambda c: V[:, VC["n_ffn"] + c:VC["n_ffn"] + c + 1],
                    lambda c: bufA[:, c, sl], [bufAb[tt]], scr)
        hn32, hn32b = C.sb("hn32", [128, 8, 128], F32)
        wr, wrb = C.sb("wr", [128, 8, 8], F32)
        G, Gb = C.sb("G", [128, 16, 8], F32)
        lg, lgb = C.sb("lg", [128, 8], F32)
        mx, mxb = C.sb("mx", [128, 8], F32)
        sm, smb = C.sb("sm", [128, 4], F32)
        C.dma(wr[:, :, :], wr_d.rearrange("(c p) n -> p c n", p=128), W=[wrb])
        for s in range(16):
            tt = s // 4
            sl = slice(s * 128, (s + 1) * 128)
            norm_fm(C, lambda c: hT[:, c, sl], [hTb[tt]], 128, 8, cst3[:, 0, :], cstb, lambda c: V[:, VC["n_ffn"] + c:VC["n_ffn"] + c + 1],
                    lambda c: hn32[:, c, :], [hn32b], scr)
            pt, pb = C.psum()
            for k in range(8):
                P.op("pe", "matmul", PK(pt[:, 0:8], hn32[:, k, :], wr[:, k, :], start=(k == 0), stop=(k == 7)), reads=[hn32b, wrb], writes=[pb])
            P.op("dve", "tensor_tensor", PK(out=lg[:, :], in0=pt[:, 0:8], in1=V[:, VC["b_router"]:VC["b_router"] + 8], op=ALU.add),
                 reads=[pb, vb], writes=[lgb])
            P.op("dve", "max", PK(out=mx[:, :], in_=lg[:, :]), reads=[lgb], writes=[mxb])
            P.op("dve", "tensor_scalar", PK(out=sm[:, 0:1], in0=mx[:, 0:1], scalar1=-1.0, scalar2=None, op0=ALU.mult), reads=[mxb], writes=[smb])
            ex, exb = scr["sg"]
            P.op("act", "activation", PK(out=ex[:, 0:8], in_=lg[:, :], func=AF.Exp, bias=sm[:, 0:1]), reads=[lgb, smb], writes=[exb])
            P.op("dve", "tensor_scalar", PK(out=lg[:, :], in0=lg[:, :], scalar1=mx[:, 1:2], scalar2=None, op0=ALU.is_ge), reads=[lgb, mxb], writes=[lgb])
            P.op("dve", "tensor_tensor", PK(out=ex[:, 0:8], in0=ex[:, 0:8], in1=lg[:, :], op=ALU.mult), reads=[exb, lgb], writes=[exb])
            P.op("dve", "reduce_sum", PK(out=sm[:, 1:2], in_=ex[:, 0:8], axis=AX.X), reads=[exb], writes=[smb])
            P.op("dve", "reciprocal", PK(out=sm[:, 2:3], in_=sm[:, 1:2]), reads=[smb], writes=[smb])
            P.op("dve", "tensor_scalar", PK(out=G[:, s, :], in0=ex[:, 0:8], scalar1=sm[:, 2:3], scalar2=None, op0=ALU.mult), reads=[exb, smb], writes=[Gb])
        P.fence(C.dummy[:, 0:1])
        gbc = sqq.rearrange("p a b c -> p (a b c)")[:, 0:NTOK]; gbcb = [Buf("gbc%d" % i) for i in range(NTT)]
        C.wbufs.append((scr["sq"][0].rearrange("p a b -> p (a b)"), Buf("wb_sq")))
        _o = _mem_off0
        while _o + 2048 <= _mem_off1:
            C.wbufs.append((C.arena[:, _o:_o + 2048].bitcast(BF16), Buf("wb_m%d" % _o)))
            _o += 2048
        dg, dgb = scr["rd"]
        for e_ in range(8):
            for tt in range(NTT):
                pt, pb = C.psum()
                for q in range(4):
                    s = tt * 4 + q
                    P.op("dve", "tensor_scalar", PK(out=dg[:, 0:128], in0=c32[:, 0, :], scalar1=G[:, s, e_:e_ + 1], scalar2=None, op0=ALU.mult),
                         reads=[c32b, Gb], writes=[dgb])
                    P.op("pe", "matmul", PK(pt[:, q * 128:(q + 1) * 128], c32[:, 1, :], dg[:, 0:128], start=True, stop=True), reads=[dgb, c32b], writes=[pb])
                P.op("act", "activation", PK(out=gbc[:, tt * TT:(tt + 1) * TT], in_=pt[:, :], func=AF.Copy), reads=[pb], writes=[gbcb[tt]])
            swiglu_ffn(C, hT, hTb, bufA, bufAb, bufH, bufHb, V, vb, None, wg_d[e_], wu_d[e_], wd_d[e_], 3584, cst3, cstb, scr, gate_bc=(gbc, gbcb))
        for tt in range(NTT):
            sl = slice(tt * TT, (tt + 1) * TT)
            C.dma(outT_d[:, sl].rearrange("(c p) n -> p c n", p=128), hT[:, :, sl], R=[hTb[tt]], is_out=True)


ARENA_WORDS = 53200


def build_fused():
    nc = bass.Bass("TRN2", target_bir_lowering=False)
    d = {}

    def I(n, s):
        d[n] = nc.dram_tensor(n, s, F32, kind="ExternalInput").ap()

    def S(n, s):
        d[n] = nc.dram_tensor(n, s, F32, kind="Internal").ap()
    I("xT", [D, T_SEQ]); I("memT", [D, 256]); I("cst", [128, 640]); I("c32A", [128, 384]); I("c32", [128, 256])
    I("mask5", [64, 320]); I("rmask", [128, SEG]); I("vecsB", [128, NVB]); I("vecsC", [128, NVC]); I("sel", [128, 2])
    I("qaug4", [4, NTOK]); I("kaug4", [8, 4, T_SEQ]); I("biasT", [128, 8 * 512]); I("lamb", [128, 256]); I("sgc", [128, 1])
    for hh in range(2):
        I("wc%d" % hh, [D, NCOLS]); I("vecsA%d" % hh, [128, NVA]); I("w_up%d" % hh, [64, 256]); I("a_up%d" % hh, [128, 256])
        I("g_upa%d" % hh, [128, 256]); I("g_upb%d" % hh, [32, 256])
        for n in ("breT", "bimT", "creT", "cimT"):
            I("%s%d" % (n, hh), [8, 128, 128])
    I("w_glu", [512, 512]); I("w_out", [D, D]); I("w_q0", [D, D]); I("w_kv0", [D, 2 * D]); I("w_o0", [D, D])
    I("ff_g", [D, 2816]); I("ff_u", [D, 2816]); I("ff_d", [2816, D]); I("w_qkv", [D, 3 * D])
    I("da_w_o", [D, D]); I("w_q1", [D, D]); I("w_kv1", [D, 2 * D]); I("w_o1", [D, D]); I("w_router", [D, 8])
    I("moe_g", [8, D, 3584]); I("moe_u", [8, D, 3584]); I("moe_d", [8, 3584, D])
    S("ymT", [D, T_SEQ]); S("h1T", [D, T_SEQ]); S("qT", [16, 64, T_SEQ]); S("kT", [16, 64, T_SEQ]); S("vtok", [T_SEQ, D])
    d["outT"] = nc.dram_tensor("outT", [D, NTOK], F32, kind="ExternalOutput").ap()
    with ExitStack() as st:
        P = Prog(nc)
        C = ACtx(nc, st, P, ARENA_WORDS)
        ymb, h1b, qb_, kb_, vtb_ = Buf("ymT"), Buf("h1T"), Buf("qT"), Buf("kT"), Buf("vtok")
        for hh in range(2):
            if hh:
                C.new_phase()
            body_A(C, d, hh, ymb)
        import os as _os
        if _os.environ.get("FSTOP") == "A":
            C.dma(d["outT"][:, 0:512], d["ymT"][:, 0:512], R=[ymb], is_out=True)
            P.emit(st)
            return nc
        for half in range(2):
            C.new_phase()
            body_B(C, d, half, (ymb, h1b, qb_, kb_, vtb_))
        C.new_phase()
        bufA, _ = C.sb("bufA_p", [128, 8, NTOK], BF16); bufAb = [Buf("bAp%d" % i) for i in range(NTT)]
        keep = C.off
        body_C1(C, d, bufA, bufAb, (h1b, qb_, kb_, vtb_))
        C.new_phase(keep=keep)
        body_C2(C, d, bufA, bufAb, h1b)
        P.emit(st)
    return nc


def pack_fused(inp, b, p):
    m = {}
    for hh in range(2):
        a = pack_A(inp, b, hh)
        if hh == 0:
            m["xT"] = a["xT"]; m["cst"] = a["cst"]; m["c32A"] = a["c32"]; m["mask5"] = a["mask5"]; m["rmask"] = a["rmask"]
        m["wc%d" % hh] = a["wc"]; m["vecsA%d" % hh] = a["vecs"]; m["w_up%d" % hh] = a["w_up"]; m["a_up%d" % hh] = a["a_up"]
        m["g_upa%d" % hh] = a["g_upa"]; m["g_upb%d" % hh] = a["g_upb"]
        for n in ("breT", "bimT", "creT", "cimT"):
            m["%s%d" % (n, hh)] = a[n]
    m["memT"] = np.ascontiguousarray(np.asarray(inp["mem"][b]).T)
    m["c32"] = c32_table(); m["vecsB"] = vecs_B(inp); m["vecsC"] = vecs_C(inp)
    sel = np.zeros((128, 2), np.float32); sel[:, p] = 1.0
    m["sel"] = sel
    m["qaug4"] = q_aug_rows(p)
    m["kaug4"] = np.stack([k_aug_rows(h) for h in range(8)])
    m["biasT"] = bias_table(p)
    m["lamb"] = np.ascontiguousarray(np.broadcast_to(np.stack([inp["da_lam_q1"][0], inp["da_lam_k1"][0], inp["da_lam_q2"][0],
                                                               inp["da_lam_k2"][0]]).reshape(1, 256), (128, 256))).astype(np.float32)
    m["sgc"] = np.asarray(inp["da_sub_gain"][0], np.float32).reshape(128, 1)
    for k_, src in (("w_glu", "s5_w_glu"), ("w_out", "hy_w_out"), ("ff_g", "ff_w_gate"), ("ff_u", "ff_w_up"), ("ff_d", "ff_w_down"),
                    ("w_qkv", "da_w_qkv"), ("da_w_o", "da_w_o"), ("w_router", "moe_w_router"), ("moe_g", "moe_w_gate"),
                    ("moe_u", "moe_w_up"), ("moe_d", "moe_w_down")):
        m[k_] = np.asarray(inp[src][0])
    for l in range(2):
        m["w_q%d" % l] = np.asarray(inp["xa_w_q"][l]); m["w_kv%d" % l] = np.asarray(inp["xa_w_kv"][l]); m["w_o%d" % l] = np.asarray(inp["xa_w_o"][l])
    return m


_CACHE = {}


def kernel(**inp):
    inp = {k: np.asarray(v) for k, v in inp.items()}
    B_, T_ = 4, 4096
    if "F" not in _CACHE:
        _CACHE["F"] = build_fused()
    cores = [(b, p) for b in range(B_) for p in range(2)]
    maps = [pack_fused(inp, b, p) for (b, p) in cores]
    res = run_bass_kernel_spmd(_CACHE["F"], maps, core_ids=list(range(8))).results
    out = np.zeros((B_, T_, 1024), np.float32)
    for i, (b, p) in enumerate(cores):
        out[b][tok_index(p)] = np.asarray(res[i]["outT"]).T
    return out
```

```python
from contextlib import ExitStack
import numpy as np
import concourse.bass as bass
import concourse.mybir as mybir
from concourse.bass_utils import run_bass_kernel_spmd

F32 = mybir.dt.float32
BF16 = mybir.dt.bfloat16
AF = mybir.ActivationFunctionType
ALU = mybir.AluOpType
AX = mybir.AxisListType

ENGS = ("pe", "act", "dve", "pool", "sp")


def PK(*a, **k):
    return (a, k)


class Buf:
    __slots__ = ("name", "w", "rs", "excl")

    def __init__(self, name, excl=False):
        self.name = name
        self.excl = excl
        self.w = None
        self.rs = []


class Op:
    __slots__ = ("eng", "fn", "deps", "is_dma", "needed", "idx", "semval", "dsem")

    def __init__(self, eng, fn, is_dma):
        self.eng = eng
        self.fn = fn
        self.deps = set()
        self.is_dma = is_dma
        self.needed = False
        self.semval = None
        self.dsem = None


class Prog:
    def __init__(self, nc, n_dma_sems=24):
        self.nc = nc
        self.ops = []
        self.n_dma_sems = n_dma_sems
        self.out_dma_ops = []
        self.fence_idx = None
        self.last = {}
        self.dmas_since = []

    def op(self, eng, fn, pack=None, reads=(), writes=(), is_dma=False, is_out=False, after=()):
        if isinstance(fn, str):
            _name, _a, _k = fn, pack[0], pack[1]
            fn = lambda e: getattr(e, _name)(*_a, **_k)
        o = Op(eng, fn, is_dma)
        o.idx = len(self.ops)
        ops = self.ops
        ex = [b for b in reads if b.excl and b not in writes]
        if ex:
            reads = [b for b in reads if not b.excl]
            writes = list(writes) + ex

        def add(d, raw):
            p = ops[d]
            if not (p.is_dma or is_dma) and p.eng == eng:
                if eng == "pe":
                    return
            o.deps.add(d)
        for b in reads:
            if b.w is not None:
                add(b.w, True)
        for b in writes:
            if b.w is not None:
                add(b.w, False)
            for r in b.rs:
                add(r, False)
        if self.fence_idx is not None:
            o.deps.add(self.fence_idx)
        for x in after:
            o.deps.add(x.idx)
        for b in reads:
            b.rs.append(o.idx)
        for b in writes:
            b.w = o.idx
            b.rs = []
        if is_dma:
            self.dmas_since.append(o.idx)
        else:
            self.last[eng] = o.idx
        self.ops.append(o)
        if is_out:
            self.out_dma_ops.append(o.idx)
        return o

    def fence(self, dummy_ap):
        deps = set(self.last.values()) | set(self.dmas_since)
        o = self.op("dve", "memset", PK(dummy_ap, 0.0))
        o.deps |= deps
        self.fence_idx = o.idx
        self.dmas_since = []
        return o

    def emit(self, stack):
        nc = self.nc
        ops = self.ops
        for o in ops:
            best = {}
            keep = set()
            for d in o.deps:
                p = ops[d]
                if p.is_dma:
                    keep.add(d)
                else:
                    if p.eng not in best or d > best[p.eng]:
                        best[p.eng] = d
            for e, d in best.items():
                keep.add(d)
            o.deps = keep
            for d in keep:
                ops[d].needed = True
        for i in self.out_dma_ops:
            ops[i].needed = True
        sems = {e: stack.enter_context(nc.semaphore("s_" + e)) for e in ENGS if e != "sp"}
        dsems = [stack.enter_context(nc.semaphore("d%d" % i)) for i in range(self.n_dma_sems)]
        cnt = {e: 0 for e in sems}
        dcnt = [0] * len(dsems)
        rr = {"sw": 0, "hw": 0}
        nsw = len(dsems) // 2
        pools = {"sw": list(range(0, nsw)), "hw": list(range(nsw, len(dsems)))}
        lastd = {}
        per_eng = {e: [] for e in ENGS}
        for o in ops:
            if o.is_dma:
                o.needed = True
                kind = "sw" if o.eng == "pool" else "hw"
                pl = pools[kind]
                k = pl[rr[kind] % len(pl)]
                rr[kind] += 1
                prev = lastd.get(k)
                if prev is not None:
                    o.deps.add(prev)
                lastd[k] = o.idx
                dcnt[k] += 16
                o.dsem = dsems[k]
                o.semval = dcnt[k]
            elif o.needed:
                cnt[o.eng] += 1
                o.semval = cnt[o.eng]
            per_eng[o.eng].append(o)
        self.stats = {e: len(per_eng[e]) for e in ENGS}
        self.stats["sem_max"] = dict(cnt)
        block = stack.enter_context(nc.Block())

        def run(engname, eng):
            seen = {}
            for o in per_eng[engname]:
                waits = []
                for d in sorted(o.deps):
                    p = ops[d]
                    s = p.dsem if p.is_dma else sems[p.eng]
                    key = id(s)
                    if seen.get(key, -1) >= p.semval:
                        continue
                    seen[key] = p.semval
                    waits.append((s, p.semval))
                emb = None
                if waits and not o.is_dma:
                    emb = waits.pop()
                for (s, v) in waits:
                    eng.wait_ge(s, v)
                ins = o.fn(eng)
                if emb is not None:
                    ins._wait_ge(emb[0], emb[1])
                if o.is_dma:
                    ins.then_inc(o.dsem, 16)
                elif o.needed:
                    ins.then_inc(sems[o.eng], 1)
            if engname == "sp":
                for i in self.out_dma_ops:
                    p = ops[i]
                    eng.wait_ge(p.dsem, p.semval)

        @block.tensor
        def _(e):
            run("pe", e)

        @block.scalar
        def _(e):
            run("act", e)

        @block.vector
        def _(e):
            run("dve", e)

        @block.gpsimd
        def _(e):
            run("pool", e)

        @block.sync
        def _(e):
            run("sp", e)


D = 1024
NTOK = 2048
TT = 512
NTT = NTOK // TT
EPS = 1e-6


class Ctx:
    def __init__(self, nc, st, P):
        self.nc, self.st, self.P = nc, st, P
        self.ps = []
        for i in range(8):
            t = st.enter_context(nc.psum_tensor("ps%d" % i, [128, 512], F32))
            self.ps.append((t, Buf("ps%d" % i, excl=True)))
        self.psi = 0
        self.epsc, self.epsb = self.sb("epsc", [128, 2], F32)
        P.op("dve", "memset", PK(self.epsc[:, 0:1], EPS), writes=[self.epsb])
        P.op("dve", "memset", PK(self.epsc[:, 1:2], 64e-5), writes=[self.epsb])
        self.wbufs = []
        self.wi = 0
        self.dmaq = 0

    def sb(self, name, shape, dt):
        t = self.st.enter_context(self.nc.sbuf_tensor(name, shape, dt))
        return t, Buf(name)

    def psum(self):
        r = self.ps[self.psi % 8]
        self.psi += 1
        return r

    def init_w(self, n, cols):
        for i in range(n):
            self.wbufs.append(self.sb("wb%d" % i, [128, cols], BF16))

    def wbuf(self):
        r = self.wbufs[self.wi % len(self.wbufs)]
        self.wi += 1
        return r

    def load_w(self, src_ap, kc, ncols):
        t, b = self.wbuf()
        view = t[:, 0:kc * ncols].rearrange("p (c n) -> p c n", c=kc)
        src = src_ap.rearrange("(c p) n -> p c n", p=128)
        self.P.op("pool", "dma_start", PK(out=view, in_=src), writes=[b], is_dma=True)
        return view, b

    def dma(self, out, in_, R=(), W=(), is_out=False, q=None):
        if q is None:
            q = "sp"
        return self.P.op(q, "dma_start", PK(out=out, in_=in_), reads=R, writes=W, is_dma=True, is_out=is_out)


def rmsnorm_fm(C, xT, xb, gcol, outT, outb, tt, scr, dim_chunks=8, extra_scale=1.0):
    P = C.P
    sq, sqb = scr["sq"]
    rs, rsb = scr["rs"]
    ones, onesb = scr["ones"]
    sl = slice(tt * TT, (tt + 1) * TT)
    pt, pb = C.psum()
    for c in range(dim_chunks):
        P.op("act", "activation", PK(out=sq[:, c, :], in_=xT[:, c, sl], func=AF.Square), reads=[xb], writes=[sqb])
    for c in range(dim_chunks):
        P.op("pe", "matmul", PK(pt[:, :], ones[:, :], sq[:, c, :], start=(c == 0), stop=(c == dim_chunks - 1)),
             reads=[sqb, onesb], writes=[pb])
    dd = dim_chunks * 128
    P.op("dve", "tensor_scalar", PK(out=rs[:, :], in0=pt[:, :], scalar1=float(dd * EPS), scalar2=-0.5, op0=ALU.add, op1=ALU.pow),
         reads=[pb], writes=[rsb])
    sc = float(np.sqrt(dd) * extra_scale)
    for c in range(dim_chunks):
        eng = "dve"
        P.op(eng, "scalar_tensor_tensor", PK(out=outT[:, c, sl], in0=xT[:, c, sl], scalar=gcol[:, c:c + 1], in1=rs[:, :],
                                                       op0=ALU.mult, op1=ALU.mult),
             reads=[xb, rsb], writes=[outb])
    return sc


def linear_fm(C, W_dram, kc, xin, xinb, n_out, evac, tts=range(NTT), col0=0, blk=512, mcols=128, xinbs=None):
    P = C.P
    nblk = (n_out + blk - 1) // blk
    oc = 0
    for bi in range(nblk):
        c0 = col0 + bi * blk
        w = min(blk, n_out - bi * blk)
        wv, wb = C.load_w(W_dram[:, c0:c0 + w], kc, w)
        for j in range(0, w, mcols):
            m = min(mcols, w - j)
            for tt in tts:
                pt, pb = C.psum()
                sl = slice(tt * TT, (tt + 1) * TT)
                for k in range(kc):
                    P.op("pe", "matmul", PK(pt[0:m, :], wv[:, k, j:j + m], xin[:, k, sl],
                                                                              start=(k == 0), stop=(k == kc - 1)),
                         reads=[wb, (xinbs[tt] if xinbs is not None else xinb)], writes=[pb])
                evac(oc, tt, pt, pb, m)
            oc += 1


def rstd_from(C, rs_ap, ps_ap, pb, rsb, eps=EPS):
    np_ = rs_ap.shape[0]
    C.P.op("act", "activation", PK(out=rs_ap, in_=ps_ap, func=AF.Sqrt, bias=C.epsc[0:np_, 0:1] if eps == EPS else C.epsc[0:np_, 1:2]),
           reads=[pb, C.epsb], writes=[rsb])
    C.P.op("dve", "reciprocal", PK(out=rs_ap, in_=rs_ap), reads=[rsb], writes=[rsb])


def norm_fm(C, xin, xb, n, dim_chunks, onesm, onesb, gcol, out, outb, scr, sq_from_psum=False):
    P = C.P
    sq, sqb = scr["sq"]
    rs, rsb = scr["rs"]
    pt, pb = C.psum()
    for c in range(dim_chunks):
        P.op("act", "activation", PK(out=sq[:, c, 0:n], in_=xin(c), func=AF.Square), reads=xb, writes=[sqb])
    for c in range(dim_chunks):
        P.op("pe", "matmul", PK(pt[:, 0:n], onesm, sq[:, c, 0:n], start=(c == 0), stop=(c == dim_chunks - 1)),
             reads=[sqb, onesb], writes=[pb])
    rstd_from(C, rs[:, 0:n], pt[:, 0:n], pb, rsb)
    for c in range(dim_chunks):
        eng = "dve"
        P.op(eng, "scalar_tensor_tensor", PK(out=out(c), in0=xin(c), scalar=gcol(c), in1=rs[:, 0:n],
                                                       op0=ALU.mult, op1=ALU.mult),
             reads=list(xb) + [rsb] + ([C.vb] if getattr(C, 'vb', None) is not None else []), writes=outb)


def mem_kv(C, memT_d, wkv_d, V, vb, col_nm, col_kg, cst, cstb, scr, name):
    P = C.P
    mT, mTb = C.sb(name + "_mT", [128, 8, 256], BF16)
    mn, mnb = C.sb(name + "_mn", [128, 8, 256], BF16)
    kf, kfb = C.sb(name + "_kf", [128, 8, 256], BF16)
    kn, knb = C.sb(name + "_kn", [128, 8, 256], BF16)
    vt, vtb = C.sb(name + "_vt", [128, 2, 1024], BF16)
    P.op("pool", "dma_start", PK(out=mT[:, :, :], in_=memT_d.rearrange("(c p) n -> p c n", p=128)), writes=[mTb], is_dma=True)
    norm_fm(C, lambda c: mT[:, c, :], [mTb], 256, 8, cst[:, 0, :], cstb, lambda c: V[:, col_nm + c:col_nm + c + 1],
            lambda c: mn[:, c, :], [mnb], scr)
    for half in range(2):
        wv, wb = C.load_w(wkv_d[:, half * 512:(half + 1) * 512], 8, 512)
        for j in range(4):
            oc = half * 4 + j
            pt, pb = C.psum()
            for k in range(8):
                P.op("pe", "matmul", PK(pt[:, 0:256], wv[:, k, j * 128:(j + 1) * 128], mn[:, k, :],
                                                                      start=(k == 0), stop=(k == 7)), reads=[wb, mnb], writes=[pb])
            P.op("dve", "tensor_copy", PK(out=kf[:, oc, :], in_=pt[:, 0:256]), reads=[pb], writes=[kfb])
    for hh in range(4):
        norm_fm(C, lambda c, hh=hh: kf[:, hh * 2 + c, :], [kfb], 256, 2, cst[:, 1, :], cstb,
                lambda c: V[:, col_kg + c:col_kg + c + 1], lambda c, hh=hh: kn[:, hh * 2 + c, :], [knb], scr)
    for half in range(2):
        wv, wb = C.load_w(wkv_d[:, 1024 + half * 512:1024 + (half + 1) * 512], 8, 512)
        for j in range(2):
            pt, pb = C.psum()
            for k in range(8):
                P.op("pe", "matmul", PK(pt[:, :], mn[:, k, j * 128:(j + 1) * 128], wv[:, k, :],
                                                                      start=(k == 0), stop=(k == 7)), reads=[wb, mnb], writes=[pb])
            P.op("act", "activation", PK(out=vt[:, j, half * 512:(half + 1) * 512], in_=pt[:, :], func=AF.Copy),
                 reads=[pb], writes=[vtb])
    return (kn, knb), (vt, vtb)


def mem_xattn(C, hT, hTb, bufA, bufAb, bufQ, bufQb, V, vb, col_nx, col_qg, wq_d, wo_d, kn, knb, vt, vtb, cst, cstb, scr, stop=0):
    P = C.P
    for tt in range(NTT):
        sl = slice(tt * TT, (tt + 1) * TT)
        norm_fm(C, lambda c: hT[:, c, sl], [hTb[tt]], TT, 8, cst[:, 0, :], cstb, lambda c: V[:, col_nx + c:col_nx + c + 1],
                lambda c: bufA[:, c, sl], [bufAb[tt]], scr)
    if stop == 16:
        return
    sqq, sqqb = scr["sqq"]
    rs, rsb = scr["rs"]

    def evac_q(oc, tt, pt, pb, m):
        sl = slice(tt * TT, (tt + 1) * TT)
        P.op("act", "activation", PK(out=sqq[:, oc % 2, tt, :], in_=pt[:, :], func=AF.Square), reads=[pb], writes=[sqqb[tt]])
        P.op("dve", "tensor_copy", PK(out=bufQ[:, oc, sl], in_=pt[:, :]), reads=[pb], writes=[bufQb[tt]])
        import os
        V17 = os.environ.get('V17', '')
        if oc % 2 == 1 and V17 != 'a':
            p2, p2b = C.psum()
            for c in range(2):
                P.op("pe", "matmul", PK(p2[:, :], cst[:, 1, :], sqq[:, c, tt, :], start=(c == 0), stop=(c == 1)),
                     reads=[sqqb[tt], cstb], writes=[p2b])
            rstd_from(C, rs[:, :], p2[:, :], p2b, rsb)
            for c in range(2):
                o2 = oc - 1 + c
                P.op("dve", "tensor_tensor", PK(out=bufQ[:, o2, sl], in0=bufQ[:, o2, sl], in1=rs[:, :], op=ALU.mult),
                     reads=[bufQb[tt], rsb], writes=[bufQb[tt]])
                P.op("act", "activation", PK(out=bufQ[:, o2, sl], in_=bufQ[:, o2, sl], func=AF.Copy, scale=V[:, col_qg + c:col_qg + c + 1]),
                     reads=[bufQb[tt], vb], writes=[bufQb[tt]])
    linear_fm(C, wq_d, 8, bufA, None, 1024, evac_q, xinbs=bufAb)
    if stop == 17:
        return
    et, etb = scr["et"]
    rd, rdb = scr["rd"]
    for hh in range(4):
        for tt in range(NTT):
            sl = slice(tt * TT, (tt + 1) * TT)
            pden, pdenb = C.psum()
            pn = [C.psum(), C.psum()]
            for j in range(2):
                psc, pscb = C.psum()
                for dc in range(2):
                    P.op("pe", "matmul", PK(psc[:, :], kn[:, hh * 2 + dc, j * 128:(j + 1) * 128], bufQ[:, hh * 2 + dc, sl],
                                                                        start=(dc == 0), stop=(dc == 1)), reads=[knb, bufQb[tt]], writes=[pscb])
                P.op("act", "activation", PK(out=et[:, j, :], in_=psc[:, :], func=AF.Exp, scale=1.0 / 16.0),
                     reads=[pscb], writes=[etb[j]])
                P.op("pe", "matmul", PK(pden[:, :], cst[:, 4, :], et[:, j, :], start=(j == 0), stop=(j == 1)),
                     reads=[etb[j], cstb], writes=[pdenb])
                for c in range(2):
                    P.op("pe", "matmul", PK(pn[c][0][:, :], vt[:, j, hh * 256 + c * 128:hh * 256 + (c + 1) * 128], et[:, j, :],
                                                            start=(j == 0), stop=(j == 1)), reads=[etb[j], vtb], writes=[pn[c][1]])
            P.op("dve", "reciprocal", PK(out=rd[:, :], in_=pden[:, :]), reads=[pdenb], writes=[rdb])
            for c in range(2):
                P.op("dve", "tensor_tensor", PK(out=bufA[:, hh * 2 + c, sl], in0=pn[c][0][:, :], in1=rd[:, :], op=ALU.mult),
                     reads=[pn[c][1], rdb], writes=[bufAb[tt]])

    if stop == 18:
        return

    def evac_o(oc, tt, pt, pb, m):
        sl = slice(tt * TT, (tt + 1) * TT)
        P.op("dve", "tensor_tensor", PK(out=hT[:, oc, sl], in0=pt[:, :], in1=hT[:, oc, sl], op=ALU.add), reads=[pb, hTb[tt]], writes=[hTb[tt]])
    linear_fm(C, wo_d, 8, bufA, None, 1024, evac_o, xinbs=bufAb)


def swiglu_ffn(C, hT, hTb, bufA, bufAb, bufH, bufHb, V, vb, col_nf, wg_d, wu_d, wd_d, dff, cst, cstb, scr, gate_bc=None):
    P = C.P
    if col_nf is not None:
        for tt in range(NTT):
            sl = slice(tt * TT, (tt + 1) * TT)
            norm_fm(C, lambda c: hT[:, c, sl], [hTb[tt]], TT, 8, cst[:, 0, :], cstb, lambda c: V[:, col_nf + c:col_nf + c + 1],
                    lambda c: bufA[:, c, sl], [bufAb[tt]], scr)
    sg, sgb = scr["sg"]
    nch = dff // 128
    GRP = bufH.shape[1]
    g0 = 0
    while g0 < nch:
        gn = min(GRP, nch - g0)
        c = 0
        while c < gn:
            cb = min(4, gn - c)
            col = (g0 + c) * 128
            wgv, wgb = C.load_w(wg_d[:, col:col + cb * 128], 8, cb * 128)
            wuv, wub = C.load_w(wu_d[:, col:col + cb * 128], 8, cb * 128)
            for j in range(cb):
                for tt in range(NTT):
                    sl = slice(tt * TT, (tt + 1) * TT)
                    pg, pgb = C.psum()
                    pu, pub = C.psum()
                    for k in range(8):
                        P.op("pe", "matmul", PK(pg[:, :], wgv[:, k, j * 128:(j + 1) * 128], bufA[:, k, sl],
                                                                                       start=(k == 0), stop=(k == 7)), reads=[wgb, bufAb[tt]], writes=[pgb])
                    for k in range(8):
                        P.op("pe", "matmul", PK(pu[:, :], wuv[:, k, j * 128:(j + 1) * 128], bufA[:, k, sl],
                                                                                       start=(k == 0), stop=(k == 7)), reads=[wub, bufAb[tt]], writes=[pub])
                    P.op("act", "activation", PK(out=sg[:, :], in_=pg[:, :], func=AF.Silu), reads=[pgb], writes=[sgb])
                    if gate_bc is not None:
                        gt, gtb = gate_bc
                        P.op("dve", "tensor_tensor", PK(out=sg[:, :], in0=sg[:, :], in1=gt[:, sl], op=ALU.mult),
                             reads=[sgb, gtb[tt]], writes=[sgb])
                    P.op("dve", "tensor_tensor", PK(out=bufH[:, c + j, sl], in0=pu[:, :], in1=sg[:, :], op=ALU.mult),
                         reads=[pub, sgb], writes=[bufHb[tt]])
            c += cb
        wblks = []
        r = 0
        while r < gn:
            rb = min(4, gn - r)
            row = (g0 + r) * 128
            wblks.append((r, rb, C.load_w(wd_d[row:row + rb * 128, :], rb, 1024)))
            r += rb
        for oc in range(8):
            for tt in range(NTT):
                sl = slice(tt * TT, (tt + 1) * TT)
                pt, pb = C.psum()
                first = True
                for (r, rb, (wv, wb)) in wblks:
                    for q in range(rb):
                        last = (r + q == gn - 1)
                        P.op("pe", "matmul", PK(pt[:, :], wv[:, q, oc * 128:(oc + 1) * 128], bufH[:, r + q, sl], start=first, stop=last),
                             reads=[wb, bufHb[tt]], writes=[pb])
                        first = False
                P.op("dve", "tensor_tensor", PK(out=hT[:, oc, sl], in0=pt[:, :], in1=hT[:, oc, sl], op=ALU.add),
                     reads=[pb, hTb[tt]], writes=[hTb[tt]])
        g0 += gn


VB = dict(b_glu=0, n_xattn=4, n_mem=12, q_gain=20, k_gain=22, n_ffn=24, n_mix1=32, da_qg=40, da_kg=41)
NVB = 42


def build_B():
    nc = bass.Bass("TRN2", target_bir_lowering=False)
    dr = lambda n, s, k="ExternalInput", dt=F32: nc.dram_tensor(n, s, dt, kind=k).ap()
    xT_d = dr("xT", [D, NTOK]); ymT_d = dr("ymT", [D, NTOK]); memT_d = dr("memT", [D, 256])
    vec_d = dr("vecs", [128, NVB]); cst_d = dr("cst", [128, 5 * 128])
    wglu_d = dr("w_glu", [512, 512]); wout_d = dr("w_out", [D, D])
    wq_d = dr("w_q", [D, D]); wkv_d = dr("w_kv", [D, 2 * D]); wo_d = dr("w_o", [D, D])
    wg_d = dr("ff_g", [D, 2816]); wu_d = dr("ff_u", [D, 2816]); wd_d = dr("ff_d", [2816, D])
    wqkv_d = dr("w_qkv", [D, 3 * D])
    h1T_d = dr("h1T", [D, NTOK], "ExternalOutput")
    qT_d = dr("qT", [16, 64, NTOK], "ExternalOutput")
    kT_d = dr("kT", [16, 64, NTOK], "ExternalOutput")
    vT_d = dr("vT", [D, NTOK], "ExternalOutput")
    with ExitStack() as st:
        P = Prog(nc)
        C = Ctx(nc, st, P)
        C.init_w(4, 4096)
        hT, _ = C.sb("hT", [128, 8, NTOK], F32); hTb = [Buf("hT%d" % i) for i in range(NTT)]
        bufA, _ = C.sb("bufA", [128, 8, NTOK], BF16); bufAb = [Buf("bA%d" % i) for i in range(NTT)]
        bufH, _ = C.sb("bufH", [128, 8, NTOK], BF16); bufHb = [Buf("bH%d" % i) for i in range(NTT)]
        V, vb = C.sb("V_sb", [128, NVB], F32); C.vb = vb
        cst3, cstb = C.sb("cst_sb", [128, 5, 128], BF16)
        scr = dict(sq=C.sb("sq", [128, 8, 512], BF16), rs=C.sb("rs", [128, 512], F32), sg=C.sb("sg", [128, 512], F32),
                   rd=C.sb("rd", [128, 512], F32))
        sqq, _ = C.sb("sqq", [128, 2, NTT, 512], BF16)
        scr["sqq"] = (sqq, [Buf("sqq%d" % i) for i in range(NTT)])
        et, _ = C.sb("et", [128, 2, 512], BF16)
        scr["et"] = (et, [Buf("et0"), Buf("et1")])
        C.dma(V[:, :], vec_d[:, :], W=[vb])
        P.op("pool", "dma_start", PK(out=cst3[:, :, :], in_=cst_d.rearrange("p (a b) -> p a b", a=5)), writes=[cstb], is_dma=True)
        for tt in range(NTT):
            sl = slice(tt * TT, (tt + 1) * TT)
            C.dma(hT[:, :, sl], xT_d[:, sl].rearrange("(c p) n -> p c n", p=128), W=[hTb[tt]])
            P.op("pool", "dma_start", PK(out=bufA[:, :, sl], in_=ymT_d[:, sl].rearrange("(c p) n -> p c n", p=128)),
                 writes=[bufAb[tt]], is_dma=True)
        import os
        STAGE = int(os.environ.get("STAGE", "9"))
        def finish():
            for tt in range(NTT):
                sl = slice(tt * TT, (tt + 1) * TT)
                C.dma(h1T_d[:, sl].rearrange("(c p) n -> p c n", p=128), hT[:, :, sl], R=[hTb[tt]], is_out=True)
            P.emit(st)
            print("B stats", P.stats)
            return nc
        if STAGE == 0:
            return finish()
        wv, wb = C.load_w(wglu_d[:, :], 4, 512)
        sg, sgb = scr["sg"]
        for tt in range(NTT):
            sl = slice(tt * TT, (tt + 1) * TT)
            pts = []
            for oc in range(4):
                pt, pb = C.psum()
                for k in range(4):
                    P.op("pe", "matmul", PK(pt[:, :], wv[:, k, oc * 128:(oc + 1) * 128], bufA[:, 4 + k, sl],
                                                                            start=(k == 0), stop=(k == 3)), reads=[wb, bufAb[tt]], writes=[pb])
                pts.append((pt, pb))
            for oc in range(4):
                pt, pb = pts[oc]
                P.op("act", "activation", PK(out=sg[:, :], in_=pt[:, :], func=AF.Sigmoid,
                                                                bias=V[:, VB["b_glu"] + oc:VB["b_glu"] + oc + 1]), reads=[pb, vb], writes=[sgb])
                P.op("dve", "tensor_tensor", PK(out=bufA[:, 4 + oc, sl], in0=bufA[:, 4 + oc, sl], in1=sg[:, :], op=ALU.mult),
                     reads=[sgb, bufAb[tt]], writes=[bufAb[tt]])

        def evac_res(oc, tt, pt, pb, m):
            sl = slice(tt * TT, (tt + 1) * TT)
            P.op("dve", "tensor_tensor", PK(out=hT[:, oc, sl], in0=pt[:, :], in1=hT[:, oc, sl], op=ALU.add), reads=[pb, hTb[tt]], writes=[hTb[tt]])
        linear_fm(C, wout_d, 8, bufA, None, 1024, evac_res, xinbs=bufAb)
        import os
        STAGE = int(os.environ.get("STAGE", "9"))
        def finish():
            for tt in range(NTT):
                sl = slice(tt * TT, (tt + 1) * TT)
                C.dma(h1T_d[:, sl].rearrange("(c p) n -> p c n", p=128), hT[:, :, sl], R=[hTb[tt]], is_out=True)
            P.emit(st)
            return nc
        if STAGE == 1:
            return finish()
        (kn, knb), (vt, vtb) = mem_kv(C, memT_d, wkv_d, V, vb, VB["n_mem"], VB["k_gain"], cst3, cstb, scr, "m0")
        if STAGE == 15:
            return finish()
        mem_xattn(C, hT, hTb, bufA, bufAb, bufH, bufHb, V, vb, VB["n_xattn"], VB["q_gain"], wq_d, wo_d, kn, knb, vt, vtb, cst3, cstb, scr, stop=STAGE)
        if STAGE in (2, 16, 17, 18):
            return finish()
        swiglu_ffn(C, hT, hTb, bufA, bufAb, bufH, bufHb, V, vb, VB["n_ffn"], wg_d, wu_d, wd_d, 2816, cst3, cstb, scr)
        for tt in range(NTT):
            sl = slice(tt * TT, (tt + 1) * TT)
            C.dma(h1T_d[:, sl].rearrange("(c p) n -> p c n", p=128), hT[:, :, sl], R=[hTb[tt]], is_out=True)
        for tt in range(NTT):
            sl = slice(tt * TT, (tt + 1) * TT)
            norm_fm(C, lambda c: hT[:, c, sl], [hTb[tt]], TT, 8, cst3[:, 0, :], cstb, lambda c: V[:, VB["n_mix1"] + c:VB["n_mix1"] + c + 1],
                    lambda c: bufA[:, c, sl], [bufAb[tt]], scr)
        qs, qsb = scr["sg"]
        qo, qob = scr["rd"]
        sq1, sq1b = C.sb("sq1", [64, 512], BF16)
        rs, rsb = scr["rs"]
        for which, (dst, gcol, scl) in enumerate(((qT_d, VB["da_qg"], 0.125), (kT_d, VB["da_kg"], 1.0))):
            def evac_qk(oc, tt, pt, pb, m, dst=dst, gcol=gcol, scl=scl):
                sl = slice(tt * TT, (tt + 1) * TT)
                P.op("act", "activation", PK(out=sq1[:, :], in_=pt[0:64, :], func=AF.Square), reads=[pb], writes=[sq1b])
                p2, p2b = C.psum()
                P.op("pe", "matmul", PK(p2[0:64, :], cst3[0:64, 2, 0:64], sq1[:, :], start=True, stop=True), reads=[sq1b, cstb], writes=[p2b])
                rstd_from(C, rs[0:64, :], p2[0:64, :], p2b, rsb)
                P.op("dve", "scalar_tensor_tensor", PK(out=qs[0:64, :], in0=pt[0:64, :], scalar=V[0:64, gcol:gcol + 1], in1=rs[0:64, :],
                                                             op0=ALU.mult, op1=ALU.mult), reads=[pb, rsb, vb], writes=[qsb])
                P.op("act", "activation", PK(out=qo[0:64, :], in_=qs[0:64, :], func=AF.Copy, scale=float(scl)), reads=[qsb], writes=[qob])
                C.dma(dst[oc, :, sl], qo[0:64, :], R=[qob], is_out=True)
            linear_fm(C, wqkv_d, 8, bufA, None, 1024, evac_qk, col0=which * 1024, mcols=64, xinbs=bufAb)
        vo, vob = scr["sg"]

        def evac_v(oc, tt, pt, pb, m):
            sl = slice(tt * TT, (tt + 1) * TT)
            P.op("act", "activation", PK(out=vo[:, :], in_=pt[:, :], func=AF.Copy), reads=[pb], writes=[vob])
            C.dma(vT_d[oc * 128:(oc + 1) * 128, sl], vo[:, :], R=[vob], is_out=True)
        linear_fm(C, wqkv_d, 8, bufA, None, 1024, evac_v, col0=2048, xinbs=bufAb)
        P.emit(st)
        print("B stats", P.stats)
    return nc


def cst_table():
    c = np.zeros((128, 5, 128), np.float32)
    c[:, 0, :] = 1.0 / 1024
    c[:, 1, :] = 1.0 / 256
    c[0:64, 2, 0:64] = 1.0 / 64
    c[64:128, 2, 64:128] = 1.0 / 64
    c[:, 3, :] = 1.0 / 128
    c[:, 4, :] = 1.0
    return c.reshape(128, 640)


def col(v):
    v = np.asarray(v, np.float32).reshape(-1)
    if v.size < 128:
        o = np.zeros((128, 1), np.float32); o[:v.size, 0] = v
        return o
    return np.ascontiguousarray(v.reshape(-1, 128).T)


def vecs_B(inp):
    V = np.zeros((128, NVB), np.float32)
    def put(name, arr):
        a = col(arr); V[:, VB[name]:VB[name] + a.shape[1]] = a
    put("b_glu", inp["s5_b_glu"][0]); put("n_xattn", inp["norm_xattn"][0]); put("n_mem", inp["norm_mem"][0])
    put("q_gain", inp["xa_q_gain"][0]); put("k_gain", inp["xa_k_gain"][0]); put("n_ffn", inp["norm_ffn"][0])
    put("n_mix1", inp["norm_mix"][1])
    put("da_qg", np.tile(np.asarray(inp["da_q_gain"][0]), 2)); put("da_kg", np.tile(np.asarray(inp["da_k_gain"][0]), 2))
    return V


def tok_index(p):
    return np.concatenate([np.arange((2 * m + p) * 512, (2 * m + p + 1) * 512) for m in range(4)])


VC = dict(n_xattn=0, n_mem=8, q_gain=16, k_gain=18, n_ffn=20, b_router=28)
NVC = 36


def build_C2():
    nc = bass.Bass("TRN2", target_bir_lowering=False)
    dr = lambda n, s, k="ExternalInput", dt=F32: nc.dram_tensor(n, s, dt, kind=k).ap()
    h1T_d = dr("h1T", [D, NTOK]); atT_d = dr("attnT", [D, NTOK]); memT_d = dr("memT", [D, 256])
    vec_d = dr("vecs", [128, NVC]); cst_d = dr("cst", [128, 5 * 128]); c32_d = dr("c32", [128, 256])
    wdo_d = dr("da_w_o", [D, D])
    wq_d = dr("w_q", [D, D]); wkv_d = dr("w_kv", [D, 2 * D]); wo_d = dr("w_o", [D, D])
    wr_d = dr("w_router", [D, 8])
    wg_d = dr("moe_g", [8, D, 3584]); wu_d = dr("moe_u", [8, D, 3584]); wd_d = dr("moe_d", [8, 3584, D])
    outT_d = dr("outT", [D, NTOK], "ExternalOutput")
    with ExitStack() as st:
        P = Prog(nc)
        C = Ctx(nc, st, P)
        C.init_w(3, 4096)
        hT, _ = C.sb("hT", [128, 8, NTOK], F32); hTb = [Buf("hT%d" % i) for i in range(NTT)]
        bufA, _ = C.sb("bufA", [128, 8, NTOK], BF16); bufAb = [Buf("bA%d" % i) for i in range(NTT)]
        bufH, _ = C.sb("bufH", [128, 8, NTOK], BF16); bufHb = [Buf("bH%d" % i) for i in range(NTT)]
        V, vb = C.sb("V_sb", [128, NVC], F32); C.vb = vb
        cst3, cstb = C.sb("cst_sb", [128, 5, 128], BF16)
        c32, c32b = C.sb("c32_sb", [128, 2, 128], F32)
        scr = dict(sq=C.sb("sq", [128, 8, 512], BF16), rs=C.sb("rs", [128, 512], F32), sg=C.sb("sg", [128, 512], F32),
                   rd=C.sb("rd", [128, 512], F32))
        sqq, _ = C.sb("sqq", [128, 2, NTT, 512], BF16)
        scr["sqq"] = (sqq, [Buf("sqq%d" % i) for i in range(NTT)])
        et, _ = C.sb("et", [128, 2, 512], BF16)
        scr["et"] = (et, [Buf("et0"), Buf("et1")])
        C.dma(V[:, :], vec_d[:, :], W=[vb])
        C.dma(c32[:, :, :], c32_d.rearrange("p (a b) -> p a b", a=2), W=[c32b])
        P.op("pool", "dma_start", PK(out=cst3[:, :, :], in_=cst_d.rearrange("p (a b) -> p a b", a=5)), writes=[cstb], is_dma=True)
        for tt in range(NTT):
            sl = slice(tt * TT, (tt + 1) * TT)
            C.dma(hT[:, :, sl], h1T_d[:, sl].rearrange("(c p) n -> p c n", p=128), W=[hTb[tt]])
            P.op("pool", "dma_start", PK(out=bufA[:, :, sl], in_=atT_d[:, sl].rearrange("(c p) n -> p c n", p=128)),
                 writes=[bufAb[tt]], is_dma=True)

        def evac_res(oc, tt, pt, pb, m):
            sl = slice(tt * TT, (tt + 1) * TT)
            P.op("dve", "tensor_tensor", PK(out=hT[:, oc, sl], in0=pt[:, :], in1=hT[:, oc, sl], op=ALU.add), reads=[pb, hTb[tt]], writes=[hTb[tt]])
        linear_fm(C, wdo_d, 8, bufA, None, 1024, evac_res, xinbs=bufAb)
        (kn, knb), (vt, vtb) = mem_kv(C, memT_d, wkv_d, V, vb, VC["n_mem"], VC["k_gain"], cst3, cstb, scr, "m1")
        mem_xattn(C, hT, hTb, bufA, bufAb, bufH, bufHb, V, vb, VC["n_xattn"], VC["q_gain"], wq_d, wo_d, kn, knb, vt, vtb, cst3, cstb, scr)
        for tt in range(NTT):
            sl = slice(tt * TT, (tt + 1) * TT)
            norm_fm(C, lambda c: hT[:, c, sl], [hTb[tt]], TT, 8, cst3[:, 0, :], cstb, lambda c: V[:, VC["n_ffn"] + c:VC["n_ffn"] + c + 1],
                    lambda c: bufA[:, c, sl], [bufAb[tt]], scr)
        hn32, hn32b = C.sb("hn32", [128, 8, 128], F32)
        wr, wrb = C.sb("wr", [128, 8, 8], F32)
        G, Gb = C.sb("G", [128, 16, 8], F32)
        lg, lgb = C.sb("lg", [128, 8], F32)
        mx, mxb = C.sb("mx", [128, 8], F32)
        sm, smb = C.sb("sm", [128, 4], F32)
        C.dma(wr[:, :, :], wr_d.rearrange("(c p) n -> p c n", p=128), W=[wrb])
        for s in range(16):
            tt = s // 4
            sl = slice(s * 128, (s + 1) * 128)
            norm_fm(C, lambda c: hT[:, c, sl], [hTb[tt]], 128, 8, cst3[:, 0, :], cstb, lambda c: V[:, VC["n_ffn"] + c:VC["n_ffn"] + c + 1],
                    lambda c: hn32[:, c, :], [hn32b], scr)
            pt, pb = C.psum()
            for k in range(8):
                P.op("pe", "matmul", PK(pt[:, 0:8], hn32[:, k, :], wr[:, k, :], start=(k == 0), stop=(k == 7)), reads=[hn32b, wrb], writes=[pb])
            P.op("dve", "tensor_tensor", PK(out=lg[:, :], in0=pt[:, 0:8], in1=V[:, VC["b_router"]:VC["b_router"] + 8], op=ALU.add),
                 reads=[pb, vb], writes=[lgb])
            P.op("dve", "max", PK(out=mx[:, :], in_=lg[:, :]), reads=[lgb], writes=[mxb])
            P.op("dve", "tensor_scalar", PK(out=sm[:, 0:1], in0=mx[:, 0:1], scalar1=-1.0, scalar2=None, op0=ALU.mult), reads=[mxb], writes=[smb])
            ex, exb = scr["sg"]
            P.op("act", "activation", PK(out=ex[:, 0:8], in_=lg[:, :], func=AF.Exp, bias=sm[:, 0:1]), reads=[lgb, smb], writes=[exb])
            P.op("dve", "tensor_scalar", PK(out=lg[:, :], in0=lg[:, :], scalar1=mx[:, 1:2], scalar2=None, op0=ALU.is_ge), reads=[lgb, mxb], writes=[lgb])
            P.op("dve", "tensor_tensor", PK(out=ex[:, 0:8], in0=ex[:, 0:8], in1=lg[:, :], op=ALU.mult), reads=[exb, lgb], writes=[exb])
            P.op("dve", "reduce_sum", PK(out=sm[:, 1:2], in_=ex[:, 0:8], axis=AX.X), reads=[exb], writes=[smb])
            P.op("dve", "reciprocal", PK(out=sm[:, 2:3], in_=sm[:, 1:2]), reads=[smb], writes=[smb])
            P.op("dve", "tensor_scalar", PK(out=G[:, s, :], in0=ex[:, 0:8], scalar1=sm[:, 2:3], scalar2=None, op0=ALU.mult), reads=[exb, smb], writes=[Gb])
        gbc, _ = C.sb("gbc", [128, NTOK], BF16); gbcb = [Buf("gbc%d" % i) for i in range(NTT)]
        dg, dgb = C.sb("dg", [128, 128], F32)
        for e_ in range(8):
            for tt in range(NTT):
                pt, pb = C.psum()
                for q in range(4):
                    s = tt * 4 + q
                    P.op("dve", "tensor_scalar", PK(out=dg[:, :], in0=c32[:, 0, :], scalar1=G[:, s, e_:e_ + 1], scalar2=None, op0=ALU.mult),
                         reads=[c32b, Gb], writes=[dgb])
                    P.op("pe", "matmul", PK(pt[:, q * 128:(q + 1) * 128], c32[:, 1, :], dg[:, :], start=True, stop=True), reads=[dgb, c32b], writes=[pb])
                P.op("act", "activation", PK(out=gbc[:, tt * TT:(tt + 1) * TT], in_=pt[:, :], func=AF.Copy), reads=[pb], writes=[gbcb[tt]])
            swiglu_ffn(C, hT, hTb, bufA, bufAb, bufH, bufHb, V, vb, None, wg_d[e_], wu_d[e_], wd_d[e_], 3584, cst3, cstb, scr, gate_bc=(gbc, gbcb))
        for tt in range(NTT):
            sl = slice(tt * TT, (tt + 1) * TT)
            C.dma(outT_d[:, sl].rearrange("(c p) n -> p c n", p=128), hT[:, :, sl], R=[hTb[tt]], is_out=True)
        P.emit(st)
    return nc


def c32_table():
    c = np.zeros((128, 2, 128), np.float32)
    c[:, 0, :] = np.eye(128, dtype=np.float32)
    c[:, 1, :] = 1.0
    return c.reshape(128, 256)


def vecs_C(inp):
    V = np.zeros((128, NVC), np.float32)
    def put(name, arr):
        a = col(arr); V[:, VC[name]:VC[name] + a.shape[1]] = a
    put("n_xattn", inp["norm_xattn"][1]); put("n_mem", inp["norm_mem"][1])
    put("q_gain", inp["xa_q_gain"][1]); put("k_gain", inp["xa_k_gain"][1]); put("n_ffn", inp["norm_ffn"][1])
    V[:, VC["b_router"]:VC["b_router"] + 8] = np.asarray(inp["moe_b_router"][0], np.float32)[None, :]
    return V


LAMBDA_INIT = 0.8 - 0.6 * float(np.exp(-0.3 * 1))


def build_C1():
    nc = bass.Bass("TRN2", target_bir_lowering=False)
    dr = lambda n, s, k="ExternalInput", dt=F32: nc.dram_tensor(n, s, dt, kind=k).ap()
    qa_d = dr("qaug", [16, 68, NTOK]); ka_d = dr("kaug", [16, 68, 4096]); vf_d = dr("vfull", [4096, D])
    bias_d = dr("biasT", [128, 8 * 512]); lamb_d = dr("lamb", [128, 4 * 64]); sgc_d = dr("sgc", [128, 1]); cst_d = dr("cst", [128, 5 * 128])
    at_d = dr("attnT", [D, NTOK], "ExternalOutput")
    with ExitStack() as st:
        P = Prog(nc)
        C = Ctx(nc, st, P)
        cst3, cstb = C.sb("cst_sb", [128, 5, 128], BF16)
        P.op("pool", "dma_start", PK(out=cst3[:, :, :], in_=cst_d.rearrange("p (a b) -> p a b", a=5)), writes=[cstb], is_dma=True)
        biasT, biasb = C.sb("bias_sb", [128, 8, 512], F32)
        C.dma(biasT[:, :, :], bias_d.rearrange("p (a b) -> p a b", a=8), W=[biasb])
        lamb, lambb = C.sb("lamb_sb", [128, 4, 64], F32)
        C.dma(lamb[:, :, :], lamb_d.rearrange("p (a b) -> p a b", a=4), W=[lambb])
        sgc, sgcb = C.sb("sgc_sb", [128, 1], F32)
        C.dma(sgc[:, :], sgc_d[:, :], W=[sgcb])
        scr = dict(sq=C.sb("sq", [128, 1, 512], BF16), rs=C.sb("rs", [128, 512], F32))
        lt, ltb = C.sb("lt", [128, 2, 64], F32)
        lc, lcb = C.sb("lc", [128, 4], F32)
        for i in range(2):
            P.op("dve", "tensor_tensor", PK(out=lt[:, i, :], in0=lamb[:, 2 * i, :], in1=lamb[:, 2 * i + 1, :], op=ALU.mult), reads=[lambb], writes=[ltb])
            P.op("dve", "reduce_sum", PK(out=lc[:, i:i + 1], in_=lt[:, i, :], axis=AX.X), reads=[ltb], writes=[lcb])
        P.op("act", "activation", PK(out=lc[:, 0:2], in_=lc[:, 0:2], func=AF.Exp), reads=[lcb], writes=[lcb])
        P.op("dve", "tensor_tensor", PK(out=lc[:, 2:3], in0=lc[:, 1:2], in1=lc[:, 0:1], op=ALU.subtract), reads=[lcb], writes=[lcb])
        P.op("dve", "tensor_scalar_add", PK(out=lc[:, 3:4], in0=lc[:, 2:3], scalar1=-LAMBDA_INIT), reads=[lcb], writes=[lcb])
        P.op("dve", "tensor_scalar", PK(out=sgc[:, :], in0=sgc[:, :], scalar1=float(1.0 - LAMBDA_INIT), scalar2=None, op0=ALU.mult), reads=[sgcb], writes=[sgcb])
        kA = [C.sb("kA%d" % i, [68, 2, 4096], BF16) for i in range(2)]
        qA = [C.sb("qA%d" % i, [68, 2, NTOK], BF16) for i in range(2)]
        vA = [C.sb("vA%d" % i, [128, 32, 128], BF16) for i in range(2)]
        et, _ = C.sb("et", [128, 4, 512], BF16); etb = [Buf("et%d" % i) for i in range(4)]
        tmp, _ = C.sb("tmp", [128, 2, 512], F32); tmpb = [Buf("tmp%d" % i) for i in range(2)]
        rd, rdb = C.sb("rd", [128, 512], F32)
        o0, o0b = C.sb("o0", [128, 512], F32)
        o1, o1b = C.sb("o1", [128, 512], F32)
        ot, otb = C.sb("ot", [128, 512], F32)
        ei = 0
        ti_ = 0
        for h in range(8):
            slope = float(2.0 ** (-(h + 1)))
            kt, ktb = kA[h % 2]; qt, qtb = qA[h % 2]; vt, vtb = vA[h % 2]
            P.op("pool", "dma_start", PK(out=kt[:, :, :], in_=ka_d[2 * h:2 * h + 2].rearrange("c r n -> r c n")), writes=[ktb], is_dma=True)
            P.op("pool", "dma_start", PK(out=qt[:, :, :], in_=qa_d[2 * h:2 * h + 2].rearrange("c r n -> r c n")), writes=[qtb], is_dma=True)
            P.op("pool", "dma_start", PK(out=vt[:, :, :], in_=vf_d[:, h * 128:(h + 1) * 128].rearrange("(j p) d -> p j d", p=128)), writes=[vtb], is_dma=True)
            for m in range(4):
                nkb = 8 * (m + 1)
                qsl = slice(m * 512, (m + 1) * 512)
                N = [C.ps[0], C.ps[1]]
                Dn = [C.ps[2], C.ps[3]]
                for j in range(nkb):
                    for c in range(2):
                        sc, scb = C.ps[4 + ((2 * j + c) % 4)]
                        P.op("pe", "matmul", PK(sc[:, :], kt[:, c, j * 128:(j + 1) * 128], qt[:, c, qsl], start=True, stop=True),
                             reads=[ktb, qtb], writes=[scb])
                        e_slot = ei % 4; ei += 1
                        if j >= nkb - 8:
                            s = j - (nkb - 8)
                            tb = ti_ % 2; ti_ += 1
                            P.op("dve", "scalar_tensor_tensor", PK(out=tmp[:, tb, :], in0=biasT[:, s, :], scalar=slope, in1=sc[:, :], op0=ALU.mult, op1=ALU.add),
                                 reads=[biasb, scb], writes=[tmpb[tb]])
                            P.op("act", "activation", PK(out=et[:, e_slot, :], in_=tmp[:, tb, :], func=AF.Exp), reads=[tmpb[tb]], writes=[etb[e_slot]])
                        else:
                            P.op("act", "activation", PK(out=et[:, e_slot, :], in_=sc[:, :], func=AF.Exp), reads=[scb], writes=[etb[e_slot]])
                        P.op("pe", "matmul", PK(N[c][0][:, :], vt[:, j, :], et[:, e_slot, :], start=(j == 0), stop=(j == nkb - 1)),
                             reads=[vtb, etb[e_slot]], writes=[N[c][1]])
                        P.op("pe", "matmul", PK(Dn[c][0][:, :], cst3[:, 4, :], et[:, e_slot, :], start=(j == 0), stop=(j == nkb - 1)),
                             reads=[cstb, etb[e_slot]], writes=[Dn[c][1]])
                P.op("dve", "reciprocal", PK(out=rd[:, :], in_=Dn[0][0][:, :]), reads=[Dn[0][1]], writes=[rdb])
                P.op("dve", "tensor_tensor", PK(out=o0[:, :], in0=N[0][0][:, :], in1=rd[:, :], op=ALU.mult), reads=[N[0][1], rdb], writes=[o0b])
                P.op("dve", "reciprocal", PK(out=rd[:, :], in_=Dn[1][0][:, :]), reads=[Dn[1][1]], writes=[rdb])
                P.op("dve", "tensor_tensor", PK(out=o1[:, :], in0=N[1][0][:, :], in1=rd[:, :], op=ALU.mult), reads=[N[1][1], rdb], writes=[o1b])
                P.op("dve", "scalar_tensor_tensor", PK(out=o0[:, :], in0=o1[:, :], scalar=lc[:, 3:4], in1=o0[:, :], op0=ALU.mult, op1=ALU.add),
                     reads=[o1b, o0b, lcb], writes=[o0b])
                sq, sqb = scr["sq"]; rs, rsb = scr["rs"]
                P.op("act", "activation", PK(out=sq[:, 0, :], in_=o0[:, :], func=AF.Square), reads=[o0b], writes=[sqb])
                pn, pnb = C.ps[4]
                P.op("pe", "matmul", PK(pn[:, :], cst3[:, 3, :], sq[:, 0, :], start=True, stop=True), reads=[sqb, cstb], writes=[pnb])
                rstd_from(C, rs[:, :], pn[:, :], pnb, rsb)
                P.op("dve", "scalar_tensor_tensor", PK(out=ot[:, :], in0=o0[:, :], scalar=sgc[:, 0:1], in1=rs[:, :], op0=ALU.mult, op1=ALU.mult),
                     reads=[o0b, rsb, sgcb], writes=[otb])
                C.dma(at_d[h * 128:(h + 1) * 128, qsl], ot[:, :], R=[otb], is_out=True)
        P.emit(st)
    return nc


def bias_table(p):
    t = np.zeros((8, 128, 512), np.float32)
    kq = np.arange(128)[:, None]
    qq = np.arange(512)[None, :]
    for s in range(8):
        if p == 0:
            r = s if s < 4 else None
            full_mask = s >= 4
        else:
            r = s - 4 if s >= 4 else None
            full_mask = False
        if full_mask:
            t[s] = -1e30
        elif r is not None:
            kpos = 128 * r + kq
            kc = kpos // 64; qc = qq // 64
            d = -2.0 * np.maximum(kpos - qq, 0).astype(np.float32)
            t[s] = np.where(kc < qc, 0.0, np.where(kc == qc, d, -1e30))
    return np.ascontiguousarray(t.transpose(1, 0, 2).reshape(128, 8 * 512))


def q_aug_rows(p):
    pos = tok_index(p)
    return np.stack([pos // 64, pos % 64, np.ones_like(pos), np.ones_like(pos)]).astype(np.float32)


def k_aug_rows(h):
    s = np.arange(4096)
    sl = 2.0 ** (-(h + 1))
    return np.stack([np.full(4096, -64.0 * sl), np.full(4096, -sl), 64.0 * sl * (s // 64), sl * (s % 64)]).astype(np.float32)


T_SEQ = 4096
SEG = 512
NSEG_FULL = T_SEQ // SEG
NCOLS = 1312
ZCH = [(0, 128), (128, 128), (256, 128), (384, 128), (512, 128), (640, 128), (768, 128), (896, 128), (1024, 32), (1056, 128), (1184, 128)]
VA = dict(n_mix=0, mu=8, w0=17, a0=19, k_k=21, k_a=23, r_k=25, ln_w=27, ln_b=29, s5_d=31, lam_re=33, lam_im=41, log_dt=49, halfpi=57)
NVA = 58
C_ID, C_B64, C_ONES = 0, 1, 2
NEG_E05 = -float(np.exp(-0.5))


def build_A(nseg=NSEG_FULL):
    nc = bass.Bass("TRN2", target_bir_lowering=False)
    dr = lambda n, s, k="ExternalInput", dt=F32: nc.dram_tensor(n, s, dt, kind=k).ap()
    xT_d = dr("xT", [D, T_SEQ]); wc_d = dr("wc", [D, NCOLS]); vec_d = dr("vecs", [128, NVA])
    cst_d = dr("cst", [128, 5 * 128]); c32_d = dr("c32", [128, 3 * 128]); msk_d = dr("mask5", [64, 320]); rmask_d = dr("rmask", [128, SEG])
    wup_d = dr("w_up", [64, 256]); aup_d = dr("a_up", [128, 256]); gupa_d = dr("g_upa", [128, 256]); gupb_d = dr("g_upb", [32, 256])
    bre_d = dr("breT", [8, 128, 128]); bim_d = dr("bimT", [8, 128, 128]); cre_d = dr("creT", [8, 128, 128]); cim_d = dr("cimT", [8, 128, 128])
    ya_d = dr("yaT", [256, T_SEQ], "ExternalOutput"); yb_d = dr("ybT", [256, T_SEQ], "ExternalOutput")
    with ExitStack() as st:
        P = Prog(nc)
        C = Ctx(nc, st, P)
        rot = [0, 1, 2, 3, 4, 5, 7]
        rs_ = [0]

        def psum():
            r = C.ps[rot[rs_[0] % len(rot)]]
            rs_[0] += 1
            return r
        C.psum = psum
        T = lambda name, shape, dt=F32: C.sb(name, shape, dt)
        V, vb = T("V_sb", [128, NVA]); C.vb = vb
        C.dma(V[:, :], vec_d[:, :], W=[vb])
        cst3, cstb = T("cst_sb", [128, 5, 128], BF16)
        P.op("pool", "dma_start", PK(out=cst3[:, :, :], in_=cst_d.rearrange("p (a b) -> p a b", a=5)), writes=[cstb], is_dma=True)
        c32, c32b = T("c32_sb", [128, 3, 128]); C.dma(c32[:, :, :], c32_d.rearrange("p (a b) -> p a b", a=3), W=[c32b])
        msk, mskb = T("msk_sb", [64, 320]); C.dma(msk[:, :], msk_d[:, :], W=[mskb])
        rmask, rmaskb = T("rmask_sb", [128, SEG]); C.dma(rmask[:, :], rmask_d[:, :], W=[rmaskb])
        wup, wupb = T("wup_sb", [64, 256]); C.dma(wup[:, :], wup_d[:, :], W=[wupb])
        aup, aupb = T("aup_sb", [128, 256]); C.dma(aup[:, :], aup_d[:, :], W=[aupb])
        gupa, gupab = T("gupa_sb", [128, 256]); C.dma(gupa[:, :], gupa_d[:, :], W=[gupab])
        gupb, gupbb = T("gupb_sb", [32, 256]); C.dma(gupb[:, :], gupb_d[:, :], W=[gupbb])
        wc, wcb = T("wc_sb", [128, 8, NCOLS], BF16)
        for k in range(8):
            P.op("pool", "dma_start", PK(out=wc[:, k, :], in_=wc_d[k * 128:(k + 1) * 128, :]), writes=[wcb], is_dma=True)
        scr = dict(sq=T("sq", [128, 8, 512], BF16), rs=T("rs", [128, 512]))
        ident = c32[:, C_ID, :]

        breT, breb = T("breT_sb", [128, 8, 128], BF16); bimT, bimb = T("bimT_sb", [128, 8, 128], BF16)
        P.op("pool", "dma_start", PK(out=breT[:, :, :], in_=bre_d.rearrange("q k m -> k q m")), writes=[breb], is_dma=True)
        P.op("pool", "dma_start", PK(out=bimT[:, :, :], in_=bim_d.rearrange("q k m -> k q m")), writes=[bimb], is_dma=True)
        creT, creb = T("creT_sb", [128, 8, 128]); cimT, cimb = T("cimT_sb", [128, 8, 128])
        C.dma(creT[:, :, :], cre_d.rearrange("q k m -> k q m"), W=[creb])
        C.dma(cimT[:, :, :], cim_d.rearrange("q k m -> k q m"), W=[cimb])
        s5c, s5cb = T("s5c", [128, 12, 8])
        lre = V[:, VA["lam_re"]:VA["lam_re"] + 8]; lim = V[:, VA["lam_im"]:VA["lam_im"] + 8]
        S = lambda i: s5c[:, i, :]
        dv = lambda name, **kw: P.op("dve", name, PK(**kw), reads=[s5cb, vb], writes=[s5cb])
        P.op("act", "activation", PK(out=S(0), in_=V[:, VA["log_dt"]:VA["log_dt"] + 8], func=AF.Exp), reads=[vb], writes=[s5cb])
        dv("tensor_tensor", out=S(1), in0=lim, in1=S(0), op=ALU.mult)
        dv("tensor_tensor", out=S(9), in0=lre, in1=S(0), op=ALU.mult)
        P.op("act", "activation", PK(out=S(2), in_=S(9), func=AF.Exp), reads=[s5cb], writes=[s5cb])
        P.op("act", "activation", PK(out=S(4), in_=S(1), func=AF.Sin, scale=0.125), reads=[s5cb], writes=[s5cb])
        P.op("act", "activation", PK(out=S(3), in_=S(1), func=AF.Sin, scale=-0.125, bias=V[:, VA["halfpi"]:VA["halfpi"] + 1]), reads=[s5cb, vb], writes=[s5cb])
        for _ in range(3):
            dv("tensor_tensor", out=S(9), in0=S(3), in1=S(3), op=ALU.mult)
            dv("tensor_tensor", out=S(10), in0=S(4), in1=S(4), op=ALU.mult)
            dv("tensor_tensor", out=S(11), in0=S(3), in1=S(4), op=ALU.mult)
            dv("tensor_tensor", out=S(3), in0=S(9), in1=S(10), op=ALU.subtract)
            dv("tensor_scalar", out=S(4), in0=S(11), scalar1=2.0, scalar2=None, op0=ALU.mult)
        dv("tensor_tensor", out=S(5), in0=S(2), in1=S(3), op=ALU.mult)
        dv("tensor_tensor", out=S(6), in0=S(2), in1=S(4), op=ALU.mult)
        dv("tensor_scalar_add", out=S(5), in0=S(5), scalar1=-1.0)
        dv("tensor_tensor", out=S(9), in0=lre, in1=lre, op=ALU.mult)
        dv("tensor_tensor", out=S(10), in0=lim, in1=lim, op=ALU.mult)
        dv("tensor_tensor", out=S(9), in0=S(9), in1=S(10), op=ALU.add)
        dv("reciprocal", out=S(9), in_=S(9))
        dv("tensor_tensor", out=S(10), in0=S(5), in1=lre, op=ALU.mult)
        dv("tensor_tensor", out=S(11), in0=S(6), in1=lim, op=ALU.mult)
        dv("tensor_tensor", out=S(10), in0=S(10), in1=S(11), op=ALU.add)
        dv("tensor_tensor", out=S(7), in0=S(10), in1=S(9), op=ALU.mult)
        dv("tensor_tensor", out=S(10), in0=S(6), in1=lre, op=ALU.mult)
        dv("tensor_tensor", out=S(11), in0=S(5), in1=lim, op=ALU.mult)
        dv("tensor_tensor", out=S(10), in0=S(10), in1=S(11), op=ALU.subtract)
        dv("tensor_tensor", out=S(8), in0=S(10), in1=S(9), op=ALU.mult)
        cpre, cpreb = T("cpre", [128, 8, 128], BF16); cpim, cpimb = T("cpim", [128, 8, 128], BF16)
        ctmp, ctmpb = T("ctmp", [128, 128])
        for q in range(8):
            cr = s5c[:, 7, q:q + 1]; ci = s5c[:, 8, q:q + 1]
            P.op("dve", "tensor_scalar", PK(out=ctmp[:, :], in0=cimT[:, q, :], scalar1=ci, scalar2=None, op0=ALU.mult), reads=[cimb, s5cb], writes=[ctmpb])
            P.op("dve", "scalar_tensor_tensor", PK(out=cpre[:, q, :], in0=creT[:, q, :], scalar=cr, in1=ctmp[:, :], op0=ALU.mult, op1=ALU.subtract),
                 reads=[creb, ctmpb, s5cb], writes=[cpreb])
            P.op("dve", "tensor_scalar", PK(out=ctmp[:, :], in0=cimT[:, q, :], scalar1=cr, scalar2=-1.0, op0=ALU.mult, op1=ALU.mult), reads=[cimb, s5cb], writes=[ctmpb])
            P.op("dve", "scalar_tensor_tensor", PK(out=ctmp[:, :], in0=creT[:, q, :], scalar=ci, in1=ctmp[:, :], op0=ALU.mult, op1=ALU.subtract),
                 reads=[creb, ctmpb, s5cb], writes=[ctmpb])
            P.op("dve", "tensor_scalar", PK(out=cpim[:, q, :], in0=ctmp[:, :], scalar1=-1.0, scalar2=None, op0=ALU.mult), reads=[ctmpb], writes=[cpimb])
        Fc, Fcb = T("Fc", [128, 8, SEG]); Fs, Fsb = T("Fs", [128, 8, SEG])
        ncol, ncolb = T("ncol", [128, 2])
        ftmp, ftmpb = T("ftmp", [128, SEG // 2])
        for q in range(8):
            P.op("dve", "tensor_copy", PK(out=Fc[:, q, 0:1], in_=s5c[:, 3, q:q + 1]), reads=[s5cb], writes=[Fcb])
            P.op("dve", "tensor_copy", PK(out=Fs[:, q, 0:1], in_=s5c[:, 4, q:q + 1]), reads=[s5cb], writes=[Fsb])
            n = 1
            while n < SEG:
                cn = Fc[:, q, n - 1:n]; sn = Fs[:, q, n - 1:n]
                P.op("dve", "tensor_scalar", PK(out=ftmp[:, 0:n], in0=Fs[:, q, 0:n], scalar1=sn, scalar2=None, op0=ALU.mult), reads=[Fsb], writes=[ftmpb])
                P.op("dve", "scalar_tensor_tensor", PK(out=Fc[:, q, n:2 * n], in0=Fc[:, q, 0:n], scalar=cn, in1=ftmp[:, 0:n], op0=ALU.mult, op1=ALU.subtract),
                     reads=[Fcb, ftmpb], writes=[Fcb])
                P.op("dve", "tensor_scalar", PK(out=ftmp[:, 0:n], in0=Fc[:, q, 0:n], scalar1=sn, scalar2=None, op0=ALU.mult), reads=[Fcb, Fsb], writes=[ftmpb])
                P.op("dve", "scalar_tensor_tensor", PK(out=Fs[:, q, n:2 * n], in0=Fs[:, q, 0:n], scalar=cn, in1=ftmp[:, 0:n], op0=ALU.mult, op1=ALU.add),
                     reads=[Fsb, Fcb, ftmpb], writes=[Fsb])
                n *= 2
        xst_re, xreb = T("xst_re", [128, 8, 2]); xst_im, ximb = T("xst_im", [128, 8, 2])
        P.op("dve", "memset", PK(xst_re[:, :, :], 0.0), writes=[xreb]); P.op("dve", "memset", PK(xst_im[:, :, :], 0.0), writes=[ximb])

        zT, _ = T("zT", [128, 11, SEG + 1]); zTb = [Buf("zT%d" % c) for c in range(11)]
        for c in range(11):
            P.op("dve", "memset", PK(zT[:, c, 0:1], 0.0), writes=[zTb[c]])
        Sst = [[T("S%d_%d" % (hp, i), [128, 64]) for i in range(2)] for hp in range(2)]
        for hp in range(2):
            P.op("dve", "memset", PK(Sst[hp][0][0][:, :], 0.0), writes=[Sst[hp][0][1]])
        sidx = [0, 0]
        xT_t, xTb = T("xT_sb", [128, 8, SEG], BF16); hn, hnb = T("hn", [128, 8, SEG], BF16)
        names = "ld a g al be km cum Gi tA tB Rb Kb Ab Bb Kh Bh rk".split()
        W_ = {n: T("w_" + n, [128, SEG]) for n in names}
        gC, gCb = T("gC", [128, 8])
        AAs, AAsb = T("AAs", [64, 320]); TM, TMb = T("TM", [64, 4, 128])
        Asq = [T("Asq%d" % i, [64, 128]) for i in range(2)]
        Zt = [T("Zt%d" % i, [64, 64]) for i in range(2)]
        Wsb, Wsbb = T("Wsb", [64, 64]); Ut0, Ut0b = T("Ut0", [64, 64]); Ut, Utb = T("Ut", [64, 64])
        Phi, Phib = T("Phi", [128, 64])
        ubuf, ubufb = T("ubuf", [128, 2, SEG], BF16)
        s5t = {n: T("s5_" + n, [128, SEG]) for n in "t1 t2 cre cim zre zim xre xim".split()}
        tw, twb = s5t["t1"]; sg0, sg0b = s5t["t2"]; sg1, sg1b = s5t["zre"]
        xreb16, xreb16b = T("xre16", [128, 8, SEG], BF16); ximb16, ximb16b = T("xim16", [128, 8, SEG], BF16)
        yo, yob = s5t["cre"]

        for seg in range(nseg):
            tsl = slice(seg * SEG, (seg + 1) * SEG)
            P.op("pool", "dma_start", PK(out=xT_t[:, :, :], in_=xT_d[:, tsl].rearrange("(c p) n -> p c n", p=128)), writes=[xTb], is_dma=True)
            norm_fm(C, lambda c: xT_t[:, c, :], [xTb], SEG, 8, cst3[:, 0, :], cstb, lambda c: V[:, VA["n_mix"] + c:VA["n_mix"] + c + 1],
                    lambda c: hn[:, c, :], [hnb], scr)
            for c, (c0, wdt) in enumerate(ZCH):
                pt, pb = C.psum()
                for k in range(8):
                    P.op("pe", "matmul", PK(pt[0:wdt, :], wc[:, k, c0:c0 + wdt], hn[:, k, :], start=(k == 0), stop=(k == 7)), reads=[wcb, hnb], writes=[pb])
                P.op("act", "activation", PK(out=zT[0:wdt, c, 1:SEG + 1], in_=pt[0:wdt, :], func=AF.Copy), reads=[pb], writes=[zTb[c]])
            tA, tAb = W_["tA"]
            for c in range(9):
                wdt = ZCH[c][1]
                P.op("dve", "tensor_tensor", PK(out=tA[0:wdt, :], in0=zT[0:wdt, c, 0:SEG], in1=zT[0:wdt, c, 1:SEG + 1], op=ALU.subtract), reads=[zTb[c]], writes=[tAb])
                P.op("dve", "tensor_copy", PK(out=zT[0:wdt, c, 0:1], in_=zT[0:wdt, c, SEG:SEG + 1]), reads=[tAb], writes=[zTb[c]])
                P.op("dve", "scalar_tensor_tensor", PK(out=zT[0:wdt, c, 1:SEG + 1], in0=tA[0:wdt, :], scalar=V[0:wdt, VA["mu"] + c:VA["mu"] + c + 1],
                                                      in1=zT[0:wdt, c, 1:SEG + 1], op0=ALU.mult, op1=ALU.add), reads=[tAb, zTb[c], vb], writes=[zTb[c]])
            Z = lambda c, lo=0, hi=128: zT[lo:hi, c, 1:SEG + 1]
            P.op("act", "activation", PK(out=tw[0:64, :], in_=Z(6, 0, 64), func=AF.Tanh), reads=[zTb[6]], writes=[twb])
            P.op("act", "activation", PK(out=sg0[:, :], in_=Z(7), func=AF.Sigmoid), reads=[zTb[7]], writes=[sg0b])
            P.op("act", "activation", PK(out=sg1[0:32, :], in_=Z(8, 0, 32), func=AF.Sigmoid), reads=[zTb[8]], writes=[sg1b])
            for hp in range(2):
                cols = slice(hp * 128, (hp + 1) * 128)
                vcol = lambda nm: V[:, VA[nm] + hp:VA[nm] + hp + 1]
                r_ = Z(hp); k_ = Z(2 + hp); v_ = Z(4 + hp)
                rb_, kb_, vb_ = zTb[hp], zTb[2 + hp], zTb[4 + hp]
                X = lambda n: W_[n][0]
                B_ = lambda n: W_[n][1]
                DV = lambda name, R, Wn, **kw: P.op("dve", name, PK(**kw), reads=R, writes=[B_(Wn)])
                pt, pb = C.psum()
                P.op("pe", "matmul", PK(pt[:, :], wup[:, cols], tw[0:64, :], start=True, stop=True), reads=[wupb, twb], writes=[pb])
                P.op("act", "activation", PK(out=X("ld")[:, :], in_=pt[:, :], func=AF.Sigmoid, bias=vcol("w0")), reads=[pb, vb], writes=[B_("ld")])
                DV("tensor_scalar", [B_("ld")], "ld", out=X("ld")[:, :], in0=X("ld")[:, :], scalar1=NEG_E05, scalar2=None, op0=ALU.mult)
                pt, pb = C.psum()
                P.op("pe", "matmul", PK(pt[:, :], aup[64:128, cols], Z(6, 64, 128), start=True, stop=True), reads=[aupb, zTb[6]], writes=[pb])
                P.op("act", "activation", PK(out=X("a")[:, :], in_=pt[:, :], func=AF.Sigmoid, bias=vcol("a0")), reads=[pb, vb], writes=[B_("a")])
                pt, pb = C.psum()
                P.op("pe", "matmul", PK(pt[:, :], gupa[:, cols], sg0[:, :], start=True, stop=False), reads=[gupab, sg0b], writes=[pb])
                P.op("pe", "matmul", PK(pt[:, :], gupb[:, cols], sg1[0:32, :], start=False, stop=True), reads=[gupbb, sg1b], writes=[pb])
                P.op("act", "activation", PK(out=X("g")[:, :], in_=pt[:, :], func=AF.Copy), reads=[pb], writes=[B_("g")])
                DV("tensor_scalar", [kb_, vb], "tA", out=X("tA")[:, :], in0=k_, scalar1=vcol("k_k"), scalar2=None, op0=ALU.mult)
                P.op("act", "activation", PK(out=X("tB")[:, :], in_=X("tA")[:, :], func=AF.Square), reads=[B_("tA")], writes=[B_("tB")])
                pt, pb = C.psum()
                P.op("pe", "matmul", PK(pt[:, :], c32[:, C_B64, :], X("tB")[:, :], start=True, stop=True), reads=[c32b, B_("tB")], writes=[pb])
                rs, rsb = scr["rs"]
                rstd_from(C, rs[:, :], pt[:, :], pb, rsb)
                DV("scalar_tensor_tensor", [B_("tA"), rsb], "al", out=X("al")[:, :], in0=X("tA")[:, :], scalar=0.125, in1=rs[:, :], op0=ALU.mult, op1=ALU.mult)
                DV("scalar_tensor_tensor", [B_("al"), B_("a")], "be", out=X("be")[:, :], in0=X("al")[:, :], scalar=-1.0, in1=X("a")[:, :], op0=ALU.mult, op1=ALU.mult)
                DV("tensor_scalar", [B_("a"), vb], "tA", out=X("tA")[:, :], in0=X("a")[:, :], scalar1=-1.0, scalar2=vcol("k_a"), op0=ALU.add, op1=ALU.mult)
                DV("scalar_tensor_tensor", [B_("tA"), kb_], "km", out=X("km")[:, :], in0=X("tA")[:, :], scalar=1.0, in1=k_, op0=ALU.add, op1=ALU.mult)
                DV("scalar_tensor_tensor", [rb_, B_("km"), vb], "rk", out=X("rk")[:, :], in0=r_, scalar=vcol("r_k"), in1=X("km")[:, :], op0=ALU.mult, op1=ALU.mult)
                DV("tensor_tensor_scan", [rmaskb, B_("ld")], "cum", out=X("cum")[:, :], data0=rmask[:, :], data1=X("ld")[:, :], initial=0.0, op0=ALU.mult, op1=ALU.add)
                cum3 = X("cum")[:, :].rearrange("p (c t) -> p c t", t=64)
                P.op("act", "activation", PK(out=X("Gi")[:, :], in_=X("cum")[:, :], func=AF.Exp), reads=[B_("cum")], writes=[B_("Gi")])
                DV("tensor_tensor", [B_("Gi"), rb_], "Rb", out=X("Rb")[:, :], in0=r_, in1=X("Gi")[:, :], op=ALU.mult)
                DV("tensor_tensor", [B_("cum"), B_("ld")], "tA", out=X("tA")[:, :], in0=X("cum")[:, :], in1=X("ld")[:, :], op=ALU.subtract)
                P.op("act", "activation", PK(out=X("Gi")[:, :], in_=X("tA")[:, :], func=AF.Exp), reads=[B_("tA")], writes=[B_("Gi")])
                DV("tensor_tensor", [B_("Gi"), B_("al")], "Ab", out=X("Ab")[:, :], in0=X("al")[:, :], in1=X("Gi")[:, :], op=ALU.mult)
                P.op("act", "activation", PK(out=X("Gi")[:, :], in_=X("cum")[:, :], func=AF.Exp, scale=-1.0), reads=[B_("cum")], writes=[B_("Gi")])
                DV("tensor_tensor", [B_("Gi"), B_("km")], "Kb", out=X("Kb")[:, :], in0=X("km")[:, :], in1=X("Gi")[:, :], op=ALU.mult)
                DV("tensor_tensor", [B_("Gi"), B_("be")], "Bb", out=X("Bb")[:, :], in0=X("be")[:, :], in1=X("Gi")[:, :], op=ALU.mult)
                tA3 = X("tA")[:, :].rearrange("p (c t) -> p c t", t=64)
                DV("tensor_tensor", [B_("cum")], "tA", out=tA3, in0=cum3[:, :, 63:64].to_broadcast([128, 8, 64]), in1=cum3, op=ALU.subtract)
                P.op("act", "activation", PK(out=X("Gi")[:, :], in_=X("tA")[:, :], func=AF.Exp), reads=[B_("tA")], writes=[B_("Gi")])
                DV("tensor_tensor", [B_("Gi"), B_("km")], "Kh", out=X("Kh")[:, :], in0=X("km")[:, :], in1=X("Gi")[:, :], op=ALU.mult)
                DV("tensor_tensor", [B_("Gi"), B_("be")], "Bh", out=X("Bh")[:, :], in0=X("be")[:, :], in1=X("Gi")[:, :], op=ALU.mult)
                P.op("act", "activation", PK(out=gC[:, :], in_=cum3[:, :, 63], func=AF.Exp), reads=[B_("cum")], writes=[gCb])
                po, pob = C.ps[6]
                for c in range(SEG // 64):
                    cs = slice(c * 64, (c + 1) * 64)
                    pt, pb = C.psum()
                    for i, (src, sb_) in enumerate(((v_, vb_), (X("Ab")[:, :], B_("Ab")), (X("Bh")[:, :], B_("Bh")), (X("Kh")[:, :], B_("Kh")))):
                        P.op("pe", "transpose", PK(pt[0:64, i * 128:(i + 1) * 128], src[:, cs], ident), reads=[sb_, c32b], writes=[pb])
                    P.op("act", "activation", PK(out=TM[:, :, :], in_=pt[0:64, :].rearrange("p (i k) -> p i k", i=4), func=AF.Copy), reads=[pb], writes=[TMb])
                    for e in range(2):
                        pr = slice(e * 64, (e + 1) * 64)
                        es = slice(e * 64, (e + 1) * 64)
                        pt, pb = C.psum()
                        Bb_, Kb_, Ab_, Rb_ = X("Bb")[pr, cs], X("Kb")[pr, cs], X("Ab")[pr, cs], X("Rb")[pr, cs]
                        P.op("pe", "matmul", PK(pt[0:64, 0:64], Bb_, Ab_, start=True, stop=True), reads=[B_("Bb"), B_("Ab")], writes=[pb])
                        P.op("pe", "matmul", PK(pt[0:64, 64:128], Bb_, Rb_, start=True, stop=True), reads=[B_("Bb"), B_("Rb")], writes=[pb])
                        P.op("pe", "matmul", PK(pt[0:64, 128:192], Kb_, Ab_, start=True, stop=True), reads=[B_("Kb"), B_("Ab")], writes=[pb])
                        P.op("pe", "matmul", PK(pt[0:64, 192:256], Kb_, Rb_, start=True, stop=True), reads=[B_("Kb"), B_("Rb")], writes=[pb])
                        P.op("pe", "matmul", PK(pt[0:64, 256:320], Ab_, Bb_, start=True, stop=True), reads=[B_("Bb"), B_("Ab")], writes=[pb])
                        P.op("dve", "tensor_tensor", PK(out=AAs[:, :], in0=pt[0:64, 0:320], in1=msk[:, :], op=ALU.mult), reads=[pb, mskb], writes=[AAsb])
                        A_ab, A_rb, A_ak, A_rk, A_abT = (AAs[:, 0:64], AAs[:, 64:128], AAs[:, 128:192], AAs[:, 192:256], AAs[:, 256:320])
                        Vt = TM[:, 0, es]; AbT = TM[:, 1, es]; BhT = TM[:, 2, es]; KhT = TM[:, 3, es]
                        zi = 0
                        P.op("dve", "tensor_tensor", PK(out=Zt[0][0][:, :], in0=A_ab, in1=ident[0:64, 0:64], op=ALU.add), reads=[AAsb, c32b], writes=[Zt[0][1]])
                        curA, curAT, curb = A_ab, A_abT, AAsb
                        for lvl in range(1, 6):
                            psq, psqb = C.psum()
                            if lvl < 5:
                                P.op("pe", "matmul", PK(psq[0:64, 0:64], curAT, curA, start=True, stop=True), reads=[curb], writes=[psqb])
                            P.op("pe", "matmul", PK(psq[0:64, 64:128], curA, curAT, start=True, stop=True), reads=[curb], writes=[psqb])
                            at, atb = Asq[lvl % 2]
                            lo = 0 if lvl < 5 else 64
                            P.op("act", "activation", PK(out=at[:, lo:128], in_=psq[0:64, lo:128], func=AF.Copy), reads=[psqb], writes=[atb])
                            curA, curAT, curb = at[:, 0:64], at[:, 64:128], atb
                            pz, pzb = C.psum()
                            P.op("pe", "matmul", PK(pz[0:64, 0:64], curAT, Zt[zi][0][:, :], start=True, stop=True), reads=[curb, Zt[zi][1]], writes=[pzb])
                            P.op("dve", "tensor_tensor", PK(out=Zt[1 - zi][0][:, :], in0=pz[0:64, 0:64], in1=Zt[zi][0][:, :], op=ALU.add),
                                 reads=[pzb, Zt[zi][1]], writes=[Zt[1 - zi][1]])
                            zi = 1 - zi
                        Tm, Tmb = Zt[zi]
                        pt, pb = C.psum()
                        P.op("pe", "matmul", PK(pt[0:64, 0:64], A_ak, Vt, start=True, stop=True), reads=[AAsb, TMb], writes=[pb])
                        P.op("act", "activation", PK(out=Wsb[:, :], in_=pt[0:64, 0:64], func=AF.Copy), reads=[pb], writes=[Wsbb])
                        pt, pb = C.psum()
                        P.op("pe", "matmul", PK(pt[0:64, 0:64], Tm[:, :], Wsb[:, :], start=True, stop=True), reads=[Tmb, Wsbb], writes=[pb])
                        P.op("pe", "matmul", PK(pt[pr, 64:128], AbT, Tm[:, :], start=True, stop=True), reads=[Tmb, TMb], writes=[pb])
                        P.op("act", "activation", PK(out=Ut0[:, :], in_=pt[0:64, 0:64], func=AF.Copy), reads=[pb], writes=[Ut0b])
                        P.op("act", "activation", PK(out=Phi[pr, :], in_=pt[pr, 64:128], func=AF.Copy), reads=[pb], writes=[Phib])
                        Sc, Scb = Sst[hp][sidx[hp] % 2] if e == 0 else Sst[hp][sidx[hp] % 2]
                        Sn, Snb = Sst[hp][(sidx[hp] + 1) % 2]
                        pt, pb = C.psum()
                        P.op("pe", "matmul", PK(pt[0:64, 0:64], Phi[pr, :], Sc[pr, :], start=True, stop=True), reads=[Phib, Scb], writes=[pb])
                        P.op("dve", "tensor_tensor", PK(out=Ut[:, :], in0=pt[0:64, 0:64], in1=Ut0[:, :], op=ALU.add), reads=[pb, Ut0b], writes=[Utb])
                        P.op("pe", "matmul", PK(po[pr, cs], Sc[pr, :], Rb_, start=True, stop=False), reads=[Scb, B_("Rb")], writes=[pob])
                        P.op("pe", "matmul", PK(po[pr, cs], Ut[:, :], A_rb, start=False, stop=False), reads=[Utb, AAsb], writes=[pob])
                        P.op("pe", "matmul", PK(po[pr, cs], Vt, A_rk, start=False, stop=True), reads=[TMb, AAsb], writes=[pob])
                        pt, pb = C.psum()
                        P.op("pe", "matmul", PK(pt[pr, 0:64], BhT, Ut[:, :], start=True, stop=False), reads=[TMb, Utb], writes=[pb])
                        P.op("pe", "matmul", PK(pt[pr, 0:64], KhT, Vt, start=False, stop=True), reads=[TMb], writes=[pb])
                        P.op("dve", "scalar_tensor_tensor", PK(out=Sn[pr, :], in0=Sc[pr, :], scalar=gC[pr, c:c + 1], in1=pt[pr, 0:64], op0=ALU.mult, op1=ALU.add),
                             reads=[Scb, gCb, pb], writes=[Snb])
                    sidx[hp] += 1
                Osb, Osbb = W_["Gi"]
                P.op("act", "activation", PK(out=Osb[:, :], in_=po[:, :], func=AF.Copy), reads=[pob], writes=[Osbb])
                pm, pmb = C.psum()
                P.op("pe", "matmul", PK(pm[:, :], c32[:, C_B64, :], Osb[:, :], start=True, stop=True), reads=[c32b, Osbb], writes=[pmb])
                DV("tensor_tensor", [Osbb, pmb], "tA", out=X("tA")[:, :], in0=Osb[:, :], in1=pm[:, :], op=ALU.subtract)
                P.op("act", "activation", PK(out=X("tB")[:, :], in_=X("tA")[:, :], func=AF.Square), reads=[B_("tA")], writes=[B_("tB")])
                pv, pvb = C.psum()
                P.op("pe", "matmul", PK(pv[:, :], c32[:, C_B64, :], X("tB")[:, :], start=True, stop=True), reads=[c32b, B_("tB")], writes=[pvb])
                rstd_from(C, rs[:, :], pv[:, :], pvb, rsb, eps=64e-5)
                DV("tensor_tensor", [B_("tA"), rsb], "tA", out=X("tA")[:, :], in0=X("tA")[:, :], in1=rs[:, :], op=ALU.mult)
                P.op("act", "activation", PK(out=X("tB")[:, :], in_=X("tA")[:, :], func=AF.Identity, scale=vcol("ln_w"), bias=vcol("ln_b")),
                     reads=[B_("tA"), vb], writes=[B_("tB")])
                pk, pkb = C.psum()
                P.op("pe", "matmul", PK(pk[:, :], c32[:, C_B64, :], X("rk")[:, :], start=True, stop=True), reads=[c32b, B_("rk")], writes=[pkb])
                DV("scalar_tensor_tensor", [pkb, vb_], "tA", out=X("tA")[:, :], in0=pk[:, :], scalar=64.0, in1=v_, op0=ALU.mult, op1=ALU.mult)
                DV("tensor_tensor", [B_("tA"), B_("tB")], "tB", out=X("tB")[:, :], in0=X("tB")[:, :], in1=X("tA")[:, :], op=ALU.add)
                DV("tensor_tensor", [B_("tB"), B_("g")], "Gi", out=Osb[:, :], in0=X("tB")[:, :], in1=X("g")[:, :], op=ALU.mult)
                C.dma(ya_d[hp * 128:(hp + 1) * 128, tsl], Osb[:, :], R=[Osbb], is_out=True)

            for uc in range(2):
                P.op("act", "activation", PK(out=ubuf[:, uc, :], in_=Z(9 + uc), func=AF.Copy), reads=[zTb[9 + uc]], writes=[ubufb])
            for q in range(8):
                uc = q // 4
                t1, t1b = s5t["t1"]; t2, t2b = s5t["t2"]
                cre_, creb_ = s5t["cre"]; cim_, cimb_ = s5t["cim"]
                zre, zreb = s5t["zre"]; zim, zimb = s5t["zim"]
                pr_, prb_ = C.psum(); pi_, pib_ = C.psum()
                P.op("pe", "matmul", PK(pr_[:, :], breT[:, q, :], ubuf[:, uc, :], start=True, stop=True), reads=[breb, ubufb], writes=[prb_])
                P.op("pe", "matmul", PK(pi_[:, :], bimT[:, q, :], ubuf[:, uc, :], start=True, stop=True), reads=[bimb, ubufb], writes=[pib_])
                fc = Fc[:, q, :]; fs = Fs[:, q, :]
                P.op("dve", "tensor_tensor", PK(out=t1[:, :], in0=pr_[:, :], in1=fc, op=ALU.mult), reads=[prb_, Fcb], writes=[t1b])
                P.op("dve", "tensor_tensor", PK(out=t2[:, :], in0=pi_[:, :], in1=fs, op=ALU.mult), reads=[pib_, Fsb], writes=[t2b])
                P.op("dve", "tensor_tensor", PK(out=cre_[:, :], in0=t1[:, :], in1=t2[:, :], op=ALU.add), reads=[t1b, t2b], writes=[creb_])
                P.op("dve", "tensor_tensor", PK(out=t1[:, :], in0=pi_[:, :], in1=fc, op=ALU.mult), reads=[pib_, Fcb], writes=[t1b])
                P.op("dve", "tensor_tensor", PK(out=t2[:, :], in0=pr_[:, :], in1=fs, op=ALU.mult), reads=[prb_, Fsb], writes=[t2b])
                P.op("dve", "tensor_tensor", PK(out=cim_[:, :], in0=t1[:, :], in1=t2[:, :], op=ALU.subtract), reads=[t1b, t2b], writes=[cimb_])
                P.op("dve", "tensor_tensor_scan", PK(out=zre[:, :], data0=s5c[:, 2, q:q + 1].to_broadcast([128, SEG]), data1=cre_[:, :], initial=xst_re[:, q, 0:1], op0=ALU.mult, op1=ALU.add),
                     reads=[s5cb, creb_, xreb], writes=[zreb])
                P.op("dve", "tensor_tensor_scan", PK(out=zim[:, :], data0=s5c[:, 2, q:q + 1].to_broadcast([128, SEG]), data1=cim_[:, :], initial=xst_im[:, q, 0:1], op0=ALU.mult, op1=ALU.add),
                     reads=[s5cb, cimb_, ximb], writes=[zimb])
                xre_, xreb_ = s5t["xre"]; xim_, ximb_ = s5t["xim"]
                P.op("dve", "tensor_tensor", PK(out=t1[:, :], in0=zre[:, :], in1=fc, op=ALU.mult), reads=[zreb, Fcb], writes=[t1b])
                P.op("dve", "tensor_tensor", PK(out=t2[:, :], in0=zim[:, :], in1=fs, op=ALU.mult), reads=[zimb, Fsb], writes=[t2b])
                P.op("dve", "tensor_tensor", PK(out=xre_[:, :], in0=t1[:, :], in1=t2[:, :], op=ALU.subtract), reads=[t1b, t2b], writes=[xreb_])
                P.op("dve", "tensor_tensor", PK(out=t1[:, :], in0=zim[:, :], in1=fc, op=ALU.mult), reads=[zimb, Fcb], writes=[t1b])
                P.op("dve", "tensor_tensor", PK(out=t2[:, :], in0=zre[:, :], in1=fs, op=ALU.mult), reads=[zreb, Fsb], writes=[t2b])
                P.op("dve", "tensor_tensor", PK(out=xim_[:, :], in0=t1[:, :], in1=t2[:, :], op=ALU.add), reads=[t1b, t2b], writes=[ximb_])
                P.op("dve", "tensor_copy", PK(out=xst_re[:, q, 0:1], in_=xre_[:, SEG - 1:SEG]), reads=[xreb_], writes=[xreb])
                P.op("dve", "tensor_copy", PK(out=xst_im[:, q, 0:1], in_=xim_[:, SEG - 1:SEG]), reads=[ximb_], writes=[ximb])
                P.op("act", "activation", PK(out=xreb16[:, q, :], in_=xre_[:, :], func=AF.Copy), reads=[xreb_], writes=[xreb16b])
                P.op("act", "activation", PK(out=ximb16[:, q, :], in_=xim_[:, :], func=AF.Copy), reads=[ximb_], writes=[ximb16b])
            for yc in range(2):
                py, pyb = C.psum()
                for qq in range(4):
                    q = yc * 4 + qq
                    P.op("pe", "matmul", PK(py[:, :], cpre[:, q, :], xreb16[:, q, :], start=(qq == 0), stop=False), reads=[cpreb, xreb16b], writes=[pyb])
                    P.op("pe", "matmul", PK(py[:, :], cpim[:, q, :], ximb16[:, q, :], start=False, stop=(qq == 3)), reads=[cpimb, ximb16b], writes=[pyb])
                t1, t1b = s5t["t1"]; t2, t2b = s5t["t2"]
                P.op("dve", "scalar_tensor_tensor", PK(out=yo[:, :], in0=Z(9 + yc), scalar=V[:, VA["s5_d"] + yc:VA["s5_d"] + yc + 1], in1=py[:, :], op0=ALU.mult, op1=ALU.add),
                     reads=[zTb[9 + yc], vb, pyb], writes=[yob])
                P.op("act", "activation", PK(out=t1[:, :], in_=yo[:, :], func=AF.Square), reads=[yob], writes=[t1b])
                P.op("dve", "tensor_scalar", PK(out=t1[:, :], in0=t1[:, :], scalar1=0.044715, scalar2=1.0, op0=ALU.mult, op1=ALU.add), reads=[t1b], writes=[t1b])
                P.op("dve", "tensor_tensor", PK(out=t1[:, :], in0=t1[:, :], in1=yo[:, :], op=ALU.mult), reads=[t1b, yob], writes=[t1b])
                P.op("act", "activation", PK(out=t2[:, :], in_=t1[:, :], func=AF.Sigmoid, scale=1.5957691216057308), reads=[t1b], writes=[t2b])
                P.op("dve", "tensor_tensor", PK(out=t2[:, :], in0=t2[:, :], in1=yo[:, :], op=ALU.mult), reads=[t2b, yob], writes=[t2b])
                C.dma(yb_d[yc * 128:(yc + 1) * 128, tsl], t2[:, :], R=[t2b], is_out=True)
        P.emit(st)
    return nc


def pack_A(inp, b, hh):
    W = np.asarray(inp["hy_w_in"][0]); mu = np.asarray(inp["rw_mu"][0])
    hc = slice(hh * 256, (hh + 1) * 256)
    idx = np.concatenate([np.arange(0, 512)[hc], 512 + np.arange(0, 512)[hc], 1024 + np.arange(0, 512)[hc],
                          np.arange(1536, 1824), 1824 + np.arange(0, 512)[hc]])
    wc = np.ascontiguousarray(W[:, idx])
    muc = np.concatenate([mu[idx[:1056]], np.zeros(96, np.float32)])
    Vt = np.zeros((128, NVA), np.float32)

    def put(name, arr):
        a = col(arr); Vt[:, VA[name]:VA[name] + a.shape[1]] = a
    put("n_mix", inp["norm_mix"][0])
    put("mu", muc)
    for nm, key in (("w0", "rw_w0"), ("a0", "rw_a0"), ("k_k", "rw_k_k"), ("k_a", "rw_k_a"), ("ln_w", "rw_ln_w"), ("ln_b", "rw_ln_b")):
        put(nm, np.asarray(inp[key][0])[hc])
    put("r_k", np.asarray(inp["rw_r_k"][0]).reshape(-1)[hc])
    put("s5_d", np.asarray(inp["s5_d"][0])[hc])
    gs = slice(hh * 16, (hh + 1) * 16)
    lre = np.asarray(inp["s5_lam_re"][0])[gs]; lim = np.asarray(inp["s5_lam_im"][0])[gs]; ldt = np.asarray(inp["s5_log_dt"][0])[gs]
    Vt[:, VA["lam_re"]:VA["lam_re"] + 8] = lre.reshape(8, 128).T
    Vt[:, VA["lam_im"]:VA["lam_im"] + 8] = lim.reshape(8, 128).T
    Vt[:, VA["log_dt"]:VA["log_dt"] + 8] = np.repeat(ldt, 64).reshape(8, 128).T
    Vt[:, VA["halfpi"]] = np.float32(np.pi / 2)
    bre = np.asarray(inp["s5_b_re"][0])[gs]; bim = np.asarray(inp["s5_b_im"][0])[gs]
    cre = np.asarray(inp["s5_c_re"][0])[gs]; cim = np.asarray(inp["s5_c_im"][0])[gs]
    breT = np.zeros((8, 128, 128), np.float32); bimT = np.zeros_like(breT); creT = np.zeros_like(breT); cimT = np.zeros_like(breT)
    for q in range(8):
        for e in range(2):
            gl = 2 * q + e
            rows = slice((gl % 8) * 16, (gl % 8) * 16 + 16)
            breT[q, rows, e * 64:(e + 1) * 64] = bre[gl].T
            bimT[q, rows, e * 64:(e + 1) * 64] = bim[gl].T
            creT[q, e * 64:(e + 1) * 64, rows] = cre[gl].T
            cimT[q, e * 64:(e + 1) * 64, rows] = cim[gl].T
    aup = np.zeros((128, 256), np.float32); aup[64:] = np.asarray(inp["rw_a_up"][0])[:, hc]
    gup = np.asarray(inp["rw_g_up"][0])[:, hc]
    c32 = np.zeros((128, 3, 128), np.float32)
    c32[:, 0, :] = np.eye(128, dtype=np.float32)
    c32[0:64, 1, 0:64] = 1.0 / 64; c32[64:, 1, 64:] = 1.0 / 64
    c32[:, 2, :] = 1.0
    s = np.arange(64)[:, None]; t = np.arange(64)[None, :]
    su = (s < t).astype(np.float32); ui = (s <= t).astype(np.float32)
    mask5 = np.concatenate([su, ui, su, ui, su.T], 1)
    rmask = np.ones((128, SEG), np.float32); rmask[:, ::64] = 0.0
    return dict(xT=np.ascontiguousarray(np.asarray(inp["x"][b]).T), wc=wc, vecs=Vt, cst=cst_table(), c32=c32.reshape(128, 384), mask5=mask5, rmask=rmask,
                w_up=np.ascontiguousarray(np.asarray(inp["rw_w_up"][0])[:, hc]), a_up=aup, g_upa=np.ascontiguousarray(gup[:128]),
                g_upb=np.ascontiguousarray(gup[128:]), breT=breT, bimT=bimT, creT=creT, cimT=cimT)


class ACtx(Ctx):
    def __init__(self, nc, st, P, words):
        self.nc, self.st, self.P = nc, st, P
        self.arena = st.enter_context(nc.sbuf_tensor("arena", [128, words], F32))
        self.words = words
        self.off = 0
        self.ps = []
        for i in range(8):
            t = st.enter_context(nc.psum_tensor("ps%d" % i, [128, 512], F32))
            self.ps.append((t, Buf("ps%d" % i, excl=True)))
        self.psi = 0
        self.wbufs = []
        self.wi = 0
        self.dmaq = 0
        self.vb = None
        self.nphase = 0
        self.epsc, self.epsb = self.sb("epsc", [128, 2], F32)
        self.dummy, _ = self.sb("dummy", [128, 2], F32)
        P.op("dve", "memset", PK(self.epsc[:, 0:1], EPS), writes=[self.epsb])
        P.op("dve", "memset", PK(self.epsc[:, 1:2], 64e-5), writes=[self.epsb])
        self.mark = self.off
        self.psum_default = self.psum

    def sb(self, name, shape, dt):
        n = 1
        for s_ in shape[1:]:
            n *= s_
        nbytes = n * (2 if dt == BF16 else 4)
        nw = (nbytes + 31) // 32 * 8
        assert self.off + nw <= self.words, ("arena overflow", name, self.off, nw, self.words)
        v = self.arena[0:shape[0], self.off:self.off + nw]
        self.off += nw
        if dt == BF16:
            v = v.bitcast(BF16)
        v = v[:, 0:n]
        if len(shape) == 3:
            v = v.rearrange("p (a b) -> p a b", a=shape[1])
        elif len(shape) == 4:
            v = v.rearrange("p (a b c) -> p a b c", a=shape[1], b=shape[2])
        return v, Buf("%s_%d" % (name, self.nphase))

    def new_phase(self, keep=None):
        self.P.fence(self.dummy[:, 0:1])
        self.off = self.mark if keep is None else keep
        self.wbufs = []
        self.wi = 0
        self.nphase += 1
        self.vb = None

    def init_w(self, n, cols):
        self.wbufs = []
        self.wi = 0
        for i in range(n):
            self.wbufs.append(self.sb("wb%d" % i, [128, cols], BF16))


def body_A(C, d, hh, ymb, nseg=NSEG_FULL):
    P = C.P
    if True:
        xT_d = d["xT"]; wc_d = d["wc%d" % hh]; vec_d = d["vecsA%d" % hh]
        cst_d = d["cst"]; c32_d = d["c32A"]; msk_d = d["mask5"]; rmask_d = d["rmask"]
        wup_d = d["w_up%d" % hh]; aup_d = d["a_up%d" % hh]; gupa_d = d["g_upa%d" % hh]; gupb_d = d["g_upb%d" % hh]
        bre_d = d["breT%d" % hh]; bim_d = d["bimT%d" % hh]; cre_d = d["creT%d" % hh]; cim_d = d["cimT%d" % hh]
        ya_d = d["ymT"][hh * 256:(hh + 1) * 256, :]; yb_d = d["ymT"][512 + hh * 256:512 + (hh + 1) * 256, :]
        rot = [0, 1, 2, 3, 4, 7]
        rs_ = [0]

        def psum():
            r = C.ps[rot[rs_[0] % len(rot)]]
            rs_[0] += 1
            return r
        C.psum = psum
        T = lambda name, shape, dt=F32: C.sb(name, shape, dt)
        V, vb = T("V_sb", [128, NVA]); C.vb = vb
        C.dma(V[:, :], vec_d[:, :], W=[vb])
        cst3, cstb = T("cst_sb", [128, 5, 128], BF16)
        P.op("pool", "dma_start", PK(out=cst3[:, :, :], in_=cst_d.rearrange("p (a b) -> p a b", a=5)), writes=[cstb], is_dma=True)
        c32, c32b = T("c32_sb", [128, 3, 128]); C.dma(c32[:, :, :], c32_d.rearrange("p (a b) -> p a b", a=3), W=[c32b])
        msk, mskb = T("msk_sb", [64, 320]); C.dma(msk[:, :], msk_d[:, :], W=[mskb])
        rmask, rmaskb = T("rmask_sb", [128, SEG]); C.dma(rmask[:, :], rmask_d[:, :], W=[rmaskb])
        wup, wupb = T("wup_sb", [64, 256]); C.dma(wup[:, :], wup_d[:, :], W=[wupb])
        aup, aupb = T("aup_sb", [128, 256]); C.dma(aup[:, :], aup_d[:, :], W=[aupb])
        gupa, gupab = T("gupa_sb", [128, 256]); C.dma(gupa[:, :], gupa_d[:, :], W=[gupab])
        gupb, gupbb = T("gupb_sb", [32, 256]); C.dma(gupb[:, :], gupb_d[:, :], W=[gupbb])
        wcL = [T("wc_sb%d" % i, [128, 8, 128], BF16) for i in range(2)]
        wci = [0]
        scr = dict(sq=T("sq", [128, 8, 512], BF16), rs=T("rs", [128, 512]))
        ident = c32[:, C_ID, :]
        names = "ld a g al be km cum Gi tA tB Rb Kb Ab Bb Kh Bh rk".split()
        W_ = {n: T("w_" + n, [128, SEG]) for n in names}

        breT, breb = T("breT_sb", [128, 8, 128], BF16); bimT, bimb = T("bimT_sb", [128, 8, 128], BF16)
        P.op("pool", "dma_start", PK(out=breT[:, :, :], in_=bre_d.rearrange("q k m -> k q m")), writes=[breb], is_dma=True)
        P.op("pool", "dma_start", PK(out=bimT[:, :, :], in_=bim_d.rearrange("q k m -> k q m")), writes=[bimb], is_dma=True)
        creT, creb = T("creT_sb", [128, 8, 128]); cimT, cimb = T("cimT_sb", [128, 8, 128])
        C.dma(creT[:, :, :], cre_d.rearrange("q k m -> k q m"), W=[creb])
        C.dma(cimT[:, :, :], cim_d.rearrange("q k m -> k q m"), W=[cimb])
        s5c, s5cb = T("s5c", [128, 12, 8])
        lre = V[:, VA["lam_re"]:VA["lam_re"] + 8]; lim = V[:, VA["lam_im"]:VA["lam_im"] + 8]
        S = lambda i: s5c[:, i, :]
        dv = lambda name, **kw: P.op("dve", name, PK(**kw), reads=[s5cb, vb], writes=[s5cb])
        P.op("act", "activation", PK(out=S(0), in_=V[:, VA["log_dt"]:VA["log_dt"] + 8], func=AF.Exp), reads=[vb], writes=[s5cb])
        dv("tensor_tensor", out=S(1), in0=lim, in1=S(0), op=ALU.mult)
        dv("tensor_tensor", out=S(9), in0=lre, in1=S(0), op=ALU.mult)
        P.op("act", "activation", PK(out=S(2), in_=S(9), func=AF.Exp), reads=[s5cb], writes=[s5cb])
        P.op("act", "activation", PK(out=S(4), in_=S(1), func=AF.Sin, scale=0.125), reads=[s5cb], writes=[s5cb])
        P.op("act", "activation", PK(out=S(3), in_=S(1), func=AF.Sin, scale=-0.125, bias=V[:, VA["halfpi"]:VA["halfpi"] + 1]), reads=[s5cb, vb], writes=[s5cb])
        for _ in range(3):
            dv("tensor_tensor", out=S(9), in0=S(3), in1=S(3), op=ALU.mult)
            dv("tensor_tensor", out=S(10), in0=S(4), in1=S(4), op=ALU.mult)
            dv("tensor_tensor", out=S(11), in0=S(3), in1=S(4), op=ALU.mult)
            dv("tensor_tensor", out=S(3), in0=S(9), in1=S(10), op=ALU.subtract)
            dv("tensor_scalar", out=S(4), in0=S(11), scalar1=2.0, scalar2=None, op0=ALU.mult)
        dv("tensor_tensor", out=S(5), in0=S(2), in1=S(3), op=ALU.mult)
        dv("tensor_tensor", out=S(6), in0=S(2), in1=S(4), op=ALU.mult)
        dv("tensor_scalar_add", out=S(5), in0=S(5), scalar1=-1.0)
        dv("tensor_tensor", out=S(9), in0=lre, in1=lre, op=ALU.mult)
        dv("tensor_tensor", out=S(10), in0=lim, in1=lim, op=ALU.mult)
        dv("tensor_tensor", out=S(9), in0=S(9), in1=S(10), op=ALU.add)
        dv("reciprocal", out=S(9), in_=S(9))
        dv("tensor_tensor", out=S(10), in0=S(5), in1=lre, op=ALU.mult)
        dv("tensor_tensor", out=S(11), in0=S(6), in1=lim, op=ALU.mult)
        dv("tensor_tensor", out=S(10), in0=S(10), in1=S(11), op=ALU.add)
        dv("tensor_tensor", out=S(7), in0=S(10), in1=S(9), op=ALU.mult)
        dv("tensor_tensor", out=S(10), in0=S(6), in1=lre, op=ALU.mult)
        dv("tensor_tensor", out=S(11), in0=S(5), in1=lim, op=ALU.mult)
        dv("tensor_tensor", out=S(10), in0=S(10), in1=S(11), op=ALU.subtract)
        dv("tensor_tensor", out=S(8), in0=S(10), in1=S(9), op=ALU.mult)
        cpre, cpreb = T("cpre", [128, 8, 128], BF16); cpim, cpimb = T("cpim", [128, 8, 128], BF16)
        ctmp = W_["tA"][0][:, 0:128]; ctmpb = W_["tA"][1]
        for q in range(8):
            cr = s5c[:, 7, q:q + 1]; ci = s5c[:, 8, q:q + 1]
            P.op("dve", "tensor_scalar", PK(out=ctmp[:, :], in0=cimT[:, q, :], scalar1=ci, scalar2=None, op0=ALU.mult), reads=[cimb, s5cb], writes=[ctmpb])
            P.op("dve", "scalar_tensor_tensor", PK(out=cpre[:, q, :], in0=creT[:, q, :], scalar=cr, in1=ctmp[:, :], op0=ALU.mult, op1=ALU.subtract),
                 reads=[creb, ctmpb, s5cb], writes=[cpreb])
            P.op("dve", "tensor_scalar", PK(out=ctmp[:, :], in0=cimT[:, q, :], scalar1=cr, scalar2=-1.0, op0=ALU.mult, op1=ALU.mult), reads=[cimb, s5cb], writes=[ctmpb])
            P.op("dve", "scalar_tensor_tensor", PK(out=ctmp[:, :], in0=creT[:, q, :], scalar=ci, in1=ctmp[:, :], op0=ALU.mult, op1=ALU.subtract),
                 reads=[creb, ctmpb, s5cb], writes=[ctmpb])
            P.op("dve", "tensor_scalar", PK(out=cpim[:, q, :], in0=ctmp[:, :], scalar1=-1.0, scalar2=None, op0=ALU.mult), reads=[ctmpb], writes=[cpimb])
        Fc, Fcb = T("Fc", [128, 8, SEG]); Fs, Fsb = T("Fs", [128, 8, SEG])
        ncol, ncolb = T("ncol", [128, 2])
        ftmp = W_["tB"][0][:, 0:SEG // 2]; ftmpb = W_["tB"][1]
        for q in range(8):
            P.op("dve", "tensor_copy", PK(out=Fc[:, q, 0:1], in_=s5c[:, 3, q:q + 1]), reads=[s5cb], writes=[Fcb])
            P.op("dve", "tensor_copy", PK(out=Fs[:, q, 0:1], in_=s5c[:, 4, q:q + 1]), reads=[s5cb], writes=[Fsb])
            n = 1
            while n < SEG:
                cn = Fc[:, q, n - 1:n]; sn = Fs[:, q, n - 1:n]
                P.op("dve", "tensor_scalar", PK(out=ftmp[:, 0:n], in0=Fs[:, q, 0:n], scalar1=sn, scalar2=None, op0=ALU.mult), reads=[Fsb], writes=[ftmpb])
                P.op("dve", "scalar_tensor_tensor", PK(out=Fc[:, q, n:2 * n], in0=Fc[:, q, 0:n], scalar=cn, in1=ftmp[:, 0:n], op0=ALU.mult, op1=ALU.subtract),
                     reads=[Fcb, ftmpb], writes=[Fcb])
                P.op("dve", "tensor_scalar", PK(out=ftmp[:, 0:n], in0=Fc[:, q, 0:n], scalar1=sn, scalar2=None, op0=ALU.mult), reads=[Fcb, Fsb], writes=[ftmpb])
                P.op("dve", "scalar_tensor_tensor", PK(out=Fs[:, q, n:2 * n], in0=Fs[:, q, 0:n], scalar=cn, in1=ftmp[:, 0:n], op0=ALU.mult, op1=ALU.add),
                     reads=[Fsb, Fcb, ftmpb], writes=[Fsb])
                n *= 2
        xst_re, xreb = T("xst_re", [128, 8, 2]); xst_im, ximb = T("xst_im", [128, 8, 2])
        P.op("dve", "memset", PK(xst_re[:, :, :], 0.0), writes=[xreb]); P.op("dve", "memset", PK(xst_im[:, :, :], 0.0), writes=[ximb])

        zT, _ = T("zT", [128, 11, SEG + 1]); zTb = [Buf("zT%d" % c) for c in range(11)]
        for c in range(11):
            P.op("dve", "memset", PK(zT[:, c, 0:1], 0.0), writes=[zTb[c]])
        Sst = [[T("S%d_%d" % (hp, i), [128, 64]) for i in range(2)] for hp in range(2)]
        for hp in range(2):
            P.op("dve", "memset", PK(Sst[hp][0][0][:, :], 0.0), writes=[Sst[hp][0][1]])
        sidx = [0, 0]
        xT_t, xTb = T("xT_sb", [128, 8, SEG], BF16); hn, hnb = T("hn", [128, 8, SEG], BF16)
        tw = xT_t[:, 0:2, :].rearrange("p a b -> p (a b)").bitcast(F32); sg0 = xT_t[:, 2:4, :].rearrange("p a b -> p (a b)").bitcast(F32)
        sg1 = xT_t[:, 4:6, :].rearrange("p a b -> p (a b)").bitcast(F32); twb = sg0b = sg1b = xTb
        gC, gCb = T("gC", [128, 8])
        NG = 4
        NU = 2 * NG
        AAsL = [T("AAs%d" % u, [64, 320]) for u in range(NU)]
        TML = [T("TM%d" % g, [64, 4, 128]) for g in range(NG)]
        AsqL = [[T("Asq%d_%d" % (u, i), [64, 128]) for i in range(2)] for u in range(NU)]
        ZtL = [[T("Zt%d_%d" % (u, i), [64, 64]) for i in range(2)] for u in range(NU)]
        WsbL = [T("Wsb%d" % u, [64, 64]) for u in range(NU)]
        Ut0L = [T("Ut0%d" % u, [64, 64]) for u in range(NU)]
        UtL = [T("Ut%d" % u, [64, 64]) for u in range(NU)]
        PhiL = [T("Phi%d" % g, [128, 64]) for g in range(NG)]
        ubuf, ubufb = T("ubuf", [128, 2, SEG], BF16)
        s5t = {n: T("s5_" + n, [128, SEG]) for n in "t1 t2 cre cim zre zim xre xim".split()}
        xreb16, _ = T("xre16", [128, 2, SEG], BF16); ximb16, _ = T("xim16", [128, 2, SEG], BF16)
        yacc, yaccb = T("yacc", [128, SEG])
        xre16bL = [Buf("xre16_0"), Buf("xre16_1")]; xim16bL = [Buf("xim16_0"), Buf("xim16_1")]
        yo, yob = s5t["cre"]

        for seg in range(nseg):
            tsl = slice(seg * SEG, (seg + 1) * SEG)
            P.op("pool", "dma_start", PK(out=xT_t[:, :, :], in_=xT_d[:, tsl].rearrange("(c p) n -> p c n", p=128)), writes=[xTb], is_dma=True)
            norm_fm(C, lambda c: xT_t[:, c, :], [xTb], SEG, 8, cst3[:, 0, :], cstb, lambda c: V[:, VA["n_mix"] + c:VA["n_mix"] + c + 1],
                    lambda c: hn[:, c, :], [hnb], scr)
            for c, (c0, wdt) in enumerate(ZCH):
                wc, wcb = wcL[wci[0] % 2]; wci[0] += 1
                P.op("pool", "dma_start", PK(out=wc[:, :, 0:wdt], in_=wc_d[:, c0:c0 + wdt].rearrange("(k p) n -> p k n", p=128)), writes=[wcb], is_dma=True)
                pt, pb = C.psum()
                for k in range(8):
                    P.op("pe", "matmul", PK(pt[0:wdt, :], wc[:, k, 0:wdt], hn[:, k, :], start=(k == 0), stop=(k == 7)), reads=[wcb, hnb], writes=[pb])
                P.op("act", "activation", PK(out=zT[0:wdt, c, 1:SEG + 1], in_=pt[0:wdt, :], func=AF.Copy), reads=[pb], writes=[zTb[c]])
            tA, tAb = W_["tA"]
            for c in range(9):
                wdt = ZCH[c][1]
                P.op("dve", "tensor_tensor", PK(out=tA[0:wdt, :], in0=zT[0:wdt, c, 0:SEG], in1=zT[0:wdt, c, 1:SEG + 1], op=ALU.subtract), reads=[zTb[c]], writes=[tAb])
                P.op("dve", "tensor_copy", PK(out=zT[0:wdt, c, 0:1], in_=zT[0:wdt, c, SEG:SEG + 1]), reads=[tAb], writes=[zTb[c]])
                P.op("dve", "scalar_tensor_tensor", PK(out=zT[0:wdt, c, 1:SEG + 1], in0=tA[0:wdt, :], scalar=V[0:wdt, VA["mu"] + c:VA["mu"] + c + 1],
                                                      in1=zT[0:wdt, c, 1:SEG + 1], op0=ALU.mult, op1=ALU.add), reads=[tAb, zTb[c], vb], writes=[zTb[c]])
            Z = lambda c, lo=0, hi=128: zT[lo:hi, c, 1:SEG + 1]
            P.op("act", "activation", PK(out=tw[0:64, :], in_=Z(6, 0, 64), func=AF.Tanh), reads=[zTb[6]], writes=[twb])
            P.op("act", "activation", PK(out=sg0[:, :], in_=Z(7), func=AF.Sigmoid), reads=[zTb[7]], writes=[sg0b])
            P.op("act", "activation", PK(out=sg1[0:32, :], in_=Z(8, 0, 32), func=AF.Sigmoid), reads=[zTb[8]], writes=[sg1b])
            for uc in range(2):
                P.op("act", "activation", PK(out=ubuf[:, uc, :], in_=Z(9 + uc), func=AF.Copy), reads=[zTb[9 + uc]], writes=[ubufb])
            def s5_pair(q):
                    uc = q // 4
                    t1, t1b = s5t["t1"]; t2, t2b = s5t["t2"]
                    cre_, creb_ = s5t["cre"]; cim_, cimb_ = s5t["cim"]
                    zre, zreb = s5t["zre"]; zim, zimb = s5t["zim"]
                    pr_, prb_ = C.psum(); pi_, pib_ = C.psum()
                    P.op("pe", "matmul", PK(pr_[:, :], breT[:, q, :], ubuf[:, uc, :], start=True, stop=True), reads=[breb, ubufb], writes=[prb_])
                    P.op("pe", "matmul", PK(pi_[:, :], bimT[:, q, :], ubuf[:, uc, :], start=True, stop=True), reads=[bimb, ubufb], writes=[pib_])
                    fc = Fc[:, q, :]; fs = Fs[:, q, :]
                    P.op("dve", "tensor_tensor", PK(out=t1[:, :], in0=pr_[:, :], in1=fc, op=ALU.mult), reads=[prb_, Fcb], writes=[t1b])
                    P.op("dve", "tensor_tensor", PK(out=t2[:, :], in0=pi_[:, :], in1=fs, op=ALU.mult), reads=[pib_, Fsb], writes=[t2b])
                    P.op("dve", "tensor_tensor", PK(out=cre_[:, :], in0=t1[:, :], in1=t2[:, :], op=ALU.add), reads=[t1b, t2b], writes=[creb_])
                    P.op("dve", "tensor_tensor", PK(out=t1[:, :], in0=pi_[:, :], in1=fc, op=ALU.mult), reads=[pib_, Fcb], writes=[t1b])
                    P.op("dve", "tensor_tensor", PK(out=t2[:, :], in0=pr_[:, :], in1=fs, op=ALU.mult), reads=[prb_, Fsb], writes=[t2b])
                    P.op("dve", "tensor_tensor", PK(out=cim_[:, :], in0=t1[:, :], in1=t2[:, :], op=ALU.subtract), reads=[t1b, t2b], writes=[cimb_])
                    P.op("dve", "tensor_tensor_scan", PK(out=zre[:, :], data0=s5c[:, 2, q:q + 1].to_broadcast([128, SEG]), data1=cre_[:, :], initial=xst_re[:, q, 0:1], op0=ALU.mult, op1=ALU.add),
                         reads=[s5cb, creb_, xreb], writes=[zreb])
                    P.op("dve", "tensor_tensor_scan", PK(out=zim[:, :], data0=s5c[:, 2, q:q + 1].to_broadcast([128, SEG]), data1=cim_[:, :], initial=xst_im[:, q, 0:1], op0=ALU.mult, op1=ALU.add),
                         reads=[s5cb, cimb_, ximb], writes=[zimb])
                    xre_, xreb_ = s5t["xre"]; xim_, ximb_ = s5t["xim"]
                    P.op("dve", "tensor_tensor", PK(out=t1[:, :], in0=zre[:, :], in1=fc, op=ALU.mult), reads=[zreb, Fcb], writes=[t1b])
                    P.op("dve", "tensor_tensor", PK(out=t2[:, :], in0=zim[:, :], in1=fs, op=ALU.mult), reads=[zimb, Fsb], writes=[t2b])
                    P.op("dve", "tensor_tensor", PK(out=xre_[:, :], in0=t1[:, :], in1=t2[:, :], op=ALU.subtract), reads=[t1b, t2b], writes=[xreb_])
                    P.op("dve", "tensor_tensor", PK(out=t1[:, :], in0=zim[:, :], in1=fc, op=ALU.mult), reads=[zimb, Fcb], writes=[t1b])
                    P.op("dve", "tensor_tensor", PK(out=t2[:, :], in0=zre[:, :], in1=fs, op=ALU.mult), reads=[zreb, Fsb], writes=[t2b])
                    P.op("dve", "tensor_tensor", PK(out=xim_[:, :], in0=t1[:, :], in1=t2[:, :], op=ALU.add), reads=[t1b, t2b], writes=[ximb_])
                    P.op("dve", "tensor_copy", PK(out=xst_re[:, q, 0:1], in_=xre_[:, SEG - 1:SEG]), reads=[xreb_], writes=[xreb])
                    P.op("dve", "tensor_copy", PK(out=xst_im[:, q, 0:1], in_=xim_[:, SEG - 1:SEG]), reads=[ximb_], writes=[ximb])
                    xb_ = q % 2
                    P.op("act", "activation", PK(out=xreb16[:, xb_, :], in_=xre_[:, :], func=AF.Copy), reads=[xreb_], writes=[xre16bL[xb_]])
                    P.op("act", "activation", PK(out=ximb16[:, xb_, :], in_=xim_[:, :], func=AF.Copy), reads=[ximb_], writes=[xim16bL[xb_]])
                    py, pyb = C.psum()
                    qq = q % 4
                    P.op("pe", "matmul", PK(py[:, :], cpre[:, q, :], xreb16[:, xb_, :], start=True, stop=False), reads=[cpreb, xre16bL[xb_]], writes=[pyb])
                    P.op("pe", "matmul", PK(py[:, :], cpim[:, q, :], ximb16[:, xb_, :], start=False, stop=True), reads=[cpimb, xim16bL[xb_]], writes=[pyb])
                    if qq == 0:
                        P.op("act", "activation", PK(out=yacc[:, :], in_=py[:, :], func=AF.Copy), reads=[pyb], writes=[yaccb])
                    else:
                        P.op("dve", "tensor_tensor", PK(out=yacc[:, :], in0=py[:, :], in1=yacc[:, :], op=ALU.add), reads=[pyb, yaccb], writes=[yaccb])
                    if qq != 3:
                        return
                    yc = q // 4
                    t1, t1b = s5t["t1"]; t2, t2b = s5t["t2"]
                    P.op("dve", "scalar_tensor_tensor", PK(out=yo[:, :], in0=Z(9 + yc), scalar=V[:, VA["s5_d"] + yc:VA["s5_d"] + yc + 1], in1=yacc[:, :], op0=ALU.mult, op1=ALU.add),
                         reads=[zTb[9 + yc], vb, yaccb], writes=[yob])
                    P.op("act", "activation", PK(out=t1[:, :], in_=yo[:, :], func=AF.Square), reads=[yob], writes=[t1b])
                    P.op("dve", "tensor_scalar", PK(out=t1[:, :], in0=t1[:, :], scalar1=0.044715, scalar2=1.0, op0=ALU.mult, op1=ALU.add), reads=[t1b], writes=[t1b])
                    P.op("dve", "tensor_tensor", PK(out=t1[:, :], in0=t1[:, :], in1=yo[:, :], op=ALU.mult), reads=[t1b, yob], writes=[t1b])
                    P.op("act", "activation", PK(out=t2[:, :], in_=t1[:, :], func=AF.Sigmoid, scale=1.5957691216057308), reads=[t1b], writes=[t2b])
                    P.op("dve", "tensor_tensor", PK(out=t2[:, :], in0=t2[:, :], in1=yo[:, :], op=ALU.mult), reads=[t2b, yob], writes=[t2b])
                    C.dma(yb_d[yc * 128:(yc + 1) * 128, tsl], t2[:, :], R=[t2b])
            s5_next = [0]
            for hp in range(2):
                cols = slice(hp * 128, (hp + 1) * 128)
                vcol = lambda nm: V[:, VA[nm] + hp:VA[nm] + hp + 1]
                r_ = Z(hp); k_ = Z(2 + hp); v_ = Z(4 + hp)
                rb_, kb_, vb_ = zTb[hp], zTb[2 + hp], zTb[4 + hp]
                X = lambda n: W_[n][0]
                B_ = lambda n: W_[n][1]
                DV = lambda name, R, Wn, **kw: P.op("dve", name, PK(**kw), reads=R, writes=[B_(Wn)])
                pt, pb = C.psum()
                P.op("pe", "matmul", PK(pt[:, :], wup[:, cols], tw[0:64, :], start=True, stop=True), reads=[wupb, twb], writes=[pb])
                P.op("act", "activation", PK(out=X("ld")[:, :], in_=pt[:, :], func=AF.Sigmoid, bias=vcol("w0")), reads=[pb, vb], writes=[B_("ld")])
                DV("tensor_scalar", [B_("ld")], "ld", out=X("ld")[:, :], in0=X("ld")[:, :], scalar1=NEG_E05, scalar2=None, op0=ALU.mult)
                pt, pb = C.psum()
                P.op("pe", "matmul", PK(pt[:, :], aup[64:128, cols], Z(6, 64, 128), start=True, stop=True), reads=[aupb, zTb[6]], writes=[pb])
                P.op("act", "activation", PK(out=X("a")[:, :], in_=pt[:, :], func=AF.Sigmoid, bias=vcol("a0")), reads=[pb, vb], writes=[B_("a")])
                pt, pb = C.psum()
                P.op("pe", "matmul", PK(pt[:, :], gupa[:, cols], sg0[:, :], start=True, stop=False), reads=[gupab, sg0b], writes=[pb])
                P.op("pe", "matmul", PK(pt[:, :], gupb[:, cols], sg1[0:32, :], start=False, stop=True), reads=[gupbb, sg1b], writes=[pb])
                P.op("act", "activation", PK(out=X("g")[:, :], in_=pt[:, :], func=AF.Copy), reads=[pb], writes=[B_("g")])
                DV("tensor_scalar", [kb_, vb], "tA", out=X("tA")[:, :], in0=k_, scalar1=vcol("k_k"), scalar2=None, op0=ALU.mult)
                P.op("act", "activation", PK(out=X("tB")[:, :], in_=X("tA")[:, :], func=AF.Square), reads=[B_("tA")], writes=[B_("tB")])
                pt, pb = C.psum()
                P.op("pe", "matmul", PK(pt[:, :], c32[:, C_B64, :], X("tB")[:, :], start=True, stop=True), reads=[c32b, B_("tB")], writes=[pb])
                rs, rsb = scr["rs"]
                rstd_from(C, rs[:, :], pt[:, :], pb, rsb)
                DV("scalar_tensor_tensor", [B_("tA"), rsb], "al", out=X("al")[:, :], in0=X("tA")[:, :], scalar=0.125, in1=rs[:, :], op0=ALU.mult, op1=ALU.mult)
                DV("scalar_tensor_tensor", [B_("al"), B_("a")], "be", out=X("be")[:, :], in0=X("al")[:, :], scalar=-1.0, in1=X("a")[:, :], op0=ALU.mult, op1=ALU.mult)
                DV("tensor_scalar", [B_("a"), vb], "tA", out=X("tA")[:, :], in0=X("a")[:, :], scalar1=-1.0, scalar2=vcol("k_a"), op0=ALU.add, op1=ALU.mult)
                DV("scalar_tensor_tensor", [B_("tA"), kb_], "km", out=X("km")[:, :], in0=X("tA")[:, :], scalar=1.0, in1=k_, op0=ALU.add, op1=ALU.mult)
                DV("scalar_tensor_tensor", [rb_, B_("km"), vb], "rk", out=X("rk")[:, :], in0=r_, scalar=vcol("r_k"), in1=X("km")[:, :], op0=ALU.mult, op1=ALU.mult)
                DV("tensor_tensor_scan", [rmaskb, B_("ld")], "cum", out=X("cum")[:, :], data0=rmask[:, :], data1=X("ld")[:, :], initial=0.0, op0=ALU.mult, op1=ALU.add)
                cum3 = X("cum")[:, :].rearrange("p (c t) -> p c t", t=64)
                P.op("act", "activation", PK(out=X("Gi")[:, :], in_=X("cum")[:, :], func=AF.Exp), reads=[B_("cum")], writes=[B_("Gi")])
                DV("tensor_tensor", [B_("Gi"), rb_], "Rb", out=X("Rb")[:, :], in0=r_, in1=X("Gi")[:, :], op=ALU.mult)
                DV("tensor_tensor", [B_("cum"), B_("ld")], "tA", out=X("tA")[:, :], in0=X("cum")[:, :], in1=X("ld")[:, :], op=ALU.subtract)
                P.op("act", "activation", PK(out=X("Gi")[:, :], in_=X("tA")[:, :], func=AF.Exp), reads=[B_("tA")], writes=[B_("Gi")])
                DV("tensor_tensor", [B_("Gi"), B_("al")], "Ab", out=X("Ab")[:, :], in0=X("al")[:, :], in1=X("Gi")[:, :], op=ALU.mult)
                P.op("act", "activation", PK(out=X("Gi")[:, :], in_=X("cum")[:, :], func=AF.Exp, scale=-1.0), reads=[B_("cum")], writes=[B_("Gi")])
                DV("tensor_tensor", [B_("Gi"), B_("km")], "Kb", out=X("Kb")[:, :], in0=X("km")[:, :], in1=X("Gi")[:, :], op=ALU.mult)
                DV("tensor_tensor", [B_("Gi"), B_("be")], "Bb", out=X("Bb")[:, :], in0=X("be")[:, :], in1=X("Gi")[:, :], op=ALU.mult)
                tA3 = X("tA")[:, :].rearrange("p (c t) -> p c t", t=64)
                DV("tensor_tensor", [B_("cum")], "tA", out=tA3, in0=cum3[:, :, 63:64].to_broadcast([128, 8, 64]), in1=cum3, op=ALU.subtract)
                P.op("act", "activation", PK(out=X("Gi")[:, :], in_=X("tA")[:, :], func=AF.Exp), reads=[B_("tA")], writes=[B_("Gi")])
                DV("tensor_tensor", [B_("Gi"), B_("km")], "Kh", out=X("Kh")[:, :], in0=X("km")[:, :], in1=X("Gi")[:, :], op=ALU.mult)
                DV("tensor_tensor", [B_("Gi"), B_("be")], "Bh", out=X("Bh")[:, :], in0=X("be")[:, :], in1=X("Gi")[:, :], op=ALU.mult)
                P.op("act", "activation", PK(out=gC[:, :], in_=cum3[:, :, 63], func=AF.Exp), reads=[B_("cum")], writes=[gCb])
                poL = [C.ps[6], C.ps[5]]
                vsrcs = ((v_, vb_), (X("Ab")[:, :], B_("Ab")), (X("Bh")[:, :], B_("Bh")), (X("Kh")[:, :], B_("Kh")))
                for g0 in range(0, SEG // 64, NG):
                    units = [(gi, e) for gi in range(NG) for e in range(2)]
                    CS = [slice((g0 + gi) * 64, (g0 + gi + 1) * 64) for gi in range(NG)]
                    PR = [slice(0, 64), slice(64, 128)]
                    for gi in range(NG):
                        pt, pb = C.psum()
                        for i, (src, sb_) in enumerate(vsrcs):
                            P.op("pe", "transpose", PK(pt[0:64, i * 128:(i + 1) * 128], src[:, CS[gi]], ident), reads=[sb_, c32b], writes=[pb])
                        P.op("act", "activation", PK(out=TML[gi][0][:, :, :], in_=pt[0:64, :].rearrange("p (i k) -> p i k", i=4), func=AF.Copy), reads=[pb], writes=[TML[gi][1]])
                    SB = 4
                    UB = [range(b0, min(NU, b0 + SB)) for b0 in range(0, NU, SB)]
                    for ub in UB:
                        pts = {}
                        for u in ub:
                            gi, e = units[u]
                            pr, cs = PR[e], CS[gi]
                            pt, pb = C.psum()
                            Bb_, Kb_, Ab_, Rb_ = X("Bb")[pr, cs], X("Kb")[pr, cs], X("Ab")[pr, cs], X("Rb")[pr, cs]
                            P.op("pe", "matmul", PK(pt[0:64, 0:64], Bb_, Ab_, start=True, stop=True), reads=[B_("Bb"), B_("Ab")], writes=[pb])
                            P.op("pe", "matmul", PK(pt[0:64, 64:128], Bb_, Rb_, start=True, stop=True), reads=[B_("Bb"), B_("Rb")], writes=[pb])
                            P.op("pe", "matmul", PK(pt[0:64, 128:192], Kb_, Ab_, start=True, stop=True), reads=[B_("Kb"), B_("Ab")], writes=[pb])
                            P.op("pe", "matmul", PK(pt[0:64, 192:256], Kb_, Rb_, start=True, stop=True), reads=[B_("Kb"), B_("Rb")], writes=[pb])
                            P.op("pe", "matmul", PK(pt[0:64, 256:320], Ab_, Bb_, start=True, stop=True), reads=[B_("Bb"), B_("Ab")], writes=[pb])
                            pts[u] = (pt, pb)
                        for u in ub:
                            pt, pb = pts[u]
                            P.op("dve", "tensor_tensor", PK(out=AAsL[u][0][:, :], in0=pt[0:64, 0:320], in1=msk[:, :], op=ALU.mult), reads=[pb, mskb], writes=[AAsL[u][1]])
                    for u in range(NU):
                        AAs, AAsb = AAsL[u]
                        P.op("dve", "tensor_tensor", PK(out=ZtL[u][0][0][:, :], in0=AAs[:, 0:64], in1=ident[0:64, 0:64], op=ALU.add), reads=[AAsb, c32b], writes=[ZtL[u][0][1]])
                    cur = [(AAsL[u][0][:, 0:64], AAsL[u][0][:, 256:320], AAsL[u][1]) for u in range(NU)]
                    zi = 0
                    for lvl in range(1, 6):
                        lo = 0 if lvl < 5 else 64
                        for ub in UB:
                            pts = {}
                            for u in ub:
                                curA, curAT, curb = cur[u]
                                psq, psqb = C.psum()
                                if lvl < 5:
                                    P.op("pe", "matmul", PK(psq[0:64, 0:64], curAT, curA, start=True, stop=True), reads=[curb], writes=[psqb])
                                P.op("pe", "matmul", PK(psq[0:64, 64:128], curA, curAT, start=True, stop=True), reads=[curb], writes=[psqb])
                                pts[u] = (psq, psqb)
                            for u in ub:
                                psq, psqb = pts[u]
                                at, atb = AsqL[u][lvl % 2]
                                P.op("act", "activation", PK(out=at[:, lo:128], in_=psq[0:64, lo:128], func=AF.Copy), reads=[psqb], writes=[atb])
                                cur[u] = (at[:, 0:64], at[:, 64:128], atb)
                        for ub in UB:
                            pts = {}
                            for u in ub:
                                pz, pzb = C.psum()
                                P.op("pe", "matmul", PK(pz[0:64, 0:64], cur[u][1], ZtL[u][zi][0][:, :], start=True, stop=True), reads=[cur[u][2], ZtL[u][zi][1]], writes=[pzb])
                                pts[u] = (pz, pzb)
                            for u in ub:
                                pz, pzb = pts[u]
                                P.op("dve", "tensor_tensor", PK(out=ZtL[u][1 - zi][0][:, :], in0=pz[0:64, 0:64], in1=ZtL[u][zi][0][:, :], op=ALU.add),
                                     reads=[pzb, ZtL[u][zi][1]], writes=[ZtL[u][1 - zi][1]])
                        zi = 1 - zi
                    for ub in UB:
                        pts = {}
                        for u in ub:
                            gi, e = units[u]
                            es = PR[e]
                            pt, pb = C.psum()
                            P.op("pe", "matmul", PK(pt[0:64, 0:64], AAsL[u][0][:, 128:192], TML[gi][0][:, 0, es], start=True, stop=True), reads=[AAsL[u][1], TML[gi][1]], writes=[pb])
                            pts[u] = (pt, pb)
                        for u in ub:
                            pt, pb = pts[u]
                            P.op("act", "activation", PK(out=WsbL[u][0][:, :], in_=pt[0:64, 0:64], func=AF.Copy), reads=[pb], writes=[WsbL[u][1]])
                    for ub in UB:
                        pts = {}
                        for u in ub:
                            gi, e = units[u]
                            pr, es = PR[e], PR[e]
                            Tm, Tmb = ZtL[u][zi]
                            pt, pb = C.psum()
                            P.op("pe", "matmul", PK(pt[0:64, 0:64], Tm[:, :], WsbL[u][0][:, :], start=True, stop=True), reads=[Tmb, WsbL[u][1]], writes=[pb])
                            P.op("pe", "matmul", PK(pt[pr, 64:128], TML[gi][0][:, 1, es], Tm[:, :], start=True, stop=True), reads=[Tmb, TML[gi][1]], writes=[pb])
                            pts[u] = (pt, pb)
                        for u in ub:
                            gi, e = units[u]
                            pt, pb = pts[u]
                            pr = PR[e]
                            P.op("act", "activation", PK(out=Ut0L[u][0][:, :], in_=pt[0:64, 0:64], func=AF.Copy), reads=[pb], writes=[Ut0L[u][1]])
                            P.op("act", "activation", PK(out=PhiL[gi][0][pr, :], in_=pt[pr, 64:128], func=AF.Copy), reads=[pb], writes=[PhiL[gi][1]])
                    for gi in range(NG):
                        c = g0 + gi
                        cs = CS[gi]
                        Sc, Scb = Sst[hp][sidx[hp] % 2]
                        Sn, Snb = Sst[hp][(sidx[hp] + 1) % 2]
                        TM, TMb = TML[gi]
                        Phi, Phib = PhiL[gi]
                        pts = []
                        for e in range(2):
                            pr = PR[e]
                            pt, pb = C.psum()
                            P.op("pe", "matmul", PK(pt[0:64, 0:64], Phi[pr, :], Sc[pr, :], start=True, stop=True), reads=[Phib, Scb], writes=[pb])
                            pts.append((pt, pb))
                        for e in range(2):
                            u = gi * 2 + e
                            pt, pb = pts[e]
                            P.op("dve", "tensor_tensor", PK(out=UtL[u][0][:, :], in0=pt[0:64, 0:64], in1=Ut0L[u][0][:, :], op=ALU.add), reads=[pb, Ut0L[u][1]], writes=[UtL[u][1]])
                        for e in range(2):
                            u = gi * 2 + e
                            pr = PR[e]
                            AAs, AAsb = AAsL[u]
                            Ut, Utb = UtL[u]
                            po, pob = poL[e]
                            mm1 = P.op("pe", "matmul", PK(po[pr, cs], Sc[pr, :], X("Rb")[pr, cs], start=True, stop=False), reads=[Scb, B_("Rb")], writes=[pob])
                            P.op("pe", "matmul", PK(po[pr, cs], Ut[:, :], AAs[:, 64:128], start=False, stop=False), reads=[Utb, AAsb], writes=[pob],
                                 after=([mm1] if e == 1 else []))
                            P.op("pe", "matmul", PK(po[pr, cs], TM[:, 0, pr], AAs[:, 192:256], start=False, stop=True), reads=[TMb, AAsb], writes=[pob])
                        pts = []
                        for e in range(2):
                            u = gi * 2 + e
                            pr = PR[e]
                            Ut, Utb = UtL[u]
                            pt, pb = C.psum()
                            P.op("pe", "matmul", PK(pt[pr, 0:64], TM[:, 2, pr], Ut[:, :], start=True, stop=False), reads=[TMb, Utb], writes=[pb])
                            P.op("pe", "matmul", PK(pt[pr, 0:64], TM[:, 3, pr], TM[:, 0, pr], start=False, stop=True), reads=[TMb], writes=[pb])
                            pts.append((pt, pb))
                        for e in range(2):
                            pr = PR[e]
                            pt, pb = pts[e]
                            P.op("dve", "scalar_tensor_tensor", PK(out=Sn[pr, :], in0=Sc[pr, :], scalar=gC[pr, c:c + 1], in1=pt[pr, 0:64], op0=ALU.mult, op1=ALU.add),
                                 reads=[Scb, gCb, pb], writes=[Snb])
                        sidx[hp] += 1
                    for _ in range(NG // 2):
                        s5_pair(s5_next[0]); s5_next[0] += 1
                Osb, Osbb = W_["Gi"]
                P.op("act", "activation", PK(out=Osb[0:64, :], in_=poL[0][0][0:64, :], func=AF.Copy), reads=[poL[0][1]], writes=[Osbb])
                P.op("act", "activation", PK(out=Osb[64:128, :], in_=poL[1][0][64:128, :], func=AF.Copy), reads=[poL[1][1]], writes=[Osbb])
                pm, pmb = C.psum()
                P.op("pe", "matmul", PK(pm[:, :], c32[:, C_B64, :], Osb[:, :], start=True, stop=True), reads=[c32b, Osbb], writes=[pmb])
                DV("tensor_tensor", [Osbb, pmb], "tA", out=X("tA")[:, :], in0=Osb[:, :], in1=pm[:, :], op=ALU.subtract)
                P.op("act", "activation", PK(out=X("tB")[:, :], in_=X("tA")[:, :], func=AF.Square), reads=[B_("tA")], writes=[B_("tB")])
                pv, pvb = C.psum()
                P.op("pe", "matmul", PK(pv[:, :], c32[:, C_B64, :], X("tB")[:, :], start=True, stop=True), reads=[c32b, B_("tB")], writes=[pvb])
                rstd_from(C, rs[:, :], pv[:, :], pvb, rsb, eps=64e-5)
                DV("tensor_tensor", [B_("tA"), rsb], "tA", out=X("tA")[:, :], in0=X("tA")[:, :], in1=rs[:, :], op=ALU.mult)
                P.op("act", "activation", PK(out=X("tB")[:, :], in_=X("tA")[:, :], func=AF.Identity, scale=vcol("ln_w"), bias=vcol("ln_b")),
                     reads=[B_("tA"), vb], writes=[B_("tB")])
                pk, pkb = C.psum()
                P.op("pe", "matmul", PK(pk[:, :], c32[:, C_B64, :], X("rk")[:, :], start=True, stop=True), reads=[c32b, B_("rk")], writes=[pkb])
                DV("scalar_tensor_tensor", [pkb, vb_], "tA", out=X("tA")[:, :], in0=pk[:, :], scalar=64.0, in1=v_, op0=ALU.mult, op1=ALU.mult)
                DV("tensor_tensor", [B_("tA"), B_("tB")], "tB", out=X("tB")[:, :], in0=X("tB")[:, :], in1=X("tA")[:, :], op=ALU.add)
                DV("tensor_tensor", [B_("tB"), B_("g")], "Gi", out=Osb[:, :], in0=X("tB")[:, :], in1=X("g")[:, :], op=ALU.mult)
                C.dma(ya_d[hp * 128:(hp + 1) * 128, tsl], Osb[:, :], R=[Osbb])

        C.psum = C.psum_default


def body_B(C, d, half, bufs):
    P = C.P
    t0 = half * NTOK
    if True:
        xT_d = d["xT"][:, t0:t0 + NTOK]; ymT_d = d["ymT"][:, t0:t0 + NTOK]; memT_d = d["memT"]
        vec_d = d["vecsB"]; cst_d = d["cst"]
        wglu_d = d["w_glu"]; wout_d = d["w_out"]; wq_d = d["w_q0"]; wkv_d = d["w_kv0"]; wo_d = d["w_o0"]
        wg_d = d["ff_g"]; wu_d = d["ff_u"]; wd_d = d["ff_d"]; wqkv_d = d["w_qkv"]
        h1T_d = d["h1T"][:, t0:t0 + NTOK]; qT_d = d["qT"][:, :, t0:t0 + NTOK]; kT_d = d["kT"][:, :, t0:t0 + NTOK]
        vtok_d = d["vtok"][t0:t0 + NTOK, :]
        ymb, h1b, qb_, kb_, vtb_ = bufs
        C.init_w(4, 4096)
        hT, _ = C.sb("hT", [128, 8, NTOK], F32); hTb = [Buf("hT%d" % i) for i in range(NTT)]
        bufA, _ = C.sb("bufA", [128, 8, NTOK], BF16); bufAb = [Buf("bA%d" % i) for i in range(NTT)]
        bufH, _ = C.sb("bufH", [128, 8, NTOK], BF16); bufHb = [Buf("bH%d" % i) for i in range(NTT)]
        V, vb = C.sb("V_sb", [128, NVB], F32); C.vb = vb
        cst3, cstb = C.sb("cst_sb", [128, 5, 128], BF16)
        scr = dict(sq=C.sb("sq", [128, 8, 512], BF16), rs=C.sb("rs", [128, 512], F32), sg=C.sb("sg", [128, 512], F32),
                   rd=C.sb("rd", [128, 512], F32))
        sqq, _ = C.sb("sqq", [128, 2, NTT, 512], BF16)
        scr["sqq"] = (sqq, [Buf("sqq%d" % i) for i in range(NTT)])
        et, _ = C.sb("et", [128, 2, 512], BF16)
        scr["et"] = (et, [Buf("et0"), Buf("et1")])
        C.dma(V[:, :], vec_d[:, :], W=[vb])
        P.op("pool", "dma_start", PK(out=cst3[:, :, :], in_=cst_d.rearrange("p (a b) -> p a b", a=5)), writes=[cstb], is_dma=True)
        for tt in range(NTT):
            sl = slice(tt * TT, (tt + 1) * TT)
            C.dma(hT[:, :, sl], xT_d[:, sl].rearrange("(c p) n -> p c n", p=128), W=[hTb[tt]])
            P.op("pool", "dma_start", PK(out=bufA[:, :, sl], in_=ymT_d[:, sl].rearrange("(c p) n -> p c n", p=128)),
                 reads=[ymb], writes=[bufAb[tt]], is_dma=True)
        wv, wb = C.load_w(wglu_d[:, :], 4, 512)
        sg, sgb = scr["sg"]
        for tt in range(NTT):
            sl = slice(tt * TT, (tt + 1) * TT)
            pts = []
            for oc in range(4):
                pt, pb = C.psum()
                for k in range(4):
                    P.op("pe", "matmul", PK(pt[:, :], wv[:, k, oc * 128:(oc + 1) * 128], bufA[:, 4 + k, sl],
                                                                            start=(k == 0), stop=(k == 3)), reads=[wb, bufAb[tt]], writes=[pb])
                pts.append((pt, pb))
            for oc in range(4):
                pt, pb = pts[oc]
                P.op("act", "activation", PK(out=sg[:, :], in_=pt[:, :], func=AF.Sigmoid,
                                                                bias=V[:, VB["b_glu"] + oc:VB["b_glu"] + oc + 1]), reads=[pb, vb], writes=[sgb])
                P.op("dve", "tensor_tensor", PK(out=bufA[:, 4 + oc, sl], in0=bufA[:, 4 + oc, sl], in1=sg[:, :], op=ALU.mult),
                     reads=[sgb, bufAb[tt]], writes=[bufAb[tt]])

        def evac_res(oc, tt, pt, pb, m):
            sl = slice(tt * TT, (tt + 1) * TT)
            P.op("dve", "tensor_tensor", PK(out=hT[:, oc, sl], in0=pt[:, :], in1=hT[:, oc, sl], op=ALU.add), reads=[pb, hTb[tt]], writes=[hTb[tt]])
        linear_fm(C, wout_d, 8, bufA, None, 1024, evac_res, xinbs=bufAb)
        (kn, knb), (vt, vtb) = mem_kv(C, memT_d, wkv_d, V, vb, VB["n_mem"], VB["k_gain"], cst3, cstb, scr, "m0")
        mem_xattn(C, hT, hTb, bufA, bufAb, bufH, bufHb, V, vb, VB["n_xattn"], VB["q_gain"], wq_d, wo_d, kn, knb, vt, vtb, cst3, cstb, scr)
        swiglu_ffn(C, hT, hTb, bufA, bufAb, bufH, bufHb, V, vb, VB["n_ffn"], wg_d, wu_d, wd_d, 2816, cst3, cstb, scr)
        for tt in range(NTT):
            sl = slice(tt * TT, (tt + 1) * TT)
            C.dma(h1T_d[:, sl].rearrange("(c p) n -> p c n", p=128), hT[:, :, sl], R=[hTb[tt]])
        for tt in range(NTT):
            sl = slice(tt * TT, (tt + 1) * TT)
            norm_fm(C, lambda c: hT[:, c, sl], [hTb[tt]], TT, 8, cst3[:, 0, :], cstb, lambda c: V[:, VB["n_mix1"] + c:VB["n_mix1"] + c + 1],
                    lambda c: bufA[:, c, sl], [bufAb[tt]], scr)
        P.fence(C.dummy[:, 0:1])
        NS = 4
        sqL = [(bufH[:, 0, i * 512:(i + 1) * 512], Buf("qk_sq%d" % i)) for i in range(NS)]
        f32v = [bufH[:, 1 + i, :].bitcast(F32) for i in range(4)]
        tl = [(f32v[i // 2][:, (i % 2) * 512:(i % 2 + 1) * 512], Buf("qk_t%d" % i)) for i in range(8)]
        rsL, qoL = tl[0:4], tl[4:8]
        qi = [0]
        P.op("dve", "tensor_scalar", PK(out=V[:, VB["da_qg"]:VB["da_qg"] + 1], in0=V[:, VB["da_qg"]:VB["da_qg"] + 1], scalar1=0.125, scalar2=None, op0=ALU.mult),
             reads=[vb], writes=[vb])
        for which, (dst, gcol) in enumerate(((qT_d, VB["da_qg"]), (kT_d, VB["da_kg"]))):
            def evac_qk(oc, tt, pt, pb, m, dst=dst, gcol=gcol):
                sl = slice(tt * TT, (tt + 1) * TT)
                k_ = qi[0] % NS; qi[0] += 1
                sq1, sq1b = sqL[k_]; rs, rsb = rsL[k_]; qo, qob = qoL[k_]
                P.op("act", "activation", PK(out=sq1, in_=pt[:, :], func=AF.Square), reads=[pb], writes=[sq1b])
                p2, p2b = C.psum()
                P.op("pe", "matmul", PK(p2[:, :], cst3[:, 2, :], sq1, start=True, stop=True), reads=[sq1b, cstb], writes=[p2b])
                rstd_from(C, rs, p2[:, :], p2b, rsb)
                P.op("dve", "scalar_tensor_tensor", PK(out=qo, in0=pt[:, :], scalar=V[:, gcol:gcol + 1], in1=rs,
                                                      op0=ALU.mult, op1=ALU.mult), reads=[pb, rsb, vb], writes=[qob])
                C.dma(dst[2 * oc:2 * oc + 2, :, sl].rearrange("c r n -> (c r) n"), qo, R=[qob])
            linear_fm(C, wqkv_d, 8, bufA, None, 1024, evac_qk, col0=which * 1024, xinbs=bufAb)
        vo, vob = scr["sg"]
        for half2 in range(2):
            wv2, wb2 = C.load_w(wqkv_d[:, 2048 + half2 * 512:2048 + (half2 + 1) * 512], 8, 512)
            for s in range(16):
                tt = s // 4
                pt, pb = C.psum()
                for k in range(8):
                    P.op("pe", "matmul", PK(pt[:, :], bufA[:, k, s * 128:(s + 1) * 128], wv2[:, k, :], start=(k == 0), stop=(k == 7)),
                         reads=[wb2, bufAb[tt]], writes=[pb])
                P.op("act", "activation", PK(out=vo[:, :], in_=pt[:, :], func=AF.Copy), reads=[pb], writes=[vob])
                C.dma(vtok_d[s * 128:(s + 1) * 128, half2 * 512:(half2 + 1) * 512], vo[:, :], R=[vob])


def body_C1(C, d, bufA, bufAb, bufs):
    P = C.P
    if True:
        qs_d = d["qT"]; ks_d = d["kT"]; vf_d = d["vtok"]; qa4_d = d["qaug4"]; ka4_d = d["kaug4"]
        bias_d = d["biasT"]; lamb_d = d["lamb"]; sgc_d = d["sgc"]; cst_d = d["cst"]; sel_d = d["sel"]
        h1b, qb_, kb_, vtb_ = bufs
        cst3, cstb = C.sb("cst_sb", [128, 5, 128], BF16)
        P.op("pool", "dma_start", PK(out=cst3[:, :, :], in_=cst_d.rearrange("p (a b) -> p a b", a=5)), writes=[cstb], is_dma=True)
        biasT, biasb = C.sb("bias_sb", [128, 8, 512], F32)
        C.dma(biasT[:, :, :], bias_d.rearrange("p (a b) -> p a b", a=8), W=[biasb])
        lamb, lambb = C.sb("lamb_sb", [128, 4, 64], F32)
        C.dma(lamb[:, :, :], lamb_d.rearrange("p (a b) -> p a b", a=4), W=[lambb])
        sgc, sgcb = C.sb("sgc_sb", [128, 1], F32)
        C.dma(sgc[:, :], sgc_d[:, :], W=[sgcb])
        scr = dict(sq=C.sb("sq", [128, 1, 512], BF16), rs=C.sb("rs", [128, 512], F32))
        lt, ltb = C.sb("lt", [128, 2, 64], F32)
        lc, lcb = C.sb("lc", [128, 4], F32)
        for i in range(2):
            P.op("dve", "tensor_tensor", PK(out=lt[:, i, :], in0=lamb[:, 2 * i, :], in1=lamb[:, 2 * i + 1, :], op=ALU.mult), reads=[lambb], writes=[ltb])
            P.op("dve", "reduce_sum", PK(out=lc[:, i:i + 1], in_=lt[:, i, :], axis=AX.X), reads=[ltb], writes=[lcb])
        P.op("act", "activation", PK(out=lc[:, 0:2], in_=lc[:, 0:2], func=AF.Exp), reads=[lcb], writes=[lcb])
        P.op("dve", "tensor_tensor", PK(out=lc[:, 2:3], in0=lc[:, 1:2], in1=lc[:, 0:1], op=ALU.subtract), reads=[lcb], writes=[lcb])
        P.op("dve", "tensor_scalar_add", PK(out=lc[:, 3:4], in0=lc[:, 2:3], scalar1=-LAMBDA_INIT), reads=[lcb], writes=[lcb])
        P.op("dve", "tensor_scalar", PK(out=sgc[:, :], in0=sgc[:, :], scalar1=float(1.0 - LAMBDA_INIT), scalar2=None, op0=ALU.mult), reads=[sgcb], writes=[sgcb])
        kA = [C.sb("kA%d" % i, [68, 2, 4096], BF16) for i in range(2)]
        qA = [C.sb("qA%d" % i, [68, 2, NTOK], BF16) for i in range(2)]
        vA = [C.sb("vA%d" % i, [128, 32, 128], BF16) for i in range(2)]
        qraw, qrawb = C.sb("qraw", [64, 2, 4096], BF16)
        sel, selb = C.sb("sel_sb", [128, 2], F32)
        C.dma(sel[:, :], sel_d[:, :], W=[selb])
        for i_ in range(2):
            for c_ in range(2):
                P.op("pool", "dma_start", PK(out=qA[i_][0][64:68, c_, :], in_=qa4_d[:, :]), writes=[qA[i_][1]], is_dma=True)
        NE_, NTMP = 8, 8
        et, _ = C.sb("et", [128, NE_, 512], BF16); etb = [Buf("et%d" % i) for i in range(NE_)]
        tmp, _ = C.sb("tmp", [128, NTMP, 512], F32); tmpb = [Buf("tmp%d" % i) for i in range(NTMP)]
        rd, rdb = C.sb("rd", [128, 512], F32)
        o0, o0b = C.sb("o0", [128, 512], F32)
        o1, o1b = C.sb("o1", [128, 512], F32)
        ot, otb = C.sb("ot", [128, 512], F32)
        ei = 0
        ti_ = [0]
        for h in range(8):
            slope = float(2.0 ** (-(h + 1)))
            kt, ktb = kA[h % 2]; qt, qtb = qA[h % 2]; vt, vtb = vA[h % 2]
            P.op("pool", "dma_start", PK(out=kt[0:64, :, :], in_=ks_d[2 * h:2 * h + 2].rearrange("c r n -> r c n")), reads=[kb_], writes=[ktb], is_dma=True)
            P.op("pool", "dma_start", PK(out=kt[64:68, 0, :], in_=ka4_d[h]), writes=[ktb], is_dma=True)
            P.op("pool", "dma_start", PK(out=kt[64:68, 1, :], in_=ka4_d[h]), writes=[ktb], is_dma=True)
            P.op("pool", "dma_start", PK(out=qraw[:, :, :], in_=qs_d[2 * h:2 * h + 2].rearrange("c r n -> r c n")), reads=[qb_], writes=[qrawb], is_dma=True)
            for c in range(2):
                q5 = qraw[:, c, :].rearrange("p (m two n) -> p m two n", two=2, n=512)
                qo4 = qt[0:64, c, :].rearrange("p (m n) -> p m n", n=512)
                P.op("dve", "tensor_scalar", PK(out=qo4, in0=q5[:, :, 0, :], scalar1=sel[0:64, 0:1], scalar2=None, op0=ALU.mult), reads=[qrawb, selb], writes=[qtb])
                P.op("dve", "scalar_tensor_tensor", PK(out=qo4, in0=q5[:, :, 1, :], scalar=sel[0:64, 1:2], in1=qo4, op0=ALU.mult, op1=ALU.add),
                     reads=[qrawb, selb, qtb], writes=[qtb])
            P.op("pool", "dma_start", PK(out=vt[:, :, :], in_=vf_d[:, h * 128:(h + 1) * 128].rearrange("(j p) d -> p j d", p=128)), reads=[vtb_], writes=[vtb], is_dma=True)
            for m in range(4):
                nkb = 8 * (m + 1)
                qsl = slice(m * 512, (m + 1) * 512)
                N = [C.ps[0], C.ps[1]]
                Dn = [C.ps[2], C.ps[3]]
                LOOK = 3
                slot_of = {}

                def stage1(j):
                    for c in range(2):
                        sc, scb = C.ps[4 + ((2 * j + c) % 4)]
                        P.op("pe", "matmul", PK(sc[:, :], kt[:, c, j * 128:(j + 1) * 128], qt[:, c, qsl], start=True, stop=True),
                             reads=[ktb, qtb], writes=[scb])
                        tb = ti_[0] % NTMP; ti_[0] += 1
                        slot_of[(j, c)] = tb
                        if j >= nkb - 8:
                            s = j - (nkb - 8)
                            P.op("dve", "scalar_tensor_tensor", PK(out=tmp[:, tb, :], in0=biasT[:, s, :], scalar=slope, in1=sc[:, :], op0=ALU.mult, op1=ALU.add),
                                 reads=[biasb, scb], writes=[tmpb[tb]])
                        else:
                            P.op("dve", "tensor_copy", PK(out=tmp[:, tb, :], in_=sc[:, :]), reads=[scb], writes=[tmpb[tb]])
                for j0 in range(min(LOOK, nkb)):
                    stage1(j0)
                for j in range(nkb):
                    if j + LOOK < nkb:
                        stage1(j + LOOK)
                    for c in range(2):
                        e_slot = ei % NE_; ei += 1
                        tb = slot_of[(j, c)]
                        P.op("act", "activation", PK(out=et[:, e_slot, :], in_=tmp[:, tb, :], func=AF.Exp), reads=[tmpb[tb]], writes=[etb[e_slot]])
                        P.op("pe", "matmul", PK(N[c][0][:, :], vt[:, j, :], et[:, e_slot, :], start=(j == 0), stop=(j == nkb - 1)),
                             reads=[vtb, etb[e_slot]], writes=[N[c][1]])
                        P.op("pe", "matmul", PK(Dn[c][0][:, :], cst3[:, 4, :], et[:, e_slot, :], start=(j == 0), stop=(j == nkb - 1)),
                             reads=[cstb, etb[e_slot]], writes=[Dn[c][1]])
                P.op("dve", "reciprocal", PK(out=rd[:, :], in_=Dn[0][0][:, :]), reads=[Dn[0][1]], writes=[rdb])
                P.op("dve", "tensor_tensor", PK(out=o0[:, :], in0=N[0][0][:, :], in1=rd[:, :], op=ALU.mult), reads=[N[0][1], rdb], writes=[o0b])
                P.op("dve", "reciprocal", PK(out=rd[:, :], in_=Dn[1][0][:, :]), reads=[Dn[1][1]], writes=[rdb])
                P.op("dve", "tensor_tensor", PK(out=o1[:, :], in0=N[1][0][:, :], in1=rd[:, :], op=ALU.mult), reads=[N[1][1], rdb], writes=[o1b])
                P.op("dve", "scalar_tensor_tensor", PK(out=o0[:, :], in0=o1[:, :], scalar=lc[:, 3:4], in1=o0[:, :], op0=ALU.mult, op1=ALU.add),
                     reads=[o1b, o0b, lcb], writes=[o0b])
                sq, sqb = scr["sq"]; rs, rsb = scr["rs"]
                P.op("act", "activation", PK(out=sq[:, 0, :], in_=o0[:, :], func=AF.Square), reads=[o0b], writes=[sqb])
                pn, pnb = C.ps[4]
                P.op("pe", "matmul", PK(pn[:, :], cst3[:, 3, :], sq[:, 0, :], start=True, stop=True), reads=[sqb, cstb], writes=[pnb])
                rstd_from(C, rs[:, :], pn[:, :], pnb, rsb)
                P.op("dve", "scalar_tensor_tensor", PK(out=bufA[:, h, qsl], in0=o0[:, :], scalar=sgc[:, 0:1], in1=rs[:, :], op0=ALU.mult, op1=ALU.mult),
                     reads=[o0b, rsb, sgcb], writes=[bufAb[m]])


def body_C2(C, d, bufA, bufAb, h1b):
    P = C.P
    if True:
        h1s_d = d["h1T"]; memT_d = d["memT"]; vec_d = d["vecsC"]; cst_d = d["cst"]; c32_d = d["c32"]; sel_d = d["sel"]
        wdo_d = d["da_w_o"]; wq_d = d["w_q1"]; wkv_d = d["w_kv1"]; wo_d = d["w_o1"]; wr_d = d["w_router"]
        wg_d = d["moe_g"]; wu_d = d["moe_u"]; wd_d = d["moe_d"]; outT_d = d["outT"]
        C.init_w(3, 4096)
        hT, _ = C.sb("hT", [128, 8, NTOK], F32); hTb = [Buf("hT%d" % i) for i in range(NTT)]
        bufH, _ = C.sb("bufH", [128, 8, NTOK], BF16); bufHb = [Buf("bH%d" % i) for i in range(NTT)]
        V, vb = C.sb("V_sb", [128, NVC], F32); C.vb = vb
        cst3, cstb = C.sb("cst_sb", [128, 5, 128], BF16)
        c32, c32b = C.sb("c32_sb", [128, 2, 128], F32)
        scr = dict(sq=C.sb("sq", [128, 8, 512], BF16), rs=C.sb("rs", [128, 512], F32), sg=C.sb("sg", [128, 512], F32),
                   rd=C.sb("rd", [128, 512], F32))
        sqq, _ = C.sb("sqq", [128, 2, NTT, 512], BF16)
        scr["sqq"] = (sqq, [Buf("sqq%d" % i) for i in range(NTT)])
        et, _ = C.sb("et", [128, 2, 512], BF16)
        scr["et"] = (et, [Buf("et0"), Buf("et1")])
        C.dma(V[:, :], vec_d[:, :], W=[vb])
        C.dma(c32[:, :, :], c32_d.rearrange("p (a b) -> p a b", a=2), W=[c32b])
        P.op("pool", "dma_start", PK(out=cst3[:, :, :], in_=cst_d.rearrange("p (a b) -> p a b", a=5)), writes=[cstb], is_dma=True)
        sel, selb = C.sb("sel_sb", [128, 2], F32)
        C.dma(sel[:, :], sel_d[:, :], W=[selb])
        htmp, _ = C.sb("htmp", [128, 2, 512], F32); htmpb = [Buf("htmp0"), Buf("htmp1")]
        hi_ = 0
        for m in range(NTT):
            sl = slice(m * TT, (m + 1) * TT)
            e0 = slice((2 * m) * TT, (2 * m + 1) * TT); e1 = slice((2 * m + 1) * TT, (2 * m + 2) * TT)
            C.dma(hT[:, :, sl], h1s_d[:, e0].rearrange("(c p) n -> p c n", p=128), R=[h1b], W=[hTb[m]])
            for c in range(8):
                hb = hi_ % 2; hi_ += 1
                C.dma(htmp[:, hb, :], h1s_d[c * 128:(c + 1) * 128, e1], R=[h1b], W=[htmpb[hb]])
                P.op("dve", "tensor_scalar", PK(out=hT[:, c, sl], in0=hT[:, c, sl], scalar1=sel[:, 0:1], scalar2=None, op0=ALU.mult), reads=[hTb[m], selb], writes=[hTb[m]])
                P.op("dve", "scalar_tensor_tensor", PK(out=hT[:, c, sl], in0=htmp[:, hb, :], scalar=sel[:, 1:2], in1=hT[:, c, sl], op0=ALU.mult, op1=ALU.add),
                     reads=[htmpb[hb], selb, hTb[m]], writes=[hTb[m]])
        def evac_res(oc, tt, pt, pb, m):
            sl = slice(tt * TT, (tt + 1) * TT)
            P.op("dve", "tensor_tensor", PK(out=hT[:, oc, sl], in0=pt[:, :], in1=hT[:, oc, sl], op=ALU.add), reads=[pb, hTb[tt]], writes=[hTb[tt]])
        linear_fm(C, wdo_d, 8, bufA, None, 1024, evac_res, xinbs=bufAb)
        _mem_off0 = C.off
        (kn, knb), (vt, vtb) = mem_kv(C, memT_d, wkv_d, V, vb, VC["n_mem"], VC["k_gain"], cst3, cstb, scr, "m1")
        _mem_off1 = C.off
        mem_xattn(C, hT, hTb, bufA, bufAb, bufH, bufHb, V, vb, VC["n_xattn"], VC["q_gain"], wq_d, wo_d, kn, knb, vt, vtb, cst3, cstb, scr)
        for tt in range(NTT):
            sl = slice(tt * TT, (tt + 1) * TT)
            norm_fm(C, lambda c: hT[:, c, sl], [hTb[tt]], TT, 8, cst3[:, 0, :], cstb, lambda c: V[:, VC["n_ffn"] + c:VC["n_ffn"] + c + 1],
                    lambda c: bufA[:, c, sl], [bufAb[tt]], scr)
        hn32, hn32b = C.sb("hn32", [128, 8, 128], F32)
        wr, wrb = C.sb("wr", [128, 8, 8], F32)
        G, Gb = C.sb("G", [128, 16, 8], F32)
        lg, lgb = C.sb("lg", [128, 8], F32)
        mx, mxb = C.sb("mx", [128, 8], F32)
        sm, smb = C.sb("sm", [128, 4], F32)
        C.dma(wr[:, :, :], wr_d.rearrange("(c p) n -> p c n", p=128), W=[wrb])
        for s in range(16):
            tt = s // 4
            sl = slice(s * 128, (s + 1) * 128)
            norm_fm(C, lambda c: hT[:, c, sl], [hTb[tt]], 128, 8, cst3[:, 0, :], cstb, lambda c: V[:, VC["n_ffn"] + c:VC["n_ffn"] + c + 1],
                    lambda c: hn32[:, c, :], [hn32b], scr)
            pt, pb = C.psum()
            for k in range(8):
                P.op("pe", "matmul", PK(pt[:, 0:8], hn32[:, k, :], wr[:, k, :], start=(k == 0), stop=(k == 7)), reads=[hn32b, wrb], writes=[pb])
            P.op("dve", "tensor_tensor", PK(out=lg[:, :], in0=pt[:, 0:8], in1=V[:, VC["b_router"]:VC["b_router"] + 8], op=ALU.add),
                 reads=[pb, vb], writes=[lgb])
            P.op("dve", "max", PK(out=mx[:, :], in_=lg[:, :]), reads=[lgb], writes=[mxb])
            P.op("dve", "tensor_scalar", PK(out=sm[:, 0:1], in0=mx[:, 0:1], scalar1=-1.0, scalar2=None, op0=ALU.mult), reads=[mxb], writes=[smb])
            ex, exb = scr["sg"]
            P.op("act", "activation", PK(out=ex[:, 0:8], in_=lg[:, :], func=AF.Exp, bias=sm[:, 0:1]), reads=[lgb, smb], writes=[exb])
            P.op("dve", "tensor_scalar", PK(out=lg[:, :], in0=lg[:, :], scalar1=mx[:, 1:2], scalar2=None, op0=ALU.is_ge), reads=[lgb, mxb], writes=[lgb])
            P.op("dve", "tensor_tensor", PK(out=ex[:, 0:8], in0=ex[:, 0:8], in1=lg[:, :], op=ALU.mult), reads=[exb, lgb], writes=[exb])
            P.op("dve", "reduce_sum", PK(out=sm[:, 1:2], in_=ex[:, 0:8], axis=AX.X), reads=[exb], writes=[smb])
            P.op("dve", "reciprocal", PK(out=sm[:, 2:3], in_=sm[:, 1:2]), reads=[smb], writes=[smb])
            P.op("dve", "tensor_scalar", PK(out=G[:, s, :], in0=ex[:, 0:8], scalar1=sm[:, 2:3], scalar2=None, op0=ALU.mult), reads=[exb, smb], writes=[Gb])
        P.fence(C.dummy[:, 0:1])
        gbc = sqq.rearrange("p a b c -> p (a b c)")[:, 0:NTOK]; gbcb = [Buf("gbc%d" % i) for i in range(NTT)]
        C.wbufs.append((scr["sq"][0].rearrange("p a b -> p (a b)"), Buf("wb_sq")))
        _o = _mem_off0
        while _o + 2048 <= _mem_off1:
            C.wbufs.append((C.arena[:, _o:_o + 2048].bitcast(BF16), Buf("wb_m%d" % _o)))
            _o += 2048
        dg, dgb = scr["rd"]
        for e_ in range(8):
            for tt in range(NTT):
                pt, pb = C.psum()
                for q in range(4):
                    s = tt * 4 + q
                    P.op("dve", "tensor_scalar", PK(out=dg[:, 0:128], in0=c32[:, 0, :], scalar1=G[:, s, e_:e_ + 1], scalar2=None, op0=ALU.mult),
                         reads=[c32b, Gb], writes=[dgb])
                    P.op("pe", "matmul", PK(pt[:, q * 128:(q + 1) * 128], c32[:, 1, :], dg[:, 0:128], start=True, stop=True), reads=[dgb, c32b], writes=[pb])
                P.op("act", "activation", PK(out=gbc[:, tt * TT:(tt + 1) * TT], in_=pt[:, :], func=AF.Copy), reads=[pb], writes=[gbcb[tt]])
            swiglu_ffn(C, hT, hTb, bufA, bufAb, bufH, bufHb, V, vb, None, wg_d[e_], wu_d[e_], wd_d[e_], 3584, cst3, cstb, scr, gate_bc=(gbc, gbcb))
        for tt in range(NTT):
            sl = slice(tt * TT, (tt + 1) * TT)
            C.dma(outT_d[:, sl].rearrange("(c p) n -> p c n", p=128), hT[:, :, sl], R=[hTb[tt]], is_out=True)


ARENA_WORDS = 53200


def build_fused():
    nc = bass.Bass("TRN2", target_bir_lowering=False)
    d = {}

    def I(n, s):
        d[n] = nc.dram_tensor(n, s, F32, kind="ExternalInput").ap()

    def S(n, s):
        d[n] = nc.dram_tensor(n, s, F32, kind="Internal").ap()
    I("xT", [D, T_SEQ]); I("memT", [D, 256]); I("cst", [128, 640]); I("c32A", [128, 384]); I("c32", [128, 256])
    I("mask5", [64, 320]); I("rmask", [128, SEG]); I("vecsB", [128, NVB]); I("vecsC", [128, NVC]); I("sel", [128, 2])
    I("qaug4", [4, NTOK]); I("kaug4", [8, 4, T_SEQ]); I("biasT", [128, 8 * 512]); I("lamb", [128, 256]); I("sgc", [128, 1])
    for hh in range(2):
        I("wc%d" % hh, [D, NCOLS]); I("vecsA%d" % hh, [128, NVA]); I("w_up%d" % hh, [64, 256]); I("a_up%d" % hh, [128, 256])
        I("g_upa%d" % hh, [128, 256]); I("g_upb%d" % hh, [32, 256])
        for n in ("breT", "bimT", "creT", "cimT"):
            I("%s%d" % (n, hh), [8, 128, 128])
    I("w_glu", [512, 512]); I("w_out", [D, D]); I("w_q0", [D, D]); I("w_kv0", [D, 2 * D]); I("w_o0", [D, D])
    I("ff_g", [D, 2816]); I("ff_u", [D, 2816]); I("ff_d", [2816, D]); I("w_qkv", [D, 3 * D])
    I("da_w_o", [D, D]); I("w_q1", [D, D]); I("w_kv1", [D, 2 * D]); I("w_o1", [D, D]); I("w_router", [D, 8])
    I("moe_g", [8, D, 3584]); I("moe_u", [8, D, 3584]); I("moe_d", [8, 3584, D])
    S("ymT", [D, T_SEQ]); S("h1T", [D, T_SEQ]); S("qT", [16, 64, T_SEQ]); S("kT", [16, 64, T_SEQ]); S("vtok", [T_SEQ, D])
    d["outT"] = nc.dram_tensor("outT", [D, NTOK], F32, kind="ExternalOutput").ap()
    with ExitStack() as st:
        P = Prog(nc)
        C = ACtx(nc, st, P, ARENA_WORDS)
        ymb, h1b, qb_, kb_, vtb_ = Buf("ymT"), Buf("h1T"), Buf("qT"), Buf("kT"), Buf("vtok")
        for hh in range(2):
            if hh:
                C.new_phase()
            body_A(C, d, hh, ymb)
        import os as _os
        if _os.environ.get("FSTOP") == "A":
            C.dma(d["outT"][:, 0:512], d["ymT"][:, 0:512], R=[ymb], is_out=True)
            P.emit(st)
            return nc
        for half in range(2):
            C.new_phase()
            body_B(C, d, half, (ymb, h1b, qb_, kb_, vtb_))
        C.new_phase()
        bufA, _ = C.sb("bufA_p", [128, 8, NTOK], BF16); bufAb = [Buf("bAp%d" % i) for i in range(NTT)]
        keep = C.off
        body_C1(C, d, bufA, bufAb, (h1b, qb_, kb_, vtb_))
        C.new_phase(keep=keep)
        body_C2(C, d, bufA, bufAb, h1b)
        P.emit(st)
    return nc


def pack_fused(inp, b, p):
    m = {}
    for hh in range(2):
        a = pack_A(inp, b, hh)
        if hh == 0:
            m["xT"] = a["xT"]; m["cst"] = a["cst"]; m["c32A"] = a["c32"]; m["mask5"] = a["mask5"]; m["rmask"] = a["rmask"]
        m["wc%d" % hh] = a["wc"]; m["vecsA%d" % hh] = a["vecs"]; m["w_up%d" % hh] = a["w_up"]; m["a_up%d" % hh] = a["a_up"]
        m["g_upa%d" % hh] = a["g_upa"]; m["g_upb%d" % hh] = a["g_upb"]
        for n in ("breT", "bimT", "creT", "cimT"):
            m["%s%d" % (n, hh)] = a[n]
    m["memT"] = np.ascontiguousarray(np.asarray(inp["mem"][b]).T)
    m["c32"] = c32_table(); m["vecsB"] = vecs_B(inp); m["vecsC"] = vecs_C(inp)
    sel = np.zeros((128, 2), np.float32); sel[:, p] = 1.0
    m["sel"] = sel
    m["qaug4"] = q_aug_rows(p)
    m["kaug4"] = np.stack([k_aug_rows(h) for h in range(8)])
    m["biasT"] = bias_table(p)
    m["lamb"] = np.ascontiguousarray(np.broadcast_to(np.stack([inp["da_lam_q1"][0], inp["da_lam_k1"][0], inp["da_lam_q2"][0],
                                                               inp["da_lam_k2"][0]]).reshape(1, 256), (128, 256))).astype(np.float32)
    m["sgc"] = np.asarray(inp["da_sub_gain"][0], np.float32).reshape(128, 1)
    for k_, src in (("w_glu", "s5_w_glu"), ("w_out", "hy_w_out"), ("ff_g", "ff_w_gate"), ("ff_u", "ff_w_up"), ("ff_d", "ff_w_down"),
                    ("w_qkv", "da_w_qkv"), ("da_w_o", "da_w_o"), ("w_router", "moe_w_router"), ("moe_g", "moe_w_gate"),
                    ("moe_u", "moe_w_up"), ("moe_d", "moe_w_down")):
        m[k_] = np.asarray(inp[src][0])
    for l in range(2):
        m["w_q%d" % l] = np.asarray(inp["xa_w_q"][l]); m["w_kv%d" % l] = np.asarray(inp["xa_w_kv"][l]); m["w_o%d" % l] = np.asarray(inp["xa_w_o"][l])
    return m


_CACHE = {}


def kernel(**inp):
    inp = {k: np.asarray(v) for k, v in inp.items()}
    B_, T_ = 4, 4096
    if "F" not in _CACHE:
        _CACHE["F"] = build_fused()
    cores = [(b, p) for b in range(B_) for p in range(2)]
    maps = [pack_fused(inp, b, p) for (b, p) in cores]
    res = run_bass_kernel_spmd(_CACHE["F"], maps, core_ids=list(range(8))).results
    out = np.zeros((B_, T_, 1024), np.float32)
    for i, (b, p) in enumerate(cores):
        out[b][tok_index(p)] = np.asarray(res[i]["outT"]).T
    return out
```
